# Optimizing a Trainium2 kernel written in Bass

```python
import math
import jax
import jax.numpy as jnp
from jax import lax
import numpy as np

D_MODEL = 1024
BATCH = 16
SEQ = 2048
DEPTH = 1

N_HEADS_A = 8
HEAD_DIM = 64
WIDTH_A = N_HEADS_A * HEAD_DIM
KV_RANK = 128
IDX_HEADS = 8
IDX_DIM = 64
TOPK_MAX = 256
N_HEADS_B = 8
WIDTH_B = N_HEADS_B * HEAD_DIM
BLOCK_Q = 128
N_BUCKETS = 32
MAX_EXACT = N_BUCKETS // 2
MAX_DISTANCE = 128
ATTN_SCALE = HEAD_DIM ** -0.5
IDX_SCALE = (IDX_HEADS ** -0.5) * (IDX_DIM ** -0.5)
N_GROUPS = 4
EXPERTS_PER_GROUP = 8
N_EXPERTS = N_GROUPS * EXPERTS_PER_GROUP
D_EXPERT = 256
TOP_K_INNER = 2
PLE_DIM = 256
EPS = 1e-6

_COLS = [
    ("q_a", WIDTH_A),
    ("c_kv", KV_RANK),
    ("q_idx", IDX_HEADS * IDX_DIM),
    ("k_idx", IDX_DIM),
    ("w_idx", IDX_HEADS),
    ("qkv_b", 3 * WIDTH_B),
    ("gate_a", D_MODEL),
    ("gate_b", D_MODEL),
]
IN_COLS = sum(c for _, c in _COLS)

kernel_name = "hybrid_dsa_stickbreak_hmoe_block"


def _rmsnorm(x, g):
    xf = x.astype(jnp.float32)
    y = xf * lax.rsqrt(jnp.mean(xf * xf, axis=-1, keepdims=True) + EPS)
    return (y * g.astype(jnp.float32)).astype(x.dtype)


def _split_cols(proj):
    out = []
    start = 0
    for _, width in _COLS:
        out.append(proj[..., start:start + width])
        start += width
    return out


def _t5_bucket(dist):
    dist = jnp.maximum(dist, 0)
    d_f = jnp.maximum(dist, 1).astype(jnp.float32)
    large = MAX_EXACT + (jnp.log(d_f / MAX_EXACT) / math.log(MAX_DISTANCE / MAX_EXACT)
                         * (N_BUCKETS - MAX_EXACT)).astype(jnp.int32)
    large = jnp.minimum(large, N_BUCKETS - 1)
    return jnp.where(dist < MAX_EXACT, dist, large)


def _dsa_branch(q, c_kv, q_idx, k_idx, w_idx, w_uk, w_uv, rel_bias):
    b, s = q.shape[0], q.shape[1]
    n_sel = min(TOPK_MAX, s // 4)
    n_blocks = s // BLOCK_Q
    q_abs = jnp.einsum("bshd,hcd->bshc", q, w_uk)
    key_pos = jnp.arange(s, dtype=jnp.int32)
    gather = jax.vmap(lambda c, i: c[i])

    def block(i):
        start = i * BLOCK_Q
        t = start + jnp.arange(BLOCK_Q, dtype=jnp.int32)
        qa = lax.dynamic_slice_in_dim(q_abs, start, BLOCK_Q, axis=1)
        qi = lax.dynamic_slice_in_dim(q_idx, start, BLOCK_Q, axis=1)
        wi = lax.dynamic_slice_in_dim(w_idx, start, BLOCK_Q, axis=1).astype(jnp.float32)
        rel = jax.nn.relu(jnp.einsum("bqhe,bse->bqhs", qi, k_idx).astype(jnp.float32))
        score = jnp.einsum("bqhs,bqh->bqs", rel, wi)
        causal = key_pos[None, :] <= t[:, None]
        score = jnp.where(causal[None], score, -jnp.inf)
        _, idx = lax.top_k(score, n_sel)
        valid = idx <= t[None, :, None]
        c_sel = gather(c_kv, idx)
        logits = jnp.einsum("bqhc,bqkc->bhqk", qa, c_sel).astype(jnp.float32) * ATTN_SCALE
        bias = rel_bias[_t5_bucket(t[None, :, None] - idx)]
        logits = logits + jnp.transpose(bias, (0, 3, 1, 2)).astype(jnp.float32)
        logits = jnp.where(valid[:, None], logits, -jnp.inf)
        probs = jax.nn.softmax(logits, axis=-1).astype(c_sel.dtype)
        return jnp.einsum("bhqk,bqkc->bqhc", probs, c_sel)

    o = lax.map(block, jnp.arange(n_blocks, dtype=jnp.int32))
    o = jnp.moveaxis(o, 0, 1).reshape(b, s, N_HEADS_A, KV_RANK)
    return jnp.einsum("bshc,hcd->bshd", o, w_uv)


def _stick_breaking_branch(q, k, v):
    b, s = q.shape[0], q.shape[1]
    n_blocks = s // BLOCK_Q
    key_pos = jnp.arange(s, dtype=jnp.int32)

    def block(i):
        start = i * BLOCK_Q
        t = start + jnp.arange(BLOCK_Q, dtype=jnp.int32)
        qb = lax.dynamic_slice_in_dim(q, start, BLOCK_Q, axis=1)
        z = jnp.einsum("bqhd,bshd->bhqs", qb, k).astype(jnp.float32) * ATTN_SCALE
        mask = (key_pos[None, :] < t[:, None])[None, None]
        log_1m = jnp.where(mask, jax.nn.log_sigmoid(-z), 0.0)
        after = lax.cumsum(log_1m, axis=3, reverse=True) - log_1m
        log_a = jax.nn.log_sigmoid(z) + after
        a = jnp.where(mask, jnp.exp(log_a), 0.0).astype(v.dtype)
        return jnp.einsum("bhqs,bshd->bqhd", a, v)

    o = lax.map(block, jnp.arange(n_blocks, dtype=jnp.int32))
    return jnp.moveaxis(o, 0, 1).reshape(b, s, N_HEADS_B, HEAD_DIM)


def _hier_moe(h, w_r1, b_r1, w_r2, b_r2, w_gate, w_up, w_down):
    b, s, d = h.shape
    hf = h.reshape(b * s, d)
    n_tok = b * s
    g_logits = jnp.matmul(hf, w_r1).astype(jnp.float32) + b_r1.astype(jnp.float32)
    g_prob = jax.nn.softmax(g_logits, axis=-1)
    g_sel = jnp.argmax(g_logits, axis=-1)
    p_g = jnp.take_along_axis(g_prob, g_sel[:, None], axis=1)[:, 0]
    e_logits = jnp.einsum("nd,gde->nge", hf, w_r2).astype(jnp.float32) + b_r2.astype(jnp.float32)
    e_sel = jnp.take_along_axis(e_logits, g_sel[:, None, None], axis=1)[:, 0]
    top_v, top_i = lax.top_k(e_sel, TOP_K_INNER)
    w_inner = jax.nn.softmax(top_v, axis=-1)
    inner = jnp.sum(jax.nn.one_hot(top_i, EXPERTS_PER_GROUP, dtype=jnp.float32) * w_inner[..., None], axis=1)
    comb = (jax.nn.one_hot(g_sel, N_GROUPS, dtype=jnp.float32)[:, :, None]
            * inner[:, None, :] * p_g[:, None, None])
    comb = comb.reshape(n_tok, N_EXPERTS).astype(h.dtype)
    out = jnp.zeros_like(hf)
    for e in range(N_EXPERTS):
        hid = jax.nn.silu(jnp.matmul(hf, w_gate[e])) * jnp.matmul(hf, w_up[e])
        out = out + comb[:, e:e + 1] * jnp.matmul(hid, w_down[e])
    return out.reshape(b, s, d)


def setup_inputs(seed: int = 0) -> dict:
    key = jax.random.key(seed)
    ks = jax.random.split(key, 24)
    f32 = jnp.float32

    def nrm(k, shape, fan_in):
        return jax.random.normal(k, shape, f32) * (fan_in ** -0.5)

    def gain(k, shape):
        return 1.0 + 0.05 * jax.random.normal(k, shape, f32)

    return {
        "x": jax.random.normal(ks[0], (BATCH, SEQ, D_MODEL), f32),
        "p": jax.random.normal(ks[1], (DEPTH, BATCH, SEQ, PLE_DIM), f32),
        "attn_norm": gain(ks[2], (DEPTH, D_MODEL)),
        "w_in": nrm(ks[3], (DEPTH, D_MODEL, IN_COLS), D_MODEL),
        "kv_norm": gain(ks[4], (DEPTH, KV_RANK)),
        "w_uk": nrm(ks[5], (DEPTH, N_HEADS_A, KV_RANK, HEAD_DIM), KV_RANK),
        "w_uv": nrm(ks[6], (DEPTH, N_HEADS_A, KV_RANK, HEAD_DIM), KV_RANK),
        "rel_bias": 0.5 * jax.random.normal(ks[7], (N_BUCKETS, N_HEADS_A), f32),
        "w_branch_a": nrm(ks[8], (DEPTH, WIDTH_A, D_MODEL), WIDTH_A),
        "w_branch_b": nrm(ks[9], (DEPTH, WIDTH_B, D_MODEL), WIDTH_B),
        "w_out": nrm(ks[10], (DEPTH, D_MODEL, D_MODEL), D_MODEL),
        "ffn_norm": gain(ks[11], (DEPTH, D_MODEL)),
        "w_r1": nrm(ks[12], (DEPTH, D_MODEL, N_GROUPS), D_MODEL),
        "b_r1": 0.01 * jax.random.normal(ks[13], (DEPTH, N_GROUPS), f32),
        "w_r2": nrm(ks[14], (DEPTH, N_GROUPS, D_MODEL, EXPERTS_PER_GROUP), D_MODEL),
        "b_r2": 0.01 * jax.random.normal(ks[15], (DEPTH, N_GROUPS, EXPERTS_PER_GROUP), f32),
        "w_gate": nrm(ks[16], (DEPTH, N_EXPERTS, D_MODEL, D_EXPERT), D_MODEL),
        "w_up": nrm(ks[17], (DEPTH, N_EXPERTS, D_MODEL, D_EXPERT), D_MODEL),
        "w_down": nrm(ks[18], (DEPTH, N_EXPERTS, D_EXPERT, D_MODEL), D_EXPERT),
        "ple_norm": gain(ks[19], (DEPTH, D_MODEL)),
        "w_ple_gate": nrm(ks[20], (DEPTH, D_MODEL, D_MODEL), D_MODEL),
        "w_ple": nrm(ks[21], (DEPTH, PLE_DIM, D_MODEL), PLE_DIM),
        "final_norm": gain(ks[22], (D_MODEL,)),
    }


def reference(x, p, attn_norm, w_in, kv_norm, w_uk, w_uv, rel_bias, w_branch_a, w_branch_b,
              w_out, ffn_norm, w_r1, b_r1, w_r2, b_r2, w_gate, w_up, w_down,
              ple_norm, w_ple_gate, w_ple, final_norm):
    b, s, _ = x.shape
    for i in range(DEPTH):
        h = _rmsnorm(x, attn_norm[i])
        proj = jnp.matmul(h, w_in[i])
        q_a, c_kv, q_idx, k_idx, w_idx, qkv_b, gate_a, gate_b = _split_cols(proj)
        q_a = q_a.reshape(b, s, N_HEADS_A, HEAD_DIM)
        c_kv = _rmsnorm(c_kv, kv_norm[i])
        q_idx = q_idx.reshape(b, s, IDX_HEADS, IDX_DIM)
        w_idx = w_idx * IDX_SCALE
        q_b, k_b, v_b = jnp.split(qkv_b.reshape(b, s, 3, N_HEADS_B, HEAD_DIM), 3, axis=2)
        o_a = _dsa_branch(q_a, c_kv, q_idx, k_idx, w_idx, w_uk[i], w_uv[i], rel_bias)
        o_b = _stick_breaking_branch(q_b[:, :, 0], k_b[:, :, 0], v_b[:, :, 0])
        y_a = jnp.matmul(o_a.reshape(b, s, WIDTH_A), w_branch_a[i])
        y_b = jnp.matmul(o_b.reshape(b, s, WIDTH_B), w_branch_b[i])
        merged = jax.nn.sigmoid(gate_a) * y_a + jax.nn.sigmoid(gate_b) * y_b
        x = x + jnp.matmul(merged, w_out[i])
        h2 = _rmsnorm(x, ffn_norm[i])
        x = x + _hier_moe(h2, w_r1[i], b_r1[i], w_r2[i], b_r2[i], w_gate[i], w_up[i], w_down[i])
        h3 = _rmsnorm(x, ple_norm[i])
        x = x + jnp.matmul(p[i], w_ple[i]) * jax.nn.sigmoid(jnp.matmul(h3, w_ple_gate[i]))
    return _rmsnorm(x, final_norm)
```

```python
import math
import types
import contextlib
import numpy as np
import concourse.bass as bass
import concourse.mybir as mybir
from concourse.bass_utils import run_bass_kernel_spmd

F32 = mybir.dt.float32
BF16 = mybir.dt.bfloat16
AF = mybir.ActivationFunctionType
ALU = mybir.AluOpType
AX = mybir.AxisListType

S = 2048
D = 1024
NT = S // 128
NCH = S // 512
ATTN_SCALE = 64 ** -0.5
IDX_SCALE = (8 ** -0.5) * (64 ** -0.5)
EPS = 1e-6
NEG = -1.0e30
N_BISECT = 20
N_EXP = 32
import os as _os
SBE = _os.environ.get("SBE", "pool")
SBLN = _os.environ.get("SBLN", "1") == "1"


class Prog:
    ENGS = ("pe", "act", "dve", "pool", "sp")

    def __init__(self, nc, same_eng_sync=True):
        self.nc = nc
        self.ops = {e: [] for e in self.ENGS}
        self.last_w = {}
        self.last_r = {}
        self.clock = {e: {} for e in self.ENGS}
        self.opclock = {}
        self.dma_count = {}
        self.dma_keys = []
        self.signaling = set()
        self.same_eng_sync = same_eng_sync
        self._bar = 0

    def _add(self, eng, fn, reads, writes, dma_key=None, n_dma=1):
        idx = len(self.ops[eng]) + 1
        deps = {}

        def need(tok):
            kind, who, n = tok
            if kind == "e" and who == eng:
                if eng in ("pe", "sp") or not self.same_eng_sync:
                    return
            k = (kind, who)
            if deps.get(k, 0) < n:
                deps[k] = n

        for r in reads:
            for tok in self.last_w.get(r, {}).values():
                need(tok)
        for w in writes:
            for tok in self.last_w.get(w, {}).values():
                need(tok)
            for tok in self.last_r.get(w, {}).values():
                need(tok)
        if dma_key is not None:
            if dma_key not in self.dma_count:
                self.dma_count[dma_key] = 0
                self.dma_keys.append(dma_key)
            prev = self.dma_count[dma_key]
            if prev > 0:
                need(("d", dma_key, prev))
            self.dma_count[dma_key] = prev + n_dma
            mytok = ("d", dma_key, prev + n_dma)
        else:
            mytok = ("e", eng, idx)
        clk = self.clock[eng]
        final = []
        for k, n in deps.items():
            if clk.get(k, 0) >= n:
                continue
            final.append((k[0], k[1], n))
        for kind, who, n in final:
            oc = self.opclock.get((kind, who, n))
            if oc:
                for k2, n2 in oc.items():
                    if clk.get(k2, 0) < n2:
                        clk[k2] = n2
            if clk.get((kind, who), 0) < n:
                clk[(kind, who)] = n
            if kind == "e":
                self.signaling.add((who, n))
        snap = dict(clk)
        if mytok[0] == "e":
            snap[("e", eng)] = idx
        self.opclock[mytok] = snap
        self.ops[eng].append((fn, final, dma_key, mytok))
        if fn is not None:
            for r in reads:
                self.last_r.setdefault(r, {})[(mytok[0], mytok[1])] = mytok
        for w in writes:
            self.last_w[w] = {(mytok[0], mytok[1]): mytok}
            self.last_r[w] = {}
        return mytok

    @staticmethod
    def _freeze(fn):
        if fn is None or getattr(fn, "__closure__", None) is None:
            return fn
        cells = []
        for c in fn.__closure__:
            try:
                cells.append(types.CellType(c.cell_contents))
            except ValueError:
                cells.append(c)
        return types.FunctionType(fn.__code__, fn.__globals__, fn.__name__, fn.__defaults__, tuple(cells))

    def op(self, eng, fn, reads=(), writes=()):
        return self._add(eng, self._freeze(fn), tuple(reads), tuple(writes))

    def dma(self, eng, key, fns, reads=(), writes=()):
        if not isinstance(fns, (list, tuple)):
            fns = [fns]
        return self._add(eng, [self._freeze(f) for f in fns], tuple(reads), tuple(writes), dma_key=key, n_dma=len(fns))

    def barrier(self, tiny_fn):
        self._bar += 1
        res = ("__barrier__", self._bar)
        allres = list(set(list(self.last_w.keys()) + list(self.last_r.keys())))
        self._add("dve", self._freeze(tiny_fn), tuple(), tuple(allres) + (res,))
        for e in ("pe", "act", "pool", "sp"):
            self._add(e, None, (res,), tuple())

    def emit(self, final_wait_tokens=()):
        nc = self.nc
        sigval = {}
        for e in self.ENGS:
            s = 0
            for i in range(1, len(self.ops[e]) + 1):
                if (e, i) in self.signaling:
                    s += 1
                    sigval[(e, i)] = s
        engobj = {"pe": nc.tensor, "act": nc.scalar, "dve": nc.vector, "pool": nc.gpsimd, "sp": nc.sync}
        with contextlib.ExitStack() as st:
            esem = {e: st.enter_context(nc.semaphore("sem_" + e)) for e in self.ENGS}
            dsem = {k: st.enter_context(nc.semaphore("dsem_%d" % i)) for i, k in enumerate(self.dma_keys)}
            block = st.enter_context(nc.Block())

            def run(e):
                eng = engobj[e]
                for i, (fn, deps, dma_key, mytok) in enumerate(self.ops[e], start=1):
                    for kind, who, n in deps:
                        if kind == "e":
                            eng.wait_ge(esem[who], sigval[(who, n)])
                        else:
                            eng.wait_ge(dsem[who], 16 * n)
                    if fn is None:
                        assert (e, i) not in self.signaling
                        continue
                    if dma_key is not None:
                        for f in fn:
                            f(eng).then_inc(dsem[dma_key], 16)
                    else:
                        ins = fn(eng)
                        if (e, i) in self.signaling:
                            ins.then_inc(esem[e], 1)
                if e == "sp":
                    for k in self.dma_keys:
                        eng.wait_ge(dsem[k], 16 * self.dma_count[k])

            block.tensor(lambda eng: run("pe"))
            block.scalar(lambda eng: run("act"))
            block.vector(lambda eng: run("dve"))
            block.gpsimd(lambda eng: run("pool"))
            block.sync(lambda eng: run("sp"))


def build_nc(NB=2, stop_after=None, dbg=False):
    nc = bass.Bass("TRN2", target_bir_lowering=False)

    def din(name, shape, dt=F32):
        return nc.dram_tensor(name, list(shape), dt, kind="ExternalInput").ap()

    x_d = din("x", [NB, S, D])
    p_d = din("p", [NB, S, 256])
    w1_d = din("w1", [128, 8 * 1280])
    widx_d = din("widx", [128, 8 * 8])
    w3_d = din("w3", [128, 8 * 1536])
    wg4_d = din("wg4", [128, 8 * 8 * 256])
    wbr_d = din("wbr", [128, 8 * 1024])
    wout_d = din("wout", [128, 8 * 1024])
    wuk_d = din("wuk", [128, 512])
    wuv_d = din("wuv", [128, 512])
    wmoe_d = din("wmoe", [N_EXP, 128, 6144])
    wr_d = din("wr", [128, 8 * 36])
    br_d = din("br", [128, 36])
    wpg_d = din("wpg", [128, 8 * 1024])
    wpl_d = din("wpl", [128, 2 * 1024])
    gat_d = din("g_attn", [128, 8])
    gff_d = din("g_ffn", [128, 8])
    gpl_d = din("g_ple", [128, 8])
    gfin_d = din("g_fin", [128, 1024])
    gkv_d = din("g_kv", [128, 1])
    btoep_d = din("btoep", [128, 8 * 640])
    b31_d = din("b31", [128, 8])
    ident_d = din("ident", [128, 128])
    cneg_d = din("cneg", [128, 128])
    smask_d = din("smask", [128, 128])
    uinc_d = din("uinc", [128, 128])
    sel_d = din("sel", [128, 256])
    out_d = nc.dram_tensor("out", [NB, S, D], F32, kind="ExternalOutput").ap()
    dbg_d = {}
    if dbg:
        for nm, shp in (("d_oaT", [128, 4 * S]), ("d_obT", [128, 4 * S]), ("d_x1", [128, NT * D])):
            dbg_d[nm] = nc.dram_tensor(nm, shp, F32, kind="ExternalOutput").ap()

    st = contextlib.ExitStack()
    with st:
        def sb(name, shape, dt):
            return st.enter_context(nc.sbuf_tensor("s_" + name, list(shape), dt))

        arA = sb("arA", [128, 16384], BF16)
        arB = sb("arB", [128, 32768], BF16)
        arC = sb("arC", [128, 16384], BF16)
        arD = sb("arD", [128, 33792], BF16)
        ident_f = sb("ident_f", [128, 128], F32)
        ident_b = sb("ident_b", [128, 128], BF16)
        cneg = sb("cneg", [128, 128], F32)
        smask = sb("smask", [128, 128], BF16)
        uinc = sb("uinc", [128, 128], BF16)
        ones_b = sb("ones_b", [128, 128], BF16)
        sel_f = sb("sel_f", [128, 256], F32)
        gB1 = sb("gB", [128, 8, 128], BF16)
        gB = [gB1, gB1, gB1]
        gpk = sb("gpk", [128, 24], F32)
        gkv = sb("gkv", [128, 1], F32)
        b31 = sb("b31", [128, 8], F32)
        brb = sb("brb", [128, 36], F32)
        wr_f = sb("wr_f", [128, 8, 36], F32)
        wuk = sb("wuk", [128, 4, 128], BF16)
        wuv = sb("wuv", [128, 512], BF16)
        widx_w = sb("widx_w", [128, 8, 8], BF16)
        small = sb("small", [128, 256], F32)
        tiny = sb("tiny", [128, 2], F32)

        psb = [st.enter_context(nc.psum_tensor("ps%d" % i, [128, 512], F32)) for i in range(8)]

        P = Prog(nc)

        def carve(ar, off_bytes, nbytes, dt, pattern=None, **kw):
            e0 = off_bytes // 2
            ap = ar[:, e0:e0 + nbytes // 2]
            if dt == F32:
                ap = ap.bitcast(F32)
            if pattern:
                ap = ap.rearrange(pattern, **kw)
            return ap

        KB = 1024
        hT = carve(arA, 0, 32 * KB, BF16, "p (k t) -> p k t", k=8)
        qaT = carve(arB, 0, 16 * KB, BF16, "p (k t) -> p k t", k=4)
        qiT = carve(arB, 16 * KB, 16 * KB, BF16, "p (k t) -> p k t", k=4)
        ckvT = carve(arB, 32 * KB, 4 * KB, BF16)
        kiT = carve(arB, 36 * KB, 4 * KB, BF16)
        Vp = carve(arB, 40 * KB, 16 * 4 * 130 * 2, BF16, "p (j m c) -> p j m c", j=16, m=4)
        widx_tm = carve(arB, 40 * KB + 16640, 512, F32, "p (i h) -> p i h", i=16)
        qbT = carve(arB, 0, 16 * KB, BF16, "p (k t) -> p k t", k=4)
        kbT = carve(arB, 16 * KB, 16 * KB, BF16, "p (k t) -> p k t", k=4)
        vb = carve(arB, 32 * KB, 16 * KB, BF16, "p (j c) -> p j c", j=16)
        qbTn = carve(arB, 48 * KB, 16 * KB, BF16, "p (k t) -> p k t", k=4)
        x1 = carve(arB, 0, 64 * KB, F32, "p (i d) -> p i d", i=16)
        oaT = carve(arC, 0, 16 * KB, BF16, "p (k t) -> p k t", k=4)
        obT = carve(arC, 16 * KB, 16 * KB, BF16, "p (k t) -> p k t", k=4)
        wmoe = [carve(arC, i * 12 * KB, 12 * KB, BF16) for i in range(2)]
        wpg = carve(arC, 0, 16 * KB, BF16, "p (k c) -> p k c", k=8)
        wpl = carve(arC, 16 * KB, 4 * KB, BF16, "p (k c) -> p k c", k=2)
        def dD(off, nbytes, dt, pattern=None, **kw):
            assert off + nbytes <= 66 * KB, (off, nbytes)
            return carve(arD, off, nbytes, dt, pattern, **kw)

        PS = lambda i: psb[i]

        def psbf(i):
            return psb[i][:].bitcast(BF16)

        def ld(key, dst, src, eng="sp", res=None, **kw):
            P.dma(eng, key, lambda e: e.dma_start(out=dst, in_=src, **kw), writes=[res or key])

        ld("c_ident", ident_f[:], ident_d[:, :])
        ld("c_cneg", cneg[:], cneg_d[:, :])
        ld("c_sel", sel_f[:], sel_d[:, :])
        ld("c_gkv", gkv[:], gkv_d[:, :])
        ld("c_b31", b31[:], b31_d[:, :])
        ld("c_br", brb[:], br_d[:, :])
        ld("c_wr", wr_f[:].rearrange("p k c -> p (k c)"), wr_d[:, :])
        ld("c_g0", gpk[:, 0:8], gat_d[:, :])
        ld("c_g1", gpk[:, 8:16], gff_d[:, :])
        ld("c_g2", gpk[:, 16:24], gpl_d[:, :])
        ld("c_smask", smask[:], smask_d[:, :], eng="pool")
        ld("c_uinc", uinc[:], uinc_d[:, :], eng="pool")
        ld("c_wuk", wuk[:].rearrange("p k c -> p (k c)"), wuk_d[:, :], eng="pool")
        ld("c_wuv", wuv[:], wuv_d[:, :], eng="pool")
        ld("c_widx", widx_w[:].rearrange("p k c -> p (k c)"), widx_d[:, :], eng="pool")
        P.op("dve", lambda e: e.tensor_copy(out=ident_b[:], in_=ident_f[:]), reads=["c_ident"], writes=["ident_b"])
        P.op("dve", lambda e: e.memset(ones_b[:], 1.0), writes=["ones_b"])
        P.op("dve", lambda e: e.memset(tiny[:], 0.0), writes=["tiny"])
        def build_gB(gi):
            for k in range(8):
                P.op("dve", lambda e, gi=gi, k=k: e.tensor_scalar(out=gB1[:, k, :], in0=ones_b[:], scalar1=gpk[:, gi * 8 + k:gi * 8 + k + 1],
                                                                  scalar2=None, op0=ALU.mult),
                     reads=["ones_b", "c_g%d" % gi], writes=["gB"])

        evac_rr = [0]

        def evac(out, in_, reads, writes, scale=None, eng=None):
            if eng is None:
                eng = ("act", "dve")[evac_rr[0] % 2]
                evac_rr[0] += 1
            if eng == "act":
                if scale is None:
                    P.op("act", lambda e: e.activation(out=out, in_=in_, func=AF.Copy), reads=reads, writes=writes)
                else:
                    P.op("act", lambda e: e.activation(out=out, in_=in_, func=AF.Copy, scale=float(scale)), reads=reads, writes=writes)
            else:
                if scale is None:
                    P.op("dve", lambda e: e.tensor_copy(out=out, in_=in_), reads=reads, writes=writes)
                else:
                    P.op("dve", lambda e: e.tensor_scalar(out=out, in0=in_, scalar1=float(scale), scalar2=None, op0=ALU.mult), reads=reads, writes=writes)

        def phase_barrier():
            P.barrier(lambda e: e.memset(tiny[:, 0:1], 0.0))

        def norm_transpose(b, src, gi, dstT, dstres, xt_bufs, xn_bufs, ps_banks, f32_side=None):
            for i in range(NT):
                sl = i % 2
                if src == "x":
                    xt = xt_bufs[sl]
                    xres = "xt%d" % sl
                    P.dma("sp", xres, lambda e, xt=xt, i=i: e.dma_start(out=xt, in_=x_d[b, i * 128:(i + 1) * 128, :]), writes=[xres])
                else:
                    xt = x1[:, i, :]
                    xres = ("x1", i)
                xn = xn_bufs[sl]
                xnres = "xn%d" % sl
                ssc = small[:, i:i + 1]
                rsc = small[:, 16 + i:17 + i]
                P.op("act", lambda e, xt=xt, xn=xn, ssc=ssc: e.activation(out=xn, in_=xt, func=AF.Square, accum_out=ssc),
                     reads=[xres], writes=[xnres, ("ss", i)])
                P.op("dve", lambda e, ssc=ssc, rsc=rsc: e.tensor_scalar(out=rsc, in0=ssc, scalar1=1.0 / D, scalar2=EPS, op0=ALU.mult, op1=ALU.add),
                     reads=[("ss", i)], writes=[("rs", i)])
                P.op("act", lambda e, rsc=rsc: e.activation(out=rsc, in_=rsc, func=AF.Sqrt), reads=[("rs", i)], writes=[("rs", i)])
                P.op("dve", lambda e, rsc=rsc: e.reciprocal(out=rsc, in_=rsc), reads=[("rs", i)], writes=[("rs", i)])
                if f32_side is None:
                    P.op("dve", lambda e, xt=xt, xn=xn, rsc=rsc: e.tensor_scalar(out=xn, in0=xt, scalar1=rsc, scalar2=None, op0=ALU.mult),
                         reads=[xres, ("rs", i)], writes=[xnres])
                    pb = ps_banks[i % len(ps_banks)]
                    pres = ("ps", pb)
                    pv = psbf(pb)[:, 0:1024].rearrange("p (k t) -> p k t", k=8)
                    for k in range(8):
                        P.op("pe", lambda e, pv=pv, xn=xn, k=k: e.transpose(out=pv[:, k, :], in_=xn[:, k * 128:(k + 1) * 128], identity=ident_b[:]),
                             reads=[xnres, "ident_b"], writes=[pres])
                    P.op("dve", lambda e, pv=pv, i=i: e.tensor_tensor(out=dstT[:, :, i * 128:(i + 1) * 128], in0=pv, in1=gB[gi][:], op=ALU.mult),
                         reads=[pres, "gB"], writes=[(dstres, i // 4)])
                else:
                    f32_side(i, xt, xres, rsc, xn, xnres)

        last_out_tokens = []
        for b in range(NB):
            xt_bufs = [dD(0, 4 * KB, F32), dD(4 * KB, 4 * KB, F32)]
            xn_bufs = [dD(8 * KB, 2 * KB, BF16), dD(10 * KB, 2 * KB, BF16)]
            w1 = dD(12 * KB, 20 * KB, BF16, "p (k c) -> p k c", k=8)
            ckv_raw = dD(32 * KB, 8 * KB, F32)
            sqb = [dD(40 * KB, 1 * KB, BF16), dD(41 * KB, 1 * KB, BF16)]
            rstd_b = [dD(42 * KB, 2 * KB, F32), dD(44 * KB, 2 * KB, F32)]
            P.dma("pool", "w1", lambda e: e.dma_start(out=w1.rearrange("p k c -> p (k c)"), in_=w1_d[:, :], max_dma_last_dim=8192), writes=["w1"])
            build_gB(0)
            norm_transpose(b, "x", 0, hT, "hT", xt_bufs, xn_bufs, [6, 7])

            if stop_after == "P0":
                break
            bank_rr = [0]

            def nb(banks):
                v = banks[bank_rr[0] % len(banks)]
                bank_rr[0] += 1
                return v

            def proj_T(w, wres, cc, c, banks):
                pb = nb(banks)
                for k in range(8):
                    P.op("pe", lambda e, pb=pb, k=k: e.matmul(PS(pb)[:, :], lhsT=w[:, k, cc * 128:(cc + 1) * 128], rhs=hT[:, k, c * 512:(c + 1) * 512],
                                                             start=(k == 0), stop=(k == 7)),
                         reads=[wres, ("hT", c)], writes=[("ps", pb)])
                return pb

            for cc in range(10):
                for c in range(NCH):
                    pb = proj_T(w1, "w1", cc, c, [0, 1, 2, 3])
                    cs = slice(c * 512, (c + 1) * 512)
                    if cc < 4:
                        evac(qaT[:, cc, cs], PS(pb)[:, :], [("ps", pb)], [("qaT", c)])
                    elif cc < 8:
                        evac(qiT[:, cc - 4, cs], PS(pb)[:, :], [("ps", pb)], [("qiT", c)])
                    elif cc == 8:
                        evac(ckv_raw[:, cs], PS(pb)[:, :], [("ps", pb)], [("ckv_raw", c)])
                    else:
                        evac(kiT[:, cs], PS(pb)[:, :], [("ps", pb)], [("kiT", c)])
            for c in range(NCH):
                cs = slice(c * 512, (c + 1) * 512)
                sq = sqb[c % 2]
                rb = rstd_b[c % 2]
                P.op("act", lambda e, sq=sq, cs=cs: e.activation(out=sq, in_=ckv_raw[:, cs], func=AF.Square), reads=[("ckv_raw", c)], writes=[("sq", c % 2)])
                pb = nb([0, 1, 2, 3])
                P.op("pe", lambda e, pb=pb, sq=sq: e.matmul(PS(pb)[:, :], lhsT=ones_b[:], rhs=sq, start=True, stop=True),
                     reads=[("sq", c % 2), "ones_b"], writes=[("ps", pb)])
                P.op("dve", lambda e, pb=pb, rb=rb: e.tensor_scalar(out=rb, in0=PS(pb)[:, :], scalar1=1.0 / 128, scalar2=EPS, op0=ALU.mult, op1=ALU.add),
                     reads=[("ps", pb)], writes=[("rb", c % 2)])
                P.op("act", lambda e, rb=rb: e.activation(out=rb, in_=rb, func=AF.Sqrt), reads=[("rb", c % 2)], writes=[("rb", c % 2)])
                P.op("dve", lambda e, rb=rb: e.reciprocal(out=rb, in_=rb), reads=[("rb", c % 2)], writes=[("rb", c % 2)])
                P.op("dve", lambda e, rb=rb, cs=cs: e.scalar_tensor_tensor(out=ckvT[:, cs], in0=ckv_raw[:, cs], scalar=gkv[:, 0:1], in1=rb, op0=ALU.mult, op1=ALU.mult),
                     reads=[("ckv_raw", c), ("rb", c % 2), "c_gkv"], writes=[("ckvT", c)])
            pbw = 4
            for i in range(NT):
                for k in range(8):
                    P.op("pe", lambda e, i=i, k=k: e.matmul(PS(pbw)[:, i * 8:(i + 1) * 8], lhsT=hT[:, k, i * 128:(i + 1) * 128], rhs=widx_w[:, k, :],
                                                         start=(k == 0), stop=(k == 7)),
                         reads=[("hT", i // 4), "c_widx"], writes=[("ps", pbw)])
            P.op("dve", lambda e: e.tensor_scalar(out=widx_tm.rearrange("p i h -> p (i h)"), in0=PS(pbw)[:, 0:128], scalar1=IDX_SCALE, scalar2=None, op0=ALU.mult),
                 reads=[("ps", pbw)], writes=["widx_tm"])
            P.op("pool", lambda e: e.memset(Vp.rearrange("p j m c -> p (j m) c")[:, :, 64:65], 1.0), writes=["Vp"])
            P.op("pool", lambda e: e.memset(Vp.rearrange("p j m c -> p (j m) c")[:, :, 129:130], 1.0), writes=["Vp"])
            for j in range(NT):
                pb = nb([0, 1, 2, 3])
                P.op("pe", lambda e, pb=pb, j=j: e.matmul(PS(pb)[:, :], lhsT=ckvT[:, j * 128:(j + 1) * 128], rhs=wuv[:], start=True, stop=True),
                     reads=[("ckvT", j // 4), "c_wuv"], writes=[("ps", pb)])
                pv = PS(pb)[:, :].rearrange("p (m h d) -> p m h d", m=4, h=2)
                evac(Vp[:, j, :, 0:64], pv[:, :, 0, :], [("ps", pb)], ["Vp"])
                evac(Vp[:, j, :, 65:129], pv[:, :, 1, :], [("ps", pb)], ["Vp"])
            phase_barrier()
            if stop_after == "P1":
                break

            score = dD(0, 8 * KB, F32)
            junk = dD(8 * KB, 4 * KB, BF16)
            mask_tm = dD(12 * KB, 4 * KB, BF16)
            maskT = dD(16 * KB, 16 * KB, BF16, "p (j t) -> p j t", j=16)
            EB = dD(32 * KB, 10 * KB, BF16, "p (h u) -> p h u", h=8)
            rbuf = [dD(42 * KB + i * KB, 1 * KB, BF16) for i in range(4)]
            Pbuf = [dD(46 * KB + i * KB, 1 * KB, BF16) for i in range(4)]
            qabs = [dD(50 * KB + i * KB, 1 * KB, BF16) for i in range(2)]
            dg = dD(52 * KB, 2 * KB, BF16, "p (h t) -> p h t", h=8)
            o_f32 = dD(54 * KB, 2 * KB, F32)
            rec = dD(56 * KB, 2 * KB, F32)
            btmp = dD(0, 10 * KB, F32, "p (h u) -> p h u", h=4)
            P.dma("sp", "btoep", lambda e: e.dma_start(out=btmp.rearrange("p h u -> p (h u)")[:, 0:2560], in_=btoep_d[:, 0:2560]), writes=["btmp"])
            P.op("act", lambda e: e.activation(out=EB[:, 0:4, :], in_=btmp[:, 0:4, :], func=AF.Exp), reads=["btmp"], writes=["EB"])
            P.dma("sp", "btoep", lambda e: e.dma_start(out=btmp.rearrange("p h u -> p (h u)")[:, 0:2560], in_=btoep_d[:, 2560:5120]), writes=["btmp"])
            P.op("act", lambda e: e.activation(out=EB[:, 4:8, :], in_=btmp[:, 0:4, :], func=AF.Exp), reads=["btmp"], writes=["EB"])
            phase_barrier()

            LO, WD, MID, CNT, TMP = 40, 41, 42, 43, 44
            rr = [0]
            for c in range(NCH):
                for tl in range(4):
                    i = 4 * c + tl
                    L = (i + 1) * 128
                    ts_ = slice(i * 128, (i + 1) * 128)
                    for h in range(8):
                        P.op("dve", lambda e, h=h, i=i: e.tensor_scalar(out=dg[:, h, :], in0=ident_b[:], scalar1=widx_tm[:, i, h:h + 1], scalar2=None, op0=ALU.mult),
                             reads=["ident_b", "widx_tm"], writes=[("dg", h)])
                    nsc = (L + 511) // 512
                    for sc in range(nsc):
                        ws = min(512, L - sc * 512)
                        spb = 4 + (sc % 2)
                        for h in range(8):
                            bp = (h % 2) * 64
                            zb = nb([0, 1, 2, 3])
                            P.op("pe", lambda e, zb=zb, h=h, bp=bp, ts_=ts_, sc=sc, ws=ws: e.matmul(
                                PS(zb)[:, 0:ws], lhsT=qiT[bp:bp + 64, h // 2, ts_], rhs=kiT[bp:bp + 64, sc * 512:sc * 512 + ws], start=True, stop=True),
                                reads=[("qiT", c), ("kiT", sc)], writes=[("ps", zb)])
                            rs = rr[0] % 4
                            rr[0] += 1
                            if rs % 2 == 0:
                                P.op("act", lambda e, zb=zb, rs=rs, ws=ws: e.activation(out=rbuf[rs][:, 0:ws], in_=PS(zb)[:, 0:ws], func=AF.Relu),
                                     reads=[("ps", zb)], writes=[("rbuf", rs)])
                            else:
                                P.op("dve", lambda e, zb=zb, rs=rs, ws=ws: e.tensor_scalar(out=rbuf[rs][:, 0:ws], in0=PS(zb)[:, 0:ws], scalar1=0.0, scalar2=None, op0=ALU.max),
                                     reads=[("ps", zb)], writes=[("rbuf", rs)])
                            P.op("pe", lambda e, spb=spb, h=h, rs=rs, ws=ws: e.matmul(PS(spb)[:, 0:ws], lhsT=dg[:, h, :], rhs=rbuf[rs][:, 0:ws], start=(h == 0), stop=(h == 7)),
                                 reads=[("dg", h), ("rbuf", rs)], writes=[("ps", spb)])
                        last = (sc == nsc - 1)
                        wcopy = ws - 128 if last else ws
                        if wcopy > 0:
                            P.op("act", lambda e, spb=spb, sc=sc, wcopy=wcopy: e.activation(out=score[:, sc * 512:sc * 512 + wcopy], in_=PS(spb)[:, 0:wcopy], func=AF.Copy),
                                 reads=[("ps", spb)], writes=["score"])
                        if last:
                            P.op("dve", lambda e, spb=spb, sc=sc, ws=ws: e.tensor_tensor(out=score[:, sc * 512 + ws - 128:sc * 512 + ws], in0=PS(spb)[:, ws - 128:ws], in1=cneg[:], op=ALU.add),
                                 reads=[("ps", spb), "c_cneg"], writes=["score"])
                    sm = lambda col: small[:, col:col + 1]
                    if i < 2:
                        P.op("dve", lambda e: e.memset(sm(LO), -1.0e29), writes=["lo"])
                    else:
                        P.op("dve", lambda e, i=i: e.tensor_reduce(out=sm(LO), in_=score[:, 0:i * 128], axis=AX.X, op=ALU.min), reads=["score"], writes=["lo"])
                        P.op("dve", lambda e, L=L: e.tensor_reduce(out=sm(WD), in_=score[:, 0:L], axis=AX.X, op=ALU.max), reads=["score"], writes=["wd"])
                        P.op("dve", lambda e: e.tensor_tensor(out=sm(WD), in0=sm(WD), in1=sm(LO), op=ALU.subtract), reads=["wd", "lo"], writes=["wd"])
                        for it in range(N_BISECT):
                            f = 0.5 ** (it + 1)
                            P.op("dve", lambda e, f=f: e.tensor_scalar(out=sm(MID), in0=sm(WD), scalar1=f, scalar2=sm(LO), op0=ALU.mult, op1=ALU.add),
                                 reads=["wd", "lo"], writes=["mid"])
                            P.op("dve", lambda e, L=L: e.tensor_scalar(out=junk[:, 0:L], in0=score[:, 0:L], scalar1=sm(MID), scalar2=None, op0=ALU.is_ge, op1=ALU.add, accum_out=sm(CNT)),
                                 reads=["score", "mid"], writes=["junk", "cnt"])
                            P.op("dve", lambda e, f=f: e.tensor_scalar(out=sm(TMP), in0=sm(CNT), scalar1=255.5, scalar2=f, op0=ALU.is_ge, op1=ALU.mult),
                                 reads=["cnt"], writes=["tmp"])
                            P.op("dve", lambda e: e.scalar_tensor_tensor(out=sm(LO), in0=sm(TMP), scalar=sm(WD), in1=sm(LO), op0=ALU.mult, op1=ALU.add),
                                 reads=["tmp", "wd", "lo"], writes=["lo"])
                    P.op("dve", lambda e, L=L: e.tensor_scalar(out=mask_tm[:, 0:L], in0=score[:, 0:L], scalar1=sm(LO), scalar2=None, op0=ALU.is_ge),
                         reads=["score", "lo"], writes=["mask_tm"])
                    for j0 in range(0, i + 1, 8):
                        n = min(8, i + 1 - j0)
                        tb = 6 + ((j0 // 8) % 2)
                        pv = psbf(tb)[:, 0:1024].rearrange("p (k t) -> p k t", k=8)
                        for jj in range(n):
                            j = j0 + jj
                            P.op("pe", lambda e, pv=pv, jj=jj, j=j: e.transpose(out=pv[:, jj, :], in_=mask_tm[:, j * 128:(j + 1) * 128], identity=ident_b[:]),
                                 reads=["mask_tm", "ident_b"], writes=[("ps", tb)])
                        evac(maskT[:, j0:j0 + n, tl * 128:(tl + 1) * 128], pv[:, 0:n, :], [("ps", tb)], [("maskT", tl)])
                cs0 = c * 512
                for h in range(8):
                    bp = (h % 2) * 64
                    m = h // 2
                    qs = h % 2
                    qb_ = nb([0, 1, 2, 3])
                    P.op("pe", lambda e, qb_=qb_, bp=bp, m=m: e.matmul(PS(qb_)[:, :], lhsT=wuk[bp:bp + 64, m, :], rhs=qaT[bp:bp + 64, m, cs0:cs0 + 512], start=True, stop=True),
                         reads=["c_wuk", ("qaT", c)], writes=[("ps", qb_)])
                    evac(qabs[qs], PS(qb_)[:, :], [("ps", qb_)], [("qabs", qs)])
                    ob = 4 + (h % 2)
                    jmax = 4 * c + 3
                    for j in range(jmax + 1):
                        col0 = max(0, j - 4 * c) * 128
                        N = 512 - col0
                        near = j >= 4 * c - 1
                        lb = nb([0, 1, 2, 3])
                        P.op("pe", lambda e, lb=lb, j=j, qs=qs, col0=col0, N=N: e.matmul(PS(lb)[:, 0:N], lhsT=ckvT[:, j * 128:(j + 1) * 128], rhs=qabs[qs][:, col0:512], start=True, stop=True),
                             reads=[("ckvT", j // 4), ("qabs", qs)], writes=[("ps", lb)])
                        pi = rr[0] % 4
                        rr[0] += 1
                        Pt = Pbuf[pi]
                        if near:
                            P.op("act", lambda e, lb=lb, Pt=Pt, N=N: e.activation(out=Pt[:, 0:N], in_=PS(lb)[:, 0:N], func=AF.Exp, scale=ATTN_SCALE),
                                 reads=[("ps", lb)], writes=[("Pbuf", pi)])
                        else:
                            P.op("act", lambda e, lb=lb, Pt=Pt, N=N, h=h: e.activation(out=Pt[:, 0:N], in_=PS(lb)[:, 0:N], func=AF.Exp, scale=ATTN_SCALE, bias=b31[:, h:h + 1]),
                                 reads=[("ps", lb), "c_b31"], writes=[("Pbuf", pi)])
                        P.op("dve", lambda e, Pt=Pt, N=N, j=j, col0=col0: e.tensor_tensor(out=Pt[:, 0:N], in0=Pt[:, 0:N], in1=maskT[:, j, col0:512], op=ALU.mult),
                             reads=[("Pbuf", pi)] + [("maskT", t) for t in range(col0 // 128, 4)], writes=[("Pbuf", pi)])
                        if near:
                            u0 = cs0 + col0 - 128 * j
                            P.op("dve", lambda e, Pt=Pt, N=N, h=h, u0=u0: e.tensor_tensor(out=Pt[:, 0:N], in0=Pt[:, 0:N], in1=EB[:, h, u0:u0 + N], op=ALU.mult),
                                 reads=[("Pbuf", pi), "EB"], writes=[("Pbuf", pi)])
                        w0 = 0 if h % 2 == 0 else 1
                        P.op("pe", lambda e, ob=ob, j=j, m=m, w0=w0, Pt=Pt, col0=col0, N=N, jmax=jmax: e.matmul(PS(ob)[:, col0:512], lhsT=Vp[:, j, m, w0:w0 + 128], rhs=Pt[:, 0:N],
                                                                                                         start=(j == 0), stop=(j == jmax)),
                             reads=["Vp", ("Pbuf", pi)], writes=[("ps", ob)])
                    P.op("act", lambda e, ob=ob: e.activation(out=o_f32, in_=PS(ob)[:, :], func=AF.Copy), reads=[("ps", ob)], writes=["o_f32"])
                    db = nb([0, 1, 2, 3])
                    P.op("pe", lambda e, db=db, h=h: e.matmul(PS(db)[:, :], lhsT=sel_f[:, (h % 2) * 128:(h % 2) * 128 + 128], rhs=o_f32, start=True, stop=True),
                         reads=["c_sel", "o_f32"], writes=[("ps", db)])
                    P.op("dve", lambda e, db=db, bp=bp: e.reciprocal(out=rec[bp:bp + 64, :], in_=PS(db)[bp:bp + 64, :]), reads=[("ps", db)], writes=["rec"])
                    P.op("dve", lambda e, bp=bp, m=m: e.tensor_tensor(out=oaT[bp:bp + 64, m, cs0:cs0 + 512], in0=o_f32[bp:bp + 64, :], in1=rec[bp:bp + 64, :], op=ALU.mult),
                         reads=["o_f32", "rec"], writes=[("oaT", c)])
            phase_barrier()
            if stop_after == "P2":
                break

            w3 = dD(0, 24 * KB, BF16, "p (k c) -> p k c", k=8)
            ebuf = [dD(24 * KB + i * 2 * KB, 2 * KB, F32) for i in range(2)]
            spbuf = [dD(28 * KB + i * KB, 1 * KB, BF16) for i in range(3)]
            Sacc = dD(31 * KB, 2 * KB, F32)
            Sbf = [dD(33 * KB + i * KB, 1 * KB, BF16) for i in range(2)]
            abuf = [dD(35 * KB + i * KB, 1 * KB, BF16) for i in range(3)]
            P.dma("pool", "w3", lambda e: e.dma_start(out=w3.rearrange("p k c -> p (k c)"), in_=w3_d[:, :], max_dma_last_dim=8192), writes=["w3"])
            if stop_after == "P3w":
                break
            for cc in range(8):
                for c in range(NCH):
                    pb = proj_T(w3, "w3", cc, c, [0, 1, 2, 3])
                    cs = slice(c * 512, (c + 1) * 512)
                    if cc < 4:
                        evac(qbT[:, cc, cs], PS(pb)[:, :], [("ps", pb)], [("qbT", c)])
                        P.op("dve", lambda e, cc=cc, cs=cs: e.tensor_scalar(out=qbTn[:, cc, cs], in0=qbT[:, cc, cs], scalar1=-ATTN_SCALE, scalar2=None, op0=ALU.mult),
                             reads=[("qbT", c)], writes=[("qbTn", c)])
                    else:
                        evac(kbT[:, cc - 4, cs], PS(pb)[:, :], [("ps", pb)], [("kbT", c)])
            if stop_after == "P3q":
                break
            for j in range(NT):
                pb = nb([0, 1, 2, 3])
                for k in range(8):
                    P.op("pe", lambda e, pb=pb, j=j, k=k: e.matmul(PS(pb)[:, :], lhsT=hT[:, k, j * 128:(j + 1) * 128], rhs=w3[:, k, 1024:1536], start=(k == 0), stop=(k == 7)),
                         reads=["w3", ("hT", j // 4)], writes=[("ps", pb)])
                evac(vb[:, j, :], PS(pb)[:, :], [("ps", pb)], [("vb", j // 4)])
            if stop_after == "P3a":
                break
            for c in range(NCH if stop_after != "P3b" else 1):
                cs0 = c * 512
                for h in range(8 if stop_after != "P3b" else 1):
                    bp = (h % 2) * 64
                    m = h // 2
                    ob = 6 + (h % 2)
                    jmax = 4 * c + 3
                    si = 0
                    for j in range(jmax, -1, -1):
                        col0 = max(0, j - 4 * c) * 128
                        N = 512 - col0
                        diag = j >= 4 * c
                        zb = nb([0, 1, 2])
                        P.op("pe", lambda e, zb=zb, bp=bp, m=m, j=j, col0=col0, N=N: e.matmul(PS(zb)[:, 0:N], lhsT=kbT[bp:bp + 64, m, j * 128:(j + 1) * 128],
                                                                                          rhs=qbT[bp:bp + 64, m, cs0 + col0:cs0 + 512], start=True, stop=True),
                             reads=[("kbT", j // 4), ("qbT", c)], writes=[("ps", zb)])
                        ei = rr[0] % 2
                        si3 = rr[0] % 3
                        rr[0] += 1
                        eb_, spt, at = ebuf[ei], spbuf[si3], abuf[si3]
                        P.op("act", lambda e, zb=zb, eb_=eb_, N=N: e.activation(out=eb_[:, 0:N], in_=PS(zb)[:, 0:N], func=AF.Exp, scale=ATTN_SCALE),
                             reads=[("ps", zb)], writes=[("ebuf", ei)])
                        P.op("act", lambda e, eb_=eb_, spt=spt, N=N: e.activation(out=spt[:, 0:N], in_=eb_[:, 0:N], func=(AF.Ln if SBLN else AF.Copy), bias=(1.0 if SBLN else 0.0), scale=1.0),
                             reads=[("ebuf", ei)], writes=[("spbuf", si3)])
                        if diag:
                            P.op(SBE, lambda e, spt=spt: e.tensor_tensor(out=spt[:, 0:128], in0=spt[:, 0:128], in1=smask[:], op=ALU.mult),
                                 reads=[("spbuf", si3), "c_smask"], writes=[("spbuf", si3)])
                        xb = 3 + (rr[0] % 2)
                        first = (j == jmax)
                        P.op("pe", lambda e, xb=xb, spt=spt, N=N: e.matmul(PS(xb)[:, 0:N], lhsT=uinc[:], rhs=spt[:, 0:N], start=True, stop=False),
                             reads=["c_uinc", ("spbuf", si3)], writes=[("ps", xb)])
                        if not first:
                            sbi = si % 2
                            P.op("pe", lambda e, xb=xb, sbi=sbi, col0=col0, N=N: e.matmul(PS(xb)[:, 0:N], lhsT=ones_b[:], rhs=Sbf[sbi][:, col0:512], start=False, stop=False),
                                 reads=["ones_b", ("Sbf", sbi)], writes=[("ps", xb)])
                        P.op("pe", lambda e, xb=xb, bp=bp, m=m, j=j, col0=col0, N=N: e.matmul(PS(xb)[:, 0:N], lhsT=kbT[bp:bp + 64, m, j * 128:(j + 1) * 128],
                                                                                          rhs=qbTn[bp:bp + 64, m, cs0 + col0:cs0 + 512], start=False, stop=True),
                             reads=[("kbT", j // 4), ("qbTn", c)], writes=[("ps", xb)])
                        P.op("act", lambda e, xb=xb, at=at, N=N: e.activation(out=at[:, 0:N], in_=PS(xb)[:, 0:N], func=AF.Exp, scale=-1.0),
                             reads=[("ps", xb)], writes=[("abuf", si3)])
                        if diag:
                            P.op("dve", lambda e, at=at: e.tensor_tensor(out=at[:, 0:128], in0=at[:, 0:128], in1=smask[:], op=ALU.mult),
                                 reads=[("abuf", si3), "c_smask"], writes=[("abuf", si3)])
                        P.op("pe", lambda e, ob=ob, j=j, m=m, at=at, col0=col0, N=N, first=first: e.matmul(PS(ob)[:, col0:512], lhsT=vb[:, j, m * 128:(m + 1) * 128], rhs=at[:, 0:N],
                                                                                                   start=first, stop=(j == 0), skip_group_check=True),
                             reads=[("vb", j // 4), ("abuf", si3)], writes=[("ps", ob)])
                        if j > 0:
                            si += 1
                            sbi = si % 2
                            if first:
                                P.op(SBE, lambda e: e.memset(Sacc[:, :], 0.0), writes=["Sacc"])
                            P.op(SBE, lambda e, spt=spt, col0=col0, N=N: e.tensor_tensor(out=Sacc[:, col0:512], in0=Sacc[:, col0:512], in1=spt[:, 0:N], op=ALU.add),
                                 reads=["Sacc", ("spbuf", si3)], writes=["Sacc"])
                            P.op(SBE, lambda e, sbi=sbi: e.tensor_copy(out=Sbf[sbi][:, :], in_=Sacc[:, :]), reads=["Sacc"], writes=[("Sbf", sbi)])
                    evac(obT[bp:bp + 64, m, cs0:cs0 + 512], PS(ob)[bp:bp + 64, :], [("ps", ob)], [("obT", c)])
            phase_barrier()
            if dbg:
                dtmp = dD(40 * KB, 16 * KB, F32)
                for nm, src, res in (("d_oaT", oaT, "oaT"), ("d_obT", obT, "obT")):
                    for q in range(2):
                        P.op("dve", lambda e, src=src, q=q: e.tensor_copy(out=dtmp, in_=src.rearrange("p k t -> p (k t)")[:, q * 4096:(q + 1) * 4096]),
                             reads=[(res, cq) for cq in range(4)], writes=["dtmp"])
                        if b == 0:
                            P.dma("sp", "dbg", lambda e, nm=nm, q=q: e.dma_start(out=dbg_d[nm][:, q * 4096:(q + 1) * 4096], in_=dtmp), reads=["dtmp"])
                phase_barrier()
            if stop_after == "P3":
                break

            mergedT = dD(0, 32 * KB, BF16, "p (k t) -> p k t", k=8)
            wout = dD(32 * KB, 16 * KB, BF16, "p (k c) -> p k c", k=8)
            wg4 = [dD(48 * KB + i * 4 * KB, 4 * KB, BF16, "p (k c) -> p k c", k=8) for i in range(2)]
            wbr = [dD(56 * KB + i * 2 * KB, 2 * KB, BF16, "p (a k c) -> p a k c", a=2, k=4) for i in range(2)]
            sgb_ = [dD(60 * KB + i * KB, 1 * KB, BF16) for i in range(2)]
            t12 = [dD(62 * KB + i * 2 * KB, 2 * KB, F32) for i in range(2)]
            P.dma("pool", "wout", lambda e: e.dma_start(out=wout.rearrange("p k c -> p (k c)"), in_=wout_d[:, :], max_dma_last_dim=8192), writes=["wout"])
            for m in range(8):
                wsl = m % 2
                P.dma("pool", "wg4_%d" % wsl, lambda e, m=m, wsl=wsl: e.dma_start(out=wg4[wsl].rearrange("p k c -> p (k c)"), in_=wg4_d[:, m * 2048:(m + 1) * 2048], max_dma_last_dim=8192),
                      writes=[("wg4", wsl)])
                P.dma("pool", "wbr_%d" % wsl, lambda e, m=m, wsl=wsl: e.dma_start(out=wbr[wsl].rearrange("p a k c -> p (a k c)"), in_=wbr_d[:, m * 1024:(m + 1) * 1024], max_dma_last_dim=8192),
                      writes=[("wbr", wsl)])
                for c in range(NCH):
                    cs = slice(c * 512, (c + 1) * 512)
                    banks = {}
                    for gi_, nm in enumerate(("ga", "gb")):
                        pb = nb([0, 1, 2, 3, 4, 5, 6, 7])
                        banks[nm] = pb
                        for k in range(8):
                            P.op("pe", lambda e, pb=pb, k=k, gi_=gi_, wsl=wsl, cs=cs: e.matmul(PS(pb)[:, :], lhsT=wg4[wsl][:, k, gi_ * 128:(gi_ + 1) * 128], rhs=hT[:, k, cs],
                                                                                        start=(k == 0), stop=(k == 7)),
                                 reads=[("wg4", wsl), ("hT", c)], writes=[("ps", pb)])
                    for a_, (nm, oT, ores) in enumerate((("ya", oaT, "oaT"), ("yb", obT, "obT"))):
                        pb = nb([0, 1, 2, 3, 4, 5, 6, 7])
                        banks[nm] = pb
                        for k in range(4):
                            P.op("pe", lambda e, pb=pb, k=k, a_=a_, wsl=wsl, oT=oT, cs=cs: e.matmul(PS(pb)[:, :], lhsT=wbr[wsl][:, a_, k, :], rhs=oT[:, k, cs], start=(k == 0), stop=(k == 3)),
                                 reads=[("wbr", wsl), (ores, c)], writes=[("ps", pb)])
                    i0 = 0
                    sa, sb_ = sgb_[i0], sgb_[i0 + 1]
                    P.op("act", lambda e, sa=sa, pb=banks["ga"]: e.activation(out=sa, in_=PS(pb)[:, :], func=AF.Sigmoid), reads=[("ps", banks["ga"])], writes=[("sg", i0)])
                    P.op("act", lambda e, sb_=sb_, pb=banks["gb"]: e.activation(out=sb_, in_=PS(pb)[:, :], func=AF.Sigmoid), reads=[("ps", banks["gb"])], writes=[("sg", i0 + 1)])
                    P.op("dve", lambda e, sa=sa, pb=banks["ya"]: e.tensor_tensor(out=t12[0], in0=sa, in1=PS(pb)[:, :], op=ALU.mult), reads=[("sg", i0), ("ps", banks["ya"])], writes=["t1"])
                    P.op("dve", lambda e, sb_=sb_, pb=banks["yb"]: e.tensor_tensor(out=t12[1], in0=sb_, in1=PS(pb)[:, :], op=ALU.mult), reads=[("sg", i0 + 1), ("ps", banks["yb"])], writes=["t2"])
                    P.op("dve", lambda e, m=m, cs=cs: e.tensor_tensor(out=mergedT[:, m, cs], in0=t12[0], in1=t12[1], op=ALU.add), reads=["t1", "t2"], writes=[("mergedT", c)])
            phase_barrier()
            for i in range(NT):
                P.dma("sp", "x1ld%d" % (i % 4), lambda e, i=i: e.dma_start(out=x1[:, i, :], in_=x_d[b, i * 128:(i + 1) * 128, :]), writes=[("x1", i)])
                pb0 = (i % 4) * 2
                for half in range(2):
                    pb = pb0 + half
                    for k in range(8):
                        P.op("pe", lambda e, pb=pb, k=k, i=i, half=half: e.matmul(PS(pb)[:, :], lhsT=mergedT[:, k, i * 128:(i + 1) * 128], rhs=wout[:, k, half * 512:(half + 1) * 512],
                                                                               start=(k == 0), stop=(k == 7)),
                             reads=[("mergedT", i // 4), "wout"], writes=[("ps", pb)])
                    P.op("dve", lambda e, pb=pb, i=i, half=half: e.tensor_tensor(out=x1[:, i, half * 512:(half + 1) * 512], in0=x1[:, i, half * 512:(half + 1) * 512], in1=PS(pb)[:, :], op=ALU.add),
                         reads=[("ps", pb), ("x1", i)], writes=[("x1", i)])
            phase_barrier()
            if dbg and b == 0:
                P.dma("sp", "dbg", lambda e: e.dma_start(out=dbg_d["d_x1"][:, :], in_=x1.rearrange("p i d -> p (i d)")), reads=[("x1", i) for i in range(NT)])
                phase_barrier()
            if stop_after == "P4":
                break

            h2T = hT
            xnf = dD(0, 4 * KB, F32)
            hTf = dD(4 * KB, 4 * KB, F32, "p (k t) -> p k t", k=8)
            comb = dD(8 * KB, 2 * KB, F32, "p (i e) -> p i e", i=16)
            elm = dD(10 * KB, 256, F32)
            m8 = dD(10 * KB + 256, 64, F32)
            eq = dD(10 * KB + 320, 256, F32)
            sgm = [dD(12 * KB + i * KB, 1 * KB, BF16) for i in range(4)]
            hid = [dD(16 * KB + i * 2 * KB, 2 * KB, BF16, "p (f t) -> p f t", f=2) for i in range(2)]
            RG, RS, RW = 60, 61, 62

            def ffn_side(i, xt, xres, rsc, xn, xnres):
                P.op("dve", lambda e, xt=xt, rsc=rsc: e.tensor_scalar(out=xnf, in0=xt, scalar1=rsc, scalar2=None, op0=ALU.mult), reads=[xres, ("rs", i)], writes=["xnf"])
                for half in range(2):
                    pb = 4 + half
                    for kk in range(4):
                        k = half * 4 + kk
                        P.op("pe", lambda e, pb=pb, kk=kk, k=k: e.transpose(out=PS(pb)[:, kk * 128:(kk + 1) * 128], in_=xnf[:, k * 128:(k + 1) * 128], identity=ident_f[:]),
                             reads=["xnf", "c_ident"], writes=[("ps", pb)])
                    pv = PS(pb)[:, :].rearrange("p (k t) -> p k t", k=4)
                    gf = gpk[:, 8 + half * 4:8 + half * 4 + 4]
                    P.op("dve", lambda e, pv=pv, half=half, i=i: e.tensor_tensor(out=h2T[:, half * 4:half * 4 + 4, i * 128:(i + 1) * 128], in0=pv, in1=gB[1][:, half * 4:half * 4 + 4, :], op=ALU.mult),
                         reads=[("ps", pb), "gB"], writes=[("hT", i // 4)])
                    P.op("dve", lambda e, pv=pv, half=half, gf=gf: e.tensor_tensor(out=hTf[:, half * 4:half * 4 + 4, :], in0=pv, in1=gf.unsqueeze(2).broadcast_to([128, 4, 128]), op=ALU.mult),
                         reads=[("ps", pb), "c_g1"], writes=["hTf"])
                for k in range(8):
                    P.op("pe", lambda e, k=k: e.matmul(PS(6)[:, 0:36], lhsT=hTf[:, k, :], rhs=wr_f[:, k, :], start=(k == 0), stop=(k == 7)),
                         reads=["hTf", "c_wr"], writes=[("ps", 6)])
                sm = lambda col: small[:, col:col + 1]
                P.op("dve", lambda e: e.tensor_tensor(out=elm[:, 0:36], in0=PS(6)[:, 0:36], in1=brb[:], op=ALU.add), reads=[("ps", 6), "c_br"], writes=["elm"])
                P.op("dve", lambda e: e.tensor_reduce(out=sm(RG), in_=elm[:, 0:4], axis=AX.X, op=ALU.max), reads=["elm"], writes=["rg"])
                P.op("dve", lambda e: e.tensor_scalar(out=eq[:, 0:4], in0=elm[:, 0:4], scalar1=sm(RG), scalar2=None, op0=ALU.is_ge), reads=["elm", "rg"], writes=["eq"])
                P.op("dve", lambda e: e.tensor_scalar(out=eq[:, 4:8], in0=eq[:, 0:4], scalar1=-1.0, scalar2=-NEG, op0=ALU.add, op1=ALU.mult), reads=["eq"], writes=["eq"])
                P.op("dve", lambda e: e.tensor_tensor(out=elm[:, 4:36].rearrange("p (g x) -> p g x", g=4), in0=elm[:, 4:36].rearrange("p (g x) -> p g x", g=4),
                                                      in1=eq[:, 4:8].unsqueeze(2).broadcast_to([128, 4, 8]), op=ALU.add), reads=["elm", "eq"], writes=["elm"])
                P.op("dve", lambda e: e.tensor_scalar(out=sm(RW), in0=sm(RG), scalar1=-1.0, scalar2=None, op0=ALU.mult), reads=["rg"], writes=["rw"])
                P.op("act", lambda e: e.activation(out=eq[:, 8:12], in_=elm[:, 0:4], func=AF.Exp, bias=sm(RW), scale=1.0, accum_out=sm(RS)), reads=["elm", "rw"], writes=["eq", "rs_"])
                P.op("dve", lambda e: e.reciprocal(out=sm(RS), in_=sm(RS)), reads=["rs_"], writes=["rs_"])
                P.op("dve", lambda e: e.max(out=m8[:, 0:8], in_=elm[:, 4:36]), reads=["elm"], writes=["m8"])
                P.op("dve", lambda e: e.tensor_tensor(out=m8[:, 8:9], in0=m8[:, 1:2], in1=m8[:, 0:1], op=ALU.subtract), reads=["m8"], writes=["m8"])
                P.op("act", lambda e: e.activation(out=m8[:, 8:9], in_=m8[:, 8:9], func=AF.Exp), reads=["m8"], writes=["m8"])
                P.op("dve", lambda e: e.tensor_scalar(out=m8[:, 8:9], in0=m8[:, 8:9], scalar1=1.0, scalar2=None, op0=ALU.add), reads=["m8"], writes=["m8"])
                P.op("dve", lambda e: e.reciprocal(out=m8[:, 9:10], in_=m8[:, 8:9]), reads=["m8"], writes=["m8"])
                P.op("dve", lambda e: e.tensor_scalar(out=m8[:, 10:11], in0=m8[:, 9:10], scalar1=-1.0, scalar2=1.0, op0=ALU.mult, op1=ALU.add), reads=["m8"], writes=["m8"])
                P.op("dve", lambda e: e.tensor_tensor(out=m8[:, 9:11], in0=m8[:, 9:11], in1=sm(RS).broadcast_to([128, 2]), op=ALU.mult), reads=["m8", "rs_"], writes=["m8"])
                P.op("dve", lambda e: e.tensor_scalar(out=eq[:, 0:32], in0=elm[:, 4:36], scalar1=m8[:, 0:1], scalar2=m8[:, 9:10], op0=ALU.is_equal, op1=ALU.mult), reads=["elm", "m8"], writes=["eq"])
                P.op("dve", lambda e: e.tensor_scalar(out=eq[:, 32:64], in0=elm[:, 4:36], scalar1=m8[:, 1:2], scalar2=m8[:, 10:11], op0=ALU.is_equal, op1=ALU.mult), reads=["elm", "m8"], writes=["eq"])
                P.op("dve", lambda e, i=i: e.tensor_tensor(out=comb[:, i, :], in0=eq[:, 0:32], in1=eq[:, 32:64], op=ALU.add), reads=["eq"], writes=["comb"])

            build_gB(1)
            norm_transpose(b, "x1", 1, h2T, "hT", None, [dD(20 * KB, 2 * KB, BF16), dD(22 * KB, 2 * KB, BF16)], None, f32_side=ffn_side)
            phase_barrier()
            for ex in range(N_EXP):
                wsl = ex % 2
                wm = wmoe[wsl]
                P.dma("pool", "wmoe%d" % wsl, lambda e, wm=wm, ex=ex: e.dma_start(out=wm, in_=wmoe_d[ex, :, :], max_dma_last_dim=8192), writes=[("wmoe", wsl)])
                wgv = wm[:, 0:2048].rearrange("p (k c) -> p k c", k=8)
                wuv_ = wm[:, 2048:4096].rearrange("p (k c) -> p k c", k=8)
                wdv = wm[:, 4096:6144].rearrange("p (f c) -> p f c", f=2)
                for c in range(NCH):
                    cs = slice(c * 512, (c + 1) * 512)
                    hs = rr[0] % 2
                    rr[0] += 1
                    for f in range(2):
                        gb_, ub_ = f * 2, f * 2 + 1
                        for k in range(8):
                            P.op("pe", lambda e, gb_=gb_, k=k, f=f, wgv=wgv, cs=cs: e.matmul(PS(gb_)[:, :], lhsT=wgv[:, k, f * 128:(f + 1) * 128], rhs=h2T[:, k, cs], start=(k == 0), stop=(k == 7)),
                                 reads=[("wmoe", wsl), ("hT", c)], writes=[("ps", gb_)])
                        for k in range(8):
                            P.op("pe", lambda e, ub_=ub_, k=k, f=f, wuv_=wuv_, cs=cs: e.matmul(PS(ub_)[:, :], lhsT=wuv_[:, k, f * 128:(f + 1) * 128], rhs=h2T[:, k, cs], start=(k == 0), stop=(k == 7)),
                                 reads=[("wmoe", wsl), ("hT", c)], writes=[("ps", ub_)])
                        sgi = (hs * 2 + f)
                        P.op("act", lambda e, gb_=gb_, sgi=sgi: e.activation(out=sgm[sgi], in_=PS(gb_)[:, :], func=AF.Silu), reads=[("ps", gb_)], writes=[("sgm", sgi)])
                        P.op("dve", lambda e, ub_=ub_, sgi=sgi, hs=hs, f=f: e.tensor_tensor(out=hid[hs][:, f, :], in0=sgm[sgi], in1=PS(ub_)[:, :], op=ALU.mult),
                             reads=[("sgm", sgi), ("ps", ub_)], writes=[("hid", hs)])
                    for tl in range(4):
                        i = c * 4 + tl
                        for half in range(2):
                            pb = 4 + (rr[0] % 4)
                            rr[0] += 1
                            for f in range(2):
                                P.op("pe", lambda e, pb=pb, f=f, hs=hs, tl=tl, wdv=wdv, half=half: e.matmul(PS(pb)[:, :], lhsT=hid[hs][:, f, tl * 128:(tl + 1) * 128], rhs=wdv[:, f, half * 512:(half + 1) * 512],
                                                                                                     start=(f == 0), stop=(f == 1)),
                                     reads=[("hid", hs), ("wmoe", wsl)], writes=[("ps", pb)])
                            P.op("dve", lambda e, pb=pb, i=i, half=half, ex=ex: e.scalar_tensor_tensor(out=x1[:, i, half * 512:(half + 1) * 512], in0=PS(pb)[:, :], scalar=comb[:, i, ex:ex + 1],
                                                                                                  in1=x1[:, i, half * 512:(half + 1) * 512], op0=ALU.mult, op1=ALU.add),
                                 reads=[("ps", pb), "comb", ("x1", i)], writes=[("x1", i)])
            phase_barrier()
            if stop_after == "P5":
                break

            h3T = hT
            P.dma("pool", "wpg", lambda e: e.dma_start(out=wpg.rearrange("p k c -> p (k c)"), in_=wpg_d[:, :], max_dma_last_dim=8192), writes=["wpg"])
            P.dma("pool", "wpl", lambda e: e.dma_start(out=wpl.rearrange("p k c -> p (k c)"), in_=wpl_d[:, :], max_dma_last_dim=8192), writes=["wpl"])
            build_gB(2)
            norm_transpose(b, "x1", 2, h3T, "hT", None, [dD(0, 2 * KB, BF16), dD(2 * KB, 2 * KB, BF16)], [6, 7])
            pt_b = [dD(4 * KB + i * KB, 1 * KB, F32) for i in range(2)]
            pT_b = [dD(6 * KB + i * 512, 512, BF16, "p (k t) -> p k t", k=2) for i in range(2)]
            sig = [dD(8 * KB + i * 2 * KB, 2 * KB, BF16) for i in range(2)]
            tmpf = [dD(12 * KB + i * 4 * KB, 4 * KB, F32) for i in range(2)]
            outb = [dD(20 * KB + i * 4 * KB, 4 * KB, F32) for i in range(2)]
            junkf = dD(28 * KB, 2 * KB, BF16)
            gfin = dD(30 * KB, 4 * KB, F32)
            P.dma("sp", "c_gfin", lambda e: e.dma_start(out=gfin, in_=gfin_d[:, :]), writes=["c_gfin"])
            for i in range(NT):
                sl = i % 2
                P.dma("sp", "pt%d" % sl, lambda e, i=i, sl=sl: e.dma_start(out=pt_b[sl], in_=p_d[b, i * 128:(i + 1) * 128, :]), writes=[("pt", sl)])
                for k in range(2):
                    P.op("pe", lambda e, k=k, sl=sl: e.transpose(out=PS(5)[:, k * 128:(k + 1) * 128], in_=pt_b[sl][:, k * 128:(k + 1) * 128], identity=ident_f[:]),
                         reads=[("pt", sl), "c_ident"], writes=[("ps", 5)])
                P.op("act", lambda e, sl=sl: e.activation(out=pT_b[sl].rearrange("p k t -> p (k t)"), in_=PS(5)[:, 0:256], func=AF.Copy), reads=[("ps", 5)], writes=[("pT", sl)])
                for half in range(2):
                    gbk = half
                    pbk = 2 + half
                    for k in range(8):
                        P.op("pe", lambda e, gbk=gbk, k=k, i=i, half=half: e.matmul(PS(gbk)[:, :], lhsT=h3T[:, k, i * 128:(i + 1) * 128], rhs=wpg[:, k, half * 512:(half + 1) * 512], start=(k == 0), stop=(k == 7)),
                             reads=[("hT", i // 4), "wpg"], writes=[("ps", gbk)])
                    for k in range(2):
                        P.op("pe", lambda e, pbk=pbk, k=k, sl=sl, half=half: e.matmul(PS(pbk)[:, :], lhsT=pT_b[sl][:, k, :], rhs=wpl[:, k, half * 512:(half + 1) * 512], start=(k == 0), stop=(k == 1)),
                             reads=[("pT", sl), "wpl"], writes=[("ps", pbk)])
                    hsl = slice(half * 512, (half + 1) * 512)
                    P.op("act", lambda e, gbk=gbk, sl=sl, hsl=hsl: e.activation(out=sig[sl][:, hsl], in_=PS(gbk)[:, :], func=AF.Sigmoid), reads=[("ps", gbk)], writes=[("sig", sl, half)])
                    P.op("dve", lambda e, pbk=pbk, sl=sl, hsl=hsl: e.tensor_tensor(out=tmpf[sl][:, hsl], in0=sig[sl][:, hsl], in1=PS(pbk)[:, :], op=ALU.mult),
                         reads=[("sig", sl, half), ("ps", pbk)], writes=[("tmpf", sl, half)])
                    P.op("dve", lambda e, sl=sl, hsl=hsl, i=i: e.tensor_tensor(out=tmpf[sl][:, hsl], in0=tmpf[sl][:, hsl], in1=x1[:, i, hsl], op=ALU.add),
                         reads=[("tmpf", sl, half), ("x1", i)], writes=[("tmpf", sl, half)])
                ssc = small[:, 64 + i:65 + i]
                P.op("act", lambda e, sl=sl, ssc=ssc: e.activation(out=junkf, in_=tmpf[sl], func=AF.Square, accum_out=ssc), reads=[("tmpf", sl, 0), ("tmpf", sl, 1)], writes=["junkf", ("fs", i)])
                P.op("dve", lambda e, ssc=ssc: e.tensor_scalar(out=ssc, in0=ssc, scalar1=1.0 / D, scalar2=EPS, op0=ALU.mult, op1=ALU.add), reads=[("fs", i)], writes=[("fs", i)])
                P.op("act", lambda e, ssc=ssc: e.activation(out=ssc, in_=ssc, func=AF.Sqrt), reads=[("fs", i)], writes=[("fs", i)])
                P.op("dve", lambda e, ssc=ssc: e.reciprocal(out=ssc, in_=ssc), reads=[("fs", i)], writes=[("fs", i)])
                P.op("dve", lambda e, sl=sl, ssc=ssc: e.scalar_tensor_tensor(out=outb[sl], in0=tmpf[sl], scalar=ssc, in1=gfin, op0=ALU.mult, op1=ALU.mult),
                     reads=[("tmpf", sl, 0), ("tmpf", sl, 1), ("fs", i), "c_gfin"], writes=[("outb", sl)])
                tok = P.dma("sp", "out%d" % sl, lambda e, sl=sl, i=i: e.dma_start(out=out_d[b, i * 128:(i + 1) * 128, :], in_=outb[sl]), reads=[("outb", sl)])
                last_out_tokens.append(tok)
            phase_barrier()

        finals = {}
        for tok in last_out_tokens:
            finals[tok[1]] = max(finals.get(tok[1], 0), tok[2])
        for k in P.dma_keys:
            if k == "dbg":
                finals[k] = P.dma_count[k]
        P.emit(final_wait_tokens=[("d", k, n) for k, n in finals.items()])
    return nc


def _pk(w, ncols=None):
    K = w.shape[0] // 128
    return np.ascontiguousarray(w.reshape(K, 128, -1).transpose(1, 0, 2).reshape(128, -1))


def _t5_bucket_np(d):
    d = np.maximum(d, 0)
    d_f = np.maximum(d, 1).astype(np.float32)
    large = 16 + (np.log(d_f / np.float32(16)) / np.float32(math.log(128 / 16)) * np.float32(16)).astype(np.int32)
    large = np.minimum(large, 31)
    return np.where(d < 16, d, large)


def prep_weights(inp):
    f = lambda a: np.ascontiguousarray(a, dtype=np.float32)
    w_in = inp["w_in"][0]
    o = {}
    c0 = 0
    sec = {}
    for nm, wdt in (("q_a", 512), ("c_kv", 128), ("q_idx", 512), ("k_idx", 64), ("w_idx", 8), ("qkv_b", 1536), ("gate_a", 1024), ("gate_b", 1024)):
        sec[nm] = w_in[:, c0:c0 + wdt]
        c0 += wdt
    w1 = np.concatenate([sec["q_a"], sec["q_idx"], sec["c_kv"], sec["k_idx"], sec["k_idx"]], axis=1)
    o["w1"] = _pk(w1)
    o["widx"] = _pk(sec["w_idx"])
    o["w3"] = _pk(sec["qkv_b"])
    ga = sec["gate_a"].reshape(1024, 8, 128)
    gb = sec["gate_b"].reshape(1024, 8, 128)
    g4 = np.concatenate([ga, gb], axis=2)
    g4 = g4.reshape(8, 128, 8, 256).transpose(1, 2, 0, 3)
    o["wg4"] = f(g4.reshape(128, -1))
    wa = inp["w_branch_a"][0].reshape(4, 128, 8, 128)
    wb = inp["w_branch_b"][0].reshape(4, 128, 8, 128)
    wbr = np.stack([wa, wb], axis=0).transpose(2, 3, 0, 1, 4)
    o["wbr"] = f(wbr.reshape(128, -1))
    o["wout"] = _pk(inp["w_out"][0])
    wuk = inp["w_uk"][0]
    wukT = wuk.transpose(0, 2, 1).reshape(4, 2, 64, 128).transpose(1, 2, 0, 3)
    o["wuk"] = f(wukT.reshape(128, 512))
    o["wuv"] = f(inp["w_uv"][0].transpose(1, 0, 2).reshape(128, 512))
    wg = inp["w_gate"][0].reshape(N_EXP, 8, 128, 256).transpose(0, 2, 1, 3).reshape(N_EXP, 128, 2048)
    wu = inp["w_up"][0].reshape(N_EXP, 8, 128, 256).transpose(0, 2, 1, 3).reshape(N_EXP, 128, 2048)
    wd = inp["w_down"][0].reshape(N_EXP, 2, 128, 1024).transpose(0, 2, 1, 3).reshape(N_EXP, 128, 2048)
    o["wmoe"] = f(np.concatenate([wg, wu, wd], axis=2))
    wr = np.concatenate([inp["w_r1"][0], inp["w_r2"][0].transpose(1, 0, 2).reshape(1024, 32)], axis=1)
    o["wr"] = _pk(wr)
    br = np.concatenate([inp["b_r1"][0], inp["b_r2"][0].reshape(32)])
    o["br"] = f(np.broadcast_to(br[None, :], (128, 36)))
    o["wpg"] = _pk(inp["w_ple_gate"][0])
    o["wpl"] = _pk(inp["w_ple"][0])
    o["g_attn"] = f(inp["attn_norm"][0].reshape(8, 128).T)
    o["g_ffn"] = f(inp["ffn_norm"][0].reshape(8, 128).T)
    o["g_ple"] = f(inp["ple_norm"][0].reshape(8, 128).T)
    o["g_fin"] = f(np.broadcast_to(inp["final_norm"][None, :], (128, 1024)))
    o["g_kv"] = f(inp["kv_norm"][0].reshape(128, 1))
    rb = inp["rel_bias"]
    s_l = np.arange(128)[:, None]
    u = np.arange(640)[None, :]
    bidx = _t5_bucket_np(u - s_l)
    o["btoep"] = f(rb[bidx].transpose(0, 2, 1).reshape(128, 8 * 640))
    o["b31"] = f(np.broadcast_to(rb[31][None, :], (128, 8)))
    o["ident"] = np.eye(128, dtype=np.float32)
    tt = np.arange(128)[:, None]
    ss = np.arange(128)[None, :]
    o["cneg"] = np.where(ss <= tt, 0.0, NEG).astype(np.float32)
    o["smask"] = (tt < ss).astype(np.float32)
    o["uinc"] = (tt >= ss).astype(np.float32)
    sel = np.zeros((128, 256), np.float32)
    sel[64, 0:128] = 1.0
    sel[63, 128:256] = 1.0
    o["sel"] = sel
    return o


_NC_CACHE = {}


def kernel(**inputs):
    inp = {k: np.asarray(v) for k, v in inputs.items()}
    n = 8
    NB = 2
    wts = prep_weights(inp)
    x = np.ascontiguousarray(inp["x"], dtype=np.float32)
    p = np.ascontiguousarray(inp["p"][0], dtype=np.float32)
    if "nc" not in _NC_CACHE:
        _NC_CACHE["nc"] = build_nc(NB=NB)
    nc = _NC_CACHE["nc"]
    in_maps = []
    for c in range(n):
        m = dict(wts)
        m["x"] = x[c * NB:(c + 1) * NB]
        m["p"] = p[c * NB:(c + 1) * NB]
        in_maps.append(m)
    res = run_bass_kernel_spmd(nc, in_maps, core_ids=list(range(n)))
    out = np.concatenate([r["out"] for r in res.results], axis=0)
    return out.astype(np.float32)
```

```python
import math
import types
import contextlib
import numpy as np
import concourse.bass as bass
import concourse.mybir as mybir
from concourse.bass_utils import run_bass_kernel_spmd

F32 = mybir.dt.float32
BF16 = mybir.dt.bfloat16
AF = mybir.ActivationFunctionType
ALU = mybir.AluOpType
AX = mybir.AxisListType

S = 2048
D = 1024
NT = S // 128
NCH = S // 512
ATTN_SCALE = 64 ** -0.5
IDX_SCALE = (8 ** -0.5) * (64 ** -0.5)
EPS = 1e-6
NEG = -1.0e30
N_BISECT = 14
N_EXP = 32
import os as _os
SBE = _os.environ.get("SBE", "pool")
SBLN = _os.environ.get("SBLN", "1") == "1"


class Prog:
    ENGS = ("pe", "act", "dve", "pool", "sp")

    def __init__(self, nc, same_eng_sync=True):
        self.nc = nc
        self.ops = {e: [] for e in self.ENGS}
        self.last_w = {}
        self.last_r = {}
        self.clock = {e: {} for e in self.ENGS}
        self.opclock = {}
        self.dma_count = {}
        self.dma_keys = []
        self.signaling = set()
        self.same_eng_sync = same_eng_sync
        self._bar = 0

    def _add(self, eng, fn, reads, writes, dma_key=None, n_dma=1):
        idx = len(self.ops[eng]) + 1
        deps = {}

        def need(tok):
            kind, who, n = tok
            if kind == "e" and who == eng:
                if eng in ("pe", "sp") or not self.same_eng_sync:
                    return
            k = (kind, who)
            if deps.get(k, 0) < n:
                deps[k] = n

        for r in reads:
            for tok in self.last_w.get(r, {}).values():
                need(tok)
        for w in writes:
            for tok in self.last_w.get(w, {}).values():
                need(tok)
            for tok in self.last_r.get(w, {}).values():
                need(tok)
        if dma_key is not None:
            if dma_key not in self.dma_count:
                self.dma_count[dma_key] = 0
                self.dma_keys.append(dma_key)
            prev = self.dma_count[dma_key]
            if prev > 0:
                need(("d", dma_key, prev))
            self.dma_count[dma_key] = prev + n_dma
            mytok = ("d", dma_key, prev + n_dma)
        else:
            mytok = ("e", eng, idx)
        clk = self.clock[eng]
        final = []
        for k, n in deps.items():
            if clk.get(k, 0) >= n:
                continue
            final.append((k[0], k[1], n))
        for kind, who, n in final:
            oc = self.opclock.get((kind, who, n))
            if oc:
                for k2, n2 in oc.items():
                    if clk.get(k2, 0) < n2:
                        clk[k2] = n2
            if clk.get((kind, who), 0) < n:
                clk[(kind, who)] = n
            if kind == "e":
                self.signaling.add((who, n))
        snap = dict(clk)
        if mytok[0] == "e":
            snap[("e", eng)] = idx
        self.opclock[mytok] = snap
        self.ops[eng].append((fn, final, dma_key, mytok))
        if fn is not None:
            for r in reads:
                self.last_r.setdefault(r, {})[(mytok[0], mytok[1])] = mytok
        for w in writes:
            self.last_w[w] = {(mytok[0], mytok[1]): mytok}
            self.last_r[w] = {}
        return mytok

    @staticmethod
    def _freeze(fn):
        if fn is None or getattr(fn, "__closure__", None) is None:
            return fn
        cells = []
        for c in fn.__closure__:
            try:
                cells.append(types.CellType(c.cell_contents))
            except ValueError:
                cells.append(c)
        return types.FunctionType(fn.__code__, fn.__globals__, fn.__name__, fn.__defaults__, tuple(cells))

    def op(self, eng, fn, reads=(), writes=()):
        return self._add(eng, self._freeze(fn), tuple(reads), tuple(writes))

    def dma(self, eng, key, fns, reads=(), writes=()):
        if not isinstance(fns, (list, tuple)):
            fns = [fns]
        return self._add(eng, [self._freeze(f) for f in fns], tuple(reads), tuple(writes), dma_key=key, n_dma=len(fns))

    def barrier(self, tiny_fn):
        self._bar += 1
        res = ("__barrier__", self._bar)
        allres = list(set(list(self.last_w.keys()) + list(self.last_r.keys())))
        self._add("dve", self._freeze(tiny_fn), tuple(), tuple(allres) + (res,))
        for e in ("pe", "act", "pool", "sp"):
            self._add(e, None, (res,), tuple())

    def emit(self, final_wait_tokens=()):
        nc = self.nc
        sigval = {}
        for e in self.ENGS:
            s = 0
            for i in range(1, len(self.ops[e]) + 1):
                if (e, i) in self.signaling:
                    s += 1
                    sigval[(e, i)] = s
        engobj = {"pe": nc.tensor, "act": nc.scalar, "dve": nc.vector, "pool": nc.gpsimd, "sp": nc.sync}
        with contextlib.ExitStack() as st:
            esem = {e: st.enter_context(nc.semaphore("sem_" + e)) for e in self.ENGS}
            dsem = {k: st.enter_context(nc.semaphore("dsem_%d" % i)) for i, k in enumerate(self.dma_keys)}
            block = st.enter_context(nc.Block())

            def run(e):
                eng = engobj[e]
                for i, (fn, deps, dma_key, mytok) in enumerate(self.ops[e], start=1):
                    for kind, who, n in deps:
                        if kind == "e":
                            eng.wait_ge(esem[who], sigval[(who, n)])
                        else:
                            eng.wait_ge(dsem[who], 16 * n)
                    if fn is None:
                        assert (e, i) not in self.signaling
                        continue
                    if dma_key is not None:
                        for f in fn:
                            f(eng).then_inc(dsem[dma_key], 16)
                    else:
                        ins = fn(eng)
                        if (e, i) in self.signaling:
                            ins.then_inc(esem[e], 1)
                if e == "sp":
                    for k in self.dma_keys:
                        eng.wait_ge(dsem[k], 16 * self.dma_count[k])

            block.tensor(lambda eng: run("pe"))
            block.scalar(lambda eng: run("act"))
            block.vector(lambda eng: run("dve"))
            block.gpsimd(lambda eng: run("pool"))
            block.sync(lambda eng: run("sp"))


def build_nc(NB=2, stop_after=None, dbg=False):
    nc = bass.Bass("TRN2", target_bir_lowering=False)

    def din(name, shape, dt=F32):
        return nc.dram_tensor(name, list(shape), dt, kind="ExternalInput").ap()

    x_d = din("x", [NB, S, D])
    p_d = din("p", [NB, S, 256])
    w1_d = din("w1", [128, 8 * 1280])
    widx_d = din("widx", [128, 8 * 8])
    w3_d = din("w3", [128, 8 * 1536])
    wg4_d = din("wg4", [128, 8 * 8 * 256])
    wbr_d = din("wbr", [128, 8 * 1024])
    wout_d = din("wout", [128, 8 * 1024])
    wuk_d = din("wuk", [128, 512])
    wuv_d = din("wuv", [128, 512])
    wmoe_d = din("wmoe", [N_EXP, 128, 6144])
    wr_d = din("wr", [128, 8 * 36])
    br_d = din("br", [128, 36])
    wpg_d = din("wpg", [128, 8 * 1024])
    wpl_d = din("wpl", [128, 2 * 1024])
    gat_d = din("g_attn", [128, 8])
    gff_d = din("g_ffn", [128, 8])
    gpl_d = din("g_ple", [128, 8])
    gfin_d = din("g_fin", [128, 1024])
    gkv_d = din("g_kv", [128, 1])
    btoep_d = din("btoep", [128, 8 * 640])
    b31_d = din("b31", [128, 8])
    ident_d = din("ident", [128, 128])
    cneg_d = din("cneg", [128, 128])
    smask_d = din("smask", [128, 128])
    uinc_d = din("uinc", [128, 128])
    sel_d = din("sel", [128, 256])
    out_d = nc.dram_tensor("out", [NB, S, D], F32, kind="ExternalOutput").ap()
    dbg_d = {}
    if dbg:
        for nm, shp in (("d_oaT", [128, 4 * S]), ("d_obT", [128, 4 * S]), ("d_x1", [128, NT * D])):
            dbg_d[nm] = nc.dram_tensor(nm, shp, F32, kind="ExternalOutput").ap()

    st = contextlib.ExitStack()
    with st:
        def sb(name, shape, dt):
            return st.enter_context(nc.sbuf_tensor("s_" + name, list(shape), dt))

        arA = sb("arA", [128, 16384], BF16)
        arB = sb("arB", [128, 32768], BF16)
        arC = sb("arC", [128, 16384], BF16)
        arD = sb("arD", [128, 33792], BF16)
        ident_f = sb("ident_f", [128, 128], F32)
        ident_b = sb("ident_b", [128, 128], BF16)
        cneg = sb("cneg", [128, 128], F32)
        smask = sb("smask", [128, 128], BF16)
        uinc = sb("uinc", [128, 128], BF16)
        ones_b = sb("ones_b", [128, 128], BF16)
        sel_f = sb("sel_f", [128, 256], F32)
        gB1 = sb("gB", [128, 8, 128], BF16)
        gB = [gB1, gB1, gB1]
        gpk = sb("gpk", [128, 24], F32)
        gkv = sb("gkv", [128, 1], F32)
        b31 = sb("b31", [128, 8], F32)
        brb = sb("brb", [128, 36], F32)
        wr_f = sb("wr_f", [128, 8, 36], F32)
        wuk = sb("wuk", [128, 4, 128], BF16)
        wuv = sb("wuv", [128, 512], BF16)
        widx_w = sb("widx_w", [128, 8, 8], BF16)
        small = sb("small", [128, 256], F32)
        tiny = sb("tiny", [128, 2], F32)

        psb = [st.enter_context(nc.psum_tensor("ps%d" % i, [128, 512], F32)) for i in range(8)]

        P = Prog(nc)

        def carve(ar, off_bytes, nbytes, dt, pattern=None, **kw):
            e0 = off_bytes // 2
            ap = ar[:, e0:e0 + nbytes // 2]
            if dt == F32:
                ap = ap.bitcast(F32)
            if pattern:
                ap = ap.rearrange(pattern, **kw)
            return ap

        KB = 1024
        hT = carve(arA, 0, 32 * KB, BF16, "p (k t) -> p k t", k=8)
        qaT = carve(arB, 0, 16 * KB, BF16, "p (k t) -> p k t", k=4)
        qiT = carve(arB, 16 * KB, 16 * KB, BF16, "p (k t) -> p k t", k=4)
        ckvT = carve(arB, 32 * KB, 4 * KB, BF16)
        kiT = carve(arB, 36 * KB, 4 * KB, BF16)
        Vp = carve(arB, 40 * KB, 16 * 4 * 130 * 2, BF16, "p (j m c) -> p j m c", j=16, m=4)
        widx_tm = carve(arB, 40 * KB + 16640, 512, F32, "p (i h) -> p i h", i=16)
        qbT = carve(arB, 0, 16 * KB, BF16, "p (k t) -> p k t", k=4)
        kbT = carve(arB, 16 * KB, 16 * KB, BF16, "p (k t) -> p k t", k=4)
        vb = carve(arB, 32 * KB, 16 * KB, BF16, "p (j c) -> p j c", j=16)
        qbTn = carve(arB, 48 * KB, 16 * KB, BF16, "p (k t) -> p k t", k=4)
        x1 = carve(arB, 0, 64 * KB, F32, "p (i d) -> p i d", i=16)
        oaT = carve(arC, 0, 16 * KB, BF16, "p (k t) -> p k t", k=4)
        obT = carve(arC, 16 * KB, 16 * KB, BF16, "p (k t) -> p k t", k=4)
        wmoe = [carve(arC, i * 12 * KB, 12 * KB, BF16) for i in range(2)]
        wpg = carve(arC, 0, 16 * KB, BF16, "p (k c) -> p k c", k=8)
        wpl = carve(arC, 16 * KB, 4 * KB, BF16, "p (k c) -> p k c", k=2)
        def dD(off, nbytes, dt, pattern=None, **kw):
            assert off + nbytes <= 66 * KB, (off, nbytes)
            return carve(arD, off, nbytes, dt, pattern, **kw)

        PS = lambda i: psb[i]

        def psbf(i):
            return psb[i][:].bitcast(BF16)

        def ld(key, dst, src, eng="sp", res=None, **kw):
            P.dma(eng, key, lambda e: e.dma_start(out=dst, in_=src, **kw), writes=[res or key])

        ld("c_ident", ident_f[:], ident_d[:, :])
        ld("c_cneg", cneg[:], cneg_d[:, :])
        ld("c_sel", sel_f[:], sel_d[:, :])
        ld("c_gkv", gkv[:], gkv_d[:, :])
        ld("c_b31", b31[:], b31_d[:, :])
        ld("c_br", brb[:], br_d[:, :])
        ld("c_wr", wr_f[:].rearrange("p k c -> p (k c)"), wr_d[:, :])
        ld("c_g0", gpk[:, 0:8], gat_d[:, :])
        ld("c_g1", gpk[:, 8:16], gff_d[:, :])
        ld("c_g2", gpk[:, 16:24], gpl_d[:, :])
        ld("c_smask", smask[:], smask_d[:, :], eng="pool")
        ld("c_uinc", uinc[:], uinc_d[:, :], eng="pool")
        ld("c_wuk", wuk[:].rearrange("p k c -> p (k c)"), wuk_d[:, :], eng="pool")
        ld("c_wuv", wuv[:], wuv_d[:, :], eng="pool")
        ld("c_widx", widx_w[:].rearrange("p k c -> p (k c)"), widx_d[:, :], eng="pool")
        P.op("dve", lambda e: e.tensor_copy(out=ident_b[:], in_=ident_f[:]), reads=["c_ident"], writes=["ident_b"])
        P.op("dve", lambda e: e.memset(ones_b[:], 1.0), writes=["ones_b"])
        P.op("dve", lambda e: e.memset(tiny[:], 0.0), writes=["tiny"])
        def build_gB(gi):
            for k in range(8):
                P.op("dve", lambda e, gi=gi, k=k: e.tensor_scalar(out=gB1[:, k, :], in0=ones_b[:], scalar1=gpk[:, gi * 8 + k:gi * 8 + k + 1],
                                                                  scalar2=None, op0=ALU.mult),
                     reads=["ones_b", "c_g%d" % gi], writes=["gB"])

        evac_rr = [0]

        def evac(out, in_, reads, writes, scale=None, eng=None):
            if eng is None:
                eng = ("act", "dve")[evac_rr[0] % 2]
                evac_rr[0] += 1
            if eng == "act":
                if scale is None:
                    P.op("act", lambda e: e.activation(out=out, in_=in_, func=AF.Copy), reads=reads, writes=writes)
                else:
                    P.op("act", lambda e: e.activation(out=out, in_=in_, func=AF.Copy, scale=float(scale)), reads=reads, writes=writes)
            else:
                if scale is None:
                    P.op("dve", lambda e: e.tensor_copy(out=out, in_=in_), reads=reads, writes=writes)
                else:
                    P.op("dve", lambda e: e.tensor_scalar(out=out, in0=in_, scalar1=float(scale), scalar2=None, op0=ALU.mult), reads=reads, writes=writes)

        def phase_barrier():
            P.barrier(lambda e: e.memset(tiny[:, 0:1], 0.0))

        def norm_transpose(b, src, gi, dstT, dstres, xt_bufs, xn_bufs, ps_banks, f32_side=None):
            for i in range(NT):
                sl = i % 2
                if src == "x":
                    xt = xt_bufs[sl]
                    xres = "xt%d" % sl
                    P.dma("sp", xres, lambda e, xt=xt, i=i: e.dma_start(out=xt, in_=x_d[b, i * 128:(i + 1) * 128, :]), writes=[xres])
                else:
                    xt = x1[:, i, :]
                    xres = ("x1", i)
                xn = xn_bufs[sl]
                xnres = "xn%d" % sl
                ssc = small[:, i:i + 1]
                rsc = small[:, 16 + i:17 + i]
                P.op("act", lambda e, xt=xt, xn=xn, ssc=ssc: e.activation(out=xn, in_=xt, func=AF.Square, accum_out=ssc),
                     reads=[xres], writes=[xnres, ("ss", i)])
                P.op("dve", lambda e, ssc=ssc, rsc=rsc: e.tensor_scalar(out=rsc, in0=ssc, scalar1=1.0 / D, scalar2=EPS, op0=ALU.mult, op1=ALU.add),
                     reads=[("ss", i)], writes=[("rs", i)])
                P.op("act", lambda e, rsc=rsc: e.activation(out=rsc, in_=rsc, func=AF.Sqrt), reads=[("rs", i)], writes=[("rs", i)])
                P.op("dve", lambda e, rsc=rsc: e.reciprocal(out=rsc, in_=rsc), reads=[("rs", i)], writes=[("rs", i)])
                if f32_side is None:
                    P.op("dve", lambda e, xt=xt, xn=xn, rsc=rsc: e.tensor_scalar(out=xn, in0=xt, scalar1=rsc, scalar2=None, op0=ALU.mult),
                         reads=[xres, ("rs", i)], writes=[xnres])
                    pb = ps_banks[i % len(ps_banks)]
                    pres = ("ps", pb)
                    pv = psbf(pb)[:, 0:1024].rearrange("p (k t) -> p k t", k=8)
                    for k in range(8):
                        P.op("pe", lambda e, pv=pv, xn=xn, k=k: e.transpose(out=pv[:, k, :], in_=xn[:, k * 128:(k + 1) * 128], identity=ident_b[:]),
                             reads=[xnres, "ident_b"], writes=[pres])
                    P.op("dve", lambda e, pv=pv, i=i: e.tensor_tensor(out=dstT[:, :, i * 128:(i + 1) * 128], in0=pv, in1=gB[gi][:], op=ALU.mult),
                         reads=[pres, "gB"], writes=[(dstres, i // 4)])
                else:
                    f32_side(i, xt, xres, rsc, xn, xnres)

        last_out_tokens = []
        for b in range(NB):
            xt_bufs = [dD(0, 4 * KB, F32), dD(4 * KB, 4 * KB, F32)]
            xn_bufs = [dD(8 * KB, 2 * KB, BF16), dD(10 * KB, 2 * KB, BF16)]
            w1 = dD(12 * KB, 20 * KB, BF16, "p (k c) -> p k c", k=8)
            ckv_raw = dD(32 * KB, 8 * KB, F32)
            sqb = [dD(40 * KB, 1 * KB, BF16), dD(41 * KB, 1 * KB, BF16)]
            rstd_b = [dD(42 * KB, 2 * KB, F32), dD(44 * KB, 2 * KB, F32)]
            P.dma("pool", "w1", lambda e: e.dma_start(out=w1.rearrange("p k c -> p (k c)"), in_=w1_d[:, :], max_dma_last_dim=8192), writes=["w1"])
            build_gB(0)
            norm_transpose(b, "x", 0, hT, "hT", xt_bufs, xn_bufs, [6, 7])

            if stop_after == "P0":
                break
            bank_rr = [0]

            def nb(banks):
                v = banks[bank_rr[0] % len(banks)]
                bank_rr[0] += 1
                return v

            def proj_T(w, wres, cc, c, banks):
                pb = nb(banks)
                for k in range(8):
                    P.op("pe", lambda e, pb=pb, k=k: e.matmul(PS(pb)[:, :], lhsT=w[:, k, cc * 128:(cc + 1) * 128], rhs=hT[:, k, c * 512:(c + 1) * 512],
                                                             start=(k == 0), stop=(k == 7)),
                         reads=[wres, ("hT", c)], writes=[("ps", pb)])
                return pb

            for cc in range(10):
                for c in range(NCH):
                    pb = proj_T(w1, "w1", cc, c, [0, 1, 2, 3])
                    cs = slice(c * 512, (c + 1) * 512)
                    if cc < 4:
                        evac(qaT[:, cc, cs], PS(pb)[:, :], [("ps", pb)], [("qaT", c)])
                    elif cc < 8:
                        evac(qiT[:, cc - 4, cs], PS(pb)[:, :], [("ps", pb)], [("qiT", c)])
                    elif cc == 8:
                        evac(ckv_raw[:, cs], PS(pb)[:, :], [("ps", pb)], [("ckv_raw", c)])
                    else:
                        evac(kiT[:, cs], PS(pb)[:, :], [("ps", pb)], [("kiT", c)])
            for c in range(NCH):
                cs = slice(c * 512, (c + 1) * 512)
                sq = sqb[c % 2]
                rb = rstd_b[c % 2]
                P.op("act", lambda e, sq=sq, cs=cs: e.activation(out=sq, in_=ckv_raw[:, cs], func=AF.Square), reads=[("ckv_raw", c)], writes=[("sq", c % 2)])
                pb = nb([0, 1, 2, 3])
                P.op("pe", lambda e, pb=pb, sq=sq: e.matmul(PS(pb)[:, :], lhsT=ones_b[:], rhs=sq, start=True, stop=True),
                     reads=[("sq", c % 2), "ones_b"], writes=[("ps", pb)])
                P.op("dve", lambda e, pb=pb, rb=rb: e.tensor_scalar(out=rb, in0=PS(pb)[:, :], scalar1=1.0 / 128, scalar2=EPS, op0=ALU.mult, op1=ALU.add),
                     reads=[("ps", pb)], writes=[("rb", c % 2)])
                P.op("act", lambda e, rb=rb: e.activation(out=rb, in_=rb, func=AF.Sqrt), reads=[("rb", c % 2)], writes=[("rb", c % 2)])
                P.op("dve", lambda e, rb=rb: e.reciprocal(out=rb, in_=rb), reads=[("rb", c % 2)], writes=[("rb", c % 2)])
                P.op("dve", lambda e, rb=rb, cs=cs: e.scalar_tensor_tensor(out=ckvT[:, cs], in0=ckv_raw[:, cs], scalar=gkv[:, 0:1], in1=rb, op0=ALU.mult, op1=ALU.mult),
                     reads=[("ckv_raw", c), ("rb", c % 2), "c_gkv"], writes=[("ckvT", c)])
            pbw = 4
            for i in range(NT):
                for k in range(8):
                    P.op("pe", lambda e, i=i, k=k: e.matmul(PS(pbw)[:, i * 8:(i + 1) * 8], lhsT=hT[:, k, i * 128:(i + 1) * 128], rhs=widx_w[:, k, :],
                                                         start=(k == 0), stop=(k == 7)),
                         reads=[("hT", i // 4), "c_widx"], writes=[("ps", pbw)])
            P.op("dve", lambda e: e.tensor_scalar(out=widx_tm.rearrange("p i h -> p (i h)"), in0=PS(pbw)[:, 0:128], scalar1=IDX_SCALE, scalar2=None, op0=ALU.mult),
                 reads=[("ps", pbw)], writes=["widx_tm"])
            P.op("pool", lambda e: e.memset(Vp.rearrange("p j m c -> p (j m) c")[:, :, 64:65], 1.0), writes=["Vp"])
            P.op("pool", lambda e: e.memset(Vp.rearrange("p j m c -> p (j m) c")[:, :, 129:130], 1.0), writes=["Vp"])
            for j in range(NT):
                pb = nb([0, 1, 2, 3])
                P.op("pe", lambda e, pb=pb, j=j: e.matmul(PS(pb)[:, :], lhsT=ckvT[:, j * 128:(j + 1) * 128], rhs=wuv[:], start=True, stop=True),
                     reads=[("ckvT", j // 4), "c_wuv"], writes=[("ps", pb)])
                pv = PS(pb)[:, :].rearrange("p (m h d) -> p m h d", m=4, h=2)
                evac(Vp[:, j, :, 0:64], pv[:, :, 0, :], [("ps", pb)], ["Vp"])
                evac(Vp[:, j, :, 65:129], pv[:, :, 1, :], [("ps", pb)], ["Vp"])
            phase_barrier()
            if stop_after == "P1":
                break

            score = dD(0, 8 * KB, F32)
            junk = dD(8 * KB, 4 * KB, BF16)
            mask_tm = dD(12 * KB, 4 * KB, BF16)
            maskT = dD(16 * KB, 16 * KB, BF16, "p (j t) -> p j t", j=16)
            EB = dD(32 * KB, 10 * KB, BF16, "p (h u) -> p h u", h=8)
            rbuf = [dD(42 * KB + i * KB, 1 * KB, BF16) for i in range(4)]
            Pbuf = [dD(46 * KB + i * KB, 1 * KB, BF16) for i in range(4)]
            qabs = [dD(50 * KB + i * KB, 1 * KB, BF16) for i in range(2)]
            dg = dD(52 * KB, 2 * KB, BF16, "p (h t) -> p h t", h=8)
            o_f32 = dD(54 * KB, 2 * KB, F32)
            rec = dD(56 * KB, 2 * KB, F32)
            btmp = dD(0, 10 * KB, F32, "p (h u) -> p h u", h=4)
            P.dma("sp", "btoep", lambda e: e.dma_start(out=btmp.rearrange("p h u -> p (h u)")[:, 0:2560], in_=btoep_d[:, 0:2560]), writes=["btmp"])
            P.op("act", lambda e: e.activation(out=EB[:, 0:4, :], in_=btmp[:, 0:4, :], func=AF.Exp), reads=["btmp"], writes=["EB"])
            P.dma("sp", "btoep", lambda e: e.dma_start(out=btmp.rearrange("p h u -> p (h u)")[:, 0:2560], in_=btoep_d[:, 2560:5120]), writes=["btmp"])
            P.op("act", lambda e: e.activation(out=EB[:, 4:8, :], in_=btmp[:, 0:4, :], func=AF.Exp), reads=["btmp"], writes=["EB"])
            phase_barrier()

            LO, WD, MID, CNT, TMP = 40, 41, 42, 43, 44
            rr = [0]
            for c in range(NCH):
                for tl in range(4):
                    i = 4 * c + tl
                    L = (i + 1) * 128
                    ts_ = slice(i * 128, (i + 1) * 128)
                    for h in range(8):
                        P.op("dve", lambda e, h=h, i=i: e.tensor_scalar(out=dg[:, h, :], in0=ident_b[:], scalar1=widx_tm[:, i, h:h + 1], scalar2=None, op0=ALU.mult),
                             reads=["ident_b", "widx_tm"], writes=[("dg", h)])
                    nsc = (L + 511) // 512
                    for sc in range(nsc):
                        ws = min(512, L - sc * 512)
                        spb = 4 + (sc % 2)
                        for h in range(8):
                            bp = (h % 2) * 64
                            zb = nb([0, 1, 2, 3])
                            P.op("pe", lambda e, zb=zb, h=h, bp=bp, ts_=ts_, sc=sc, ws=ws: e.matmul(
                                PS(zb)[:, 0:ws], lhsT=qiT[bp:bp + 64, h // 2, ts_], rhs=kiT[bp:bp + 64, sc * 512:sc * 512 + ws], start=True, stop=True),
                                reads=[("qiT", c), ("kiT", sc)], writes=[("ps", zb)])
                            rs = rr[0] % 4
                            rr[0] += 1
                            if True:
                                P.op("act", lambda e, zb=zb, rs=rs, ws=ws: e.activation(out=rbuf[rs][:, 0:ws], in_=PS(zb)[:, 0:ws], func=AF.Relu),
                                     reads=[("ps", zb)], writes=[("rbuf", rs)])
                            else:
                                P.op("dve", lambda e, zb=zb, rs=rs, ws=ws: e.tensor_scalar(out=rbuf[rs][:, 0:ws], in0=PS(zb)[:, 0:ws], scalar1=0.0, scalar2=None, op0=ALU.max),
                                     reads=[("ps", zb)], writes=[("rbuf", rs)])
                            P.op("pe", lambda e, spb=spb, h=h, rs=rs, ws=ws: e.matmul(PS(spb)[:, 0:ws], lhsT=dg[:, h, :], rhs=rbuf[rs][:, 0:ws], start=(h == 0), stop=(h == 7)),
                                 reads=[("dg", h), ("rbuf", rs)], writes=[("ps", spb)])
                        last = (sc == nsc - 1)
                        wcopy = ws - 128 if last else ws
                        if wcopy > 0:
                            P.op("act", lambda e, spb=spb, sc=sc, wcopy=wcopy: e.activation(out=score[:, sc * 512:sc * 512 + wcopy], in_=PS(spb)[:, 0:wcopy], func=AF.Copy),
                                 reads=[("ps", spb)], writes=["score"])
                        if last:
                            P.op("dve", lambda e, spb=spb, sc=sc, ws=ws: e.tensor_tensor(out=score[:, sc * 512 + ws - 128:sc * 512 + ws], in0=PS(spb)[:, ws - 128:ws], in1=cneg[:], op=ALU.add),
                                 reads=[("ps", spb), "c_cneg"], writes=["score"])
                    sm = lambda col: small[:, col:col + 1]
                    if i < 2:
                        P.op("dve", lambda e: e.memset(sm(LO), -1.0e29), writes=["lo"])
                    else:
                        P.op("dve", lambda e, i=i: e.tensor_reduce(out=sm(LO), in_=score[:, 0:i * 128], axis=AX.X, op=ALU.min), reads=["score"], writes=["lo"])
                        P.op("dve", lambda e, L=L: e.tensor_reduce(out=sm(WD), in_=score[:, 0:L], axis=AX.X, op=ALU.max), reads=["score"], writes=["wd"])
                        P.op("dve", lambda e: e.tensor_tensor(out=sm(WD), in0=sm(WD), in1=sm(LO), op=ALU.subtract), reads=["wd", "lo"], writes=["wd"])
                        for it in range(N_BISECT):
                            f = 0.5 ** (it + 1)
                            P.op("dve", lambda e, f=f: e.tensor_scalar(out=sm(MID), in0=sm(WD), scalar1=f, scalar2=sm(LO), op0=ALU.mult, op1=ALU.add),
                                 reads=["wd", "lo"], writes=["mid"])
                            P.op("dve", lambda e, L=L: e.tensor_scalar(out=junk[:, 0:L], in0=score[:, 0:L], scalar1=sm(MID), scalar2=None, op0=ALU.is_ge, op1=ALU.add, accum_out=sm(CNT)),
                                 reads=["score", "mid"], writes=["junk", "cnt"])
                            P.op("dve", lambda e, f=f: e.tensor_scalar(out=sm(TMP), in0=sm(CNT), scalar1=255.5, scalar2=f, op0=ALU.is_ge, op1=ALU.mult),
                                 reads=["cnt"], writes=["tmp"])
                            P.op("dve", lambda e: e.scalar_tensor_tensor(out=sm(LO), in0=sm(TMP), scalar=sm(WD), in1=sm(LO), op0=ALU.mult, op1=ALU.add),
                                 reads=["tmp", "wd", "lo"], writes=["lo"])
                    P.op("dve", lambda e, L=L: e.tensor_scalar(out=mask_tm[:, 0:L], in0=score[:, 0:L], scalar1=sm(LO), scalar2=None, op0=ALU.is_ge),
                         reads=["score", "lo"], writes=["mask_tm"])
                    for j0 in range(0, i + 1, 8):
                        n = min(8, i + 1 - j0)
                        tb = 6 + ((j0 // 8) % 2)
                        pv = psbf(tb)[:, 0:1024].rearrange("p (k t) -> p k t", k=8)
                        for jj in range(n):
                            j = j0 + jj
                            P.op("pe", lambda e, pv=pv, jj=jj, j=j: e.transpose(out=pv[:, jj, :], in_=mask_tm[:, j * 128:(j + 1) * 128], identity=ident_b[:]),
                                 reads=["mask_tm", "ident_b"], writes=[("ps", tb)])
                        evac(maskT[:, j0:j0 + n, tl * 128:(tl + 1) * 128], pv[:, 0:n, :], [("ps", tb)], [("maskT", tl)])
                cs0 = c * 512
                for h in range(8):
                    bp = (h % 2) * 64
                    m = h // 2
                    qs = h % 2
                    qb_ = nb([0, 1, 2, 3])
                    P.op("pe", lambda e, qb_=qb_, bp=bp, m=m: e.matmul(PS(qb_)[:, :], lhsT=wuk[bp:bp + 64, m, :], rhs=qaT[bp:bp + 64, m, cs0:cs0 + 512], start=True, stop=True),
                         reads=["c_wuk", ("qaT", c)], writes=[("ps", qb_)])
                    evac(qabs[qs], PS(qb_)[:, :], [("ps", qb_)], [("qabs", qs)])
                    ob = 4 + (h % 2)
                    jmax = 4 * c + 3
                    for j in range(jmax + 1):
                        col0 = max(0, j - 4 * c) * 128
                        N = 512 - col0
                        near = j >= 4 * c - 1
                        lb = nb([0, 1, 2, 3])
                        P.op("pe", lambda e, lb=lb, j=j, qs=qs, col0=col0, N=N: e.matmul(PS(lb)[:, 0:N], lhsT=ckvT[:, j * 128:(j + 1) * 128], rhs=qabs[qs][:, col0:512], start=True, stop=True),
                             reads=[("ckvT", j // 4), ("qabs", qs)], writes=[("ps", lb)])
                        pi = rr[0] % 4
                        rr[0] += 1
                        Pt = Pbuf[pi]
                        if near:
                            P.op("act", lambda e, lb=lb, Pt=Pt, N=N: e.activation(out=Pt[:, 0:N], in_=PS(lb)[:, 0:N], func=AF.Exp, scale=ATTN_SCALE),
                                 reads=[("ps", lb)], writes=[("Pbuf", pi)])
                        else:
                            P.op("act", lambda e, lb=lb, Pt=Pt, N=N, h=h: e.activation(out=Pt[:, 0:N], in_=PS(lb)[:, 0:N], func=AF.Exp, scale=ATTN_SCALE, bias=b31[:, h:h + 1]),
                                 reads=[("ps", lb), "c_b31"], writes=[("Pbuf", pi)])
                        P.op("dve", lambda e, Pt=Pt, N=N, j=j, col0=col0: e.tensor_tensor(out=Pt[:, 0:N], in0=Pt[:, 0:N], in1=maskT[:, j, col0:512], op=ALU.mult),
                             reads=[("Pbuf", pi)] + [("maskT", t) for t in range(col0 // 128, 4)], writes=[("Pbuf", pi)])
                        if near:
                            u0 = cs0 + col0 - 128 * j
                            P.op("dve", lambda e, Pt=Pt, N=N, h=h, u0=u0: e.tensor_tensor(out=Pt[:, 0:N], in0=Pt[:, 0:N], in1=EB[:, h, u0:u0 + N], op=ALU.mult),
                                 reads=[("Pbuf", pi), "EB"], writes=[("Pbuf", pi)])
                        w0 = 0 if h % 2 == 0 else 1
                        P.op("pe", lambda e, ob=ob, j=j, m=m, w0=w0, Pt=Pt, col0=col0, N=N, jmax=jmax: e.matmul(PS(ob)[:, col0:512], lhsT=Vp[:, j, m, w0:w0 + 128], rhs=Pt[:, 0:N],
                                                                                                         start=(j == 0), stop=(j == jmax)),
                             reads=["Vp", ("Pbuf", pi)], writes=[("ps", ob)])
                    P.op("act", lambda e, ob=ob: e.activation(out=o_f32, in_=PS(ob)[:, :], func=AF.Copy), reads=[("ps", ob)], writes=["o_f32"])
                    db = nb([0, 1, 2, 3])
                    P.op("pe", lambda e, db=db, h=h: e.matmul(PS(db)[:, :], lhsT=sel_f[:, (h % 2) * 128:(h % 2) * 128 + 128], rhs=o_f32, start=True, stop=True),
                         reads=["c_sel", "o_f32"], writes=[("ps", db)])
                    P.op("act", lambda e, db=db, bp=bp: e.activation(out=rec[bp:bp + 64, :], in_=PS(db)[bp:bp + 64, :], func=AF.Ln), reads=[("ps", db)], writes=["rec"])
                    P.op("act", lambda e, bp=bp: e.activation(out=rec[bp:bp + 64, :], in_=rec[bp:bp + 64, :], func=AF.Exp, scale=-1.0), reads=["rec"], writes=["rec"])
                    P.op("dve", lambda e, bp=bp, m=m: e.tensor_tensor(out=oaT[bp:bp + 64, m, cs0:cs0 + 512], in0=o_f32[bp:bp + 64, :], in1=rec[bp:bp + 64, :], op=ALU.mult),
                         reads=["o_f32", "rec"], writes=[("oaT", c)])
            phase_barrier()
            if stop_after == "P2":
                break

            w3 = dD(0, 24 * KB, BF16, "p (k c) -> p k c", k=8)
            ebuf = [dD(24 * KB + i * 2 * KB, 2 * KB, F32) for i in range(4)]
            spbuf = [dD(32 * KB + i * KB, 1 * KB, BF16) for i in range(6)]
            tbuf = [dD(38 * KB + i * 2 * KB, 2 * KB, F32) for i in range(3)]
            abuf = [dD(44 * KB + i * KB, 1 * KB, BF16) for i in range(4)]
            sbrr = [0, 0, 0, 0]
            P.dma("pool", "w3", lambda e: e.dma_start(out=w3.rearrange("p k c -> p (k c)"), in_=w3_d[:, :], max_dma_last_dim=8192), writes=["w3"])
            if stop_after == "P3w":
                break
            for cc in range(8):
                for c in range(NCH):
                    pb = proj_T(w3, "w3", cc, c, [0, 1, 2, 3])
                    cs = slice(c * 512, (c + 1) * 512)
                    if cc < 4:
                        evac(qbT[:, cc, cs], PS(pb)[:, :], [("ps", pb)], [("qbT", c)])
                    else:
                        evac(kbT[:, cc - 4, cs], PS(pb)[:, :], [("ps", pb)], [("kbT", c)])
            if stop_after == "P3q":
                break
            for j in range(NT):
                pb = nb([0, 1, 2, 3])
                for k in range(8):
                    P.op("pe", lambda e, pb=pb, j=j, k=k: e.matmul(PS(pb)[:, :], lhsT=hT[:, k, j * 128:(j + 1) * 128], rhs=w3[:, k, 1024:1536], start=(k == 0), stop=(k == 7)),
                         reads=["w3", ("hT", j // 4)], writes=[("ps", pb)])
                evac(vb[:, j, :], PS(pb)[:, :], [("ps", pb)], [("vb", j // 4)])
            if stop_after == "P3a":
                break
            for c in range(NCH):
                cs0 = c * 512
                jmax = 4 * c + 3
                for hp in range(4):
                    m = hp
                    units = [(j, hh) for j in range(jmax, -1, -1) for hh in range(2)]
                    stt = {}
                    prev_sp = {0: None, 1: None}

                    def S1(u):
                        j, hh = u
                        bp = hh * 64
                        col0 = max(0, j - 4 * c) * 128
                        N = 512 - col0
                        zb = nb([0, 1, 2, 5])
                        P.op("pe", lambda e, zb=zb, bp=bp, j=j, col0=col0, N=N: e.matmul(PS(zb)[:, 0:N], lhsT=kbT[bp:bp + 64, m, j * 128:(j + 1) * 128],
                                                                                    rhs=qbT[bp:bp + 64, m, cs0 + col0:cs0 + 512], start=True, stop=True),
                             reads=[("kbT", j // 4), ("qbT", c)], writes=[("ps", zb)])
                        ei = sbrr[0] % 4
                        si = sbrr[1] % 6
                        sbrr[0] += 1
                        sbrr[1] += 1
                        eb_, spt = ebuf[ei], spbuf[si]
                        P.op("act", lambda e, zb=zb, eb_=eb_, N=N: e.activation(out=eb_[:, 0:N], in_=PS(zb)[:, 0:N], func=AF.Exp, scale=ATTN_SCALE),
                             reads=[("ps", zb)], writes=[("ebuf", ei)])
                        if j >= 4 * c:
                            P.op("dve", lambda e, eb_=eb_: e.tensor_tensor(out=eb_[:, 0:128], in0=eb_[:, 0:128], in1=smask[:], op=ALU.mult),
                                 reads=[("ebuf", ei), "c_smask"], writes=[("ebuf", ei)])
                        P.op("act", lambda e, eb_=eb_, spt=spt, N=N: e.activation(out=spt[:, 0:N], in_=eb_[:, 0:N], func=AF.Ln, bias=1.0, scale=1.0),
                             reads=[("ebuf", ei)], writes=[("spbuf", si)])
                        stt[u] = dict(ei=ei, si=si, col0=col0, N=N)

                    def S2(u):
                        j, hh = u
                        d = stt[u]
                        xb = 3 + hh
                        col0, N = d["col0"], d["N"]
                        spt = spbuf[d["si"]]
                        pv = prev_sp[hh]
                        if pv is not None:
                            psi, pcol0, pN = pv
                            P.op("pe", lambda e, xb=xb, psi=psi, pcol0=pcol0, pN=pN: e.matmul(PS(xb)[:, pcol0:512], lhsT=smask[:], rhs=spbuf[psi][:, 0:pN], start=False, stop=True, skip_group_check=True),
                                 reads=["c_smask", ("spbuf", psi)], writes=[("ps", xb)])
                        P.op("pe", lambda e, xb=xb, spt=spt, col0=col0, N=N, first=(pv is None): e.matmul(PS(xb)[:, col0:512], lhsT=uinc[:], rhs=spt[:, 0:N], start=first, stop=True, skip_group_check=True),
                             reads=["c_uinc", ("spbuf", d["si"])], writes=[("ps", xb)])
                        prev_sp[hh] = (d["si"], col0, N)
                        ti = sbrr[2] % 3
                        ai = sbrr[3] % 4
                        sbrr[2] += 1
                        sbrr[3] += 1
                        d["ai"] = ai
                        P.op("act", lambda e, xb=xb, ti=ti, col0=col0, N=N: e.activation(out=tbuf[ti][:, 0:N], in_=PS(xb)[:, col0:512], func=AF.Exp, scale=-1.0),
                             reads=[("ps", xb)], writes=[("tbuf", ti)])
                        P.op("dve", lambda e, ti=ti, ai=ai, ei=d["ei"], N=N: e.tensor_tensor(out=abuf[ai][:, 0:N], in0=tbuf[ti][:, 0:N], in1=ebuf[ei][:, 0:N], op=ALU.mult),
                             reads=[("tbuf", ti), ("ebuf", d["ei"])], writes=[("abuf", ai)])

                    def S3(u):
                        j, hh = u
                        d = stt[u]
                        ob = 6 + hh
                        col0, N = d["col0"], d["N"]
                        P.op("pe", lambda e, ob=ob, j=j, ai=d["ai"], col0=col0, N=N: e.matmul(PS(ob)[:, col0:512], lhsT=vb[:, j, m * 128:(m + 1) * 128], rhs=abuf[ai][:, 0:N],
                                                                                       start=(j == jmax), stop=(j == 0), skip_group_check=True),
                             reads=[("vb", j // 4), ("abuf", d["ai"])], writes=[("ps", ob)])

                    nu = len(units)
                    for k in range(nu + 2):
                        if k < nu:
                            S1(units[k])
                        if 0 <= k - 1 < nu:
                            S2(units[k - 1])
                        if 0 <= k - 2 < nu:
                            S3(units[k - 2])
                    for hh in range(2):
                        bp = hh * 64
                        evac(obT[bp:bp + 64, m, cs0:cs0 + 512], PS(6 + hh)[bp:bp + 64, :], [("ps", 6 + hh)], [("obT", c)])
            phase_barrier()
            if dbg:
                dtmp = dD(48 * KB, 16 * KB, F32)
                for nm, src, res in (("d_oaT", oaT, "oaT"), ("d_obT", obT, "obT")):
                    for q in range(2):
                        P.op("dve", lambda e, src=src, q=q: e.tensor_copy(out=dtmp, in_=src.rearrange("p k t -> p (k t)")[:, q * 4096:(q + 1) * 4096]),
                             reads=[(res, cq) for cq in range(4)], writes=["dtmp"])
                        if b == 0:
                            P.dma("sp", "dbg", lambda e, nm=nm, q=q: e.dma_start(out=dbg_d[nm][:, q * 4096:(q + 1) * 4096], in_=dtmp), reads=["dtmp"])
                phase_barrier()
            if stop_after == "P3":
                break

            mergedT = dD(0, 32 * KB, BF16, "p (k t) -> p k t", k=8)
            wout = dD(32 * KB, 16 * KB, BF16, "p (k c) -> p k c", k=8)
            wg4 = [dD(48 * KB + i * 4 * KB, 4 * KB, BF16, "p (k c) -> p k c", k=8) for i in range(2)]
            wbr = [dD(56 * KB + i * 2 * KB, 2 * KB, BF16, "p (a k c) -> p a k c", a=2, k=4) for i in range(2)]
            sgb_ = [dD(60 * KB + i * KB, 1 * KB, BF16) for i in range(2)]
            t12 = [dD(62 * KB + i * 2 * KB, 2 * KB, F32) for i in range(2)]
            P.dma("pool", "wout", lambda e: e.dma_start(out=wout.rearrange("p k c -> p (k c)"), in_=wout_d[:, :], max_dma_last_dim=8192), writes=["wout"])
            for m in range(8):
                wsl = m % 2
                P.dma("pool", "wg4_%d" % wsl, lambda e, m=m, wsl=wsl: e.dma_start(out=wg4[wsl].rearrange("p k c -> p (k c)"), in_=wg4_d[:, m * 2048:(m + 1) * 2048], max_dma_last_dim=8192),
                      writes=[("wg4", wsl)])
                P.dma("pool", "wbr_%d" % wsl, lambda e, m=m, wsl=wsl: e.dma_start(out=wbr[wsl].rearrange("p a k c -> p (a k c)"), in_=wbr_d[:, m * 1024:(m + 1) * 1024], max_dma_last_dim=8192),
                      writes=[("wbr", wsl)])
                for c in range(NCH):
                    cs = slice(c * 512, (c + 1) * 512)
                    banks = {}
                    for gi_, nm in enumerate(("ga", "gb")):
                        pb = nb([0, 1, 2, 3, 4, 5, 6, 7])
                        banks[nm] = pb
                        for k in range(8):
                            P.op("pe", lambda e, pb=pb, k=k, gi_=gi_, wsl=wsl, cs=cs: e.matmul(PS(pb)[:, :], lhsT=wg4[wsl][:, k, gi_ * 128:(gi_ + 1) * 128], rhs=hT[:, k, cs],
                                                                                        start=(k == 0), stop=(k == 7)),
                                 reads=[("wg4", wsl), ("hT", c)], writes=[("ps", pb)])
                    for a_, (nm, oT, ores) in enumerate((("ya", oaT, "oaT"), ("yb", obT, "obT"))):
                        pb = nb([0, 1, 2, 3, 4, 5, 6, 7])
                        banks[nm] = pb
                        for k in range(4):
                            P.op("pe", lambda e, pb=pb, k=k, a_=a_, wsl=wsl, oT=oT, cs=cs: e.matmul(PS(pb)[:, :], lhsT=wbr[wsl][:, a_, k, :], rhs=oT[:, k, cs], start=(k == 0), stop=(k == 3)),
                                 reads=[("wbr", wsl), (ores, c)], writes=[("ps", pb)])
                    i0 = 0
                    sa, sb_ = sgb_[i0], sgb_[i0 + 1]
                    P.op("act", lambda e, sa=sa, pb=banks["ga"]: e.activation(out=sa, in_=PS(pb)[:, :], func=AF.Sigmoid), reads=[("ps", banks["ga"])], writes=[("sg", i0)])
                    P.op("act", lambda e, sb_=sb_, pb=banks["gb"]: e.activation(out=sb_, in_=PS(pb)[:, :], func=AF.Sigmoid), reads=[("ps", banks["gb"])], writes=[("sg", i0 + 1)])
                    P.op("dve", lambda e, sa=sa, pb=banks["ya"]: e.tensor_tensor(out=t12[0], in0=sa, in1=PS(pb)[:, :], op=ALU.mult), reads=[("sg", i0), ("ps", banks["ya"])], writes=["t1"])
                    P.op("dve", lambda e, sb_=sb_, pb=banks["yb"]: e.tensor_tensor(out=t12[1], in0=sb_, in1=PS(pb)[:, :], op=ALU.mult), reads=[("sg", i0 + 1), ("ps", banks["yb"])], writes=["t2"])
                    P.op("dve", lambda e, m=m, cs=cs: e.tensor_tensor(out=mergedT[:, m, cs], in0=t12[0], in1=t12[1], op=ALU.add), reads=["t1", "t2"], writes=[("mergedT", c)])
            phase_barrier()
            for i in range(NT):
                P.dma("sp", "x1ld%d" % (i % 4), lambda e, i=i: e.dma_start(out=x1[:, i, :], in_=x_d[b, i * 128:(i + 1) * 128, :]), writes=[("x1", i)])
                pb0 = (i % 4) * 2
                for half in range(2):
                    pb = pb0 + half
                    for k in range(8):
                        P.op("pe", lambda e, pb=pb, k=k, i=i, half=half: e.matmul(PS(pb)[:, :], lhsT=mergedT[:, k, i * 128:(i + 1) * 128], rhs=wout[:, k, half * 512:(half + 1) * 512],
                                                                               start=(k == 0), stop=(k == 7)),
                             reads=[("mergedT", i // 4), "wout"], writes=[("ps", pb)])
                    P.op("dve", lambda e, pb=pb, i=i, half=half: e.tensor_tensor(out=x1[:, i, half * 512:(half + 1) * 512], in0=x1[:, i, half * 512:(half + 1) * 512], in1=PS(pb)[:, :], op=ALU.add),
                         reads=[("ps", pb), ("x1", i)], writes=[("x1", i)])
            phase_barrier()
            if dbg and b == 0:
                P.dma("sp", "dbg", lambda e: e.dma_start(out=dbg_d["d_x1"][:, :], in_=x1.rearrange("p i d -> p (i d)")), reads=[("x1", i) for i in range(NT)])
                phase_barrier()
            if stop_after == "P4":
                break

            h2T = hT
            xnf = dD(0, 4 * KB, F32)
            hTf = dD(4 * KB, 4 * KB, F32, "p (k t) -> p k t", k=8)
            comb = dD(8 * KB, 2 * KB, F32, "p (i e) -> p i e", i=16)
            elm = dD(10 * KB, 256, F32)
            m8 = dD(10 * KB + 256, 64, F32)
            eq = dD(10 * KB + 320, 256, F32)
            sgm = [dD(12 * KB + i * KB, 1 * KB, BF16) for i in range(4)]
            hid = [dD(16 * KB + i * 2 * KB, 2 * KB, BF16, "p (f t) -> p f t", f=2) for i in range(2)]
            RG, RS, RW = 60, 61, 62

            def ffn_side(i, xt, xres, rsc, xn, xnres):
                P.op("dve", lambda e, xt=xt, rsc=rsc: e.tensor_scalar(out=xnf, in0=xt, scalar1=rsc, scalar2=None, op0=ALU.mult), reads=[xres, ("rs", i)], writes=["xnf"])
                for half in range(2):
                    pb = 4 + half
                    for kk in range(4):
                        k = half * 4 + kk
                        P.op("pe", lambda e, pb=pb, kk=kk, k=k: e.transpose(out=PS(pb)[:, kk * 128:(kk + 1) * 128], in_=xnf[:, k * 128:(k + 1) * 128], identity=ident_f[:]),
                             reads=["xnf", "c_ident"], writes=[("ps", pb)])
                    pv = PS(pb)[:, :].rearrange("p (k t) -> p k t", k=4)
                    gf = gpk[:, 8 + half * 4:8 + half * 4 + 4]
                    P.op("dve", lambda e, pv=pv, half=half, i=i: e.tensor_tensor(out=h2T[:, half * 4:half * 4 + 4, i * 128:(i + 1) * 128], in0=pv, in1=gB[1][:, half * 4:half * 4 + 4, :], op=ALU.mult),
                         reads=[("ps", pb), "gB"], writes=[("hT", i // 4)])
                    P.op("dve", lambda e, pv=pv, half=half, gf=gf: e.tensor_tensor(out=hTf[:, half * 4:half * 4 + 4, :], in0=pv, in1=gf.unsqueeze(2).broadcast_to([128, 4, 128]), op=ALU.mult),
                         reads=[("ps", pb), "c_g1"], writes=["hTf"])
                for k in range(8):
                    P.op("pe", lambda e, k=k: e.matmul(PS(6)[:, 0:36], lhsT=hTf[:, k, :], rhs=wr_f[:, k, :], start=(k == 0), stop=(k == 7)),
                         reads=["hTf", "c_wr"], writes=[("ps", 6)])
                sm = lambda col: small[:, col:col + 1]
                P.op("dve", lambda e: e.tensor_tensor(out=elm[:, 0:36], in0=PS(6)[:, 0:36], in1=brb[:], op=ALU.add), reads=[("ps", 6), "c_br"], writes=["elm"])
                P.op("dve", lambda e: e.tensor_reduce(out=sm(RG), in_=elm[:, 0:4], axis=AX.X, op=ALU.max), reads=["elm"], writes=["rg"])
                P.op("dve", lambda e: e.tensor_scalar(out=eq[:, 0:4], in0=elm[:, 0:4], scalar1=sm(RG), scalar2=None, op0=ALU.is_ge), reads=["elm", "rg"], writes=["eq"])
                P.op("dve", lambda e: e.tensor_scalar(out=eq[:, 4:8], in0=eq[:, 0:4], scalar1=-1.0, scalar2=-NEG, op0=ALU.add, op1=ALU.mult), reads=["eq"], writes=["eq"])
                P.op("dve", lambda e: e.tensor_tensor(out=elm[:, 4:36].rearrange("p (g x) -> p g x", g=4), in0=elm[:, 4:36].rearrange("p (g x) -> p g x", g=4),
                                                      in1=eq[:, 4:8].unsqueeze(2).broadcast_to([128, 4, 8]), op=ALU.add), reads=["elm", "eq"], writes=["elm"])
                P.op("dve", lambda e: e.tensor_scalar(out=sm(RW), in0=sm(RG), scalar1=-1.0, scalar2=None, op0=ALU.mult), reads=["rg"], writes=["rw"])
                P.op("act", lambda e: e.activation(out=eq[:, 8:12], in_=elm[:, 0:4], func=AF.Exp, bias=sm(RW), scale=1.0, accum_out=sm(RS)), reads=["elm", "rw"], writes=["eq", "rs_"])
                P.op("dve", lambda e: e.reciprocal(out=sm(RS), in_=sm(RS)), reads=["rs_"], writes=["rs_"])
                P.op("dve", lambda e: e.max(out=m8[:, 0:8], in_=elm[:, 4:36]), reads=["elm"], writes=["m8"])
                P.op("dve", lambda e: e.tensor_tensor(out=m8[:, 8:9], in0=m8[:, 1:2], in1=m8[:, 0:1], op=ALU.subtract), reads=["m8"], writes=["m8"])
                P.op("act", lambda e: e.activation(out=m8[:, 8:9], in_=m8[:, 8:9], func=AF.Exp), reads=["m8"], writes=["m8"])
                P.op("dve", lambda e: e.tensor_scalar(out=m8[:, 8:9], in0=m8[:, 8:9], scalar1=1.0, scalar2=None, op0=ALU.add), reads=["m8"], writes=["m8"])
                P.op("dve", lambda e: e.reciprocal(out=m8[:, 9:10], in_=m8[:, 8:9]), reads=["m8"], writes=["m8"])
                P.op("dve", lambda e: e.tensor_scalar(out=m8[:, 10:11], in0=m8[:, 9:10], scalar1=-1.0, scalar2=1.0, op0=ALU.mult, op1=ALU.add), reads=["m8"], writes=["m8"])
                P.op("dve", lambda e: e.tensor_tensor(out=m8[:, 9:11], in0=m8[:, 9:11], in1=sm(RS).broadcast_to([128, 2]), op=ALU.mult), reads=["m8", "rs_"], writes=["m8"])
                P.op("dve", lambda e: e.tensor_scalar(out=eq[:, 0:32], in0=elm[:, 4:36], scalar1=m8[:, 0:1], scalar2=m8[:, 9:10], op0=ALU.is_equal, op1=ALU.mult), reads=["elm", "m8"], writes=["eq"])
                P.op("dve", lambda e: e.tensor_scalar(out=eq[:, 32:64], in0=elm[:, 4:36], scalar1=m8[:, 1:2], scalar2=m8[:, 10:11], op0=ALU.is_equal, op1=ALU.mult), reads=["elm", "m8"], writes=["eq"])
                P.op("dve", lambda e, i=i: e.tensor_tensor(out=comb[:, i, :], in0=eq[:, 0:32], in1=eq[:, 32:64], op=ALU.add), reads=["eq"], writes=["comb"])

            build_gB(1)
            norm_transpose(b, "x1", 1, h2T, "hT", None, [dD(20 * KB, 2 * KB, BF16), dD(22 * KB, 2 * KB, BF16)], None, f32_side=ffn_side)
            phase_barrier()
            for ex in range(N_EXP):
                wsl = ex % 2
                wm = wmoe[wsl]
                P.dma("pool", "wmoe%d" % wsl, lambda e, wm=wm, ex=ex: e.dma_start(out=wm, in_=wmoe_d[ex, :, :], max_dma_last_dim=8192), writes=[("wmoe", wsl)])
                wgv = wm[:, 0:2048].rearrange("p (k c) -> p k c", k=8)
                wuv_ = wm[:, 2048:4096].rearrange("p (k c) -> p k c", k=8)
                wdv = wm[:, 4096:6144].rearrange("p (f c) -> p f c", f=2)
                for c in range(NCH):
                    cs = slice(c * 512, (c + 1) * 512)
                    hs = rr[0] % 2
                    rr[0] += 1
                    for f in range(2):
                        gb_, ub_ = f * 2, f * 2 + 1
                        for k in range(8):
                            P.op("pe", lambda e, gb_=gb_, k=k, f=f, wgv=wgv, cs=cs: e.matmul(PS(gb_)[:, :], lhsT=wgv[:, k, f * 128:(f + 1) * 128], rhs=h2T[:, k, cs], start=(k == 0), stop=(k == 7)),
                                 reads=[("wmoe", wsl), ("hT", c)], writes=[("ps", gb_)])
                        for k in range(8):
                            P.op("pe", lambda e, ub_=ub_, k=k, f=f, wuv_=wuv_, cs=cs: e.matmul(PS(ub_)[:, :], lhsT=wuv_[:, k, f * 128:(f + 1) * 128], rhs=h2T[:, k, cs], start=(k == 0), stop=(k == 7)),
                                 reads=[("wmoe", wsl), ("hT", c)], writes=[("ps", ub_)])
                        sgi = (hs * 2 + f)
                        P.op("act", lambda e, gb_=gb_, sgi=sgi: e.activation(out=sgm[sgi], in_=PS(gb_)[:, :], func=AF.Silu), reads=[("ps", gb_)], writes=[("sgm", sgi)])
                        P.op("dve", lambda e, ub_=ub_, sgi=sgi, hs=hs, f=f: e.tensor_tensor(out=hid[hs][:, f, :], in0=sgm[sgi], in1=PS(ub_)[:, :], op=ALU.mult),
                             reads=[("sgm", sgi), ("ps", ub_)], writes=[("hid", hs)])
                    for tl in range(4):
                        i = c * 4 + tl
                        for half in range(2):
                            pb = 4 + (rr[0] % 4)
                            rr[0] += 1
                            for f in range(2):
                                P.op("pe", lambda e, pb=pb, f=f, hs=hs, tl=tl, wdv=wdv, half=half: e.matmul(PS(pb)[:, :], lhsT=hid[hs][:, f, tl * 128:(tl + 1) * 128], rhs=wdv[:, f, half * 512:(half + 1) * 512],
                                                                                                     start=(f == 0), stop=(f == 1)),
                                     reads=[("hid", hs), ("wmoe", wsl)], writes=[("ps", pb)])
                            P.op("dve", lambda e, pb=pb, i=i, half=half, ex=ex: e.scalar_tensor_tensor(out=x1[:, i, half * 512:(half + 1) * 512], in0=PS(pb)[:, :], scalar=comb[:, i, ex:ex + 1],
                                                                                                  in1=x1[:, i, half * 512:(half + 1) * 512], op0=ALU.mult, op1=ALU.add),
                                 reads=[("ps", pb), "comb", ("x1", i)], writes=[("x1", i)])
            phase_barrier()
            if stop_after == "P5":
                break

            h3T = hT
            P.dma("pool", "wpg", lambda e: e.dma_start(out=wpg.rearrange("p k c -> p (k c)"), in_=wpg_d[:, :], max_dma_last_dim=8192), writes=["wpg"])
            P.dma("pool", "wpl", lambda e: e.dma_start(out=wpl.rearrange("p k c -> p (k c)"), in_=wpl_d[:, :], max_dma_last_dim=8192), writes=["wpl"])
            build_gB(2)
            norm_transpose(b, "x1", 2, h3T, "hT", None, [dD(0, 2 * KB, BF16), dD(2 * KB, 2 * KB, BF16)], [6, 7])
            pt_b = [dD(4 * KB + i * KB, 1 * KB, F32) for i in range(2)]
            pT_b = [dD(6 * KB + i * 512, 512, BF16, "p (k t) -> p k t", k=2) for i in range(2)]
            sig = [dD(8 * KB + i * 2 * KB, 2 * KB, BF16) for i in range(2)]
            tmpf = [dD(12 * KB + i * 4 * KB, 4 * KB, F32) for i in range(2)]
            outb = [dD(20 * KB + i * 4 * KB, 4 * KB, F32) for i in range(2)]
            junkf = dD(28 * KB, 2 * KB, BF16)
            gfin = dD(30 * KB, 4 * KB, F32)
            P.dma("sp", "c_gfin", lambda e: e.dma_start(out=gfin, in_=gfin_d[:, :]), writes=["c_gfin"])
            for i in range(NT):
                sl = i % 2
                P.dma("sp", "pt%d" % sl, lambda e, i=i, sl=sl: e.dma_start(out=pt_b[sl], in_=p_d[b, i * 128:(i + 1) * 128, :]), writes=[("pt", sl)])
                for k in range(2):
                    P.op("pe", lambda e, k=k, sl=sl: e.transpose(out=PS(5)[:, k * 128:(k + 1) * 128], in_=pt_b[sl][:, k * 128:(k + 1) * 128], identity=ident_f[:]),
                         reads=[("pt", sl), "c_ident"], writes=[("ps", 5)])
                P.op("act", lambda e, sl=sl: e.activation(out=pT_b[sl].rearrange("p k t -> p (k t)"), in_=PS(5)[:, 0:256], func=AF.Copy), reads=[("ps", 5)], writes=[("pT", sl)])
                for half in range(2):
                    gbk = half
                    pbk = 2 + half
                    for k in range(8):
                        P.op("pe", lambda e, gbk=gbk, k=k, i=i, half=half: e.matmul(PS(gbk)[:, :], lhsT=h3T[:, k, i * 128:(i + 1) * 128], rhs=wpg[:, k, half * 512:(half + 1) * 512], start=(k == 0), stop=(k == 7)),
                             reads=[("hT", i // 4), "wpg"], writes=[("ps", gbk)])
                    for k in range(2):
                        P.op("pe", lambda e, pbk=pbk, k=k, sl=sl, half=half: e.matmul(PS(pbk)[:, :], lhsT=pT_b[sl][:, k, :], rhs=wpl[:, k, half * 512:(half + 1) * 512], start=(k == 0), stop=(k == 1)),
                             reads=[("pT", sl), "wpl"], writes=[("ps", pbk)])
                    hsl = slice(half * 512, (half + 1) * 512)
                    P.op("act", lambda e, gbk=gbk, sl=sl, hsl=hsl: e.activation(out=sig[sl][:, hsl], in_=PS(gbk)[:, :], func=AF.Sigmoid), reads=[("ps", gbk)], writes=[("sig", sl, half)])
                    P.op("dve", lambda e, pbk=pbk, sl=sl, hsl=hsl: e.tensor_tensor(out=tmpf[sl][:, hsl], in0=sig[sl][:, hsl], in1=PS(pbk)[:, :], op=ALU.mult),
                         reads=[("sig", sl, half), ("ps", pbk)], writes=[("tmpf", sl, half)])
                    P.op("dve", lambda e, sl=sl, hsl=hsl, i=i: e.tensor_tensor(out=tmpf[sl][:, hsl], in0=tmpf[sl][:, hsl], in1=x1[:, i, hsl], op=ALU.add),
                         reads=[("tmpf", sl, half), ("x1", i)], writes=[("tmpf", sl, half)])
                ssc = small[:, 64 + i:65 + i]
                P.op("act", lambda e, sl=sl, ssc=ssc: e.activation(out=junkf, in_=tmpf[sl], func=AF.Square, accum_out=ssc), reads=[("tmpf", sl, 0), ("tmpf", sl, 1)], writes=["junkf", ("fs", i)])
                P.op("dve", lambda e, ssc=ssc: e.tensor_scalar(out=ssc, in0=ssc, scalar1=1.0 / D, scalar2=EPS, op0=ALU.mult, op1=ALU.add), reads=[("fs", i)], writes=[("fs", i)])
                P.op("act", lambda e, ssc=ssc: e.activation(out=ssc, in_=ssc, func=AF.Sqrt), reads=[("fs", i)], writes=[("fs", i)])
                P.op("dve", lambda e, ssc=ssc: e.reciprocal(out=ssc, in_=ssc), reads=[("fs", i)], writes=[("fs", i)])
                P.op("dve", lambda e, sl=sl, ssc=ssc: e.scalar_tensor_tensor(out=outb[sl], in0=tmpf[sl], scalar=ssc, in1=gfin, op0=ALU.mult, op1=ALU.mult),
                     reads=[("tmpf", sl, 0), ("tmpf", sl, 1), ("fs", i), "c_gfin"], writes=[("outb", sl)])
                tok = P.dma("sp", "out%d" % sl, lambda e, sl=sl, i=i: e.dma_start(out=out_d[b, i * 128:(i + 1) * 128, :], in_=outb[sl]), reads=[("outb", sl)])
                last_out_tokens.append(tok)
            phase_barrier()

        finals = {}
        for tok in last_out_tokens:
            finals[tok[1]] = max(finals.get(tok[1], 0), tok[2])
        for k in P.dma_keys:
            if k == "dbg":
                finals[k] = P.dma_count[k]
        P.emit(final_wait_tokens=[("d", k, n) for k, n in finals.items()])
    return nc


def _pk(w, ncols=None):
    K = w.shape[0] // 128
    return np.ascontiguousarray(w.reshape(K, 128, -1).transpose(1, 0, 2).reshape(128, -1))


def _t5_bucket_np(d):
    d = np.maximum(d, 0)
    d_f = np.maximum(d, 1).astype(np.float32)
    large = 16 + (np.log(d_f / np.float32(16)) / np.float32(math.log(128 / 16)) * np.float32(16)).astype(np.int32)
    large = np.minimum(large, 31)
    return np.where(d < 16, d, large)


def prep_weights(inp):
    f = lambda a: np.ascontiguousarray(a, dtype=np.float32)
    w_in = inp["w_in"][0]
    o = {}
    c0 = 0
    sec = {}
    for nm, wdt in (("q_a", 512), ("c_kv", 128), ("q_idx", 512), ("k_idx", 64), ("w_idx", 8), ("qkv_b", 1536), ("gate_a", 1024), ("gate_b", 1024)):
        sec[nm] = w_in[:, c0:c0 + wdt]
        c0 += wdt
    w1 = np.concatenate([sec["q_a"], sec["q_idx"], sec["c_kv"], sec["k_idx"], sec["k_idx"]], axis=1)
    o["w1"] = _pk(w1)
    o["widx"] = _pk(sec["w_idx"])
    o["w3"] = _pk(sec["qkv_b"])
    ga = sec["gate_a"].reshape(1024, 8, 128)
    gb = sec["gate_b"].reshape(1024, 8, 128)
    g4 = np.concatenate([ga, gb], axis=2)
    g4 = g4.reshape(8, 128, 8, 256).transpose(1, 2, 0, 3)
    o["wg4"] = f(g4.reshape(128, -1))
    wa = inp["w_branch_a"][0].reshape(4, 128, 8, 128)
    wb = inp["w_branch_b"][0].reshape(4, 128, 8, 128)
    wbr = np.stack([wa, wb], axis=0).transpose(2, 3, 0, 1, 4)
    o["wbr"] = f(wbr.reshape(128, -1))
    o["wout"] = _pk(inp["w_out"][0])
    wuk = inp["w_uk"][0]
    wukT = wuk.transpose(0, 2, 1).reshape(4, 2, 64, 128).transpose(1, 2, 0, 3)
    o["wuk"] = f(wukT.reshape(128, 512))
    o["wuv"] = f(inp["w_uv"][0].transpose(1, 0, 2).reshape(128, 512))
    wg = inp["w_gate"][0].reshape(N_EXP, 8, 128, 256).transpose(0, 2, 1, 3).reshape(N_EXP, 128, 2048)
    wu = inp["w_up"][0].reshape(N_EXP, 8, 128, 256).transpose(0, 2, 1, 3).reshape(N_EXP, 128, 2048)
    wd = inp["w_down"][0].reshape(N_EXP, 2, 128, 1024).transpose(0, 2, 1, 3).reshape(N_EXP, 128, 2048)
    o["wmoe"] = f(np.concatenate([wg, wu, wd], axis=2))
    wr = np.concatenate([inp["w_r1"][0], inp["w_r2"][0].transpose(1, 0, 2).reshape(1024, 32)], axis=1)
    o["wr"] = _pk(wr)
    br = np.concatenate([inp["b_r1"][0], inp["b_r2"][0].reshape(32)])
    o["br"] = f(np.broadcast_to(br[None, :], (128, 36)))
    o["wpg"] = _pk(inp["w_ple_gate"][0])
    o["wpl"] = _pk(inp["w_ple"][0])
    o["g_attn"] = f(inp["attn_norm"][0].reshape(8, 128).T)
    o["g_ffn"] = f(inp["ffn_norm"][0].reshape(8, 128).T)
    o["g_ple"] = f(inp["ple_norm"][0].reshape(8, 128).T)
    o["g_fin"] = f(np.broadcast_to(inp["final_norm"][None, :], (128, 1024)))
    o["g_kv"] = f(inp["kv_norm"][0].reshape(128, 1))
    rb = inp["rel_bias"]
    s_l = np.arange(128)[:, None]
    u = np.arange(640)[None, :]
    bidx = _t5_bucket_np(u - s_l)
    o["btoep"] = f(rb[bidx].transpose(0, 2, 1).reshape(128, 8 * 640))
    o["b31"] = f(np.broadcast_to(rb[31][None, :], (128, 8)))
    o["ident"] = np.eye(128, dtype=np.float32)
    tt = np.arange(128)[:, None]
    ss = np.arange(128)[None, :]
    o["cneg"] = np.where(ss <= tt, 0.0, NEG).astype(np.float32)
    o["smask"] = (tt < ss).astype(np.float32)
    o["uinc"] = (tt >= ss).astype(np.float32)
    sel = np.zeros((128, 256), np.float32)
    sel[64, 0:128] = 1.0
    sel[63, 128:256] = 1.0
    o["sel"] = sel
    return o


_NC_CACHE = {}


def kernel(**inputs):
    inp = {k: np.asarray(v) for k, v in inputs.items()}
    n = 8
    NB = 2
    wts = prep_weights(inp)
    x = np.ascontiguousarray(inp["x"], dtype=np.float32)
    p = np.ascontiguousarray(inp["p"][0], dtype=np.float32)
    if "nc" not in _NC_CACHE:
        _NC_CACHE["nc"] = build_nc(NB=NB)
    nc = _NC_CACHE["nc"]
    in_maps = []
    for c in range(n):
        m = dict(wts)
        m["x"] = x[c * NB:(c + 1) * NB]
        m["p"] = p[c * NB:(c + 1) * NB]
        in_maps.append(m)
    res = run_bass_kernel_spmd(nc, in_maps, core_ids=list(range(n)))
    out = np.concatenate([r["out"] for r in res.results], axis=0)
    return out.astype(np.float32)
```

```python
import math
import types
import contextlib
import numpy as np
import concourse.bass as bass
import concourse.mybir as mybir
from concourse.bass_utils import run_bass_kernel_spmd

F32 = mybir.dt.float32
BF16 = mybir.dt.bfloat16
AF = mybir.ActivationFunctionType
ALU = mybir.AluOpType
AX = mybir.AxisListType

S = 2048
D = 1024
NT = S // 128
NCH = S // 512
ATTN_SCALE = 64 ** -0.5
IDX_SCALE = (8 ** -0.5) * (64 ** -0.5)
EPS = 1e-6
NEG = -1.0e30
N_BISECT = 14
N_EXP = 32
import os as _os
SBE = _os.environ.get("SBE", "pool")
SBLN = _os.environ.get("SBLN", "1") == "1"


class Prog:
    ENGS = ("pe", "act", "dve", "pool", "sp")

    def __init__(self, nc, same_eng_sync=True):
        self.nc = nc
        self.ops = {e: [] for e in self.ENGS}
        self.last_w = {}
        self.last_r = {}
        self.clock = {e: {} for e in self.ENGS}
        self.opclock = {}
        self.dma_count = {}
        self.dma_keys = []
        self.signaling = set()
        self.same_eng_sync = same_eng_sync
        self._bar = 0

    def _add(self, eng, fn, reads, writes, dma_key=None, n_dma=1):
        idx = len(self.ops[eng]) + 1
        deps = {}

        def need(tok):
            kind, who, n = tok
            if kind == "e" and who == eng:
                if eng in ("pe", "sp") or not self.same_eng_sync:
                    return
            k = (kind, who)
            if deps.get(k, 0) < n:
                deps[k] = n

        for r in reads:
            for tok in self.last_w.get(r, {}).values():
                need(tok)
        for w in writes:
            for tok in self.last_w.get(w, {}).values():
                need(tok)
            for tok in self.last_r.get(w, {}).values():
                need(tok)
        if dma_key is not None:
            if dma_key not in self.dma_count:
                self.dma_count[dma_key] = 0
                self.dma_keys.append(dma_key)
            prev = self.dma_count[dma_key]
            if prev > 0:
                need(("d", dma_key, prev))
            self.dma_count[dma_key] = prev + n_dma
            mytok = ("d", dma_key, prev + n_dma)
        else:
            mytok = ("e", eng, idx)
        clk = self.clock[eng]
        final = []
        for k, n in deps.items():
            if clk.get(k, 0) >= n:
                continue
            final.append((k[0], k[1], n))
        for kind, who, n in final:
            oc = self.opclock.get((kind, who, n))
            if oc:
                for k2, n2 in oc.items():
                    if clk.get(k2, 0) < n2:
                        clk[k2] = n2
            if clk.get((kind, who), 0) < n:
                clk[(kind, who)] = n
            if kind == "e":
                self.signaling.add((who, n))
        snap = dict(clk)
        if mytok[0] == "e":
            snap[("e", eng)] = idx
        self.opclock[mytok] = snap
        self.ops[eng].append((fn, final, dma_key, mytok))
        if fn is not None:
            for r in reads:
                self.last_r.setdefault(r, {})[(mytok[0], mytok[1])] = mytok
        for w in writes:
            self.last_w[w] = {(mytok[0], mytok[1]): mytok}
            self.last_r[w] = {}
        return mytok

    @staticmethod
    def _freeze(fn):
        if fn is None or getattr(fn, "__closure__", None) is None:
            return fn
        cells = []
        for c in fn.__closure__:
            try:
                cells.append(types.CellType(c.cell_contents))
            except ValueError:
                cells.append(c)
        return types.FunctionType(fn.__code__, fn.__globals__, fn.__name__, fn.__defaults__, tuple(cells))

    def op(self, eng, fn, reads=(), writes=()):
        return self._add(eng, self._freeze(fn), tuple(reads), tuple(writes))

    def dma(self, eng, key, fns, reads=(), writes=()):
        if not isinstance(fns, (list, tuple)):
            fns = [fns]
        return self._add(eng, [self._freeze(f) for f in fns], tuple(reads), tuple(writes), dma_key=key, n_dma=len(fns))

    def barrier(self, tiny_fn):
        self._bar += 1
        res = ("__barrier__", self._bar)
        allres = list(set(list(self.last_w.keys()) + list(self.last_r.keys())))
        self._add("dve", self._freeze(tiny_fn), tuple(), tuple(allres) + (res,))
        for e in ("pe", "act", "pool", "sp"):
            self._add(e, None, (res,), tuple())

    def emit(self, final_wait_tokens=()):
        nc = self.nc
        sigval = {}
        for e in self.ENGS:
            s = 0
            for i in range(1, len(self.ops[e]) + 1):
                if (e, i) in self.signaling:
                    s += 1
                    sigval[(e, i)] = s
        engobj = {"pe": nc.tensor, "act": nc.scalar, "dve": nc.vector, "pool": nc.gpsimd, "sp": nc.sync}
        with contextlib.ExitStack() as st:
            esem = {e: st.enter_context(nc.semaphore("sem_" + e)) for e in self.ENGS}
            dsem = {k: st.enter_context(nc.semaphore("dsem_%d" % i)) for i, k in enumerate(self.dma_keys)}
            block = st.enter_context(nc.Block())

            def run(e):
                eng = engobj[e]
                for i, (fn, deps, dma_key, mytok) in enumerate(self.ops[e], start=1):
                    for kind, who, n in deps:
                        if kind == "e":
                            eng.wait_ge(esem[who], sigval[(who, n)])
                        else:
                            eng.wait_ge(dsem[who], 16 * n)
                    if fn is None:
                        assert (e, i) not in self.signaling
                        continue
                    if dma_key is not None:
                        for f in fn:
                            f(eng).then_inc(dsem[dma_key], 16)
                    else:
                        ins = fn(eng)
                        if (e, i) in self.signaling:
                            ins.then_inc(esem[e], 1)
                if e == "sp":
                    for k in self.dma_keys:
                        eng.wait_ge(dsem[k], 16 * self.dma_count[k])

            block.tensor(lambda eng: run("pe"))
            block.scalar(lambda eng: run("act"))
            block.vector(lambda eng: run("dve"))
            block.gpsimd(lambda eng: run("pool"))
            block.sync(lambda eng: run("sp"))


def build_nc(NB=2, stop_after=None, dbg=False):
    nc = bass.Bass("TRN2", target_bir_lowering=False)

    def din(name, shape, dt=F32):
        return nc.dram_tensor(name, list(shape), dt, kind="ExternalInput").ap()

    x_d = din("x", [NB, S, D])
    p_d = din("p", [NB, S, 256])
    w1_d = din("w1", [128, 8 * 1280])
    widx_d = din("widx", [128, 8 * 8])
    w3_d = din("w3", [128, 8 * 1536])
    wg4_d = din("wg4", [128, 8 * 8 * 256])
    wbr_d = din("wbr", [128, 8 * 1024])
    wout_d = din("wout", [128, 8 * 1024])
    wuk_d = din("wuk", [128, 512])
    wuv_d = din("wuv", [128, 512])
    wmoe_d = din("wmoe", [N_EXP, 128, 6144])
    wr_d = din("wr", [128, 8 * 36])
    br_d = din("br", [128, 36])
    wpg_d = din("wpg", [128, 8 * 1024])
    wpl_d = din("wpl", [128, 2 * 1024])
    gat_d = din("g_attn", [128, 8])
    gff_d = din("g_ffn", [128, 8])
    gpl_d = din("g_ple", [128, 8])
    gfin_d = din("g_fin", [128, 1024])
    gkv_d = din("g_kv", [128, 1])
    btoep_d = din("btoep", [128, 8 * 640])
    b31_d = din("b31", [128, 8])
    ident_d = din("ident", [128, 128])
    cneg_d = din("cneg", [128, 128])
    smask_d = din("smask", [128, 128])
    uinc_d = din("uinc", [128, 128])
    sel_d = din("sel", [128, 256])
    out_d = nc.dram_tensor("out", [NB, S, D], F32, kind="ExternalOutput").ap()
    dbg_d = {}
    if dbg:
        for nm, shp in (("d_oaT", [128, 4 * S]), ("d_obT", [128, 4 * S]), ("d_x1", [128, NT * D])):
            dbg_d[nm] = nc.dram_tensor(nm, shp, F32, kind="ExternalOutput").ap()

    st = contextlib.ExitStack()
    with st:
        def sb(name, shape, dt):
            return st.enter_context(nc.sbuf_tensor("s_" + name, list(shape), dt))

        arA = sb("arA", [128, 16384], BF16)
        arB = sb("arB", [128, 32768], BF16)
        arC = sb("arC", [128, 16384], BF16)
        arD = sb("arD", [128, 33792], BF16)
        ident_f = sb("ident_f", [128, 128], F32)
        ident_b = sb("ident_b", [128, 128], BF16)
        cneg = sb("cneg", [128, 128], F32)
        smask = sb("smask", [128, 128], BF16)
        uinc = sb("uinc", [128, 128], BF16)
        ones_b = sb("ones_b", [128, 128], BF16)
        sel_f = sb("sel_f", [128, 256], F32)
        gB1 = sb("gB", [128, 8, 128], BF16)
        gB = [gB1, gB1, gB1]
        gpk = sb("gpk", [128, 24], F32)
        gkv = sb("gkv", [128, 1], F32)
        b31 = sb("b31", [128, 8], F32)
        brb = sb("brb", [128, 36], F32)
        wr_f = sb("wr_f", [128, 8, 36], F32)
        wuk = sb("wuk", [128, 4, 128], BF16)
        wuv = sb("wuv", [128, 512], BF16)
        widx_w = sb("widx_w", [128, 8, 8], BF16)
        small = sb("small", [128, 256], F32)
        tiny = sb("tiny", [128, 2], F32)

        psb = [st.enter_context(nc.psum_tensor("ps%d" % i, [128, 512], F32)) for i in range(8)]

        P = Prog(nc)

        def carve(ar, off_bytes, nbytes, dt, pattern=None, **kw):
            e0 = off_bytes // 2
            ap = ar[:, e0:e0 + nbytes // 2]
            if dt == F32:
                ap = ap.bitcast(F32)
            if pattern:
                ap = ap.rearrange(pattern, **kw)
            return ap

        KB = 1024
        hT = carve(arA, 0, 32 * KB, BF16, "p (k t) -> p k t", k=8)
        qaT = carve(arB, 0, 16 * KB, BF16, "p (k t) -> p k t", k=4)
        qiT = carve(arB, 16 * KB, 16 * KB, BF16, "p (k t) -> p k t", k=4)
        ckvT = carve(arB, 32 * KB, 4 * KB, BF16)
        kiT = carve(arB, 36 * KB, 4 * KB, BF16)
        Vp = carve(arB, 40 * KB, 16 * 4 * 130 * 2, BF16, "p (j m c) -> p j m c", j=16, m=4)
        widx_tm = carve(arB, 40 * KB + 16640, 512, F32, "p (i h) -> p i h", i=16)
        qbT = carve(arB, 0, 16 * KB, BF16, "p (k t) -> p k t", k=4)
        kbT = carve(arB, 16 * KB, 16 * KB, BF16, "p (k t) -> p k t", k=4)
        vb = carve(arB, 32 * KB, 16 * KB, BF16, "p (j c) -> p j c", j=16)
        qbTn = carve(arB, 48 * KB, 16 * KB, BF16, "p (k t) -> p k t", k=4)
        x1 = carve(arB, 0, 64 * KB, F32, "p (i d) -> p i d", i=16)
        oaT = carve(arC, 0, 16 * KB, BF16, "p (k t) -> p k t", k=4)
        obT = carve(arC, 16 * KB, 16 * KB, BF16, "p (k t) -> p k t", k=4)
        wmoe = [carve(arC, i * 12 * KB, 12 * KB, BF16) for i in range(2)]
        wpg = carve(arC, 0, 16 * KB, BF16, "p (k c) -> p k c", k=8)
        wpl = carve(arC, 16 * KB, 4 * KB, BF16, "p (k c) -> p k c", k=2)
        def dD(off, nbytes, dt, pattern=None, **kw):
            assert off + nbytes <= 66 * KB, (off, nbytes)
            return carve(arD, off, nbytes, dt, pattern, **kw)

        PS = lambda i: psb[i]

        def psbf(i):
            return psb[i][:].bitcast(BF16)

        def ld(key, dst, src, eng="sp", res=None, **kw):
            P.dma(eng, key, lambda e: e.dma_start(out=dst, in_=src, **kw), writes=[res or key])

        ld("c_ident", ident_f[:], ident_d[:, :])
        ld("c_cneg", cneg[:], cneg_d[:, :])
        ld("c_sel", sel_f[:], sel_d[:, :])
        ld("c_gkv", gkv[:], gkv_d[:, :])
        ld("c_b31", b31[:], b31_d[:, :])
        ld("c_br", brb[:], br_d[:, :])
        ld("c_wr", wr_f[:].rearrange("p k c -> p (k c)"), wr_d[:, :])
        ld("c_g0", gpk[:, 0:8], gat_d[:, :])
        ld("c_g1", gpk[:, 8:16], gff_d[:, :])
        ld("c_g2", gpk[:, 16:24], gpl_d[:, :])
        ld("c_smask", smask[:], smask_d[:, :], eng="pool")
        ld("c_uinc", uinc[:], uinc_d[:, :], eng="pool")
        ld("c_wuk", wuk[:].rearrange("p k c -> p (k c)"), wuk_d[:, :], eng="pool")
        ld("c_wuv", wuv[:], wuv_d[:, :], eng="pool")
        ld("c_widx", widx_w[:].rearrange("p k c -> p (k c)"), widx_d[:, :], eng="pool")
        P.op("dve", lambda e: e.tensor_copy(out=ident_b[:], in_=ident_f[:]), reads=["c_ident"], writes=["ident_b"])
        P.op("dve", lambda e: e.memset(ones_b[:], 1.0), writes=["ones_b"])
        P.op("dve", lambda e: e.memset(tiny[:], 0.0), writes=["tiny"])
        def build_gB(gi):
            for k in range(8):
                P.op("dve", lambda e, gi=gi, k=k: e.tensor_scalar(out=gB1[:, k, :], in0=ones_b[:], scalar1=gpk[:, gi * 8 + k:gi * 8 + k + 1],
                                                                  scalar2=None, op0=ALU.mult),
                     reads=["ones_b", "c_g%d" % gi], writes=["gB"])

        evac_rr = [0]

        def evac(out, in_, reads, writes, scale=None, eng=None):
            if eng is None:
                eng = ("act", "dve")[evac_rr[0] % 2]
                evac_rr[0] += 1
            if eng == "act":
                if scale is None:
                    P.op("act", lambda e: e.activation(out=out, in_=in_, func=AF.Copy), reads=reads, writes=writes)
                else:
                    P.op("act", lambda e: e.activation(out=out, in_=in_, func=AF.Copy, scale=float(scale)), reads=reads, writes=writes)
            else:
                if scale is None:
                    P.op("dve", lambda e: e.tensor_copy(out=out, in_=in_), reads=reads, writes=writes)
                else:
                    P.op("dve", lambda e: e.tensor_scalar(out=out, in0=in_, scalar1=float(scale), scalar2=None, op0=ALU.mult), reads=reads, writes=writes)

        def phase_barrier():
            P.barrier(lambda e: e.memset(tiny[:, 0:1], 0.0))

        def norm_transpose(b, src, gi, dstT, dstres, xt_bufs, xn_bufs, ps_banks, f32_side=None):
            for i in range(NT):
                sl = i % 2
                if src == "x":
                    xt = xt_bufs[sl]
                    xres = "xt%d" % sl
                    P.dma("sp", xres, lambda e, xt=xt, i=i: e.dma_start(out=xt, in_=x_d[b, i * 128:(i + 1) * 128, :]), writes=[xres])
                else:
                    xt = x1[:, i, :]
                    xres = ("x1", i)
                xn = xn_bufs[sl]
                xnres = "xn%d" % sl
                ssc = small[:, i:i + 1]
                rsc = small[:, 16 + i:17 + i]
                P.op("act", lambda e, xt=xt, xn=xn, ssc=ssc: e.activation(out=xn, in_=xt, func=AF.Square, accum_out=ssc),
                     reads=[xres], writes=[xnres, ("ss", i)])
                P.op("dve", lambda e, ssc=ssc, rsc=rsc: e.tensor_scalar(out=rsc, in0=ssc, scalar1=1.0 / D, scalar2=EPS, op0=ALU.mult, op1=ALU.add),
                     reads=[("ss", i)], writes=[("rs", i)])
                P.op("act", lambda e, rsc=rsc: e.activation(out=rsc, in_=rsc, func=AF.Sqrt), reads=[("rs", i)], writes=[("rs", i)])
                P.op("dve", lambda e, rsc=rsc: e.reciprocal(out=rsc, in_=rsc), reads=[("rs", i)], writes=[("rs", i)])
                if f32_side is None:
                    P.op("dve", lambda e, xt=xt, xn=xn, rsc=rsc: e.tensor_scalar(out=xn, in0=xt, scalar1=rsc, scalar2=None, op0=ALU.mult),
                         reads=[xres, ("rs", i)], writes=[xnres])
                    pb = ps_banks[i % len(ps_banks)]
                    pres = ("ps", pb)
                    pv = psbf(pb)[:, 0:1024].rearrange("p (k t) -> p k t", k=8)
                    for k in range(8):
                        P.op("pe", lambda e, pv=pv, xn=xn, k=k: e.transpose(out=pv[:, k, :], in_=xn[:, k * 128:(k + 1) * 128], identity=ident_b[:]),
                             reads=[xnres, "ident_b"], writes=[pres])
                    P.op("dve", lambda e, pv=pv, i=i: e.tensor_tensor(out=dstT[:, :, i * 128:(i + 1) * 128], in0=pv, in1=gB[gi][:], op=ALU.mult),
                         reads=[pres, "gB"], writes=[(dstres, i // 4)])
                else:
                    f32_side(i, xt, xres, rsc, xn, xnres)

        last_out_tokens = []
        for b in range(NB):
            xt_bufs = [dD(0, 4 * KB, F32), dD(4 * KB, 4 * KB, F32)]
            xn_bufs = [dD(8 * KB, 2 * KB, BF16), dD(10 * KB, 2 * KB, BF16)]
            w1 = dD(12 * KB, 20 * KB, BF16, "p (k c) -> p k c", k=8)
            ckv_raw = dD(32 * KB, 8 * KB, F32)
            sqb = [dD(40 * KB, 1 * KB, BF16), dD(41 * KB, 1 * KB, BF16)]
            rstd_b = [dD(42 * KB, 2 * KB, F32), dD(44 * KB, 2 * KB, F32)]
            P.dma("pool", "w1", lambda e: e.dma_start(out=w1.rearrange("p k c -> p (k c)"), in_=w1_d[:, :], max_dma_last_dim=8192), writes=["w1"])
            build_gB(0)
            norm_transpose(b, "x", 0, hT, "hT", xt_bufs, xn_bufs, [6, 7])

            if stop_after == "P0":
                break
            bank_rr = [0]

            def nb(banks):
                v = banks[bank_rr[0] % len(banks)]
                bank_rr[0] += 1
                return v

            def proj_T(w, wres, cc, c, banks):
                pb = nb(banks)
                for k in range(8):
                    P.op("pe", lambda e, pb=pb, k=k: e.matmul(PS(pb)[:, :], lhsT=w[:, k, cc * 128:(cc + 1) * 128], rhs=hT[:, k, c * 512:(c + 1) * 512],
                                                             start=(k == 0), stop=(k == 7)),
                         reads=[wres, ("hT", c)], writes=[("ps", pb)])
                return pb

            for cc in range(10):
                for c in range(NCH):
                    pb = proj_T(w1, "w1", cc, c, [0, 1, 2, 3])
                    cs = slice(c * 512, (c + 1) * 512)
                    if cc < 4:
                        evac(qaT[:, cc, cs], PS(pb)[:, :], [("ps", pb)], [("qaT", c)])
                    elif cc < 8:
                        evac(qiT[:, cc - 4, cs], PS(pb)[:, :], [("ps", pb)], [("qiT", c)])
                    elif cc == 8:
                        evac(ckv_raw[:, cs], PS(pb)[:, :], [("ps", pb)], [("ckv_raw", c)])
                    else:
                        evac(kiT[:, cs], PS(pb)[:, :], [("ps", pb)], [("kiT", c)])
            for c in range(NCH):
                cs = slice(c * 512, (c + 1) * 512)
                sq = sqb[c % 2]
                rb = rstd_b[c % 2]
                P.op("act", lambda e, sq=sq, cs=cs: e.activation(out=sq, in_=ckv_raw[:, cs], func=AF.Square), reads=[("ckv_raw", c)], writes=[("sq", c % 2)])
                pb = nb([0, 1, 2, 3])
                P.op("pe", lambda e, pb=pb, sq=sq: e.matmul(PS(pb)[:, :], lhsT=ones_b[:], rhs=sq, start=True, stop=True),
                     reads=[("sq", c % 2), "ones_b"], writes=[("ps", pb)])
                P.op("dve", lambda e, pb=pb, rb=rb: e.tensor_scalar(out=rb, in0=PS(pb)[:, :], scalar1=1.0 / 128, scalar2=EPS, op0=ALU.mult, op1=ALU.add),
                     reads=[("ps", pb)], writes=[("rb", c % 2)])
                P.op("act", lambda e, rb=rb: e.activation(out=rb, in_=rb, func=AF.Sqrt), reads=[("rb", c % 2)], writes=[("rb", c % 2)])
                P.op("dve", lambda e, rb=rb: e.reciprocal(out=rb, in_=rb), reads=[("rb", c % 2)], writes=[("rb", c % 2)])
                P.op("dve", lambda e, rb=rb, cs=cs: e.scalar_tensor_tensor(out=ckvT[:, cs], in0=ckv_raw[:, cs], scalar=gkv[:, 0:1], in1=rb, op0=ALU.mult, op1=ALU.mult),
                     reads=[("ckv_raw", c), ("rb", c % 2), "c_gkv"], writes=[("ckvT", c)])
            pbw = 4
            for i in range(NT):
                for k in range(8):
                    P.op("pe", lambda e, i=i, k=k: e.matmul(PS(pbw)[:, i * 8:(i + 1) * 8], lhsT=hT[:, k, i * 128:(i + 1) * 128], rhs=widx_w[:, k, :],
                                                         start=(k == 0), stop=(k == 7)),
                         reads=[("hT", i // 4), "c_widx"], writes=[("ps", pbw)])
            P.op("dve", lambda e: e.tensor_scalar(out=widx_tm.rearrange("p i h -> p (i h)"), in0=PS(pbw)[:, 0:128], scalar1=IDX_SCALE, scalar2=None, op0=ALU.mult),
                 reads=[("ps", pbw)], writes=["widx_tm"])
            P.op("pool", lambda e: e.memset(Vp.rearrange("p j m c -> p (j m) c")[:, :, 64:65], 1.0), writes=["Vp"])
            P.op("pool", lambda e: e.memset(Vp.rearrange("p j m c -> p (j m) c")[:, :, 129:130], 1.0), writes=["Vp"])
            for j in range(NT):
                pb = nb([0, 1, 2, 3])
                P.op("pe", lambda e, pb=pb, j=j: e.matmul(PS(pb)[:, :], lhsT=ckvT[:, j * 128:(j + 1) * 128], rhs=wuv[:], start=True, stop=True),
                     reads=[("ckvT", j // 4), "c_wuv"], writes=[("ps", pb)])
                pv = PS(pb)[:, :].rearrange("p (m h d) -> p m h d", m=4, h=2)
                evac(Vp[:, j, :, 0:64], pv[:, :, 0, :], [("ps", pb)], ["Vp"])
                evac(Vp[:, j, :, 65:129], pv[:, :, 1, :], [("ps", pb)], ["Vp"])
            phase_barrier()
            if stop_after == "P1":
                break

            NEGM = 30000.0
            score = [dD(0, 8 * KB, F32), dD(8 * KB, 8 * KB, F32), carve(arC, 16 * KB, 8 * KB, F32), carve(arC, 24 * KB, 8 * KB, F32)]
            junk = dD(16 * KB, 4 * KB, BF16)
            mask_tm = dD(20 * KB, 4 * KB, BF16)
            maskT = dD(24 * KB, 16 * KB, BF16, "p (j t) -> p j t", j=16)
            B8 = dD(40 * KB, 10 * KB, BF16, "p (h u) -> p h u", h=8)
            rbuf = [dD(50 * KB + i * KB, 1 * KB, BF16) for i in range(4)]
            Pbuf = [dD(54 * KB + i * KB, 1 * KB, BF16) for i in range(4)]
            qabs = [dD(58 * KB + i * KB, 1 * KB, BF16) for i in range(2)]
            dg = dD(60 * KB, 2 * KB, BF16, "p (h t) -> p h t", h=8)
            o_f32 = dD(62 * KB, 2 * KB, F32)
            rec = dD(64 * KB, 2 * KB, F32)
            btmp = dD(0, 10 * KB, F32, "p (h u) -> p h u", h=4)
            for hh in range(2):
                P.dma("sp", "btoep", lambda e, hh=hh: e.dma_start(out=btmp.rearrange("p h u -> p (h u)")[:, 0:2560], in_=btoep_d[:, hh * 2560:(hh + 1) * 2560]), writes=["btmp"])
                P.op("dve", lambda e, hh=hh: e.tensor_scalar(out=B8[:, hh * 4:hh * 4 + 4, :], in0=btmp[:, 0:4, :], scalar1=1.0 / ATTN_SCALE, scalar2=None, op0=ALU.mult),
                     reads=["btmp"], writes=["B8"])
            phase_barrier()

            LO, WD, MID, CNT, TMP = 40, 44, 48, 52, 56
            rr = [0]
            smr = lambda base, a0, a1: small[:, base + a0:base + a1]

            def scores_chunk(c):
                for tl in range(4):
                    i = 4 * c + tl
                    L = (i + 1) * 128
                    ts_ = slice(i * 128, (i + 1) * 128)
                    sct = score[tl]
                    for h in range(8):
                        P.op("dve", lambda e, h=h, i=i: e.tensor_scalar(out=dg[:, h, :], in0=ident_b[:], scalar1=widx_tm[:, i, h:h + 1], scalar2=None, op0=ALU.mult),
                             reads=["ident_b", "widx_tm"], writes=[("dg", h)])
                    nsc = (L + 511) // 512
                    for sc in range(nsc):
                        ws = min(512, L - sc * 512)
                        spb = 4 + (sc % 2)
                        for h in range(8):
                            bp = (h % 2) * 64
                            zb = nb([0, 1, 2, 3])
                            P.op("pe", lambda e, zb=zb, h=h, bp=bp, ts_=ts_, sc=sc, ws=ws: e.matmul(
                                PS(zb)[:, 0:ws], lhsT=qiT[bp:bp + 64, h // 2, ts_], rhs=kiT[bp:bp + 64, sc * 512:sc * 512 + ws], start=True, stop=True),
                                reads=[("qiT", c), ("kiT", sc)], writes=[("ps", zb)])
                            rs = rr[0] % 4
                            rr[0] += 1
                            P.op("act", lambda e, zb=zb, rs=rs, ws=ws: e.activation(out=rbuf[rs][:, 0:ws], in_=PS(zb)[:, 0:ws], func=AF.Relu),
                                 reads=[("ps", zb)], writes=[("rbuf", rs)])
                            P.op("pe", lambda e, spb=spb, h=h, rs=rs, ws=ws: e.matmul(PS(spb)[:, 0:ws], lhsT=dg[:, h, :], rhs=rbuf[rs][:, 0:ws], start=(h == 0), stop=(h == 7)),
                                 reads=[("dg", h), ("rbuf", rs)], writes=[("ps", spb)])
                        last = (sc == nsc - 1)
                        wcopy = ws - 128 if last else ws
                        if wcopy > 0:
                            P.op("act", lambda e, spb=spb, sc=sc, wcopy=wcopy, sct=sct: e.activation(out=sct[:, sc * 512:sc * 512 + wcopy], in_=PS(spb)[:, 0:wcopy], func=AF.Copy),
                                 reads=[("ps", spb)], writes=[("score", tl)])
                        if last:
                            P.op("dve", lambda e, spb=spb, sc=sc, ws=ws, sct=sct: e.tensor_tensor(out=sct[:, sc * 512 + ws - 128:sc * 512 + ws], in0=PS(spb)[:, ws - 128:ws], in1=cneg[:], op=ALU.add),
                                 reads=[("ps", spb), "c_cneg"], writes=[("score", tl)])

            def bisect_chunk(c):
                act = [tl for tl in range(4) if 4 * c + tl >= 2]
                for tl in range(4):
                    i = 4 * c + tl
                    L = (i + 1) * 128
                    if i < 2:
                        P.op("dve", lambda e, tl=tl: e.memset(smr(LO, tl, tl + 1), -1.0e29), writes=[("lo", tl)])
                    else:
                        P.op("dve", lambda e, tl=tl, i=i: e.tensor_reduce(out=smr(LO, tl, tl + 1), in_=score[tl][:, 0:i * 128], axis=AX.X, op=ALU.min), reads=[("score", tl)], writes=[("lo", tl)])
                        P.op("dve", lambda e, tl=tl, L=L: e.tensor_reduce(out=smr(WD, tl, tl + 1), in_=score[tl][:, 0:L], axis=AX.X, op=ALU.max), reads=[("score", tl)], writes=[("wd", tl)])
                if not act:
                    return
                a0, a1 = act[0], act[-1] + 1
                R = lambda nm: [(nm, t) for t in range(a0, a1)]
                P.op("dve", lambda e: e.tensor_tensor(out=smr(WD, a0, a1), in0=smr(WD, a0, a1), in1=smr(LO, a0, a1), op=ALU.subtract), reads=R("wd") + R("lo"), writes=R("wd"))
                for it in range(N_BISECT):
                    f = 0.5 ** (it + 1)
                    P.op("dve", lambda e, f=f: e.scalar_tensor_tensor(out=smr(MID, a0, a1), in0=smr(WD, a0, a1), scalar=f, in1=smr(LO, a0, a1), op0=ALU.mult, op1=ALU.add),
                         reads=R("wd") + R("lo"), writes=R("mid"))
                    for tl in act:
                        L = (4 * c + tl + 1) * 128
                        jb, jres = ((junk, "junk"), (mask_tm, "mask_tm"))[tl % 2]
                        P.op("dve", lambda e, tl=tl, L=L, jb=jb: e.tensor_scalar(out=jb[:, 0:L], in0=score[tl][:, 0:L], scalar1=smr(MID, tl, tl + 1), scalar2=None, op0=ALU.is_ge, op1=ALU.add,
                                                                         accum_out=smr(CNT, tl, tl + 1)),
                             reads=[("score", tl), ("mid", tl)], writes=[("cnt", tl), jres])
                    P.op("dve", lambda e, f=f: e.tensor_scalar(out=smr(TMP, a0, a1), in0=smr(CNT, a0, a1), scalar1=255.5, scalar2=f, op0=ALU.is_ge, op1=ALU.mult),
                         reads=R("cnt"), writes=["tmp4"])
                    P.op("dve", lambda e: e.tensor_tensor(out=smr(TMP, a0, a1), in0=smr(TMP, a0, a1), in1=smr(WD, a0, a1), op=ALU.mult), reads=["tmp4"] + R("wd"), writes=["tmp4"])
                    P.op("dve", lambda e: e.tensor_tensor(out=smr(LO, a0, a1), in0=smr(LO, a0, a1), in1=smr(TMP, a0, a1), op=ALU.add), reads=["tmp4"] + R("lo"), writes=R("lo"))

            def maskgen_chunk(c):
                for tl in range(4):
                    i = 4 * c + tl
                    L = (i + 1) * 128
                    P.op("dve", lambda e, tl=tl, L=L: e.tensor_scalar(out=mask_tm[:, 0:L], in0=score[tl][:, 0:L], scalar1=smr(LO, tl, tl + 1), scalar2=-NEGM, op0=ALU.is_lt, op1=ALU.mult),
                         reads=[("score", tl), ("lo", tl)], writes=["mask_tm"])
                    for j0 in range(0, i + 1, 8):
                        n = min(8, i + 1 - j0)
                        tb = 6 + ((j0 // 8) % 2)
                        pv = psbf(tb)[:, 0:1024].rearrange("p (k t) -> p k t", k=8)
                        for jj in range(n):
                            j = j0 + jj
                            P.op("pe", lambda e, pv=pv, jj=jj, j=j: e.transpose(out=pv[:, jj, :], in_=mask_tm[:, j * 128:(j + 1) * 128], identity=ident_b[:]),
                                 reads=["mask_tm", "ident_b"], writes=[("ps", tb)])
                        evac(maskT[:, j0:j0 + n, tl * 128:(tl + 1) * 128], pv[:, 0:n, :], [("ps", tb)], [("maskT", tl)])

            def attention_chunk(c):
                cs0 = c * 512
                jmax = 4 * c + 3

                def emit_qabs(h):
                    bp = (h % 2) * 64
                    qb_ = nb([0, 1, 2, 3])
                    P.op("pe", lambda e, qb_=qb_, bp=bp, h=h: e.matmul(PS(qb_)[:, :], lhsT=wuk[bp:bp + 64, h // 2, :], rhs=qaT[bp:bp + 64, h // 2, cs0:cs0 + 512], start=True, stop=True),
                         reads=["c_wuk", ("qaT", c)], writes=[("ps", qb_)])
                    evac(qabs[h % 2], PS(qb_)[:, :], [("ps", qb_)], [("qabs", h % 2)])

                def emit_tail(h):
                    bp = (h % 2) * 64
                    ob = 4 + (h % 2)
                    P.op("act", lambda e, ob=ob: e.activation(out=o_f32, in_=PS(ob)[:, :], func=AF.Copy), reads=[("ps", ob)], writes=["o_f32"])
                    db = nb([0, 1, 2, 3])
                    P.op("pe", lambda e, db=db, h=h: e.matmul(PS(db)[:, :], lhsT=sel_f[:, (h % 2) * 128:(h % 2) * 128 + 128], rhs=o_f32, start=True, stop=True),
                         reads=["c_sel", "o_f32"], writes=[("ps", db)])
                    P.op("act", lambda e, db=db, bp=bp: e.activation(out=rec[bp:bp + 64, :], in_=PS(db)[bp:bp + 64, :], func=AF.Ln), reads=[("ps", db)], writes=["rec"])
                    P.op("act", lambda e, bp=bp: e.activation(out=rec[bp:bp + 64, :], in_=rec[bp:bp + 64, :], func=AF.Exp, scale=-1.0), reads=["rec"], writes=["rec"])
                    P.op("dve", lambda e, bp=bp, h=h: e.tensor_tensor(out=oaT[bp:bp + 64, h // 2, cs0:cs0 + 512], in0=o_f32[bp:bp + 64, :], in1=rec[bp:bp + 64, :], op=ALU.mult),
                         reads=["o_f32", "rec"], writes=[("oaT", c)])

                emit_qabs(0)
                pending_tail = None
                for h in range(8):
                    m = h // 2
                    qs = h % 2
                    ob = 4 + (h % 2)
                    if h < 7:
                        emit_qabs(h + 1)
                    stt = {}

                    def A(j):
                        col0 = max(0, j - 4 * c) * 128
                        N = 512 - col0
                        near = j >= 4 * c - 1
                        lb = nb([0, 1, 2, 3])
                        P.op("pe", lambda e, lb=lb, j=j, col0=col0, N=N: e.matmul(PS(lb)[:, 0:N], lhsT=ckvT[:, j * 128:(j + 1) * 128], rhs=qabs[qs][:, col0:512], start=True, stop=False),
                             reads=[("ckvT", j // 4), ("qabs", qs)], writes=[("ps", lb)])
                        P.op("pe", lambda e, lb=lb, j=j, col0=col0, N=N, near=near: e.matmul(PS(lb)[:, 0:N], lhsT=ident_b[:], rhs=maskT[:, j, col0:512], start=False, stop=(not near)),
                             reads=["ident_b"] + [("maskT", t) for t in range(col0 // 128, 4)], writes=[("ps", lb)])
                        if near:
                            u0 = cs0 + col0 - 128 * j
                            P.op("pe", lambda e, lb=lb, u0=u0, N=N: e.matmul(PS(lb)[:, 0:N], lhsT=ident_b[:], rhs=B8[:, h, u0:u0 + N], start=False, stop=True),
                                 reads=["ident_b", "B8"], writes=[("ps", lb)])
                        pi = rr[0] % 4
                        rr[0] += 1
                        Pt = Pbuf[pi]
                        if near:
                            P.op("act", lambda e, lb=lb, Pt=Pt, N=N: e.activation(out=Pt[:, 0:N], in_=PS(lb)[:, 0:N], func=AF.Exp, scale=ATTN_SCALE),
                                 reads=[("ps", lb)], writes=[("Pbuf", pi)])
                        else:
                            P.op("act", lambda e, lb=lb, Pt=Pt, N=N: e.activation(out=Pt[:, 0:N], in_=PS(lb)[:, 0:N], func=AF.Exp, scale=ATTN_SCALE, bias=b31[:, h:h + 1]),
                                 reads=[("ps", lb), "c_b31"], writes=[("Pbuf", pi)])
                        stt[j] = (pi, col0, N)

                    def B(j):
                        pi, col0, N = stt[j]
                        w0 = 0 if h % 2 == 0 else 1
                        P.op("pe", lambda e, j=j, w0=w0, pi=pi, col0=col0, N=N: e.matmul(PS(ob)[:, col0:512], lhsT=Vp[:, j, m, w0:w0 + 128], rhs=Pbuf[pi][:, 0:N],
                                                                                  start=(j == 0), stop=(j == jmax)),
                             reads=["Vp", ("Pbuf", pi)], writes=[("ps", ob)])

                    for k in range(jmax + 3):
                        if k <= jmax:
                            A(k)
                        if 0 <= k - 2 <= jmax:
                            B(k - 2)
                        if k == 2 and pending_tail is not None:
                            emit_tail(pending_tail)
                            pending_tail = None
                    pending_tail = h
                emit_tail(pending_tail)

            scores_chunk(0)
            bisect_chunk(0)
            maskgen_chunk(0)
            for c in range(NCH):
                if c + 1 < NCH:
                    scores_chunk(c + 1)
                    bisect_chunk(c + 1)
                attention_chunk(c)
                if c + 1 < NCH:
                    maskgen_chunk(c + 1)
            phase_barrier()
            if stop_after == "P2":
                break

            w3 = dD(0, 24 * KB, BF16, "p (k c) -> p k c", k=8)
            ebuf = [dD(24 * KB + i * 2 * KB, 2 * KB, F32) for i in range(4)]
            spbuf = [dD(32 * KB + i * KB, 1 * KB, BF16) for i in range(6)]
            tbuf = [dD(38 * KB + i * 2 * KB, 2 * KB, F32) for i in range(3)]
            abuf = [dD(44 * KB + i * KB, 1 * KB, BF16) for i in range(4)]
            sbrr = [0, 0, 0, 0]
            P.dma("pool", "w3", lambda e: e.dma_start(out=w3.rearrange("p k c -> p (k c)"), in_=w3_d[:, :], max_dma_last_dim=8192), writes=["w3"])
            if stop_after == "P3w":
                break
            for cc in range(8):
                for c in range(NCH):
                    pb = proj_T(w3, "w3", cc, c, [0, 1, 2, 3])
                    cs = slice(c * 512, (c + 1) * 512)
                    if cc < 4:
                        evac(qbT[:, cc, cs], PS(pb)[:, :], [("ps", pb)], [("qbT", c)])
                    else:
                        evac(kbT[:, cc - 4, cs], PS(pb)[:, :], [("ps", pb)], [("kbT", c)])
            if stop_after == "P3q":
                break
            for j in range(NT):
                pb = nb([0, 1, 2, 3])
                for k in range(8):
                    P.op("pe", lambda e, pb=pb, j=j, k=k: e.matmul(PS(pb)[:, :], lhsT=hT[:, k, j * 128:(j + 1) * 128], rhs=w3[:, k, 1024:1536], start=(k == 0), stop=(k == 7)),
                         reads=["w3", ("hT", j // 4)], writes=[("ps", pb)])
                evac(vb[:, j, :], PS(pb)[:, :], [("ps", pb)], [("vb", j // 4)])
            if stop_after == "P3a":
                break
            for c in range(NCH):
                cs0 = c * 512
                jmax = 4 * c + 3
                for hp in range(4):
                    m = hp
                    units = [(j, hh) for j in range(jmax, -1, -1) for hh in range(2)]
                    stt = {}
                    prev_sp = {0: None, 1: None}

                    def S1(u):
                        j, hh = u
                        bp = hh * 64
                        col0 = max(0, j - 4 * c) * 128
                        N = 512 - col0
                        zb = nb([0, 1, 2, 5])
                        P.op("pe", lambda e, zb=zb, bp=bp, j=j, col0=col0, N=N: e.matmul(PS(zb)[:, 0:N], lhsT=kbT[bp:bp + 64, m, j * 128:(j + 1) * 128],
                                                                                    rhs=qbT[bp:bp + 64, m, cs0 + col0:cs0 + 512], start=True, stop=True),
                             reads=[("kbT", j // 4), ("qbT", c)], writes=[("ps", zb)])
                        ei = sbrr[0] % 4
                        si = sbrr[1] % 6
                        sbrr[0] += 1
                        sbrr[1] += 1
                        eb_, spt = ebuf[ei], spbuf[si]
                        P.op("act", lambda e, zb=zb, eb_=eb_, N=N: e.activation(out=eb_[:, 0:N], in_=PS(zb)[:, 0:N], func=AF.Exp, scale=ATTN_SCALE),
                             reads=[("ps", zb)], writes=[("ebuf", ei)])
                        if j >= 4 * c:
                            P.op("dve", lambda e, eb_=eb_: e.tensor_tensor(out=eb_[:, 0:128], in0=eb_[:, 0:128], in1=smask[:], op=ALU.mult),
                                 reads=[("ebuf", ei), "c_smask"], writes=[("ebuf", ei)])
                        P.op("act", lambda e, eb_=eb_, spt=spt, N=N: e.activation(out=spt[:, 0:N], in_=eb_[:, 0:N], func=AF.Ln, bias=1.0, scale=1.0),
                             reads=[("ebuf", ei)], writes=[("spbuf", si)])
                        stt[u] = dict(ei=ei, si=si, col0=col0, N=N)

                    def S2(u):
                        j, hh = u
                        d = stt[u]
                        xb = 3 + hh
                        col0, N = d["col0"], d["N"]
                        spt = spbuf[d["si"]]
                        pv = prev_sp[hh]
                        if pv is not None:
                            psi, pcol0, pN = pv
                            P.op("pe", lambda e, xb=xb, psi=psi, pcol0=pcol0, pN=pN: e.matmul(PS(xb)[:, pcol0:512], lhsT=smask[:], rhs=spbuf[psi][:, 0:pN], start=False, stop=True, skip_group_check=True),
                                 reads=["c_smask", ("spbuf", psi)], writes=[("ps", xb)])
                        P.op("pe", lambda e, xb=xb, spt=spt, col0=col0, N=N, first=(pv is None): e.matmul(PS(xb)[:, col0:512], lhsT=uinc[:], rhs=spt[:, 0:N], start=first, stop=True, skip_group_check=True),
                             reads=["c_uinc", ("spbuf", d["si"])], writes=[("ps", xb)])
                        prev_sp[hh] = (d["si"], col0, N)
                        ti = sbrr[2] % 3
                        ai = sbrr[3] % 4
                        sbrr[2] += 1
                        sbrr[3] += 1
                        d["ai"] = ai
                        P.op("act", lambda e, xb=xb, ti=ti, col0=col0, N=N: e.activation(out=tbuf[ti][:, 0:N], in_=PS(xb)[:, col0:512], func=AF.Exp, scale=-1.0),
                             reads=[("ps", xb)], writes=[("tbuf", ti)])
                        P.op("dve", lambda e, ti=ti, ai=ai, ei=d["ei"], N=N: e.tensor_tensor(out=abuf[ai][:, 0:N], in0=tbuf[ti][:, 0:N], in1=ebuf[ei][:, 0:N], op=ALU.mult),
                             reads=[("tbuf", ti), ("ebuf", d["ei"])], writes=[("abuf", ai)])

                    def S3(u):
                        j, hh = u
                        d = stt[u]
                        ob = 6 + hh
                        col0, N = d["col0"], d["N"]
                        P.op("pe", lambda e, ob=ob, j=j, ai=d["ai"], col0=col0, N=N: e.matmul(PS(ob)[:, col0:512], lhsT=vb[:, j, m * 128:(m + 1) * 128], rhs=abuf[ai][:, 0:N],
                                                                                       start=(j == jmax), stop=(j == 0), skip_group_check=True),
                             reads=[("vb", j // 4), ("abuf", d["ai"])], writes=[("ps", ob)])

                    nu = len(units)
                    for k in range(nu + 2):
                        if k < nu:
                            S1(units[k])
                        if 0 <= k - 1 < nu:
                            S2(units[k - 1])
                        if 0 <= k - 2 < nu:
                            S3(units[k - 2])
                    for hh in range(2):
                        bp = hh * 64
                        evac(obT[bp:bp + 64, m, cs0:cs0 + 512], PS(6 + hh)[bp:bp + 64, :], [("ps", 6 + hh)], [("obT", c)])
            phase_barrier()
            if dbg:
                dtmp = dD(48 * KB, 16 * KB, F32)
                for nm, src, res in (("d_oaT", oaT, "oaT"), ("d_obT", obT, "obT")):
                    for q in range(2):
                        P.op("dve", lambda e, src=src, q=q: e.tensor_copy(out=dtmp, in_=src.rearrange("p k t -> p (k t)")[:, q * 4096:(q + 1) * 4096]),
                             reads=[(res, cq) for cq in range(4)], writes=["dtmp"])
                        if b == 0:
                            P.dma("sp", "dbg", lambda e, nm=nm, q=q: e.dma_start(out=dbg_d[nm][:, q * 4096:(q + 1) * 4096], in_=dtmp), reads=["dtmp"])
                phase_barrier()
            if stop_after == "P3":
                break

            mergedT = dD(0, 32 * KB, BF16, "p (k t) -> p k t", k=8)
            wout = dD(32 * KB, 16 * KB, BF16, "p (k c) -> p k c", k=8)
            wg4 = [dD(48 * KB + i * 4 * KB, 4 * KB, BF16, "p (k c) -> p k c", k=8) for i in range(2)]
            wbr = [dD(56 * KB + i * 2 * KB, 2 * KB, BF16, "p (a k c) -> p a k c", a=2, k=4) for i in range(2)]
            sgb_ = [dD(60 * KB + i * KB, 1 * KB, BF16) for i in range(2)]
            t12 = [dD(62 * KB + i * 2 * KB, 2 * KB, F32) for i in range(2)]
            P.dma("pool", "wout", lambda e: e.dma_start(out=wout.rearrange("p k c -> p (k c)"), in_=wout_d[:, :], max_dma_last_dim=8192), writes=["wout"])
            for m in range(8):
                wsl = m % 2
                P.dma("pool", "wg4_%d" % wsl, lambda e, m=m, wsl=wsl: e.dma_start(out=wg4[wsl].rearrange("p k c -> p (k c)"), in_=wg4_d[:, m * 2048:(m + 1) * 2048], max_dma_last_dim=8192),
                      writes=[("wg4", wsl)])
                P.dma("pool", "wbr_%d" % wsl, lambda e, m=m, wsl=wsl: e.dma_start(out=wbr[wsl].rearrange("p a k c -> p (a k c)"), in_=wbr_d[:, m * 1024:(m + 1) * 1024], max_dma_last_dim=8192),
                      writes=[("wbr", wsl)])
                for c in range(NCH):
                    cs = slice(c * 512, (c + 1) * 512)
                    banks = {}
                    for gi_, nm in enumerate(("ga", "gb")):
                        pb = nb([0, 1, 2, 3, 4, 5, 6, 7])
                        banks[nm] = pb
                        for k in range(8):
                            P.op("pe", lambda e, pb=pb, k=k, gi_=gi_, wsl=wsl, cs=cs: e.matmul(PS(pb)[:, :], lhsT=wg4[wsl][:, k, gi_ * 128:(gi_ + 1) * 128], rhs=hT[:, k, cs],
                                                                                        start=(k == 0), stop=(k == 7)),
                                 reads=[("wg4", wsl), ("hT", c)], writes=[("ps", pb)])
                    for a_, (nm, oT, ores) in enumerate((("ya", oaT, "oaT"), ("yb", obT, "obT"))):
                        pb = nb([0, 1, 2, 3, 4, 5, 6, 7])
                        banks[nm] = pb
                        for k in range(4):
                            P.op("pe", lambda e, pb=pb, k=k, a_=a_, wsl=wsl, oT=oT, cs=cs: e.matmul(PS(pb)[:, :], lhsT=wbr[wsl][:, a_, k, :], rhs=oT[:, k, cs], start=(k == 0), stop=(k == 3)),
                                 reads=[("wbr", wsl), (ores, c)], writes=[("ps", pb)])
                    i0 = 0
                    sa, sb_ = sgb_[i0], sgb_[i0 + 1]
                    P.op("act", lambda e, sa=sa, pb=banks["ga"]: e.activation(out=sa, in_=PS(pb)[:, :], func=AF.Sigmoid), reads=[("ps", banks["ga"])], writes=[("sg", i0)])
                    P.op("act", lambda e, sb_=sb_, pb=banks["gb"]: e.activation(out=sb_, in_=PS(pb)[:, :], func=AF.Sigmoid), reads=[("ps", banks["gb"])], writes=[("sg", i0 + 1)])
                    P.op("dve", lambda e, sa=sa, pb=banks["ya"]: e.tensor_tensor(out=t12[0], in0=sa, in1=PS(pb)[:, :], op=ALU.mult), reads=[("sg", i0), ("ps", banks["ya"])], writes=["t1"])
                    P.op("dve", lambda e, sb_=sb_, pb=banks["yb"]: e.tensor_tensor(out=t12[1], in0=sb_, in1=PS(pb)[:, :], op=ALU.mult), reads=[("sg", i0 + 1), ("ps", banks["yb"])], writes=["t2"])
                    P.op("dve", lambda e, m=m, cs=cs: e.tensor_tensor(out=mergedT[:, m, cs], in0=t12[0], in1=t12[1], op=ALU.add), reads=["t1", "t2"], writes=[("mergedT", c)])
            phase_barrier()
            for i in range(NT):
                P.dma("sp", "x1ld%d" % (i % 4), lambda e, i=i: e.dma_start(out=x1[:, i, :], in_=x_d[b, i * 128:(i + 1) * 128, :]), writes=[("x1", i)])
                pb0 = (i % 4) * 2
                for half in range(2):
                    pb = pb0 + half
                    for k in range(8):
                        P.op("pe", lambda e, pb=pb, k=k, i=i, half=half: e.matmul(PS(pb)[:, :], lhsT=mergedT[:, k, i * 128:(i + 1) * 128], rhs=wout[:, k, half * 512:(half + 1) * 512],
                                                                               start=(k == 0), stop=(k == 7)),
                             reads=[("mergedT", i // 4), "wout"], writes=[("ps", pb)])
                    P.op("dve", lambda e, pb=pb, i=i, half=half: e.tensor_tensor(out=x1[:, i, half * 512:(half + 1) * 512], in0=x1[:, i, half * 512:(half + 1) * 512], in1=PS(pb)[:, :], op=ALU.add),
                         reads=[("ps", pb), ("x1", i)], writes=[("x1", i)])
            phase_barrier()
            if dbg and b == 0:
                P.dma("sp", "dbg", lambda e: e.dma_start(out=dbg_d["d_x1"][:, :], in_=x1.rearrange("p i d -> p (i d)")), reads=[("x1", i) for i in range(NT)])
                phase_barrier()
            if stop_after == "P4":
                break

            h2T = hT
            xnf = dD(0, 4 * KB, F32)
            hTf = dD(4 * KB, 4 * KB, F32, "p (k t) -> p k t", k=8)
            comb = dD(8 * KB, 2 * KB, F32, "p (i e) -> p i e", i=16)
            elm = dD(10 * KB, 256, F32)
            m8 = dD(10 * KB + 256, 64, F32)
            eq = dD(10 * KB + 320, 256, F32)
            sgm = [dD(12 * KB + i * KB, 1 * KB, BF16) for i in range(4)]
            hid = [dD(16 * KB + i * 2 * KB, 2 * KB, BF16, "p (f t) -> p f t", f=2) for i in range(2)]
            RG, RS, RW = 60, 61, 62

            def ffn_side(i, xt, xres, rsc, xn, xnres):
                P.op("dve", lambda e, xt=xt, rsc=rsc: e.tensor_scalar(out=xnf, in0=xt, scalar1=rsc, scalar2=None, op0=ALU.mult), reads=[xres, ("rs", i)], writes=["xnf"])
                for half in range(2):
                    pb = 4 + half
                    for kk in range(4):
                        k = half * 4 + kk
                        P.op("pe", lambda e, pb=pb, kk=kk, k=k: e.transpose(out=PS(pb)[:, kk * 128:(kk + 1) * 128], in_=xnf[:, k * 128:(k + 1) * 128], identity=ident_f[:]),
                             reads=["xnf", "c_ident"], writes=[("ps", pb)])
                    pv = PS(pb)[:, :].rearrange("p (k t) -> p k t", k=4)
                    gf = gpk[:, 8 + half * 4:8 + half * 4 + 4]
                    P.op("dve", lambda e, pv=pv, half=half, i=i: e.tensor_tensor(out=h2T[:, half * 4:half * 4 + 4, i * 128:(i + 1) * 128], in0=pv, in1=gB[1][:, half * 4:half * 4 + 4, :], op=ALU.mult),
                         reads=[("ps", pb), "gB"], writes=[("hT", i // 4)])
                    P.op("dve", lambda e, pv=pv, half=half, gf=gf: e.tensor_tensor(out=hTf[:, half * 4:half * 4 + 4, :], in0=pv, in1=gf.unsqueeze(2).broadcast_to([128, 4, 128]), op=ALU.mult),
                         reads=[("ps", pb), "c_g1"], writes=["hTf"])
                for k in range(8):
                    P.op("pe", lambda e, k=k: e.matmul(PS(6)[:, 0:36], lhsT=hTf[:, k, :], rhs=wr_f[:, k, :], start=(k == 0), stop=(k == 7)),
                         reads=["hTf", "c_wr"], writes=[("ps", 6)])
                sm = lambda col: small[:, col:col + 1]
                P.op("dve", lambda e: e.tensor_tensor(out=elm[:, 0:36], in0=PS(6)[:, 0:36], in1=brb[:], op=ALU.add), reads=[("ps", 6), "c_br"], writes=["elm"])
                P.op("dve", lambda e: e.tensor_reduce(out=sm(RG), in_=elm[:, 0:4], axis=AX.X, op=ALU.max), reads=["elm"], writes=["rg"])
                P.op("dve", lambda e: e.tensor_scalar(out=eq[:, 0:4], in0=elm[:, 0:4], scalar1=sm(RG), scalar2=None, op0=ALU.is_ge), reads=["elm", "rg"], writes=["eq"])
                P.op("dve", lambda e: e.tensor_scalar(out=eq[:, 4:8], in0=eq[:, 0:4], scalar1=-1.0, scalar2=-NEG, op0=ALU.add, op1=ALU.mult), reads=["eq"], writes=["eq"])
                P.op("dve", lambda e: e.tensor_tensor(out=elm[:, 4:36].rearrange("p (g x) -> p g x", g=4), in0=elm[:, 4:36].rearrange("p (g x) -> p g x", g=4),
                                                      in1=eq[:, 4:8].unsqueeze(2).broadcast_to([128, 4, 8]), op=ALU.add), reads=["elm", "eq"], writes=["elm"])
                P.op("dve", lambda e: e.tensor_scalar(out=sm(RW), in0=sm(RG), scalar1=-1.0, scalar2=None, op0=ALU.mult), reads=["rg"], writes=["rw"])
                P.op("act", lambda e: e.activation(out=eq[:, 8:12], in_=elm[:, 0:4], func=AF.Exp, bias=sm(RW), scale=1.0, accum_out=sm(RS)), reads=["elm", "rw"], writes=["eq", "rs_"])
                P.op("dve", lambda e: e.reciprocal(out=sm(RS), in_=sm(RS)), reads=["rs_"], writes=["rs_"])
                P.op("dve", lambda e: e.max(out=m8[:, 0:8], in_=elm[:, 4:36]), reads=["elm"], writes=["m8"])
                P.op("dve", lambda e: e.tensor_tensor(out=m8[:, 8:9], in0=m8[:, 1:2], in1=m8[:, 0:1], op=ALU.subtract), reads=["m8"], writes=["m8"])
                P.op("act", lambda e: e.activation(out=m8[:, 8:9], in_=m8[:, 8:9], func=AF.Exp), reads=["m8"], writes=["m8"])
                P.op("dve", lambda e: e.tensor_scalar(out=m8[:, 8:9], in0=m8[:, 8:9], scalar1=1.0, scalar2=None, op0=ALU.add), reads=["m8"], writes=["m8"])
                P.op("dve", lambda e: e.reciprocal(out=m8[:, 9:10], in_=m8[:, 8:9]), reads=["m8"], writes=["m8"])
                P.op("dve", lambda e: e.tensor_scalar(out=m8[:, 10:11], in0=m8[:, 9:10], scalar1=-1.0, scalar2=1.0, op0=ALU.mult, op1=ALU.add), reads=["m8"], writes=["m8"])
                P.op("dve", lambda e: e.tensor_tensor(out=m8[:, 9:11], in0=m8[:, 9:11], in1=sm(RS).broadcast_to([128, 2]), op=ALU.mult), reads=["m8", "rs_"], writes=["m8"])
                P.op("dve", lambda e: e.tensor_scalar(out=eq[:, 0:32], in0=elm[:, 4:36], scalar1=m8[:, 0:1], scalar2=m8[:, 9:10], op0=ALU.is_equal, op1=ALU.mult), reads=["elm", "m8"], writes=["eq"])
                P.op("dve", lambda e: e.tensor_scalar(out=eq[:, 32:64], in0=elm[:, 4:36], scalar1=m8[:, 1:2], scalar2=m8[:, 10:11], op0=ALU.is_equal, op1=ALU.mult), reads=["elm", "m8"], writes=["eq"])
                P.op("dve", lambda e, i=i: e.tensor_tensor(out=comb[:, i, :], in0=eq[:, 0:32], in1=eq[:, 32:64], op=ALU.add), reads=["eq"], writes=["comb"])

            build_gB(1)
            norm_transpose(b, "x1", 1, h2T, "hT", None, [dD(20 * KB, 2 * KB, BF16), dD(22 * KB, 2 * KB, BF16)], None, f32_side=ffn_side)
            phase_barrier()
            for ex in range(N_EXP):
                wsl = ex % 2
                wm = wmoe[wsl]
                P.dma("pool", "wmoe%d" % wsl, lambda e, wm=wm, ex=ex: e.dma_start(out=wm, in_=wmoe_d[ex, :, :], max_dma_last_dim=8192), writes=[("wmoe", wsl)])
                wgv = wm[:, 0:2048].rearrange("p (k c) -> p k c", k=8)
                wuv_ = wm[:, 2048:4096].rearrange("p (k c) -> p k c", k=8)
                wdv = wm[:, 4096:6144].rearrange("p (f c) -> p f c", f=2)
                for c in range(NCH):
                    cs = slice(c * 512, (c + 1) * 512)
                    hs = rr[0] % 2
                    rr[0] += 1
                    for f in range(2):
                        gb_, ub_ = f * 2, f * 2 + 1
                        for k in range(8):
                            P.op("pe", lambda e, gb_=gb_, k=k, f=f, wgv=wgv, cs=cs: e.matmul(PS(gb_)[:, :], lhsT=wgv[:, k, f * 128:(f + 1) * 128], rhs=h2T[:, k, cs], start=(k == 0), stop=(k == 7)),
                                 reads=[("wmoe", wsl), ("hT", c)], writes=[("ps", gb_)])
                        for k in range(8):
                            P.op("pe", lambda e, ub_=ub_, k=k, f=f, wuv_=wuv_, cs=cs: e.matmul(PS(ub_)[:, :], lhsT=wuv_[:, k, f * 128:(f + 1) * 128], rhs=h2T[:, k, cs], start=(k == 0), stop=(k == 7)),
                                 reads=[("wmoe", wsl), ("hT", c)], writes=[("ps", ub_)])
                        sgi = (hs * 2 + f)
                        P.op("act", lambda e, gb_=gb_, sgi=sgi: e.activation(out=sgm[sgi], in_=PS(gb_)[:, :], func=AF.Silu), reads=[("ps", gb_)], writes=[("sgm", sgi)])
                        P.op("dve", lambda e, ub_=ub_, sgi=sgi, hs=hs, f=f: e.tensor_tensor(out=hid[hs][:, f, :], in0=sgm[sgi], in1=PS(ub_)[:, :], op=ALU.mult),
                             reads=[("sgm", sgi), ("ps", ub_)], writes=[("hid", hs)])
                    for tl in range(4):
                        i = c * 4 + tl
                        for half in range(2):
                            pb = 4 + (rr[0] % 4)
                            rr[0] += 1
                            for f in range(2):
                                P.op("pe", lambda e, pb=pb, f=f, hs=hs, tl=tl, wdv=wdv, half=half: e.matmul(PS(pb)[:, :], lhsT=hid[hs][:, f, tl * 128:(tl + 1) * 128], rhs=wdv[:, f, half * 512:(half + 1) * 512],
                                                                                                     start=(f == 0), stop=(f == 1)),
                                     reads=[("hid", hs), ("wmoe", wsl)], writes=[("ps", pb)])
                            P.op("dve", lambda e, pb=pb, i=i, half=half, ex=ex: e.scalar_tensor_tensor(out=x1[:, i, half * 512:(half + 1) * 512], in0=PS(pb)[:, :], scalar=comb[:, i, ex:ex + 1],
                                                                                                  in1=x1[:, i, half * 512:(half + 1) * 512], op0=ALU.mult, op1=ALU.add),
                                 reads=[("ps", pb), "comb", ("x1", i)], writes=[("x1", i)])
            phase_barrier()
            if stop_after == "P5":
                break

            h3T = hT
            P.dma("pool", "wpg", lambda e: e.dma_start(out=wpg.rearrange("p k c -> p (k c)"), in_=wpg_d[:, :], max_dma_last_dim=8192), writes=["wpg"])
            P.dma("pool", "wpl", lambda e: e.dma_start(out=wpl.rearrange("p k c -> p (k c)"), in_=wpl_d[:, :], max_dma_last_dim=8192), writes=["wpl"])
            build_gB(2)
            norm_transpose(b, "x1", 2, h3T, "hT", None, [dD(0, 2 * KB, BF16), dD(2 * KB, 2 * KB, BF16)], [6, 7])
            pt_b = [dD(4 * KB + i * KB, 1 * KB, F32) for i in range(2)]
            pT_b = [dD(6 * KB + i * 512, 512, BF16, "p (k t) -> p k t", k=2) for i in range(2)]
            sig = [dD(8 * KB + i * 2 * KB, 2 * KB, BF16) for i in range(2)]
            tmpf = [dD(12 * KB + i * 4 * KB, 4 * KB, F32) for i in range(2)]
            outb = [dD(20 * KB + i * 4 * KB, 4 * KB, F32) for i in range(2)]
            junkf = dD(28 * KB, 2 * KB, BF16)
            gfin = dD(30 * KB, 4 * KB, F32)
            P.dma("sp", "c_gfin", lambda e: e.dma_start(out=gfin, in_=gfin_d[:, :]), writes=["c_gfin"])
            for i in range(NT):
                sl = i % 2
                P.dma("sp", "pt%d" % sl, lambda e, i=i, sl=sl: e.dma_start(out=pt_b[sl], in_=p_d[b, i * 128:(i + 1) * 128, :]), writes=[("pt", sl)])
                for k in range(2):
                    P.op("pe", lambda e, k=k, sl=sl: e.transpose(out=PS(5)[:, k * 128:(k + 1) * 128], in_=pt_b[sl][:, k * 128:(k + 1) * 128], identity=ident_f[:]),
                         reads=[("pt", sl), "c_ident"], writes=[("ps", 5)])
                P.op("act", lambda e, sl=sl: e.activation(out=pT_b[sl].rearrange("p k t -> p (k t)"), in_=PS(5)[:, 0:256], func=AF.Copy), reads=[("ps", 5)], writes=[("pT", sl)])
                for half in range(2):
                    gbk = half
                    pbk = 2 + half
                    for k in range(8):
                        P.op("pe", lambda e, gbk=gbk, k=k, i=i, half=half: e.matmul(PS(gbk)[:, :], lhsT=h3T[:, k, i * 128:(i + 1) * 128], rhs=wpg[:, k, half * 512:(half + 1) * 512], start=(k == 0), stop=(k == 7)),
                             reads=[("hT", i // 4), "wpg"], writes=[("ps", gbk)])
                    for k in range(2):
                        P.op("pe", lambda e, pbk=pbk, k=k, sl=sl, half=half: e.matmul(PS(pbk)[:, :], lhsT=pT_b[sl][:, k, :], rhs=wpl[:, k, half * 512:(half + 1) * 512], start=(k == 0), stop=(k == 1)),
                             reads=[("pT", sl), "wpl"], writes=[("ps", pbk)])
                    hsl = slice(half * 512, (half + 1) * 512)
                    P.op("act", lambda e, gbk=gbk, sl=sl, hsl=hsl: e.activation(out=sig[sl][:, hsl], in_=PS(gbk)[:, :], func=AF.Sigmoid), reads=[("ps", gbk)], writes=[("sig", sl, half)])
                    P.op("dve", lambda e, pbk=pbk, sl=sl, hsl=hsl: e.tensor_tensor(out=tmpf[sl][:, hsl], in0=sig[sl][:, hsl], in1=PS(pbk)[:, :], op=ALU.mult),
                         reads=[("sig", sl, half), ("ps", pbk)], writes=[("tmpf", sl, half)])
                    P.op("dve", lambda e, sl=sl, hsl=hsl, i=i: e.tensor_tensor(out=tmpf[sl][:, hsl], in0=tmpf[sl][:, hsl], in1=x1[:, i, hsl], op=ALU.add),
                         reads=[("tmpf", sl, half), ("x1", i)], writes=[("tmpf", sl, half)])
                ssc = small[:, 64 + i:65 + i]
                P.op("act", lambda e, sl=sl, ssc=ssc: e.activation(out=junkf, in_=tmpf[sl], func=AF.Square, accum_out=ssc), reads=[("tmpf", sl, 0), ("tmpf", sl, 1)], writes=["junkf", ("fs", i)])
                P.op("dve", lambda e, ssc=ssc: e.tensor_scalar(out=ssc, in0=ssc, scalar1=1.0 / D, scalar2=EPS, op0=ALU.mult, op1=ALU.add), reads=[("fs", i)], writes=[("fs", i)])
                P.op("act", lambda e, ssc=ssc: e.activation(out=ssc, in_=ssc, func=AF.Sqrt), reads=[("fs", i)], writes=[("fs", i)])
                P.op("dve", lambda e, ssc=ssc: e.reciprocal(out=ssc, in_=ssc), reads=[("fs", i)], writes=[("fs", i)])
                P.op("dve", lambda e, sl=sl, ssc=ssc: e.scalar_tensor_tensor(out=outb[sl], in0=tmpf[sl], scalar=ssc, in1=gfin, op0=ALU.mult, op1=ALU.mult),
                     reads=[("tmpf", sl, 0), ("tmpf", sl, 1), ("fs", i), "c_gfin"], writes=[("outb", sl)])
                tok = P.dma("sp", "out%d" % sl, lambda e, sl=sl, i=i: e.dma_start(out=out_d[b, i * 128:(i + 1) * 128, :], in_=outb[sl]), reads=[("outb", sl)])
                last_out_tokens.append(tok)
            phase_barrier()

        finals = {}
        for tok in last_out_tokens:
            finals[tok[1]] = max(finals.get(tok[1], 0), tok[2])
        for k in P.dma_keys:
            if k == "dbg":
                finals[k] = P.dma_count[k]
        P.emit(final_wait_tokens=[("d", k, n) for k, n in finals.items()])
    return nc


def _pk(w, ncols=None):
    K = w.shape[0] // 128
    return np.ascontiguousarray(w.reshape(K, 128, -1).transpose(1, 0, 2).reshape(128, -1))


def _t5_bucket_np(d):
    d = np.maximum(d, 0)
    d_f = np.maximum(d, 1).astype(np.float32)
    large = 16 + (np.log(d_f / np.float32(16)) / np.float32(math.log(128 / 16)) * np.float32(16)).astype(np.int32)
    large = np.minimum(large, 31)
    return np.where(d < 16, d, large)


def prep_weights(inp):
    f = lambda a: np.ascontiguousarray(a, dtype=np.float32)
    w_in = inp["w_in"][0]
    o = {}
    c0 = 0
    sec = {}
    for nm, wdt in (("q_a", 512), ("c_kv", 128), ("q_idx", 512), ("k_idx", 64), ("w_idx", 8), ("qkv_b", 1536), ("gate_a", 1024), ("gate_b", 1024)):
        sec[nm] = w_in[:, c0:c0 + wdt]
        c0 += wdt
    w1 = np.concatenate([sec["q_a"], sec["q_idx"], sec["c_kv"], sec["k_idx"], sec["k_idx"]], axis=1)
    o["w1"] = _pk(w1)
    o["widx"] = _pk(sec["w_idx"])
    o["w3"] = _pk(sec["qkv_b"])
    ga = sec["gate_a"].reshape(1024, 8, 128)
    gb = sec["gate_b"].reshape(1024, 8, 128)
    g4 = np.concatenate([ga, gb], axis=2)
    g4 = g4.reshape(8, 128, 8, 256).transpose(1, 2, 0, 3)
    o["wg4"] = f(g4.reshape(128, -1))
    wa = inp["w_branch_a"][0].reshape(4, 128, 8, 128)
    wb = inp["w_branch_b"][0].reshape(4, 128, 8, 128)
    wbr = np.stack([wa, wb], axis=0).transpose(2, 3, 0, 1, 4)
    o["wbr"] = f(wbr.reshape(128, -1))
    o["wout"] = _pk(inp["w_out"][0])
    wuk = inp["w_uk"][0]
    wukT = wuk.transpose(0, 2, 1).reshape(4, 2, 64, 128).transpose(1, 2, 0, 3)
    o["wuk"] = f(wukT.reshape(128, 512))
    o["wuv"] = f(inp["w_uv"][0].transpose(1, 0, 2).reshape(128, 512))
    wg = inp["w_gate"][0].reshape(N_EXP, 8, 128, 256).transpose(0, 2, 1, 3).reshape(N_EXP, 128, 2048)
    wu = inp["w_up"][0].reshape(N_EXP, 8, 128, 256).transpose(0, 2, 1, 3).reshape(N_EXP, 128, 2048)
    wd = inp["w_down"][0].reshape(N_EXP, 2, 128, 1024).transpose(0, 2, 1, 3).reshape(N_EXP, 128, 2048)
    o["wmoe"] = f(np.concatenate([wg, wu, wd], axis=2))
    wr = np.concatenate([inp["w_r1"][0], inp["w_r2"][0].transpose(1, 0, 2).reshape(1024, 32)], axis=1)
    o["wr"] = _pk(wr)
    br = np.concatenate([inp["b_r1"][0], inp["b_r2"][0].reshape(32)])
    o["br"] = f(np.broadcast_to(br[None, :], (128, 36)))
    o["wpg"] = _pk(inp["w_ple_gate"][0])
    o["wpl"] = _pk(inp["w_ple"][0])
    o["g_attn"] = f(inp["attn_norm"][0].reshape(8, 128).T)
    o["g_ffn"] = f(inp["ffn_norm"][0].reshape(8, 128).T)
    o["g_ple"] = f(inp["ple_norm"][0].reshape(8, 128).T)
    o["g_fin"] = f(np.broadcast_to(inp["final_norm"][None, :], (128, 1024)))
    o["g_kv"] = f(inp["kv_norm"][0].reshape(128, 1))
    rb = inp["rel_bias"]
    s_l = np.arange(128)[:, None]
    u = np.arange(640)[None, :]
    bidx = _t5_bucket_np(u - s_l)
    o["btoep"] = f(rb[bidx].transpose(0, 2, 1).reshape(128, 8 * 640))
    o["b31"] = f(np.broadcast_to(rb[31][None, :], (128, 8)))
    o["ident"] = np.eye(128, dtype=np.float32)
    tt = np.arange(128)[:, None]
    ss = np.arange(128)[None, :]
    o["cneg"] = np.where(ss <= tt, 0.0, NEG).astype(np.float32)
    o["smask"] = (tt < ss).astype(np.float32)
    o["uinc"] = (tt >= ss).astype(np.float32)
    sel = np.zeros((128, 256), np.float32)
    sel[64, 0:128] = 1.0
    sel[63, 128:256] = 1.0
    o["sel"] = sel
    return o


_NC_CACHE = {}


def kernel(**inputs):
    inp = {k: np.asarray(v) for k, v in inputs.items()}
    n = 8
    NB = 2
    wts = prep_weights(inp)
    x = np.ascontiguousarray(inp["x"], dtype=np.float32)
    p = np.ascontiguousarray(inp["p"][0], dtype=np.float32)
    if "nc" not in _NC_CACHE:
        _NC_CACHE["nc"] = build_nc(NB=NB)
    nc = _NC_CACHE["nc"]
    in_maps = []
    for c in range(n):
        m = dict(wts)
        m["x"] = x[c * NB:(c + 1) * NB]
        m["p"] = p[c * NB:(c + 1) * NB]
        in_maps.append(m)
    res = run_bass_kernel_spmd(nc, in_maps, core_ids=list(range(n)))
    out = np.concatenate([r["out"] for r in res.results], axis=0)
    return out.astype(np.float32)
```

```python
import math
import types
import contextlib
import numpy as np
import concourse.bass as bass
import concourse.mybir as mybir
from concourse.bass_utils import run_bass_kernel_spmd

F32 = mybir.dt.float32
BF16 = mybir.dt.bfloat16
AF = mybir.ActivationFunctionType
ALU = mybir.AluOpType
AX = mybir.AxisListType

S = 2048
D = 1024
NT = S // 128
NCH = S // 512
ATTN_SCALE = 64 ** -0.5
IDX_SCALE = (8 ** -0.5) * (64 ** -0.5)
EPS = 1e-6
NEG = -1.0e30
N_BISECT = 14
N_EXP = 32
import os as _os
SBE = _os.environ.get("SBE", "pool")
SBLN = _os.environ.get("SBLN", "1") == "1"


class Prog:
    ENGS = ("pe", "act", "dve", "pool", "sp")

    def __init__(self, nc, same_eng_sync=True):
        self.nc = nc
        self.ops = {e: [] for e in self.ENGS}
        self.last_w = {}
        self.last_r = {}
        self.clock = {e: {} for e in self.ENGS}
        self.opclock = {}
        self.dma_count = {}
        self.dma_keys = []
        self.signaling = set()
        self.same_eng_sync = same_eng_sync
        self._bar = 0

    def _add(self, eng, fn, reads, writes, dma_key=None, n_dma=1):
        idx = len(self.ops[eng]) + 1
        deps = {}

        def need(tok):
            kind, who, n = tok
            if kind == "e" and who == eng:
                if eng in ("pe", "sp") or not self.same_eng_sync:
                    return
            k = (kind, who)
            if deps.get(k, 0) < n:
                deps[k] = n

        for r in reads:
            for tok in self.last_w.get(r, {}).values():
                need(tok)
        for w in writes:
            for tok in self.last_w.get(w, {}).values():
                need(tok)
            for tok in self.last_r.get(w, {}).values():
                need(tok)
        if dma_key is not None:
            if dma_key not in self.dma_count:
                self.dma_count[dma_key] = 0
                self.dma_keys.append(dma_key)
            prev = self.dma_count[dma_key]
            if prev > 0:
                need(("d", dma_key, prev))
            self.dma_count[dma_key] = prev + n_dma
            mytok = ("d", dma_key, prev + n_dma)
        else:
            mytok = ("e", eng, idx)
        clk = self.clock[eng]
        final = []
        for k, n in deps.items():
            if clk.get(k, 0) >= n:
                continue
            final.append((k[0], k[1], n))
        for kind, who, n in final:
            oc = self.opclock.get((kind, who, n))
            if oc:
                for k2, n2 in oc.items():
                    if clk.get(k2, 0) < n2:
                        clk[k2] = n2
            if clk.get((kind, who), 0) < n:
                clk[(kind, who)] = n
            if kind == "e":
                self.signaling.add((who, n))
        snap = dict(clk)
        if mytok[0] == "e":
            snap[("e", eng)] = idx
        self.opclock[mytok] = snap
        self.ops[eng].append((fn, final, dma_key, mytok))
        if fn is not None:
            for r in reads:
                self.last_r.setdefault(r, {})[(mytok[0], mytok[1])] = mytok
        for w in writes:
            self.last_w[w] = {(mytok[0], mytok[1]): mytok}
            self.last_r[w] = {}
        return mytok

    @staticmethod
    def _freeze(fn):
        if fn is None or getattr(fn, "__closure__", None) is None:
            return fn
        cells = []
        for c in fn.__closure__:
            try:
                cells.append(types.CellType(c.cell_contents))
            except ValueError:
                cells.append(c)
        return types.FunctionType(fn.__code__, fn.__globals__, fn.__name__, fn.__defaults__, tuple(cells))

    def op(self, eng, fn, reads=(), writes=()):
        return self._add(eng, self._freeze(fn), tuple(reads), tuple(writes))

    def dma(self, eng, key, fns, reads=(), writes=()):
        if not isinstance(fns, (list, tuple)):
            fns = [fns]
        return self._add(eng, [self._freeze(f) for f in fns], tuple(reads), tuple(writes), dma_key=key, n_dma=len(fns))

    def barrier(self, tiny_fn):
        self._bar += 1
        res = ("__barrier__", self._bar)
        allres = list(set(list(self.last_w.keys()) + list(self.last_r.keys())))
        self._add("dve", self._freeze(tiny_fn), tuple(), tuple(allres) + (res,))
        for e in ("pe", "act", "pool", "sp"):
            self._add(e, None, (res,), tuple())

    def emit(self, final_wait_tokens=()):
        nc = self.nc
        sigval = {}
        for e in self.ENGS:
            s = 0
            for i in range(1, len(self.ops[e]) + 1):
                if (e, i) in self.signaling:
                    s += 1
                    sigval[(e, i)] = s
        engobj = {"pe": nc.tensor, "act": nc.scalar, "dve": nc.vector, "pool": nc.gpsimd, "sp": nc.sync}
        with contextlib.ExitStack() as st:
            esem = {e: st.enter_context(nc.semaphore("sem_" + e)) for e in self.ENGS}
            dsem = {k: st.enter_context(nc.semaphore("dsem_%d" % i)) for i, k in enumerate(self.dma_keys)}
            block = st.enter_context(nc.Block())

            def run(e):
                eng = engobj[e]
                for i, (fn, deps, dma_key, mytok) in enumerate(self.ops[e], start=1):
                    for kind, who, n in deps:
                        if kind == "e":
                            eng.wait_ge(esem[who], sigval[(who, n)])
                        else:
                            eng.wait_ge(dsem[who], 16 * n)
                    if fn is None:
                        assert (e, i) not in self.signaling
                        continue
                    if dma_key is not None:
                        for f in fn:
                            f(eng).then_inc(dsem[dma_key], 16)
                    else:
                        ins = fn(eng)
                        if (e, i) in self.signaling:
                            ins.then_inc(esem[e], 1)
                if e == "sp":
                    for k in self.dma_keys:
                        eng.wait_ge(dsem[k], 16 * self.dma_count[k])

            block.tensor(lambda eng: run("pe"))
            block.scalar(lambda eng: run("act"))
            block.vector(lambda eng: run("dve"))
            block.gpsimd(lambda eng: run("pool"))
            block.sync(lambda eng: run("sp"))


def build_nc(NB=2, stop_after=None, dbg=False):
    nc = bass.Bass("TRN2", target_bir_lowering=False)

    def din(name, shape, dt=F32):
        return nc.dram_tensor(name, list(shape), dt, kind="ExternalInput").ap()

    x_d = din("x", [NB, S, D])
    p_d = din("p", [NB, S, 256])
    w1_d = din("w1", [128, 8 * 1280])
    widx_d = din("widx", [128, 8 * 8])
    w3_d = din("w3", [128, 8 * 1536])
    wg4_d = din("wg4", [128, 8 * 8 * 256])
    wbr_d = din("wbr", [128, 8 * 1024])
    wout_d = din("wout", [128, 8 * 1024])
    wuk_d = din("wuk", [128, 512])
    wuv_d = din("wuv", [128, 512])
    wmoe_d = din("wmoe", [N_EXP, 128, 6144])
    wr_d = din("wr", [128, 8 * 36])
    br_d = din("br", [128, 36])
    wpg_d = din("wpg", [128, 8 * 1024])
    wpl_d = din("wpl", [128, 2 * 1024])
    gat_d = din("g_attn", [128, 8])
    gff_d = din("g_ffn", [128, 8])
    gpl_d = din("g_ple", [128, 8])
    gfin_d = din("g_fin", [128, 1024])
    gkv_d = din("g_kv", [128, 1])
    btoep_d = din("btoep", [128, 8 * 640])
    b31_d = din("b31", [128, 8])
    ident_d = din("ident", [128, 128])
    cneg_d = din("cneg", [128, 128])
    smask_d = din("smask", [128, 128])
    uinc_d = din("uinc", [128, 128])
    sel_d = din("sel", [128, 256])
    out_d = nc.dram_tensor("out", [NB, S, D], F32, kind="ExternalOutput").ap()
    dbg_d = {}
    if dbg:
        for nm, shp in (("d_oaT", [128, 4 * S]), ("d_obT", [128, 4 * S]), ("d_x1", [128, NT * D])):
            dbg_d[nm] = nc.dram_tensor(nm, shp, F32, kind="ExternalOutput").ap()

    st = contextlib.ExitStack()
    with st:
        def sb(name, shape, dt):
            return st.enter_context(nc.sbuf_tensor("s_" + name, list(shape), dt))

        arA = sb("arA", [128, 16384], BF16)
        arB = sb("arB", [128, 32768], BF16)
        arC = sb("arC", [128, 16384], BF16)
        arD = sb("arD", [128, 33792], BF16)
        ident_f = sb("ident_f", [128, 128], F32)
        ident_b = sb("ident_b", [128, 128], BF16)
        cneg = sb("cneg", [128, 128], F32)
        smask = sb("smask", [128, 128], BF16)
        uinc = sb("uinc", [128, 128], BF16)
        ones_b = sb("ones_b", [128, 128], BF16)
        sel_f = sb("sel_f", [128, 256], F32)
        gB1 = sb("gB", [128, 8, 128], BF16)
        gB = [gB1, gB1, gB1]
        gpk = sb("gpk", [128, 24], F32)
        gkv = sb("gkv", [128, 1], F32)
        b31 = sb("b31", [128, 8], F32)
        brb = sb("brb", [128, 36], F32)
        wr_f = sb("wr_f", [128, 8, 36], F32)
        wuk = sb("wuk", [128, 4, 128], BF16)
        wuv = sb("wuv", [128, 512], BF16)
        widx_w = sb("widx_w", [128, 8, 8], BF16)
        small = sb("small", [128, 256], F32)
        tiny = sb("tiny", [128, 2], F32)

        psb = [st.enter_context(nc.psum_tensor("ps%d" % i, [128, 512], F32)) for i in range(8)]

        P = Prog(nc)

        def carve(ar, off_bytes, nbytes, dt, pattern=None, **kw):
            e0 = off_bytes // 2
            ap = ar[:, e0:e0 + nbytes // 2]
            if dt == F32:
                ap = ap.bitcast(F32)
            if pattern:
                ap = ap.rearrange(pattern, **kw)
            return ap

        KB = 1024
        hT = carve(arA, 0, 32 * KB, BF16, "p (k t) -> p k t", k=8)
        qaT = carve(arB, 0, 16 * KB, BF16, "p (k t) -> p k t", k=4)
        qiT = carve(arB, 16 * KB, 16 * KB, BF16, "p (k t) -> p k t", k=4)
        ckvT = carve(arB, 32 * KB, 4 * KB, BF16)
        kiT = carve(arB, 36 * KB, 4 * KB, BF16)
        Vp = carve(arB, 40 * KB, 16 * 4 * 130 * 2, BF16, "p (j m c) -> p j m c", j=16, m=4)
        widx_tm = carve(arB, 40 * KB + 16640, 512, F32, "p (i h) -> p i h", i=16)
        qbT = carve(arB, 0, 16 * KB, BF16, "p (k t) -> p k t", k=4)
        kbT = carve(arB, 16 * KB, 16 * KB, BF16, "p (k t) -> p k t", k=4)
        vb = carve(arB, 32 * KB, 16 * KB, BF16, "p (j c) -> p j c", j=16)
        qbTn = carve(arB, 48 * KB, 16 * KB, BF16, "p (k t) -> p k t", k=4)
        x1 = carve(arB, 0, 64 * KB, F32, "p (i d) -> p i d", i=16)
        oaT = carve(arC, 0, 16 * KB, BF16, "p (k t) -> p k t", k=4)
        obT = carve(arC, 16 * KB, 16 * KB, BF16, "p (k t) -> p k t", k=4)
        wmoe = [carve(arC, i * 12 * KB, 12 * KB, BF16) for i in range(2)]
        wpg = carve(arC, 0, 16 * KB, BF16, "p (k c) -> p k c", k=8)
        wpl = carve(arC, 16 * KB, 4 * KB, BF16, "p (k c) -> p k c", k=2)
        def dD(off, nbytes, dt, pattern=None, **kw):
            assert off + nbytes <= 66 * KB, (off, nbytes)
            return carve(arD, off, nbytes, dt, pattern, **kw)

        PS = lambda i: psb[i]

        def psbf(i):
            return psb[i][:].bitcast(BF16)

        def ld(key, dst, src, eng="sp", res=None, **kw):
            P.dma(eng, key, lambda e: e.dma_start(out=dst, in_=src, **kw), writes=[res or key])

        ld("c_ident", ident_f[:], ident_d[:, :])
        ld("c_cneg", cneg[:], cneg_d[:, :])
        ld("c_sel", sel_f[:], sel_d[:, :])
        ld("c_gkv", gkv[:], gkv_d[:, :])
        ld("c_b31", b31[:], b31_d[:, :])
        ld("c_br", brb[:], br_d[:, :])
        ld("c_wr", wr_f[:].rearrange("p k c -> p (k c)"), wr_d[:, :])
        ld("c_g0", gpk[:, 0:8], gat_d[:, :])
        ld("c_g1", gpk[:, 8:16], gff_d[:, :])
        ld("c_g2", gpk[:, 16:24], gpl_d[:, :])
        ld("c_smask", smask[:], smask_d[:, :], eng="pool")
        ld("c_uinc", uinc[:], uinc_d[:, :], eng="pool")
        ld("c_wuk", wuk[:].rearrange("p k c -> p (k c)"), wuk_d[:, :], eng="pool")
        ld("c_wuv", wuv[:], wuv_d[:, :], eng="pool")
        ld("c_widx", widx_w[:].rearrange("p k c -> p (k c)"), widx_d[:, :], eng="pool")
        P.op("dve", lambda e: e.tensor_copy(out=ident_b[:], in_=ident_f[:]), reads=["c_ident"], writes=["ident_b"])
        P.op("dve", lambda e: e.memset(ones_b[:], 1.0), writes=["ones_b"])
        P.op("dve", lambda e: e.memset(tiny[:], 0.0), writes=["tiny"])
        def build_gB(gi):
            for k in range(8):
                P.op("dve", lambda e, gi=gi, k=k: e.tensor_scalar(out=gB1[:, k, :], in0=ones_b[:], scalar1=gpk[:, gi * 8 + k:gi * 8 + k + 1],
                                                                  scalar2=None, op0=ALU.mult),
                     reads=["ones_b", "c_g%d" % gi], writes=["gB"])

        evac_rr = [0]

        def evac(out, in_, reads, writes, scale=None, eng=None):
            if eng is None:
                eng = ("act", "dve")[evac_rr[0] % 2]
                evac_rr[0] += 1
            if eng == "act":
                if scale is None:
                    P.op("act", lambda e: e.activation(out=out, in_=in_, func=AF.Copy), reads=reads, writes=writes)
                else:
                    P.op("act", lambda e: e.activation(out=out, in_=in_, func=AF.Copy, scale=float(scale)), reads=reads, writes=writes)
            else:
                if scale is None:
                    P.op("dve", lambda e: e.tensor_copy(out=out, in_=in_), reads=reads, writes=writes)
                else:
                    P.op("dve", lambda e: e.tensor_scalar(out=out, in0=in_, scalar1=float(scale), scalar2=None, op0=ALU.mult), reads=reads, writes=writes)

        def phase_barrier():
            P.barrier(lambda e: e.memset(tiny[:, 0:1], 0.0))

        def norm_transpose(b, src, gi, dstT, dstres, xt_bufs, xn_bufs, ps_banks, f32_side=None):
            for i in range(NT):
                sl = i % 2
                if src == "x":
                    xt = xt_bufs[sl]
                    xres = "xt%d" % sl
                    P.dma("sp", xres, lambda e, xt=xt, i=i: e.dma_start(out=xt, in_=x_d[b, i * 128:(i + 1) * 128, :]), writes=[xres])
                else:
                    xt = x1[:, i, :]
                    xres = ("x1", i)
                xn = xn_bufs[sl]
                xnres = "xn%d" % sl
                ssc = small[:, i:i + 1]
                rsc = small[:, 16 + i:17 + i]
                P.op("act", lambda e, xt=xt, xn=xn, ssc=ssc: e.activation(out=xn, in_=xt, func=AF.Square, accum_out=ssc),
                     reads=[xres], writes=[xnres, ("ss", i)])
                P.op("dve", lambda e, ssc=ssc, rsc=rsc: e.tensor_scalar(out=rsc, in0=ssc, scalar1=1.0 / D, scalar2=EPS, op0=ALU.mult, op1=ALU.add),
                     reads=[("ss", i)], writes=[("rs", i)])
                P.op("act", lambda e, rsc=rsc: e.activation(out=rsc, in_=rsc, func=AF.Sqrt), reads=[("rs", i)], writes=[("rs", i)])
                P.op("dve", lambda e, rsc=rsc: e.reciprocal(out=rsc, in_=rsc), reads=[("rs", i)], writes=[("rs", i)])
                if f32_side is None:
                    P.op("dve", lambda e, xt=xt, xn=xn, rsc=rsc: e.tensor_scalar(out=xn, in0=xt, scalar1=rsc, scalar2=None, op0=ALU.mult),
                         reads=[xres, ("rs", i)], writes=[xnres])
                    pb = ps_banks[i % len(ps_banks)]
                    pres = ("ps", pb)
                    pv = psbf(pb)[:, 0:1024].rearrange("p (k t) -> p k t", k=8)
                    for k in range(8):
                        P.op("pe", lambda e, pv=pv, xn=xn, k=k: e.transpose(out=pv[:, k, :], in_=xn[:, k * 128:(k + 1) * 128], identity=ident_b[:]),
                             reads=[xnres, "ident_b"], writes=[pres])
                    P.op("dve", lambda e, pv=pv, i=i: e.tensor_tensor(out=dstT[:, :, i * 128:(i + 1) * 128], in0=pv, in1=gB[gi][:], op=ALU.mult),
                         reads=[pres, "gB"], writes=[(dstres, i // 4)])
                else:
                    f32_side(i, xt, xres, rsc, xn, xnres)

        last_out_tokens = []
        for b in range(NB):
            xt_bufs = [dD(0, 4 * KB, F32), dD(4 * KB, 4 * KB, F32)]
            xn_bufs = [dD(8 * KB, 2 * KB, BF16), dD(10 * KB, 2 * KB, BF16)]
            w1 = dD(12 * KB, 20 * KB, BF16, "p (k c) -> p k c", k=8)
            ckv_raw = dD(32 * KB, 8 * KB, F32)
            sqb = [dD(40 * KB, 1 * KB, BF16), dD(41 * KB, 1 * KB, BF16)]
            rstd_b = [dD(42 * KB, 2 * KB, F32), dD(44 * KB, 2 * KB, F32)]
            P.dma("pool", "w1", lambda e: e.dma_start(out=w1.rearrange("p k c -> p (k c)"), in_=w1_d[:, :], max_dma_last_dim=8192), writes=["w1"])
            build_gB(0)
            norm_transpose(b, "x", 0, hT, "hT", xt_bufs, xn_bufs, [6, 7])

            if stop_after == "P0":
                break
            bank_rr = [0]

            def nb(banks):
                v = banks[bank_rr[0] % len(banks)]
                bank_rr[0] += 1
                return v

            def proj_T(w, wres, cc, c, banks):
                pb = nb(banks)
                for k in range(8):
                    P.op("pe", lambda e, pb=pb, k=k: e.matmul(PS(pb)[:, :], lhsT=w[:, k, cc * 128:(cc + 1) * 128], rhs=hT[:, k, c * 512:(c + 1) * 512],
                                                             start=(k == 0), stop=(k == 7)),
                         reads=[wres, ("hT", c)], writes=[("ps", pb)])
                return pb

            for cc in range(10):
                for c in range(NCH):
                    pb = proj_T(w1, "w1", cc, c, [0, 1, 2, 3])
                    cs = slice(c * 512, (c + 1) * 512)
                    if cc < 4:
                        evac(qaT[:, cc, cs], PS(pb)[:, :], [("ps", pb)], [("qaT", c)])
                    elif cc < 8:
                        evac(qiT[:, cc - 4, cs], PS(pb)[:, :], [("ps", pb)], [("qiT", c)])
                    elif cc == 8:
                        evac(ckv_raw[:, cs], PS(pb)[:, :], [("ps", pb)], [("ckv_raw", c)])
                    else:
                        evac(kiT[:, cs], PS(pb)[:, :], [("ps", pb)], [("kiT", c)])
            for c in range(NCH):
                cs = slice(c * 512, (c + 1) * 512)
                sq = sqb[c % 2]
                rb = rstd_b[c % 2]
                P.op("act", lambda e, sq=sq, cs=cs: e.activation(out=sq, in_=ckv_raw[:, cs], func=AF.Square), reads=[("ckv_raw", c)], writes=[("sq", c % 2)])
                pb = nb([0, 1, 2, 3])
                P.op("pe", lambda e, pb=pb, sq=sq: e.matmul(PS(pb)[:, :], lhsT=ones_b[:], rhs=sq, start=True, stop=True),
                     reads=[("sq", c % 2), "ones_b"], writes=[("ps", pb)])
                P.op("dve", lambda e, pb=pb, rb=rb: e.tensor_scalar(out=rb, in0=PS(pb)[:, :], scalar1=1.0 / 128, scalar2=EPS, op0=ALU.mult, op1=ALU.add),
                     reads=[("ps", pb)], writes=[("rb", c % 2)])
                P.op("act", lambda e, rb=rb: e.activation(out=rb, in_=rb, func=AF.Sqrt), reads=[("rb", c % 2)], writes=[("rb", c % 2)])
                P.op("dve", lambda e, rb=rb: e.reciprocal(out=rb, in_=rb), reads=[("rb", c % 2)], writes=[("rb", c % 2)])
                P.op("dve", lambda e, rb=rb, cs=cs: e.scalar_tensor_tensor(out=ckvT[:, cs], in0=ckv_raw[:, cs], scalar=gkv[:, 0:1], in1=rb, op0=ALU.mult, op1=ALU.mult),
                     reads=[("ckv_raw", c), ("rb", c % 2), "c_gkv"], writes=[("ckvT", c)])
            pbw = 4
            for i in range(NT):
                for k in range(8):
                    P.op("pe", lambda e, i=i, k=k: e.matmul(PS(pbw)[:, i * 8:(i + 1) * 8], lhsT=hT[:, k, i * 128:(i + 1) * 128], rhs=widx_w[:, k, :],
                                                         start=(k == 0), stop=(k == 7)),
                         reads=[("hT", i // 4), "c_widx"], writes=[("ps", pbw)])
            P.op("dve", lambda e: e.tensor_scalar(out=widx_tm.rearrange("p i h -> p (i h)"), in0=PS(pbw)[:, 0:128], scalar1=IDX_SCALE, scalar2=None, op0=ALU.mult),
                 reads=[("ps", pbw)], writes=["widx_tm"])
            P.op("pool", lambda e: e.memset(Vp.rearrange("p j m c -> p (j m) c")[:, :, 64:65], 1.0), writes=["Vp"])
            P.op("pool", lambda e: e.memset(Vp.rearrange("p j m c -> p (j m) c")[:, :, 129:130], 1.0), writes=["Vp"])
            for j in range(NT):
                pb = nb([0, 1, 2, 3])
                P.op("pe", lambda e, pb=pb, j=j: e.matmul(PS(pb)[:, :], lhsT=ckvT[:, j * 128:(j + 1) * 128], rhs=wuv[:], start=True, stop=True),
                     reads=[("ckvT", j // 4), "c_wuv"], writes=[("ps", pb)])
                pv = PS(pb)[:, :].rearrange("p (m h d) -> p m h d", m=4, h=2)
                evac(Vp[:, j, :, 0:64], pv[:, :, 0, :], [("ps", pb)], ["Vp"])
                evac(Vp[:, j, :, 65:129], pv[:, :, 1, :], [("ps", pb)], ["Vp"])
            phase_barrier()
            if stop_after == "P1":
                break

            NEGM = 30000.0
            score = [dD(0, 8 * KB, F32), dD(8 * KB, 8 * KB, F32), carve(arC, 16 * KB, 8 * KB, F32), carve(arC, 24 * KB, 8 * KB, F32)]
            junk = dD(16 * KB, 4 * KB, BF16)
            mask_tm = dD(20 * KB, 4 * KB, BF16)
            maskT = dD(24 * KB, 16 * KB, BF16, "p (j t) -> p j t", j=16)
            B8 = dD(40 * KB, 10 * KB, BF16, "p (h u) -> p h u", h=8)
            rbuf = [dD(50 * KB + i * KB, 1 * KB, BF16) for i in range(4)]
            Pbuf = [dD(54 * KB + i * KB, 1 * KB, BF16) for i in range(4)]
            qabs = [dD(58 * KB + i * KB, 1 * KB, BF16) for i in range(2)]
            dg = dD(60 * KB, 2 * KB, BF16, "p (h t) -> p h t", h=8)
            o_f32 = dD(62 * KB, 2 * KB, F32)
            rec = dD(64 * KB, 2 * KB, F32)
            btmp = dD(0, 10 * KB, F32, "p (h u) -> p h u", h=4)
            for hh in range(2):
                P.dma("sp", "btoep", lambda e, hh=hh: e.dma_start(out=btmp.rearrange("p h u -> p (h u)")[:, 0:2560], in_=btoep_d[:, hh * 2560:(hh + 1) * 2560]), writes=["btmp"])
                P.op("dve", lambda e, hh=hh: e.tensor_scalar(out=B8[:, hh * 4:hh * 4 + 4, :], in0=btmp[:, 0:4, :], scalar1=1.0 / ATTN_SCALE, scalar2=None, op0=ALU.mult),
                     reads=["btmp"], writes=["B8"])
            phase_barrier()

            LO, WD, MID, CNT, TMP = 40, 44, 48, 52, 56
            rr = [0]
            smr = lambda base, a0, a1: small[:, base + a0:base + a1]

            def scores_chunk(c):
                units = []
                for tl in range(4):
                    L = (4 * c + tl + 1) * 128
                    nsc = (L + 511) // 512
                    for sc in range(nsc):
                        for h in range(8):
                            units.append((tl, sc, h, nsc))
                stt = {}

                def A(u):
                    tl, sc, h, nsc = u
                    i = 4 * c + tl
                    L = (i + 1) * 128
                    ws = min(512, L - sc * 512)
                    bp = (h % 2) * 64
                    zb = nb([0, 1, 2, 3])
                    P.op("pe", lambda e: e.matmul(PS(zb)[:, 0:ws], lhsT=qiT[bp:bp + 64, h // 2, i * 128:(i + 1) * 128], rhs=kiT[bp:bp + 64, sc * 512:sc * 512 + ws], start=True, stop=True),
                         reads=[("qiT", c), ("kiT", sc)], writes=[("ps", zb)])
                    rs = rr[0] % 4
                    rr[0] += 1
                    if rs % 2 == 0:
                        P.op("act", lambda e: e.activation(out=rbuf[rs][:, 0:ws], in_=PS(zb)[:, 0:ws], func=AF.Relu), reads=[("ps", zb)], writes=[("rbuf", rs)])
                    else:
                        P.op("dve", lambda e: e.tensor_scalar(out=rbuf[rs][:, 0:ws], in0=PS(zb)[:, 0:ws], scalar1=0.0, scalar2=None, op0=ALU.max), reads=[("ps", zb)], writes=[("rbuf", rs)])
                    stt[u] = (rs, ws)

                def B(u):
                    tl, sc, h, nsc = u
                    i = 4 * c + tl
                    rs, ws = stt[u]
                    sct = score[tl]
                    spb = 4 + (sc % 2)
                    if sc == 0 and h == 0:
                        for hh in range(8):
                            P.op("dve", lambda e, hh=hh: e.tensor_scalar(out=dg[:, hh, :], in0=ident_b[:], scalar1=widx_tm[:, i, hh:hh + 1], scalar2=None, op0=ALU.mult),
                                 reads=["ident_b", "widx_tm"], writes=[("dg", hh)])
                    P.op("pe", lambda e: e.matmul(PS(spb)[:, 0:ws], lhsT=dg[:, h, :], rhs=rbuf[rs][:, 0:ws], start=(h == 0), stop=(h == 7)),
                         reads=[("dg", h), ("rbuf", rs)], writes=[("ps", spb)])
                    if h == 7:
                        last = (sc == nsc - 1)
                        wcopy = ws - 128 if last else ws
                        if wcopy > 0:
                            P.op("act", lambda e: e.activation(out=sct[:, sc * 512:sc * 512 + wcopy], in_=PS(spb)[:, 0:wcopy], func=AF.Copy),
                                 reads=[("ps", spb)], writes=[("score", tl)])
                        if last:
                            P.op("dve", lambda e: e.tensor_tensor(out=sct[:, sc * 512 + ws - 128:sc * 512 + ws], in0=PS(spb)[:, ws - 128:ws], in1=cneg[:], op=ALU.add),
                                 reads=[("ps", spb), "c_cneg"], writes=[("score", tl)])

                nu = len(units)
                for k in range(nu + 2):
                    if k < nu:
                        A(units[k])
                    if 0 <= k - 2 < nu:
                        B(units[k - 2])

            def bisect_chunk(c):
                act = [tl for tl in range(4) if 4 * c + tl >= 2]
                for tl in range(4):
                    i = 4 * c + tl
                    L = (i + 1) * 128
                    if i < 2:
                        P.op("dve", lambda e, tl=tl: e.memset(smr(LO, tl, tl + 1), -1.0e29), writes=[("lo", tl)])
                    else:
                        P.op("dve", lambda e, tl=tl, i=i: e.tensor_reduce(out=smr(LO, tl, tl + 1), in_=score[tl][:, 0:i * 128], axis=AX.X, op=ALU.min), reads=[("score", tl)], writes=[("lo", tl)])
                        P.op("dve", lambda e, tl=tl, L=L: e.tensor_reduce(out=smr(WD, tl, tl + 1), in_=score[tl][:, 0:L], axis=AX.X, op=ALU.max), reads=[("score", tl)], writes=[("wd", tl)])
                if not act:
                    return
                a0, a1 = act[0], act[-1] + 1
                R = lambda nm: [(nm, t) for t in range(a0, a1)]
                P.op("dve", lambda e: e.tensor_tensor(out=smr(WD, a0, a1), in0=smr(WD, a0, a1), in1=smr(LO, a0, a1), op=ALU.subtract), reads=R("wd") + R("lo"), writes=R("wd"))
                for it in range(N_BISECT):
                    f = 0.5 ** (it + 1)
                    P.op("dve", lambda e, f=f: e.scalar_tensor_tensor(out=smr(MID, a0, a1), in0=smr(WD, a0, a1), scalar=f, in1=smr(LO, a0, a1), op0=ALU.mult, op1=ALU.add),
                         reads=R("wd") + R("lo"), writes=R("mid"))
                    for tl in act:
                        L = (4 * c + tl + 1) * 128
                        jb, jres = ((junk, "junk"), (mask_tm, "mask_tm"))[tl % 2]
                        P.op("dve", lambda e, tl=tl, L=L, jb=jb: e.tensor_scalar(out=jb[:, 0:L], in0=score[tl][:, 0:L], scalar1=smr(MID, tl, tl + 1), scalar2=None, op0=ALU.is_ge, op1=ALU.add,
                                                                         accum_out=smr(CNT, tl, tl + 1)),
                             reads=[("score", tl), ("mid", tl)], writes=[("cnt", tl), jres])
                    P.op("dve", lambda e, f=f: e.tensor_scalar(out=smr(TMP, a0, a1), in0=smr(CNT, a0, a1), scalar1=255.5, scalar2=f, op0=ALU.is_ge, op1=ALU.mult),
                         reads=R("cnt"), writes=["tmp4"])
                    P.op("dve", lambda e: e.tensor_tensor(out=smr(TMP, a0, a1), in0=smr(TMP, a0, a1), in1=smr(WD, a0, a1), op=ALU.mult), reads=["tmp4"] + R("wd"), writes=["tmp4"])
                    P.op("dve", lambda e: e.tensor_tensor(out=smr(LO, a0, a1), in0=smr(LO, a0, a1), in1=smr(TMP, a0, a1), op=ALU.add), reads=["tmp4"] + R("lo"), writes=R("lo"))

            def maskgen_chunk(c):
                for tl in range(4):
                    i = 4 * c + tl
                    L = (i + 1) * 128
                    P.op("dve", lambda e, tl=tl, L=L: e.tensor_scalar(out=mask_tm[:, 0:L], in0=score[tl][:, 0:L], scalar1=smr(LO, tl, tl + 1), scalar2=-NEGM, op0=ALU.is_lt, op1=ALU.mult),
                         reads=[("score", tl), ("lo", tl)], writes=["mask_tm"])
                    for j0 in range(0, i + 1, 8):
                        n = min(8, i + 1 - j0)
                        tb = 6 + ((j0 // 8) % 2)
                        pv = psbf(tb)[:, 0:1024].rearrange("p (k t) -> p k t", k=8)
                        for jj in range(n):
                            j = j0 + jj
                            P.op("pe", lambda e, pv=pv, jj=jj, j=j: e.transpose(out=pv[:, jj, :], in_=mask_tm[:, j * 128:(j + 1) * 128], identity=ident_b[:]),
                                 reads=["mask_tm", "ident_b"], writes=[("ps", tb)])
                        evac(maskT[:, j0:j0 + n, tl * 128:(tl + 1) * 128], pv[:, 0:n, :], [("ps", tb)], [("maskT", tl)])

            def attention_chunk(c):
                cs0 = c * 512
                jmax = 4 * c + 3

                def emit_qabs(h):
                    bp = (h % 2) * 64
                    qb_ = nb([0, 1, 2, 3])
                    P.op("pe", lambda e, qb_=qb_, bp=bp, h=h: e.matmul(PS(qb_)[:, :], lhsT=wuk[bp:bp + 64, h // 2, :], rhs=qaT[bp:bp + 64, h // 2, cs0:cs0 + 512], start=True, stop=True),
                         reads=["c_wuk", ("qaT", c)], writes=[("ps", qb_)])
                    evac(qabs[h % 2], PS(qb_)[:, :], [("ps", qb_)], [("qabs", h % 2)])

                def emit_tail(h):
                    bp = (h % 2) * 64
                    ob = 4 + (h % 2)
                    P.op("act", lambda e, ob=ob: e.activation(out=o_f32, in_=PS(ob)[:, :], func=AF.Copy), reads=[("ps", ob)], writes=["o_f32"])
                    db = nb([0, 1, 2, 3])
                    P.op("pe", lambda e, db=db, h=h: e.matmul(PS(db)[:, :], lhsT=sel_f[:, (h % 2) * 128:(h % 2) * 128 + 128], rhs=o_f32, start=True, stop=True),
                         reads=["c_sel", "o_f32"], writes=[("ps", db)])
                    P.op("act", lambda e, db=db, bp=bp: e.activation(out=rec[bp:bp + 64, :], in_=PS(db)[bp:bp + 64, :], func=AF.Ln), reads=[("ps", db)], writes=["rec"])
                    P.op("act", lambda e, bp=bp: e.activation(out=rec[bp:bp + 64, :], in_=rec[bp:bp + 64, :], func=AF.Exp, scale=-1.0), reads=["rec"], writes=["rec"])
                    P.op("dve", lambda e, bp=bp, h=h: e.tensor_tensor(out=oaT[bp:bp + 64, h // 2, cs0:cs0 + 512], in0=o_f32[bp:bp + 64, :], in1=rec[bp:bp + 64, :], op=ALU.mult),
                         reads=["o_f32", "rec"], writes=[("oaT", c)])

                emit_qabs(0)
                pending_tail = None
                for h in range(8):
                    m = h // 2
                    qs = h % 2
                    ob = 4 + (h % 2)
                    if h < 7:
                        emit_qabs(h + 1)
                    stt = {}

                    def A(j):
                        col0 = max(0, j - 4 * c) * 128
                        N = 512 - col0
                        near = j >= 4 * c - 1
                        lb = nb([0, 1, 2, 3])
                        P.op("pe", lambda e, lb=lb, j=j, col0=col0, N=N: e.matmul(PS(lb)[:, 0:N], lhsT=ckvT[:, j * 128:(j + 1) * 128], rhs=qabs[qs][:, col0:512], start=True, stop=False),
                             reads=[("ckvT", j // 4), ("qabs", qs)], writes=[("ps", lb)])
                        P.op("pe", lambda e, lb=lb, j=j, col0=col0, N=N, near=near: e.matmul(PS(lb)[:, 0:N], lhsT=ident_b[:], rhs=maskT[:, j, col0:512], start=False, stop=(not near)),
                             reads=["ident_b"] + [("maskT", t) for t in range(col0 // 128, 4)], writes=[("ps", lb)])
                        if near:
                            u0 = cs0 + col0 - 128 * j
                            P.op("pe", lambda e, lb=lb, u0=u0, N=N: e.matmul(PS(lb)[:, 0:N], lhsT=ident_b[:], rhs=B8[:, h, u0:u0 + N], start=False, stop=True),
                                 reads=["ident_b", "B8"], writes=[("ps", lb)])
                        pi = rr[0] % 4
                        rr[0] += 1
                        Pt = Pbuf[pi]
                        if near:
                            P.op("act", lambda e, lb=lb, Pt=Pt, N=N: e.activation(out=Pt[:, 0:N], in_=PS(lb)[:, 0:N], func=AF.Exp, scale=ATTN_SCALE),
                                 reads=[("ps", lb)], writes=[("Pbuf", pi)])
                        else:
                            P.op("act", lambda e, lb=lb, Pt=Pt, N=N: e.activation(out=Pt[:, 0:N], in_=PS(lb)[:, 0:N], func=AF.Exp, scale=ATTN_SCALE, bias=b31[:, h:h + 1]),
                                 reads=[("ps", lb), "c_b31"], writes=[("Pbuf", pi)])
                        stt[j] = (pi, col0, N)

                    def B(j):
                        pi, col0, N = stt[j]
                        w0 = 0 if h % 2 == 0 else 1
                        P.op("pe", lambda e, j=j, w0=w0, pi=pi, col0=col0, N=N: e.matmul(PS(ob)[:, col0:512], lhsT=Vp[:, j, m, w0:w0 + 128], rhs=Pbuf[pi][:, 0:N],
                                                                                  start=(j == 0), stop=(j == jmax)),
                             reads=["Vp", ("Pbuf", pi)], writes=[("ps", ob)])

                    for k in range(jmax + 3):
                        if k <= jmax:
                            A(k)
                        if 0 <= k - 2 <= jmax:
                            B(k - 2)
                        if k == 2 and pending_tail is not None:
                            emit_tail(pending_tail)
                            pending_tail = None
                    pending_tail = h
                emit_tail(pending_tail)

            scores_chunk(0)
            bisect_chunk(0)
            maskgen_chunk(0)
            for c in range(NCH):
                if c + 1 < NCH:
                    scores_chunk(c + 1)
                    bisect_chunk(c + 1)
                attention_chunk(c)
                if c + 1 < NCH:
                    maskgen_chunk(c + 1)
            phase_barrier()
            if stop_after == "P2":
                break

            w3 = dD(0, 24 * KB, BF16, "p (k c) -> p k c", k=8)
            ebuf = [dD(24 * KB + i * 2 * KB, 2 * KB, F32) for i in range(4)]
            spbuf = [dD(32 * KB + i * KB, 1 * KB, BF16) for i in range(6)]
            tbuf = [dD(38 * KB + i * 2 * KB, 2 * KB, F32) for i in range(3)]
            abuf = [dD(44 * KB + i * KB, 1 * KB, BF16) for i in range(4)]
            sbrr = [0, 0, 0, 0]
            P.dma("pool", "w3", lambda e: e.dma_start(out=w3.rearrange("p k c -> p (k c)"), in_=w3_d[:, :], max_dma_last_dim=8192), writes=["w3"])
            if stop_after == "P3w":
                break
            for cc in range(8):
                for c in range(NCH):
                    pb = proj_T(w3, "w3", cc, c, [0, 1, 2, 3])
                    cs = slice(c * 512, (c + 1) * 512)
                    if cc < 4:
                        evac(qbT[:, cc, cs], PS(pb)[:, :], [("ps", pb)], [("qbT", c)])
                    else:
                        evac(kbT[:, cc - 4, cs], PS(pb)[:, :], [("ps", pb)], [("kbT", c)])
            if stop_after == "P3q":
                break
            for j in range(NT):
                pb = nb([0, 1, 2, 3])
                for k in range(8):
                    P.op("pe", lambda e, pb=pb, j=j, k=k: e.matmul(PS(pb)[:, :], lhsT=hT[:, k, j * 128:(j + 1) * 128], rhs=w3[:, k, 1024:1536], start=(k == 0), stop=(k == 7)),
                         reads=["w3", ("hT", j // 4)], writes=[("ps", pb)])
                evac(vb[:, j, :], PS(pb)[:, :], [("ps", pb)], [("vb", j // 4)])
            if stop_after == "P3a":
                break
            for c in range(NCH):
                cs0 = c * 512
                jmax = 4 * c + 3
                for hp in range(4):
                    m = hp
                    units = [(j, hh) for j in range(jmax, -1, -1) for hh in range(2)]
                    stt = {}
                    prev_sp = {0: None, 1: None}

                    def S1(u):
                        j, hh = u
                        bp = hh * 64
                        col0 = max(0, j - 4 * c) * 128
                        N = 512 - col0
                        zb = nb([0, 1, 2, 5])
                        P.op("pe", lambda e, zb=zb, bp=bp, j=j, col0=col0, N=N: e.matmul(PS(zb)[:, 0:N], lhsT=kbT[bp:bp + 64, m, j * 128:(j + 1) * 128],
                                                                                    rhs=qbT[bp:bp + 64, m, cs0 + col0:cs0 + 512], start=True, stop=True),
                             reads=[("kbT", j // 4), ("qbT", c)], writes=[("ps", zb)])
                        ei = sbrr[0] % 4
                        si = sbrr[1] % 6
                        sbrr[0] += 1
                        sbrr[1] += 1
                        eb_, spt = ebuf[ei], spbuf[si]
                        P.op("act", lambda e, zb=zb, eb_=eb_, N=N: e.activation(out=eb_[:, 0:N], in_=PS(zb)[:, 0:N], func=AF.Exp, scale=ATTN_SCALE),
                             reads=[("ps", zb)], writes=[("ebuf", ei)])
                        if j >= 4 * c:
                            P.op("dve", lambda e, eb_=eb_: e.tensor_tensor(out=eb_[:, 0:128], in0=eb_[:, 0:128], in1=smask[:], op=ALU.mult),
                                 reads=[("ebuf", ei), "c_smask"], writes=[("ebuf", ei)])
                        P.op("act", lambda e, eb_=eb_, spt=spt, N=N: e.activation(out=spt[:, 0:N], in_=eb_[:, 0:N], func=AF.Ln, bias=1.0, scale=1.0),
                             reads=[("ebuf", ei)], writes=[("spbuf", si)])
                        stt[u] = dict(ei=ei, si=si, col0=col0, N=N)

                    def S2(u):
                        j, hh = u
                        d = stt[u]
                        xb = 3 + hh
                        col0, N = d["col0"], d["N"]
                        spt = spbuf[d["si"]]
                        pv = prev_sp[hh]
                        if pv is not None:
                            psi, pcol0, pN = pv
                            P.op("pe", lambda e, xb=xb, psi=psi, pcol0=pcol0, pN=pN: e.matmul(PS(xb)[:, pcol0:512], lhsT=smask[:], rhs=spbuf[psi][:, 0:pN], start=False, stop=True, skip_group_check=True),
                                 reads=["c_smask", ("spbuf", psi)], writes=[("ps", xb)])
                        P.op("pe", lambda e, xb=xb, spt=spt, col0=col0, N=N, first=(pv is None): e.matmul(PS(xb)[:, col0:512], lhsT=uinc[:], rhs=spt[:, 0:N], start=first, stop=True, skip_group_check=True),
                             reads=["c_uinc", ("spbuf", d["si"])], writes=[("ps", xb)])
                        prev_sp[hh] = (d["si"], col0, N)
                        ti = sbrr[2] % 3
                        ai = sbrr[3] % 4
                        sbrr[2] += 1
                        sbrr[3] += 1
                        d["ai"] = ai
                        P.op("act", lambda e, xb=xb, ti=ti, col0=col0, N=N: e.activation(out=tbuf[ti][:, 0:N], in_=PS(xb)[:, col0:512], func=AF.Exp, scale=-1.0),
                             reads=[("ps", xb)], writes=[("tbuf", ti)])
                        P.op("dve", lambda e, ti=ti, ai=ai, ei=d["ei"], N=N: e.tensor_tensor(out=abuf[ai][:, 0:N], in0=tbuf[ti][:, 0:N], in1=ebuf[ei][:, 0:N], op=ALU.mult),
                             reads=[("tbuf", ti), ("ebuf", d["ei"])], writes=[("abuf", ai)])

                    def S3(u):
                        j, hh = u
                        d = stt[u]
                        ob = 6 + hh
                        col0, N = d["col0"], d["N"]
                        P.op("pe", lambda e, ob=ob, j=j, ai=d["ai"], col0=col0, N=N: e.matmul(PS(ob)[:, col0:512], lhsT=vb[:, j, m * 128:(m + 1) * 128], rhs=abuf[ai][:, 0:N],
                                                                                       start=(j == jmax), stop=(j == 0), skip_group_check=True),
                             reads=[("vb", j // 4), ("abuf", d["ai"])], writes=[("ps", ob)])

                    nu = len(units)
                    for k in range(nu + 2):
                        if k < nu:
                            S1(units[k])
                        if 0 <= k - 1 < nu:
                            S2(units[k - 1])
                        if 0 <= k - 2 < nu:
                            S3(units[k - 2])
                    for hh in range(2):
                        bp = hh * 64
                        evac(obT[bp:bp + 64, m, cs0:cs0 + 512], PS(6 + hh)[bp:bp + 64, :], [("ps", 6 + hh)], [("obT", c)])
            phase_barrier()
            if dbg:
                dtmp = dD(48 * KB, 16 * KB, F32)
                for nm, src, res in (("d_oaT", oaT, "oaT"), ("d_obT", obT, "obT")):
                    for q in range(2):
                        P.op("dve", lambda e, src=src, q=q: e.tensor_copy(out=dtmp, in_=src.rearrange("p k t -> p (k t)")[:, q * 4096:(q + 1) * 4096]),
                             reads=[(res, cq) for cq in range(4)], writes=["dtmp"])
                        if b == 0:
                            P.dma("sp", "dbg", lambda e, nm=nm, q=q: e.dma_start(out=dbg_d[nm][:, q * 4096:(q + 1) * 4096], in_=dtmp), reads=["dtmp"])
                phase_barrier()
            if stop_after == "P3":
                break

            mergedT = dD(0, 32 * KB, BF16, "p (k t) -> p k t", k=8)
            wout = dD(32 * KB, 16 * KB, BF16, "p (k c) -> p k c", k=8)
            wg4 = [dD(48 * KB + i * 4 * KB, 4 * KB, BF16, "p (k c) -> p k c", k=8) for i in range(2)]
            wbr = [dD(56 * KB + i * 2 * KB, 2 * KB, BF16, "p (a k c) -> p a k c", a=2, k=4) for i in range(2)]
            sgb_ = [dD(60 * KB + i * KB, 1 * KB, BF16) for i in range(2)]
            t12 = [dD(62 * KB + i * 2 * KB, 2 * KB, F32) for i in range(2)]
            P.dma("pool", "wout", lambda e: e.dma_start(out=wout.rearrange("p k c -> p (k c)"), in_=wout_d[:, :], max_dma_last_dim=8192), writes=["wout"])
            for m in range(8):
                wsl = m % 2
                P.dma("pool", "wg4_%d" % wsl, lambda e, m=m, wsl=wsl: e.dma_start(out=wg4[wsl].rearrange("p k c -> p (k c)"), in_=wg4_d[:, m * 2048:(m + 1) * 2048], max_dma_last_dim=8192),
                      writes=[("wg4", wsl)])
                P.dma("pool", "wbr_%d" % wsl, lambda e, m=m, wsl=wsl: e.dma_start(out=wbr[wsl].rearrange("p a k c -> p (a k c)"), in_=wbr_d[:, m * 1024:(m + 1) * 1024], max_dma_last_dim=8192),
                      writes=[("wbr", wsl)])
                for c in range(NCH):
                    cs = slice(c * 512, (c + 1) * 512)
                    banks = {}
                    for gi_, nm in enumerate(("ga", "gb")):
                        pb = nb([0, 1, 2, 3, 4, 5, 6, 7])
                        banks[nm] = pb
                        for k in range(8):
                            P.op("pe", lambda e, pb=pb, k=k, gi_=gi_, wsl=wsl, cs=cs: e.matmul(PS(pb)[:, :], lhsT=wg4[wsl][:, k, gi_ * 128:(gi_ + 1) * 128], rhs=hT[:, k, cs],
                                                                                        start=(k == 0), stop=(k == 7)),
                                 reads=[("wg4", wsl), ("hT", c)], writes=[("ps", pb)])
                    for a_, (nm, oT, ores) in enumerate((("ya", oaT, "oaT"), ("yb", obT, "obT"))):
                        pb = nb([0, 1, 2, 3, 4, 5, 6, 7])
                        banks[nm] = pb
                        for k in range(4):
                            P.op("pe", lambda e, pb=pb, k=k, a_=a_, wsl=wsl, oT=oT, cs=cs: e.matmul(PS(pb)[:, :], lhsT=wbr[wsl][:, a_, k, :], rhs=oT[:, k, cs], start=(k == 0), stop=(k == 3)),
                                 reads=[("wbr", wsl), (ores, c)], writes=[("ps", pb)])
                    i0 = 0
                    sa, sb_ = sgb_[i0], sgb_[i0 + 1]
                    P.op("act", lambda e, sa=sa, pb=banks["ga"]: e.activation(out=sa, in_=PS(pb)[:, :], func=AF.Sigmoid), reads=[("ps", banks["ga"])], writes=[("sg", i0)])
                    P.op("act", lambda e, sb_=sb_, pb=banks["gb"]: e.activation(out=sb_, in_=PS(pb)[:, :], func=AF.Sigmoid), reads=[("ps", banks["gb"])], writes=[("sg", i0 + 1)])
                    P.op("dve", lambda e, sa=sa, pb=banks["ya"]: e.tensor_tensor(out=t12[0], in0=sa, in1=PS(pb)[:, :], op=ALU.mult), reads=[("sg", i0), ("ps", banks["ya"])], writes=["t1"])
                    P.op("dve", lambda e, sb_=sb_, pb=banks["yb"]: e.tensor_tensor(out=t12[1], in0=sb_, in1=PS(pb)[:, :], op=ALU.mult), reads=[("sg", i0 + 1), ("ps", banks["yb"])], writes=["t2"])
                    P.op("dve", lambda e, m=m, cs=cs: e.tensor_tensor(out=mergedT[:, m, cs], in0=t12[0], in1=t12[1], op=ALU.add), reads=["t1", "t2"], writes=[("mergedT", c)])
            phase_barrier()
            for i in range(NT):
                P.dma("sp", "x1ld%d" % (i % 4), lambda e, i=i: e.dma_start(out=x1[:, i, :], in_=x_d[b, i * 128:(i + 1) * 128, :]), writes=[("x1", i)])
                pb0 = (i % 4) * 2
                for half in range(2):
                    pb = pb0 + half
                    for k in range(8):
                        P.op("pe", lambda e, pb=pb, k=k, i=i, half=half: e.matmul(PS(pb)[:, :], lhsT=mergedT[:, k, i * 128:(i + 1) * 128], rhs=wout[:, k, half * 512:(half + 1) * 512],
                                                                               start=(k == 0), stop=(k == 7)),
                             reads=[("mergedT", i // 4), "wout"], writes=[("ps", pb)])
                    P.op("dve", lambda e, pb=pb, i=i, half=half: e.tensor_tensor(out=x1[:, i, half * 512:(half + 1) * 512], in0=x1[:, i, half * 512:(half + 1) * 512], in1=PS(pb)[:, :], op=ALU.add),
                         reads=[("ps", pb), ("x1", i)], writes=[("x1", i)])
            phase_barrier()
            if dbg and b == 0:
                P.dma("sp", "dbg", lambda e: e.dma_start(out=dbg_d["d_x1"][:, :], in_=x1.rearrange("p i d -> p (i d)")), reads=[("x1", i) for i in range(NT)])
                phase_barrier()
            if stop_after == "P4":
                break

            h2T = hT
            xnf = dD(0, 4 * KB, F32)
            hTf = dD(4 * KB, 4 * KB, F32, "p (k t) -> p k t", k=8)
            comb = dD(8 * KB, 2 * KB, F32, "p (i e) -> p i e", i=16)
            elm = dD(10 * KB, 256, F32)
            m8 = dD(10 * KB + 256, 64, F32)
            eq = dD(10 * KB + 320, 256, F32)
            sgm = [dD(12 * KB + i * KB, 1 * KB, BF16) for i in range(4)]
            hid = [dD(16 * KB + i * 2 * KB, 2 * KB, BF16, "p (f t) -> p f t", f=2) for i in range(2)]
            RG, RS, RW = 60, 61, 62

            def ffn_side(i, xt, xres, rsc, xn, xnres):
                P.op("dve", lambda e, xt=xt, rsc=rsc: e.tensor_scalar(out=xnf, in0=xt, scalar1=rsc, scalar2=None, op0=ALU.mult), reads=[xres, ("rs", i)], writes=["xnf"])
                for half in range(2):
                    pb = 4 + half
                    for kk in range(4):
                        k = half * 4 + kk
                        P.op("pe", lambda e, pb=pb, kk=kk, k=k: e.transpose(out=PS(pb)[:, kk * 128:(kk + 1) * 128], in_=xnf[:, k * 128:(k + 1) * 128], identity=ident_f[:]),
                             reads=["xnf", "c_ident"], writes=[("ps", pb)])
                    pv = PS(pb)[:, :].rearrange("p (k t) -> p k t", k=4)
                    gf = gpk[:, 8 + half * 4:8 + half * 4 + 4]
                    P.op("dve", lambda e, pv=pv, half=half, i=i: e.tensor_tensor(out=h2T[:, half * 4:half * 4 + 4, i * 128:(i + 1) * 128], in0=pv, in1=gB[1][:, half * 4:half * 4 + 4, :], op=ALU.mult),
                         reads=[("ps", pb), "gB"], writes=[("hT", i // 4)])
                    P.op("dve", lambda e, pv=pv, half=half, gf=gf: e.tensor_tensor(out=hTf[:, half * 4:half * 4 + 4, :], in0=pv, in1=gf.unsqueeze(2).broadcast_to([128, 4, 128]), op=ALU.mult),
                         reads=[("ps", pb), "c_g1"], writes=["hTf"])
                for k in range(8):
                    P.op("pe", lambda e, k=k: e.matmul(PS(6)[:, 0:36], lhsT=hTf[:, k, :], rhs=wr_f[:, k, :], start=(k == 0), stop=(k == 7)),
                         reads=["hTf", "c_wr"], writes=[("ps", 6)])
                sm = lambda col: small[:, col:col + 1]
                P.op("dve", lambda e: e.tensor_tensor(out=elm[:, 0:36], in0=PS(6)[:, 0:36], in1=brb[:], op=ALU.add), reads=[("ps", 6), "c_br"], writes=["elm"])
                P.op("dve", lambda e: e.tensor_reduce(out=sm(RG), in_=elm[:, 0:4], axis=AX.X, op=ALU.max), reads=["elm"], writes=["rg"])
                P.op("dve", lambda e: e.tensor_scalar(out=eq[:, 0:4], in0=elm[:, 0:4], scalar1=sm(RG), scalar2=None, op0=ALU.is_ge), reads=["elm", "rg"], writes=["eq"])
                P.op("dve", lambda e: e.tensor_scalar(out=eq[:, 4:8], in0=eq[:, 0:4], scalar1=-1.0, scalar2=-NEG, op0=ALU.add, op1=ALU.mult), reads=["eq"], writes=["eq"])
                P.op("dve", lambda e: e.tensor_tensor(out=elm[:, 4:36].rearrange("p (g x) -> p g x", g=4), in0=elm[:, 4:36].rearrange("p (g x) -> p g x", g=4),
                                                      in1=eq[:, 4:8].unsqueeze(2).broadcast_to([128, 4, 8]), op=ALU.add), reads=["elm", "eq"], writes=["elm"])
                P.op("dve", lambda e: e.tensor_scalar(out=sm(RW), in0=sm(RG), scalar1=-1.0, scalar2=None, op0=ALU.mult), reads=["rg"], writes=["rw"])
                P.op("act", lambda e: e.activation(out=eq[:, 8:12], in_=elm[:, 0:4], func=AF.Exp, bias=sm(RW), scale=1.0, accum_out=sm(RS)), reads=["elm", "rw"], writes=["eq", "rs_"])
                P.op("dve", lambda e: e.reciprocal(out=sm(RS), in_=sm(RS)), reads=["rs_"], writes=["rs_"])
                P.op("dve", lambda e: e.max(out=m8[:, 0:8], in_=elm[:, 4:36]), reads=["elm"], writes=["m8"])
                P.op("dve", lambda e: e.tensor_tensor(out=m8[:, 8:9], in0=m8[:, 1:2], in1=m8[:, 0:1], op=ALU.subtract), reads=["m8"], writes=["m8"])
                P.op("act", lambda e: e.activation(out=m8[:, 8:9], in_=m8[:, 8:9], func=AF.Exp), reads=["m8"], writes=["m8"])
                P.op("dve", lambda e: e.tensor_scalar(out=m8[:, 8:9], in0=m8[:, 8:9], scalar1=1.0, scalar2=None, op0=ALU.add), reads=["m8"], writes=["m8"])
                P.op("dve", lambda e: e.reciprocal(out=m8[:, 9:10], in_=m8[:, 8:9]), reads=["m8"], writes=["m8"])
                P.op("dve", lambda e: e.tensor_scalar(out=m8[:, 10:11], in0=m8[:, 9:10], scalar1=-1.0, scalar2=1.0, op0=ALU.mult, op1=ALU.add), reads=["m8"], writes=["m8"])
                P.op("dve", lambda e: e.tensor_tensor(out=m8[:, 9:11], in0=m8[:, 9:11], in1=sm(RS).broadcast_to([128, 2]), op=ALU.mult), reads=["m8", "rs_"], writes=["m8"])
                P.op("dve", lambda e: e.tensor_scalar(out=eq[:, 0:32], in0=elm[:, 4:36], scalar1=m8[:, 0:1], scalar2=m8[:, 9:10], op0=ALU.is_equal, op1=ALU.mult), reads=["elm", "m8"], writes=["eq"])
                P.op("dve", lambda e: e.tensor_scalar(out=eq[:, 32:64], in0=elm[:, 4:36], scalar1=m8[:, 1:2], scalar2=m8[:, 10:11], op0=ALU.is_equal, op1=ALU.mult), reads=["elm", "m8"], writes=["eq"])
                P.op("dve", lambda e, i=i: e.tensor_tensor(out=comb[:, i, :], in0=eq[:, 0:32], in1=eq[:, 32:64], op=ALU.add), reads=["eq"], writes=["comb"])

            build_gB(1)
            norm_transpose(b, "x1", 1, h2T, "hT", None, [dD(20 * KB, 2 * KB, BF16), dD(22 * KB, 2 * KB, BF16)], None, f32_side=ffn_side)
            phase_barrier()
            def wviews(ex):
                wm = wmoe[ex % 2]
                return (wm[:, 0:2048].rearrange("p (k c) -> p k c", k=8), wm[:, 2048:4096].rearrange("p (k c) -> p k c", k=8),
                        wm[:, 4096:6144].rearrange("p (f c) -> p f c", f=2))

            def moe_dma(ex):
                wsl = ex % 2
                P.dma("pool", "wmoe%d" % wsl, lambda e, wsl=wsl, ex=ex: e.dma_start(out=wmoe[wsl], in_=wmoe_d[ex, :, :], max_dma_last_dim=8192), writes=[("wmoe", wsl)])

            def GU(n, f):
                ex, c = n // NCH, n % NCH
                wsl = ex % 2
                wgv, wuv_, wdv = wviews(ex)
                cs = slice(c * 512, (c + 1) * 512)
                hs = n % 2
                gb_, ub_ = f * 2, f * 2 + 1
                for k in range(8):
                    P.op("pe", lambda e, k=k: e.matmul(PS(gb_)[:, :], lhsT=wgv[:, k, f * 128:(f + 1) * 128], rhs=h2T[:, k, cs], start=(k == 0), stop=(k == 7)),
                         reads=[("wmoe", wsl), ("hT", c)], writes=[("ps", gb_)])
                for k in range(8):
                    P.op("pe", lambda e, k=k: e.matmul(PS(ub_)[:, :], lhsT=wuv_[:, k, f * 128:(f + 1) * 128], rhs=h2T[:, k, cs], start=(k == 0), stop=(k == 7)),
                         reads=[("wmoe", wsl), ("hT", c)], writes=[("ps", ub_)])
                sgi = hs * 2 + f
                P.op("act", lambda e: e.activation(out=sgm[sgi], in_=PS(gb_)[:, :], func=AF.Silu), reads=[("ps", gb_)], writes=[("sgm", sgi)])
                P.op("dve", lambda e: e.tensor_tensor(out=hid[hs][:, f, :], in0=sgm[sgi], in1=PS(ub_)[:, :], op=ALU.mult),
                     reads=[("sgm", sgi), ("ps", ub_)], writes=[("hid", hs, f)])

            def DOWN(n, tiles):
                ex, c = n // NCH, n % NCH
                wsl = ex % 2
                wgv, wuv_, wdv = wviews(ex)
                hs = n % 2
                for tl in tiles:
                    i = c * 4 + tl
                    for half in range(2):
                        pb = 4 + (rr[0] % 4)
                        rr[0] += 1
                        for f in range(2):
                            P.op("pe", lambda e, f=f: e.matmul(PS(pb)[:, :], lhsT=hid[hs][:, f, tl * 128:(tl + 1) * 128], rhs=wdv[:, f, half * 512:(half + 1) * 512],
                                                             start=(f == 0), stop=(f == 1)),
                                 reads=[("hid", hs, f), ("wmoe", wsl)], writes=[("ps", pb)])
                        P.op("dve", lambda e: e.scalar_tensor_tensor(out=x1[:, i, half * 512:(half + 1) * 512], in0=PS(pb)[:, :], scalar=comb[:, i, ex:ex + 1],
                                                                      in1=x1[:, i, half * 512:(half + 1) * 512], op0=ALU.mult, op1=ALU.add),
                             reads=[("ps", pb), "comb", ("x1", i)], writes=[("x1", i)])

            NSTEP = N_EXP * NCH
            moe_dma(0)
            GU(0, 0)
            GU(0, 1)
            for n in range(NSTEP):
                if n % NCH == 0 and n // NCH + 1 < N_EXP:
                    moe_dma(n // NCH + 1)
                if n + 1 < NSTEP:
                    GU(n + 1, 0)
                DOWN(n, (0, 1))
                if n + 1 < NSTEP:
                    GU(n + 1, 1)
                DOWN(n, (2, 3))
            phase_barrier()
            if stop_after == "P5":
                break

            h3T = hT
            P.dma("pool", "wpg", lambda e: e.dma_start(out=wpg.rearrange("p k c -> p (k c)"), in_=wpg_d[:, :], max_dma_last_dim=8192), writes=["wpg"])
            P.dma("pool", "wpl", lambda e: e.dma_start(out=wpl.rearrange("p k c -> p (k c)"), in_=wpl_d[:, :], max_dma_last_dim=8192), writes=["wpl"])
            build_gB(2)
            norm_transpose(b, "x1", 2, h3T, "hT", None, [dD(0, 2 * KB, BF16), dD(2 * KB, 2 * KB, BF16)], [6, 7])
            pt_b = [dD(4 * KB + i * KB, 1 * KB, F32) for i in range(2)]
            pT_b = [dD(6 * KB + i * 512, 512, BF16, "p (k t) -> p k t", k=2) for i in range(2)]
            sig = [dD(8 * KB + i * 2 * KB, 2 * KB, BF16) for i in range(2)]
            tmpf = [dD(12 * KB + i * 4 * KB, 4 * KB, F32) for i in range(2)]
            outb = [dD(20 * KB + i * 4 * KB, 4 * KB, F32) for i in range(2)]
            junkf = dD(28 * KB, 2 * KB, BF16)
            gfin = dD(30 * KB, 4 * KB, F32)
            P.dma("sp", "c_gfin", lambda e: e.dma_start(out=gfin, in_=gfin_d[:, :]), writes=["c_gfin"])
            for i in range(NT):
                sl = i % 2
                P.dma("sp", "pt%d" % sl, lambda e, i=i, sl=sl: e.dma_start(out=pt_b[sl], in_=p_d[b, i * 128:(i + 1) * 128, :]), writes=[("pt", sl)])
                for k in range(2):
                    P.op("pe", lambda e, k=k, sl=sl: e.transpose(out=PS(5)[:, k * 128:(k + 1) * 128], in_=pt_b[sl][:, k * 128:(k + 1) * 128], identity=ident_f[:]),
                         reads=[("pt", sl), "c_ident"], writes=[("ps", 5)])
                P.op("act", lambda e, sl=sl: e.activation(out=pT_b[sl].rearrange("p k t -> p (k t)"), in_=PS(5)[:, 0:256], func=AF.Copy), reads=[("ps", 5)], writes=[("pT", sl)])
                for half in range(2):
                    gbk = half
                    pbk = 2 + half
                    for k in range(8):
                        P.op("pe", lambda e, gbk=gbk, k=k, i=i, half=half: e.matmul(PS(gbk)[:, :], lhsT=h3T[:, k, i * 128:(i + 1) * 128], rhs=wpg[:, k, half * 512:(half + 1) * 512], start=(k == 0), stop=(k == 7)),
                             reads=[("hT", i // 4), "wpg"], writes=[("ps", gbk)])
                    for k in range(2):
                        P.op("pe", lambda e, pbk=pbk, k=k, sl=sl, half=half: e.matmul(PS(pbk)[:, :], lhsT=pT_b[sl][:, k, :], rhs=wpl[:, k, half * 512:(half + 1) * 512], start=(k == 0), stop=(k == 1)),
                             reads=[("pT", sl), "wpl"], writes=[("ps", pbk)])
                    hsl = slice(half * 512, (half + 1) * 512)
                    P.op("act", lambda e, gbk=gbk, sl=sl, hsl=hsl: e.activation(out=sig[sl][:, hsl], in_=PS(gbk)[:, :], func=AF.Sigmoid), reads=[("ps", gbk)], writes=[("sig", sl, half)])
                    P.op("dve", lambda e, pbk=pbk, sl=sl, hsl=hsl: e.tensor_tensor(out=tmpf[sl][:, hsl], in0=sig[sl][:, hsl], in1=PS(pbk)[:, :], op=ALU.mult),
                         reads=[("sig", sl, half), ("ps", pbk)], writes=[("tmpf", sl, half)])
                    P.op("dve", lambda e, sl=sl, hsl=hsl, i=i: e.tensor_tensor(out=tmpf[sl][:, hsl], in0=tmpf[sl][:, hsl], in1=x1[:, i, hsl], op=ALU.add),
                         reads=[("tmpf", sl, half), ("x1", i)], writes=[("tmpf", sl, half)])
                ssc = small[:, 64 + i:65 + i]
                P.op("act", lambda e, sl=sl, ssc=ssc: e.activation(out=junkf, in_=tmpf[sl], func=AF.Square, accum_out=ssc), reads=[("tmpf", sl, 0), ("tmpf", sl, 1)], writes=["junkf", ("fs", i)])
                P.op("dve", lambda e, ssc=ssc: e.tensor_scalar(out=ssc, in0=ssc, scalar1=1.0 / D, scalar2=EPS, op0=ALU.mult, op1=ALU.add), reads=[("fs", i)], writes=[("fs", i)])
                P.op("act", lambda e, ssc=ssc: e.activation(out=ssc, in_=ssc, func=AF.Sqrt), reads=[("fs", i)], writes=[("fs", i)])
                P.op("dve", lambda e, ssc=ssc: e.reciprocal(out=ssc, in_=ssc), reads=[("fs", i)], writes=[("fs", i)])
                P.op("dve", lambda e, sl=sl, ssc=ssc: e.scalar_tensor_tensor(out=outb[sl], in0=tmpf[sl], scalar=ssc, in1=gfin, op0=ALU.mult, op1=ALU.mult),
                     reads=[("tmpf", sl, 0), ("tmpf", sl, 1), ("fs", i), "c_gfin"], writes=[("outb", sl)])
                tok = P.dma("sp", "out%d" % sl, lambda e, sl=sl, i=i: e.dma_start(out=out_d[b, i * 128:(i + 1) * 128, :], in_=outb[sl]), reads=[("outb", sl)])
                last_out_tokens.append(tok)
            phase_barrier()

        finals = {}
        for tok in last_out_tokens:
            finals[tok[1]] = max(finals.get(tok[1], 0), tok[2])
        for k in P.dma_keys:
            if k == "dbg":
                finals[k] = P.dma_count[k]
        P.emit(final_wait_tokens=[("d", k, n) for k, n in finals.items()])
    return nc


def _pk(w, ncols=None):
    K = w.shape[0] // 128
    return np.ascontiguousarray(w.reshape(K, 128, -1).transpose(1, 0, 2).reshape(128, -1))


def _t5_bucket_np(d):
    d = np.maximum(d, 0)
    d_f = np.maximum(d, 1).astype(np.float32)
    large = 16 + (np.log(d_f / np.float32(16)) / np.float32(math.log(128 / 16)) * np.float32(16)).astype(np.int32)
    large = np.minimum(large, 31)
    return np.where(d < 16, d, large)


def prep_weights(inp):
    f = lambda a: np.ascontiguousarray(a, dtype=np.float32)
    w_in = inp["w_in"][0]
    o = {}
    c0 = 0
    sec = {}
    for nm, wdt in (("q_a", 512), ("c_kv", 128), ("q_idx", 512), ("k_idx", 64), ("w_idx", 8), ("qkv_b", 1536), ("gate_a", 1024), ("gate_b", 1024)):
        sec[nm] = w_in[:, c0:c0 + wdt]
        c0 += wdt
    w1 = np.concatenate([sec["q_a"], sec["q_idx"], sec["c_kv"], sec["k_idx"], sec["k_idx"]], axis=1)
    o["w1"] = _pk(w1)
    o["widx"] = _pk(sec["w_idx"])
    o["w3"] = _pk(sec["qkv_b"])
    ga = sec["gate_a"].reshape(1024, 8, 128)
    gb = sec["gate_b"].reshape(1024, 8, 128)
    g4 = np.concatenate([ga, gb], axis=2)
    g4 = g4.reshape(8, 128, 8, 256).transpose(1, 2, 0, 3)
    o["wg4"] = f(g4.reshape(128, -1))
    wa = inp["w_branch_a"][0].reshape(4, 128, 8, 128)
    wb = inp["w_branch_b"][0].reshape(4, 128, 8, 128)
    wbr = np.stack([wa, wb], axis=0).transpose(2, 3, 0, 1, 4)
    o["wbr"] = f(wbr.reshape(128, -1))
    o["wout"] = _pk(inp["w_out"][0])
    wuk = inp["w_uk"][0]
    wukT = wuk.transpose(0, 2, 1).reshape(4, 2, 64, 128).transpose(1, 2, 0, 3)
    o["wuk"] = f(wukT.reshape(128, 512))
    o["wuv"] = f(inp["w_uv"][0].transpose(1, 0, 2).reshape(128, 512))
    wg = inp["w_gate"][0].reshape(N_EXP, 8, 128, 256).transpose(0, 2, 1, 3).reshape(N_EXP, 128, 2048)
    wu = inp["w_up"][0].reshape(N_EXP, 8, 128, 256).transpose(0, 2, 1, 3).reshape(N_EXP, 128, 2048)
    wd = inp["w_down"][0].reshape(N_EXP, 2, 128, 1024).transpose(0, 2, 1, 3).reshape(N_EXP, 128, 2048)
    o["wmoe"] = f(np.concatenate([wg, wu, wd], axis=2))
    wr = np.concatenate([inp["w_r1"][0], inp["w_r2"][0].transpose(1, 0, 2).reshape(1024, 32)], axis=1)
    o["wr"] = _pk(wr)
    br = np.concatenate([inp["b_r1"][0], inp["b_r2"][0].reshape(32)])
    o["br"] = f(np.broadcast_to(br[None, :], (128, 36)))
    o["wpg"] = _pk(inp["w_ple_gate"][0])
    o["wpl"] = _pk(inp["w_ple"][0])
    o["g_attn"] = f(inp["attn_norm"][0].reshape(8, 128).T)
    o["g_ffn"] = f(inp["ffn_norm"][0].reshape(8, 128).T)
    o["g_ple"] = f(inp["ple_norm"][0].reshape(8, 128).T)
    o["g_fin"] = f(np.broadcast_to(inp["final_norm"][None, :], (128, 1024)))
    o["g_kv"] = f(inp["kv_norm"][0].reshape(128, 1))
    rb = inp["rel_bias"]
    s_l = np.arange(128)[:, None]
    u = np.arange(640)[None, :]
    bidx = _t5_bucket_np(u - s_l)
    o["btoep"] = f(rb[bidx].transpose(0, 2, 1).reshape(128, 8 * 640))
    o["b31"] = f(np.broadcast_to(rb[31][None, :], (128, 8)))
    o["ident"] = np.eye(128, dtype=np.float32)
    tt = np.arange(128)[:, None]
    ss = np.arange(128)[None, :]
    o["cneg"] = np.where(ss <= tt, 0.0, NEG).astype(np.float32)
    o["smask"] = (tt < ss).astype(np.float32)
    o["uinc"] = (tt >= ss).astype(np.float32)
    sel = np.zeros((128, 256), np.float32)
    sel[64, 0:128] = 1.0
    sel[63, 128:256] = 1.0
    o["sel"] = sel
    return o


_NC_CACHE = {}


def kernel(**inputs):
    inp = {k: np.asarray(v) for k, v in inputs.items()}
    n = 8
    NB = 2
    wts = prep_weights(inp)
    x = np.ascontiguousarray(inp["x"], dtype=np.float32)
    p = np.ascontiguousarray(inp["p"][0], dtype=np.float32)
    if "nc" not in _NC_CACHE:
        _NC_CACHE["nc"] = build_nc(NB=NB)
    nc = _NC_CACHE["nc"]
    in_maps = []
    for c in range(n):
        m = dict(wts)
        m["x"] = x[c * NB:(c + 1) * NB]
        m["p"] = p[c * NB:(c + 1) * NB]
        in_maps.append(m)
    res = run_bass_kernel_spmd(nc, in_maps, core_ids=list(range(n)))
    out = np.concatenate([r["out"] for r in res.results], axis=0)
    return out.astype(np.float32)
```

```python
import math
import types
import contextlib
import numpy as np
import concourse.bass as bass
import concourse.mybir as mybir
from concourse.bass_utils import run_bass_kernel_spmd

F32 = mybir.dt.float32
BF16 = mybir.dt.bfloat16
AF = mybir.ActivationFunctionType
ALU = mybir.AluOpType
AX = mybir.AxisListType

S = 2048
D = 1024
NT = S // 128
NCH = S // 512
ATTN_SCALE = 64 ** -0.5
IDX_SCALE = (8 ** -0.5) * (64 ** -0.5)
EPS = 1e-6
NEG = -1.0e30
N_BISECT = 14
N_EXP = 32
import os as _os
SBE = _os.environ.get("SBE", "pool")
SBLN = _os.environ.get("SBLN", "1") == "1"


class Prog:
    ENGS = ("pe", "act", "dve", "pool", "sp")

    def __init__(self, nc, same_eng_sync=True):
        self.nc = nc
        self.ops = {e: [] for e in self.ENGS}
        self.last_w = {}
        self.last_r = {}
        self.clock = {e: {} for e in self.ENGS}
        self.opclock = {}
        self.dma_count = {}
        self.dma_keys = []
        self.signaling = set()
        self.same_eng_sync = same_eng_sync
        self._bar = 0

    def _add(self, eng, fn, reads, writes, dma_key=None, n_dma=1):
        idx = len(self.ops[eng]) + 1
        deps = {}

        def need(tok):
            kind, who, n = tok
            if kind == "e" and who == eng:
                if eng in ("pe", "sp") or not self.same_eng_sync:
                    return
            k = (kind, who)
            if deps.get(k, 0) < n:
                deps[k] = n

        for r in reads:
            for tok in self.last_w.get(r, {}).values():
                need(tok)
        for w in writes:
            for tok in self.last_w.get(w, {}).values():
                need(tok)
            for tok in self.last_r.get(w, {}).values():
                need(tok)
        if dma_key is not None:
            if dma_key not in self.dma_count:
                self.dma_count[dma_key] = 0
                self.dma_keys.append(dma_key)
            prev = self.dma_count[dma_key]
            if prev > 0:
                need(("d", dma_key, prev))
            self.dma_count[dma_key] = prev + n_dma
            mytok = ("d", dma_key, prev + n_dma)
        else:
            mytok = ("e", eng, idx)
        clk = self.clock[eng]
        final = []
        for k, n in deps.items():
            if clk.get(k, 0) >= n:
                continue
            final.append((k[0], k[1], n))
        for kind, who, n in final:
            oc = self.opclock.get((kind, who, n))
            if oc:
                for k2, n2 in oc.items():
                    if clk.get(k2, 0) < n2:
                        clk[k2] = n2
            if clk.get((kind, who), 0) < n:
                clk[(kind, who)] = n
            if kind == "e":
                self.signaling.add((who, n))
        snap = dict(clk)
        if mytok[0] == "e":
            snap[("e", eng)] = idx
        self.opclock[mytok] = snap
        self.ops[eng].append((fn, final, dma_key, mytok))
        if fn is not None:
            for r in reads:
                self.last_r.setdefault(r, {})[(mytok[0], mytok[1])] = mytok
        for w in writes:
            self.last_w[w] = {(mytok[0], mytok[1]): mytok}
            self.last_r[w] = {}
        return mytok

    @staticmethod
    def _freeze(fn):
        if fn is None or getattr(fn, "__closure__", None) is None:
            return fn
        cells = []
        for c in fn.__closure__:
            try:
                cells.append(types.CellType(c.cell_contents))
            except ValueError:
                cells.append(c)
        return types.FunctionType(fn.__code__, fn.__globals__, fn.__name__, fn.__defaults__, tuple(cells))

    def op(self, eng, fn, reads=(), writes=()):
        return self._add(eng, self._freeze(fn), tuple(reads), tuple(writes))

    def dma(self, eng, key, fns, reads=(), writes=()):
        if not isinstance(fns, (list, tuple)):
            fns = [fns]
        return self._add(eng, [self._freeze(f) for f in fns], tuple(reads), tuple(writes), dma_key=key, n_dma=len(fns))

    def barrier(self, tiny_fn):
        self._bar += 1
        res = ("__barrier__", self._bar)
        allres = list(set(list(self.last_w.keys()) + list(self.last_r.keys())))
        self._add("dve", self._freeze(tiny_fn), tuple(), tuple(allres) + (res,))
        for e in ("pe", "act", "pool", "sp"):
            self._add(e, None, (res,), tuple())

    def emit(self, final_wait_tokens=()):
        nc = self.nc
        sigval = {}
        for e in self.ENGS:
            s = 0
            for i in range(1, len(self.ops[e]) + 1):
                if (e, i) in self.signaling:
                    s += 1
                    sigval[(e, i)] = s
        engobj = {"pe": nc.tensor, "act": nc.scalar, "dve": nc.vector, "pool": nc.gpsimd, "sp": nc.sync}
        with contextlib.ExitStack() as st:
            esem = {e: st.enter_context(nc.semaphore("sem_" + e)) for e in self.ENGS}
            dsem = {k: st.enter_context(nc.semaphore("dsem_%d" % i)) for i, k in enumerate(self.dma_keys)}
            block = st.enter_context(nc.Block())

            def run(e):
                eng = engobj[e]
                for i, (fn, deps, dma_key, mytok) in enumerate(self.ops[e], start=1):
                    for kind, who, n in deps:
                        if kind == "e":
                            eng.wait_ge(esem[who], sigval[(who, n)])
                        else:
                            eng.wait_ge(dsem[who], 16 * n)
                    if fn is None:
                        assert (e, i) not in self.signaling
                        continue
                    if dma_key is not None:
                        for f in fn:
                            f(eng).then_inc(dsem[dma_key], 16)
                    else:
                        ins = fn(eng)
                        if (e, i) in self.signaling:
                            ins.then_inc(esem[e], 1)
                if e == "sp":
                    for k in self.dma_keys:
                        eng.wait_ge(dsem[k], 16 * self.dma_count[k])

            block.tensor(lambda eng: run("pe"))
            block.scalar(lambda eng: run("act"))
            block.vector(lambda eng: run("dve"))
            block.gpsimd(lambda eng: run("pool"))
            block.sync(lambda eng: run("sp"))


def build_nc(NB=2, stop_after=None, dbg=False):
    nc = bass.Bass("TRN2", target_bir_lowering=False)

    def din(name, shape, dt=F32):
        return nc.dram_tensor(name, list(shape), dt, kind="ExternalInput").ap()

    x_d = din("x", [NB, S, D])
    p_d = din("p", [NB, S, 256])
    w1_d = din("w1", [128, 8 * 1280])
    widx_d = din("widx", [128, 8 * 8])
    w3_d = din("w3", [128, 8 * 1536])
    wg4_d = din("wg4", [128, 8 * 8 * 256])
    wbr_d = din("wbr", [128, 8 * 1024])
    wout_d = din("wout", [128, 8 * 1024])
    wuk_d = din("wuk", [128, 512])
    wuv_d = din("wuv", [128, 512])
    wmoe_d = din("wmoe", [N_EXP, 128, 6144])
    wr_d = din("wr", [128, 8 * 36])
    br_d = din("br", [128, 36])
    wpg_d = din("wpg", [128, 8 * 1024])
    wpl_d = din("wpl", [128, 2 * 1024])
    gat_d = din("g_attn", [128, 8])
    gff_d = din("g_ffn", [128, 8])
    gpl_d = din("g_ple", [128, 8])
    gfin_d = din("g_fin", [128, 1024])
    gkv_d = din("g_kv", [128, 1])
    btoep_d = din("btoep", [128, 8 * 640])
    b31_d = din("b31", [128, 8])
    ident_d = din("ident", [128, 128])
    cneg_d = din("cneg", [128, 128])
    smask_d = din("smask", [128, 128])
    uinc_d = din("uinc", [128, 128])
    sel_d = din("sel", [128, 256])
    out_d = nc.dram_tensor("out", [NB, S, D], F32, kind="ExternalOutput").ap()
    dbg_d = {}
    if dbg:
        for nm, shp in (("d_oaT", [128, 4 * S]), ("d_obT", [128, 4 * S]), ("d_x1", [128, NT * D])):
            dbg_d[nm] = nc.dram_tensor(nm, shp, F32, kind="ExternalOutput").ap()

    st = contextlib.ExitStack()
    with st:
        def sb(name, shape, dt):
            return st.enter_context(nc.sbuf_tensor("s_" + name, list(shape), dt))

        arA = sb("arA", [128, 16384], BF16)
        arB = sb("arB", [128, 32768], BF16)
        arC = sb("arC", [128, 16384], BF16)
        arD = sb("arD", [128, 33792], BF16)
        ident_f = sb("ident_f", [128, 128], F32)
        ident_b = sb("ident_b", [128, 128], BF16)
        cneg = sb("cneg", [128, 128], F32)
        smask = sb("smask", [128, 128], BF16)
        uinc = sb("uinc", [128, 128], BF16)
        ones_b = sb("ones_b", [128, 128], BF16)
        sel_f = sb("sel_f", [128, 256], F32)
        gB1 = sb("gB", [128, 8, 128], BF16)
        gB = [gB1, gB1, gB1]
        gpk = sb("gpk", [128, 24], F32)
        gkv = sb("gkv", [128, 1], F32)
        b31 = sb("b31", [128, 8], F32)
        brb = sb("brb", [128, 36], F32)
        wr_f = sb("wr_f", [128, 8, 36], F32)
        wuk = sb("wuk", [128, 4, 128], BF16)
        wuv = sb("wuv", [128, 512], BF16)
        widx_w = sb("widx_w", [128, 8, 8], BF16)
        small = sb("small", [128, 256], F32)
        tiny = sb("tiny", [128, 2], F32)

        psb = [st.enter_context(nc.psum_tensor("ps%d" % i, [128, 512], F32)) for i in range(8)]

        P = Prog(nc)

        def carve(ar, off_bytes, nbytes, dt, pattern=None, **kw):
            e0 = off_bytes // 2
            ap = ar[:, e0:e0 + nbytes // 2]
            if dt == F32:
                ap = ap.bitcast(F32)
            if pattern:
                ap = ap.rearrange(pattern, **kw)
            return ap

        KB = 1024
        hT = carve(arA, 0, 32 * KB, BF16, "p (k t) -> p k t", k=8)
        qaT = carve(arB, 0, 16 * KB, BF16, "p (k t) -> p k t", k=4)
        qiT = carve(arB, 16 * KB, 16 * KB, BF16, "p (k t) -> p k t", k=4)
        ckvT = carve(arB, 32 * KB, 4 * KB, BF16)
        kiT = carve(arB, 36 * KB, 4 * KB, BF16)
        Vp = carve(arB, 40 * KB, 16 * 4 * 130 * 2, BF16, "p (j m c) -> p j m c", j=16, m=4)
        widx_tm = carve(arB, 40 * KB + 16640, 512, F32, "p (i h) -> p i h", i=16)
        qbT = carve(arB, 0, 16 * KB, BF16, "p (k t) -> p k t", k=4)
        kbT = carve(arB, 16 * KB, 16 * KB, BF16, "p (k t) -> p k t", k=4)
        vb = carve(arB, 32 * KB, 16 * KB, BF16, "p (j c) -> p j c", j=16)
        qbTn = carve(arB, 48 * KB, 16 * KB, BF16, "p (k t) -> p k t", k=4)
        x1 = carve(arB, 0, 64 * KB, F32, "p (i d) -> p i d", i=16)
        oaT = carve(arC, 0, 16 * KB, BF16, "p (k t) -> p k t", k=4)
        obT = carve(arC, 16 * KB, 16 * KB, BF16, "p (k t) -> p k t", k=4)
        wmoe = [carve(arC, i * 12 * KB, 12 * KB, BF16) for i in range(2)]
        wpg = carve(arC, 0, 16 * KB, BF16, "p (k c) -> p k c", k=8)
        wpl = carve(arC, 16 * KB, 4 * KB, BF16, "p (k c) -> p k c", k=2)
        def dD(off, nbytes, dt, pattern=None, **kw):
            assert off + nbytes <= 66 * KB, (off, nbytes)
            return carve(arD, off, nbytes, dt, pattern, **kw)

        PS = lambda i: psb[i]

        def psbf(i):
            return psb[i][:].bitcast(BF16)

        def ld(key, dst, src, eng="sp", res=None, **kw):
            P.dma(eng, key, lambda e: e.dma_start(out=dst, in_=src, **kw), writes=[res or key])

        ld("c_ident", ident_f[:], ident_d[:, :])
        ld("c_cneg", cneg[:], cneg_d[:, :])
        ld("c_sel", sel_f[:], sel_d[:, :])
        ld("c_gkv", gkv[:], gkv_d[:, :])
        ld("c_b31", b31[:], b31_d[:, :])
        ld("c_br", brb[:], br_d[:, :])
        ld("c_wr", wr_f[:].rearrange("p k c -> p (k c)"), wr_d[:, :])
        ld("c_g0", gpk[:, 0:8], gat_d[:, :])
        ld("c_g1", gpk[:, 8:16], gff_d[:, :])
        ld("c_g2", gpk[:, 16:24], gpl_d[:, :])
        ld("c_smask", smask[:], smask_d[:, :], eng="pool")
        ld("c_uinc", uinc[:], uinc_d[:, :], eng="pool")
        ld("c_wuk", wuk[:].rearrange("p k c -> p (k c)"), wuk_d[:, :], eng="pool")
        ld("c_wuv", wuv[:], wuv_d[:, :], eng="pool")
        ld("c_widx", widx_w[:].rearrange("p k c -> p (k c)"), widx_d[:, :], eng="pool")
        P.op("dve", lambda e: e.tensor_copy(out=ident_b[:], in_=ident_f[:]), reads=["c_ident"], writes=["ident_b"])
        P.op("dve", lambda e: e.memset(ones_b[:], 1.0), writes=["ones_b"])
        P.op("dve", lambda e: e.memset(tiny[:], 0.0), writes=["tiny"])
        def build_gB(gi):
            for k in range(8):
                P.op("dve", lambda e, gi=gi, k=k: e.tensor_scalar(out=gB1[:, k, :], in0=ones_b[:], scalar1=gpk[:, gi * 8 + k:gi * 8 + k + 1],
                                                                  scalar2=None, op0=ALU.mult),
                     reads=["ones_b", "c_g%d" % gi], writes=["gB"])

        evac_rr = [0]

        def evac(out, in_, reads, writes, scale=None, eng=None):
            if eng is None:
                eng = ("act", "dve")[evac_rr[0] % 2]
                evac_rr[0] += 1
            if eng == "act":
                if scale is None:
                    P.op("act", lambda e: e.activation(out=out, in_=in_, func=AF.Copy), reads=reads, writes=writes)
                else:
                    P.op("act", lambda e: e.activation(out=out, in_=in_, func=AF.Copy, scale=float(scale)), reads=reads, writes=writes)
            else:
                if scale is None:
                    P.op("dve", lambda e: e.tensor_copy(out=out, in_=in_), reads=reads, writes=writes)
                else:
                    P.op("dve", lambda e: e.tensor_scalar(out=out, in0=in_, scalar1=float(scale), scalar2=None, op0=ALU.mult), reads=reads, writes=writes)

        def phase_barrier():
            P.barrier(lambda e: e.memset(tiny[:, 0:1], 0.0))

        def norm_transpose(b, src, gi, dstT, dstres, xt_bufs, xn_bufs, ps_banks, f32_side=None):
            for i in range(NT):
                sl = i % 2
                if src == "x":
                    xt = xt_bufs[sl]
                    xres = "xt%d" % sl
                    P.dma("sp", xres, lambda e, xt=xt, i=i: e.dma_start(out=xt, in_=x_d[b, i * 128:(i + 1) * 128, :]), writes=[xres])
                else:
                    xt = x1[:, i, :]
                    xres = ("x1", i)
                xn = xn_bufs[sl]
                xnres = "xn%d" % sl
                ssc = small[:, i:i + 1]
                rsc = small[:, 16 + i:17 + i]
                P.op("act", lambda e, xt=xt, xn=xn, ssc=ssc: e.activation(out=xn, in_=xt, func=AF.Square, accum_out=ssc),
                     reads=[xres], writes=[xnres, ("ss", i)])
                P.op("dve", lambda e, ssc=ssc, rsc=rsc: e.tensor_scalar(out=rsc, in0=ssc, scalar1=1.0 / D, scalar2=EPS, op0=ALU.mult, op1=ALU.add),
                     reads=[("ss", i)], writes=[("rs", i)])
                P.op("act", lambda e, rsc=rsc: e.activation(out=rsc, in_=rsc, func=AF.Sqrt), reads=[("rs", i)], writes=[("rs", i)])
                P.op("dve", lambda e, rsc=rsc: e.reciprocal(out=rsc, in_=rsc), reads=[("rs", i)], writes=[("rs", i)])
                if f32_side is None:
                    P.op("dve", lambda e, xt=xt, xn=xn, rsc=rsc: e.tensor_scalar(out=xn, in0=xt, scalar1=rsc, scalar2=None, op0=ALU.mult),
                         reads=[xres, ("rs", i)], writes=[xnres])
                    pb = ps_banks[i % len(ps_banks)]
                    pres = ("ps", pb)
                    pv = psbf(pb)[:, 0:1024].rearrange("p (k t) -> p k t", k=8)
                    for k in range(8):
                        P.op("pe", lambda e, pv=pv, xn=xn, k=k: e.transpose(out=pv[:, k, :], in_=xn[:, k * 128:(k + 1) * 128], identity=ident_b[:]),
                             reads=[xnres, "ident_b"], writes=[pres])
                    P.op("dve", lambda e, pv=pv, i=i: e.tensor_tensor(out=dstT[:, :, i * 128:(i + 1) * 128], in0=pv, in1=gB[gi][:], op=ALU.mult),
                         reads=[pres, "gB"], writes=[(dstres, i // 4)])
                else:
                    f32_side(i, xt, xres, rsc, xn, xnres)

        last_out_tokens = []
        for b in range(NB):
            xt_bufs = [dD(0, 4 * KB, F32), dD(4 * KB, 4 * KB, F32)]
            xn_bufs = [dD(8 * KB, 2 * KB, BF16), dD(10 * KB, 2 * KB, BF16)]
            w1 = dD(12 * KB, 20 * KB, BF16, "p (k c) -> p k c", k=8)
            ckv_raw = dD(32 * KB, 8 * KB, F32)
            sqb = [dD(40 * KB, 1 * KB, BF16), dD(41 * KB, 1 * KB, BF16)]
            rstd_b = [dD(42 * KB, 2 * KB, F32), dD(44 * KB, 2 * KB, F32)]
            P.dma("pool", "w1", lambda e: e.dma_start(out=w1.rearrange("p k c -> p (k c)"), in_=w1_d[:, :], max_dma_last_dim=8192), writes=["w1"])
            build_gB(0)
            norm_transpose(b, "x", 0, hT, "hT", xt_bufs, xn_bufs, [6, 7])

            if stop_after == "P0":
                break
            bank_rr = [0]

            def nb(banks):
                v = banks[bank_rr[0] % len(banks)]
                bank_rr[0] += 1
                return v

            def proj_T(w, wres, cc, c, banks):
                pb = nb(banks)
                for k in range(8):
                    P.op("pe", lambda e, pb=pb, k=k: e.matmul(PS(pb)[:, :], lhsT=w[:, k, cc * 128:(cc + 1) * 128], rhs=hT[:, k, c * 512:(c + 1) * 512],
                                                             start=(k == 0), stop=(k == 7)),
                         reads=[wres, ("hT", c)], writes=[("ps", pb)])
                return pb

            for cc in range(10):
                for c in range(NCH):
                    pb = proj_T(w1, "w1", cc, c, [0, 1, 2, 3])
                    cs = slice(c * 512, (c + 1) * 512)
                    if cc < 4:
                        evac(qaT[:, cc, cs], PS(pb)[:, :], [("ps", pb)], [("qaT", c)])
                    elif cc < 8:
                        evac(qiT[:, cc - 4, cs], PS(pb)[:, :], [("ps", pb)], [("qiT", c)])
                    elif cc == 8:
                        evac(ckv_raw[:, cs], PS(pb)[:, :], [("ps", pb)], [("ckv_raw", c)])
                    else:
                        evac(kiT[:, cs], PS(pb)[:, :], [("ps", pb)], [("kiT", c)])
            for c in range(NCH):
                cs = slice(c * 512, (c + 1) * 512)
                sq = sqb[c % 2]
                rb = rstd_b[c % 2]
                P.op("act", lambda e, sq=sq, cs=cs: e.activation(out=sq, in_=ckv_raw[:, cs], func=AF.Square), reads=[("ckv_raw", c)], writes=[("sq", c % 2)])
                pb = nb([0, 1, 2, 3])
                P.op("pe", lambda e, pb=pb, sq=sq: e.matmul(PS(pb)[:, :], lhsT=ones_b[:], rhs=sq, start=True, stop=True),
                     reads=[("sq", c % 2), "ones_b"], writes=[("ps", pb)])
                P.op("dve", lambda e, pb=pb, rb=rb: e.tensor_scalar(out=rb, in0=PS(pb)[:, :], scalar1=1.0 / 128, scalar2=EPS, op0=ALU.mult, op1=ALU.add),
                     reads=[("ps", pb)], writes=[("rb", c % 2)])
                P.op("act", lambda e, rb=rb: e.activation(out=rb, in_=rb, func=AF.Sqrt), reads=[("rb", c % 2)], writes=[("rb", c % 2)])
                P.op("dve", lambda e, rb=rb: e.reciprocal(out=rb, in_=rb), reads=[("rb", c % 2)], writes=[("rb", c % 2)])
                P.op("dve", lambda e, rb=rb, cs=cs: e.scalar_tensor_tensor(out=ckvT[:, cs], in0=ckv_raw[:, cs], scalar=gkv[:, 0:1], in1=rb, op0=ALU.mult, op1=ALU.mult),
                     reads=[("ckv_raw", c), ("rb", c % 2), "c_gkv"], writes=[("ckvT", c)])
            pbw = 4
            for i in range(NT):
                for k in range(8):
                    P.op("pe", lambda e, i=i, k=k: e.matmul(PS(pbw)[:, i * 8:(i + 1) * 8], lhsT=hT[:, k, i * 128:(i + 1) * 128], rhs=widx_w[:, k, :],
                                                         start=(k == 0), stop=(k == 7)),
                         reads=[("hT", i // 4), "c_widx"], writes=[("ps", pbw)])
            P.op("dve", lambda e: e.tensor_scalar(out=widx_tm.rearrange("p i h -> p (i h)"), in0=PS(pbw)[:, 0:128], scalar1=IDX_SCALE, scalar2=None, op0=ALU.mult),
                 reads=[("ps", pbw)], writes=["widx_tm"])
            P.op("pool", lambda e: e.memset(Vp.rearrange("p j m c -> p (j m) c")[:, :, 64:65], 1.0), writes=["Vp"])
            P.op("pool", lambda e: e.memset(Vp.rearrange("p j m c -> p (j m) c")[:, :, 129:130], 1.0), writes=["Vp"])
            for j in range(NT):
                pb = nb([0, 1, 2, 3])
                P.op("pe", lambda e, pb=pb, j=j: e.matmul(PS(pb)[:, :], lhsT=ckvT[:, j * 128:(j + 1) * 128], rhs=wuv[:], start=True, stop=True),
                     reads=[("ckvT", j // 4), "c_wuv"], writes=[("ps", pb)])
                pv = PS(pb)[:, :].rearrange("p (m h d) -> p m h d", m=4, h=2)
                evac(Vp[:, j, :, 0:64], pv[:, :, 0, :], [("ps", pb)], ["Vp"])
                evac(Vp[:, j, :, 65:129], pv[:, :, 1, :], [("ps", pb)], ["Vp"])
            phase_barrier()
            if stop_after == "P1":
                break

            NEGM = 30000.0
            score = [dD(0, 8 * KB, F32), dD(8 * KB, 8 * KB, F32), carve(arC, 16 * KB, 8 * KB, F32), carve(arC, 24 * KB, 8 * KB, F32)]
            junk = dD(16 * KB, 4 * KB, BF16)
            mask_tm = dD(20 * KB, 4 * KB, BF16)
            maskT = dD(24 * KB, 16 * KB, BF16, "p (j t) -> p j t", j=16)
            B8 = dD(40 * KB, 10 * KB, BF16, "p (h u) -> p h u", h=8)
            rbuf = [dD(50 * KB + i * KB, 1 * KB, BF16) for i in range(4)]
            Pbuf = [dD(54 * KB + i * KB, 1 * KB, BF16) for i in range(4)]
            qabs = [dD(58 * KB + i * KB, 1 * KB, BF16) for i in range(2)]
            dg = dD(60 * KB, 2 * KB, BF16, "p (h t) -> p h t", h=8)
            o_f32 = dD(62 * KB, 2 * KB, F32)
            rec = dD(64 * KB, 2 * KB, F32)
            btmp = dD(0, 10 * KB, F32, "p (h u) -> p h u", h=4)
            for hh in range(2):
                P.dma("sp", "btoep", lambda e, hh=hh: e.dma_start(out=btmp.rearrange("p h u -> p (h u)")[:, 0:2560], in_=btoep_d[:, hh * 2560:(hh + 1) * 2560]), writes=["btmp"])
                P.op("dve", lambda e, hh=hh: e.tensor_scalar(out=B8[:, hh * 4:hh * 4 + 4, :], in0=btmp[:, 0:4, :], scalar1=1.0 / ATTN_SCALE, scalar2=None, op0=ALU.mult),
                     reads=["btmp"], writes=["B8"])
            phase_barrier()

            LO, WD, MID, CNT, TMP = 40, 44, 48, 52, 56
            rr = [0]
            smr = lambda base, a0, a1: small[:, base + a0:base + a1]

            def scores_chunk(c):
                units = []
                for tl in range(4):
                    L = (4 * c + tl + 1) * 128
                    nsc = (L + 511) // 512
                    for sc in range(nsc):
                        for h in range(8):
                            units.append((tl, sc, h, nsc))
                stt = {}

                def A(u):
                    tl, sc, h, nsc = u
                    i = 4 * c + tl
                    L = (i + 1) * 128
                    ws = min(512, L - sc * 512)
                    bp = (h % 2) * 64
                    zb = nb([0, 1, 2, 3])
                    P.op("pe", lambda e: e.matmul(PS(zb)[:, 0:ws], lhsT=qiT[bp:bp + 64, h // 2, i * 128:(i + 1) * 128], rhs=kiT[bp:bp + 64, sc * 512:sc * 512 + ws], start=True, stop=True),
                         reads=[("qiT", c), ("kiT", sc)], writes=[("ps", zb)])
                    rs = rr[0] % 4
                    rr[0] += 1
                    if rs % 2 == 0:
                        P.op("act", lambda e: e.activation(out=rbuf[rs][:, 0:ws], in_=PS(zb)[:, 0:ws], func=AF.Relu), reads=[("ps", zb)], writes=[("rbuf", rs)])
                    else:
                        P.op("dve", lambda e: e.tensor_scalar(out=rbuf[rs][:, 0:ws], in0=PS(zb)[:, 0:ws], scalar1=0.0, scalar2=None, op0=ALU.max), reads=[("ps", zb)], writes=[("rbuf", rs)])
                    stt[u] = (rs, ws)

                def B(u):
                    tl, sc, h, nsc = u
                    i = 4 * c + tl
                    rs, ws = stt[u]
                    sct = score[tl]
                    spb = 4 + (sc % 2)
                    if sc == 0 and h == 0:
                        for hh in range(8):
                            P.op("dve", lambda e, hh=hh: e.tensor_scalar(out=dg[:, hh, :], in0=ident_b[:], scalar1=widx_tm[:, i, hh:hh + 1], scalar2=None, op0=ALU.mult),
                                 reads=["ident_b", "widx_tm"], writes=[("dg", hh)])
                    P.op("pe", lambda e: e.matmul(PS(spb)[:, 0:ws], lhsT=dg[:, h, :], rhs=rbuf[rs][:, 0:ws], start=(h == 0), stop=(h == 7)),
                         reads=[("dg", h), ("rbuf", rs)], writes=[("ps", spb)])
                    if h == 7:
                        last = (sc == nsc - 1)
                        wcopy = ws - 128 if last else ws
                        if wcopy > 0:
                            P.op("act", lambda e: e.activation(out=sct[:, sc * 512:sc * 512 + wcopy], in_=PS(spb)[:, 0:wcopy], func=AF.Copy),
                                 reads=[("ps", spb)], writes=[("score", tl)])
                        if last:
                            P.op("dve", lambda e: e.tensor_tensor(out=sct[:, sc * 512 + ws - 128:sc * 512 + ws], in0=PS(spb)[:, ws - 128:ws], in1=cneg[:], op=ALU.add),
                                 reads=[("ps", spb), "c_cneg"], writes=[("score", tl)])

                nu = len(units)
                for k in range(nu + 2):
                    if k < nu:
                        A(units[k])
                    if 0 <= k - 2 < nu:
                        B(units[k - 2])

            def bisect_chunk(c):
                act = [tl for tl in range(4) if 4 * c + tl >= 2]
                for tl in range(4):
                    i = 4 * c + tl
                    L = (i + 1) * 128
                    if i < 2:
                        P.op("dve", lambda e, tl=tl: e.memset(smr(LO, tl, tl + 1), -1.0e29), writes=[("lo", tl)])
                    else:
                        P.op("dve", lambda e, tl=tl, i=i: e.tensor_reduce(out=smr(LO, tl, tl + 1), in_=score[tl][:, 0:i * 128], axis=AX.X, op=ALU.min), reads=[("score", tl)], writes=[("lo", tl)])
                        P.op("dve", lambda e, tl=tl, L=L: e.tensor_reduce(out=smr(WD, tl, tl + 1), in_=score[tl][:, 0:L], axis=AX.X, op=ALU.max), reads=[("score", tl)], writes=[("wd", tl)])
                if not act:
                    return
                a0, a1 = act[0], act[-1] + 1
                R = lambda nm: [(nm, t) for t in range(a0, a1)]
                P.op("dve", lambda e: e.tensor_tensor(out=smr(WD, a0, a1), in0=smr(WD, a0, a1), in1=smr(LO, a0, a1), op=ALU.subtract), reads=R("wd") + R("lo"), writes=R("wd"))
                for it in range(N_BISECT):
                    f = 0.5 ** (it + 1)
                    P.op("dve", lambda e, f=f: e.scalar_tensor_tensor(out=smr(MID, a0, a1), in0=smr(WD, a0, a1), scalar=f, in1=smr(LO, a0, a1), op0=ALU.mult, op1=ALU.add),
                         reads=R("wd") + R("lo"), writes=R("mid"))
                    for tl in act:
                        L = (4 * c + tl + 1) * 128
                        jb, jres = ((junk, "junk"), (mask_tm, "mask_tm"))[tl % 2]
                        P.op("dve", lambda e, tl=tl, L=L, jb=jb: e.tensor_scalar(out=jb[:, 0:L], in0=score[tl][:, 0:L], scalar1=smr(MID, tl, tl + 1), scalar2=None, op0=ALU.is_ge, op1=ALU.add,
                                                                         accum_out=smr(CNT, tl, tl + 1)),
                             reads=[("score", tl), ("mid", tl)], writes=[("cnt", tl), jres])
                    P.op("dve", lambda e, f=f: e.tensor_scalar(out=smr(TMP, a0, a1), in0=smr(CNT, a0, a1), scalar1=255.5, scalar2=f, op0=ALU.is_ge, op1=ALU.mult),
                         reads=R("cnt"), writes=["tmp4"])
                    P.op("dve", lambda e: e.tensor_tensor(out=smr(TMP, a0, a1), in0=smr(TMP, a0, a1), in1=smr(WD, a0, a1), op=ALU.mult), reads=["tmp4"] + R("wd"), writes=["tmp4"])
                    P.op("dve", lambda e: e.tensor_tensor(out=smr(LO, a0, a1), in0=smr(LO, a0, a1), in1=smr(TMP, a0, a1), op=ALU.add), reads=["tmp4"] + R("lo"), writes=R("lo"))

            def maskgen_chunk(c):
                for tl in range(4):
                    i = 4 * c + tl
                    L = (i + 1) * 128
                    P.op("dve", lambda e, tl=tl, L=L: e.tensor_scalar(out=mask_tm[:, 0:L], in0=score[tl][:, 0:L], scalar1=smr(LO, tl, tl + 1), scalar2=-NEGM, op0=ALU.is_lt, op1=ALU.mult),
                         reads=[("score", tl), ("lo", tl)], writes=["mask_tm"])
                    for j0 in range(0, i + 1, 8):
                        n = min(8, i + 1 - j0)
                        tb = 6 + ((j0 // 8) % 2)
                        pv = psbf(tb)[:, 0:1024].rearrange("p (k t) -> p k t", k=8)
                        for jj in range(n):
                            j = j0 + jj
                            P.op("pe", lambda e, pv=pv, jj=jj, j=j: e.transpose(out=pv[:, jj, :], in_=mask_tm[:, j * 128:(j + 1) * 128], identity=ident_b[:]),
                                 reads=["mask_tm", "ident_b"], writes=[("ps", tb)])
                        evac(maskT[:, j0:j0 + n, tl * 128:(tl + 1) * 128], pv[:, 0:n, :], [("ps", tb)], [("maskT", tl)])

            def attention_chunk(c):
                cs0 = c * 512
                jmax = 4 * c + 3

                def emit_qabs(h):
                    bp = (h % 2) * 64
                    qb_ = nb([0, 1, 2, 3])
                    P.op("pe", lambda e, qb_=qb_, bp=bp, h=h: e.matmul(PS(qb_)[:, :], lhsT=wuk[bp:bp + 64, h // 2, :], rhs=qaT[bp:bp + 64, h // 2, cs0:cs0 + 512], start=True, stop=True),
                         reads=["c_wuk", ("qaT", c)], writes=[("ps", qb_)])
                    evac(qabs[h % 2], PS(qb_)[:, :], [("ps", qb_)], [("qabs", h % 2)], eng="act")

                def emit_tail(h):
                    bp = (h % 2) * 64
                    ob = 4 + (h % 2)
                    dbk = 6 + (h % 2)
                    P.op("act", lambda e: e.activation(out=o_f32[bp:bp + 64, :], in_=PS(ob)[bp:bp + 64, :], func=AF.Copy), reads=[("ps", ob)], writes=[("o_f32", h % 2)])
                    P.op("act", lambda e: e.activation(out=rec[bp:bp + 64, :], in_=PS(dbk)[bp:bp + 64, :], func=AF.Ln), reads=[("ps", dbk)], writes=[("rec", h % 2)])
                    P.op("act", lambda e: e.activation(out=rec[bp:bp + 64, :], in_=rec[bp:bp + 64, :], func=AF.Exp, scale=-1.0), reads=[("rec", h % 2)], writes=[("rec", h % 2)])
                    P.op("pool", lambda e: e.tensor_tensor(out=oaT[bp:bp + 64, h // 2, cs0:cs0 + 512], in0=o_f32[bp:bp + 64, :], in1=rec[bp:bp + 64, :], op=ALU.mult),
                         reads=[("o_f32", h % 2), ("rec", h % 2)], writes=[("oaT", c)])

                emit_qabs(0)
                pending_tail = None
                for h in range(8):
                    m = h // 2
                    qs = h % 2
                    ob = 4 + (h % 2)
                    if h < 7:
                        emit_qabs(h + 1)
                    stt = {}

                    def A(j):
                        col0 = max(0, j - 4 * c) * 128
                        N = 512 - col0
                        near = j >= 4 * c - 1
                        lb = nb([0, 1, 2, 3])
                        P.op("pe", lambda e, lb=lb, j=j, col0=col0, N=N: e.matmul(PS(lb)[:, 0:N], lhsT=ckvT[:, j * 128:(j + 1) * 128], rhs=qabs[qs][:, col0:512], start=True, stop=False),
                             reads=[("ckvT", j // 4), ("qabs", qs)], writes=[("ps", lb)])
                        P.op("pe", lambda e, lb=lb, j=j, col0=col0, N=N, near=near: e.matmul(PS(lb)[:, 0:N], lhsT=ident_b[:], rhs=maskT[:, j, col0:512], start=False, stop=(not near)),
                             reads=["ident_b"] + [("maskT", t) for t in range(col0 // 128, 4)], writes=[("ps", lb)])
                        if near:
                            u0 = cs0 + col0 - 128 * j
                            P.op("pe", lambda e, lb=lb, u0=u0, N=N: e.matmul(PS(lb)[:, 0:N], lhsT=ident_b[:], rhs=B8[:, h, u0:u0 + N], start=False, stop=True),
                                 reads=["ident_b", "B8"], writes=[("ps", lb)])
                        pi = rr[0] % 4
                        rr[0] += 1
                        Pt = Pbuf[pi]
                        if near:
                            P.op("act", lambda e, lb=lb, Pt=Pt, N=N: e.activation(out=Pt[:, 0:N], in_=PS(lb)[:, 0:N], func=AF.Exp, scale=ATTN_SCALE),
                                 reads=[("ps", lb)], writes=[("Pbuf", pi)])
                        else:
                            P.op("act", lambda e, lb=lb, Pt=Pt, N=N: e.activation(out=Pt[:, 0:N], in_=PS(lb)[:, 0:N], func=AF.Exp, scale=ATTN_SCALE, bias=b31[:, h:h + 1]),
                                 reads=[("ps", lb), "c_b31"], writes=[("Pbuf", pi)])
                        stt[j] = (pi, col0, N)

                    def B(j):
                        pi, col0, N = stt[j]
                        w0 = 0 if h % 2 == 0 else 1
                        P.op("pe", lambda e, j=j, w0=w0, pi=pi, col0=col0, N=N: e.matmul(PS(ob)[:, col0:512], lhsT=Vp[:, j, m, w0:w0 + 128], rhs=Pbuf[pi][:, 0:N],
                                                                                  start=(j == 0), stop=(j == jmax)),
                             reads=["Vp", ("Pbuf", pi)], writes=[("ps", ob)])
                        dbk = 6 + (h % 2)
                        P.op("pe", lambda e, j=j, pi=pi, col0=col0, N=N: e.matmul(PS(dbk)[:, col0:512], lhsT=ones_b[:], rhs=Pbuf[pi][:, 0:N], start=(j == 0), stop=(j == jmax)),
                             reads=["ones_b", ("Pbuf", pi)], writes=[("ps", dbk)])

                    for k in range(jmax + 3):
                        if k <= jmax:
                            A(k)
                        if 0 <= k - 2 <= jmax:
                            B(k - 2)
                        if k == 2 and pending_tail is not None:
                            emit_tail(pending_tail)
                            pending_tail = None
                    pending_tail = h
                emit_tail(pending_tail)

            scores_chunk(0)
            bisect_chunk(0)
            maskgen_chunk(0)
            for c in range(NCH):
                if c + 1 < NCH:
                    scores_chunk(c + 1)
                    bisect_chunk(c + 1)
                attention_chunk(c)
                if c + 1 < NCH:
                    maskgen_chunk(c + 1)
            phase_barrier()
            if stop_after == "P2":
                break

            w3 = dD(0, 24 * KB, BF16, "p (k c) -> p k c", k=8)
            ebuf = [dD(24 * KB + i * 2 * KB, 2 * KB, F32) for i in range(4)]
            spbuf = [dD(32 * KB + i * KB, 1 * KB, BF16) for i in range(6)]
            tbuf = [dD(38 * KB + i * 2 * KB, 2 * KB, F32) for i in range(3)]
            abuf = [dD(44 * KB + i * KB, 1 * KB, BF16) for i in range(4)]
            sbrr = [0, 0, 0, 0]
            P.dma("pool", "w3", lambda e: e.dma_start(out=w3.rearrange("p k c -> p (k c)"), in_=w3_d[:, :], max_dma_last_dim=8192), writes=["w3"])
            if stop_after == "P3w":
                break
            for cc in range(8):
                for c in range(NCH):
                    pb = proj_T(w3, "w3", cc, c, [0, 1, 2, 3])
                    cs = slice(c * 512, (c + 1) * 512)
                    if cc < 4:
                        evac(qbT[:, cc, cs], PS(pb)[:, :], [("ps", pb)], [("qbT", c)])
                    else:
                        evac(kbT[:, cc - 4, cs], PS(pb)[:, :], [("ps", pb)], [("kbT", c)])
            if stop_after == "P3q":
                break
            for j in range(NT):
                pb = nb([0, 1, 2, 3])
                for k in range(8):
                    P.op("pe", lambda e, pb=pb, j=j, k=k: e.matmul(PS(pb)[:, :], lhsT=hT[:, k, j * 128:(j + 1) * 128], rhs=w3[:, k, 1024:1536], start=(k == 0), stop=(k == 7)),
                         reads=["w3", ("hT", j // 4)], writes=[("ps", pb)])
                evac(vb[:, j, :], PS(pb)[:, :], [("ps", pb)], [("vb", j // 4)])
            if stop_after == "P3a":
                break
            for c in range(NCH):
                cs0 = c * 512
                jmax = 4 * c + 3
                for hp in range(4):
                    m = hp
                    units = [(j, hh) for j in range(jmax, -1, -1) for hh in range(2)]
                    stt = {}
                    prev_sp = {0: None, 1: None}

                    def S1(u):
                        j, hh = u
                        bp = hh * 64
                        col0 = max(0, j - 4 * c) * 128
                        N = 512 - col0
                        zb = nb([0, 1, 2, 5])
                        P.op("pe", lambda e, zb=zb, bp=bp, j=j, col0=col0, N=N: e.matmul(PS(zb)[:, 0:N], lhsT=kbT[bp:bp + 64, m, j * 128:(j + 1) * 128],
                                                                                    rhs=qbT[bp:bp + 64, m, cs0 + col0:cs0 + 512], start=True, stop=True),
                             reads=[("kbT", j // 4), ("qbT", c)], writes=[("ps", zb)])
                        ei = sbrr[0] % 4
                        si = sbrr[1] % 6
                        sbrr[0] += 1
                        sbrr[1] += 1
                        eb_, spt = ebuf[ei], spbuf[si]
                        P.op("act", lambda e, zb=zb, eb_=eb_, N=N: e.activation(out=eb_[:, 0:N], in_=PS(zb)[:, 0:N], func=AF.Exp, scale=ATTN_SCALE),
                             reads=[("ps", zb)], writes=[("ebuf", ei)])
                        if j >= 4 * c:
                            P.op("dve", lambda e, eb_=eb_: e.tensor_tensor(out=eb_[:, 0:128], in0=eb_[:, 0:128], in1=smask[:], op=ALU.mult),
                                 reads=[("ebuf", ei), "c_smask"], writes=[("ebuf", ei)])
                        P.op("act", lambda e, eb_=eb_, spt=spt, N=N: e.activation(out=spt[:, 0:N], in_=eb_[:, 0:N], func=AF.Ln, bias=1.0, scale=1.0),
                             reads=[("ebuf", ei)], writes=[("spbuf", si)])
                        stt[u] = dict(ei=ei, si=si, col0=col0, N=N)

                    def S2(u):
                        j, hh = u
                        d = stt[u]
                        xb = 3 + hh
                        col0, N = d["col0"], d["N"]
                        spt = spbuf[d["si"]]
                        pv = prev_sp[hh]
                        if pv is not None:
                            psi, pcol0, pN = pv
                            P.op("pe", lambda e, xb=xb, psi=psi, pcol0=pcol0, pN=pN: e.matmul(PS(xb)[:, pcol0:512], lhsT=smask[:], rhs=spbuf[psi][:, 0:pN], start=False, stop=True, skip_group_check=True),
                                 reads=["c_smask", ("spbuf", psi)], writes=[("ps", xb)])
                        P.op("pe", lambda e, xb=xb, spt=spt, col0=col0, N=N, first=(pv is None): e.matmul(PS(xb)[:, col0:512], lhsT=uinc[:], rhs=spt[:, 0:N], start=first, stop=True, skip_group_check=True),
                             reads=["c_uinc", ("spbuf", d["si"])], writes=[("ps", xb)])
                        prev_sp[hh] = (d["si"], col0, N)
                        ti = sbrr[2] % 3
                        ai = sbrr[3] % 4
                        sbrr[2] += 1
                        sbrr[3] += 1
                        d["ai"] = ai
                        P.op("act", lambda e, xb=xb, ti=ti, col0=col0, N=N: e.activation(out=tbuf[ti][:, 0:N], in_=PS(xb)[:, col0:512], func=AF.Exp, scale=-1.0),
                             reads=[("ps", xb)], writes=[("tbuf", ti)])
                        P.op("dve", lambda e, ti=ti, ai=ai, ei=d["ei"], N=N: e.tensor_tensor(out=abuf[ai][:, 0:N], in0=tbuf[ti][:, 0:N], in1=ebuf[ei][:, 0:N], op=ALU.mult),
                             reads=[("tbuf", ti), ("ebuf", d["ei"])], writes=[("abuf", ai)])

                    def S3(u):
                        j, hh = u
                        d = stt[u]
                        ob = 6 + hh
                        col0, N = d["col0"], d["N"]
                        P.op("pe", lambda e, ob=ob, j=j, ai=d["ai"], col0=col0, N=N: e.matmul(PS(ob)[:, col0:512], lhsT=vb[:, j, m * 128:(m + 1) * 128], rhs=abuf[ai][:, 0:N],
                                                                                       start=(j == jmax), stop=(j == 0), skip_group_check=True),
                             reads=[("vb", j // 4), ("abuf", d["ai"])], writes=[("ps", ob)])

                    nu = len(units)
                    for k in range(nu + 2):
                        if k < nu:
                            S1(units[k])
                        if 0 <= k - 1 < nu:
                            S2(units[k - 1])
                        if 0 <= k - 2 < nu:
                            S3(units[k - 2])
                    for hh in range(2):
                        bp = hh * 64
                        evac(obT[bp:bp + 64, m, cs0:cs0 + 512], PS(6 + hh)[bp:bp + 64, :], [("ps", 6 + hh)], [("obT", c)])
            phase_barrier()
            if dbg:
                dtmp = dD(48 * KB, 16 * KB, F32)
                for nm, src, res in (("d_oaT", oaT, "oaT"), ("d_obT", obT, "obT")):
                    for q in range(2):
                        P.op("dve", lambda e, src=src, q=q: e.tensor_copy(out=dtmp, in_=src.rearrange("p k t -> p (k t)")[:, q * 4096:(q + 1) * 4096]),
                             reads=[(res, cq) for cq in range(4)], writes=["dtmp"])
                        if b == 0:
                            P.dma("sp", "dbg", lambda e, nm=nm, q=q: e.dma_start(out=dbg_d[nm][:, q * 4096:(q + 1) * 4096], in_=dtmp), reads=["dtmp"])
                phase_barrier()
            if stop_after == "P3":
                break

            mergedT = dD(0, 32 * KB, BF16, "p (k t) -> p k t", k=8)
            wout = dD(32 * KB, 16 * KB, BF16, "p (k c) -> p k c", k=8)
            wg4 = [dD(48 * KB + i * 4 * KB, 4 * KB, BF16, "p (k c) -> p k c", k=8) for i in range(2)]
            wbr = [dD(56 * KB + i * 2 * KB, 2 * KB, BF16, "p (a k c) -> p a k c", a=2, k=4) for i in range(2)]
            sgb_ = [dD(60 * KB + i * KB, 1 * KB, BF16) for i in range(2)]
            t12 = [dD(62 * KB + i * 2 * KB, 2 * KB, F32) for i in range(2)]
            P.dma("pool", "wout", lambda e: e.dma_start(out=wout.rearrange("p k c -> p (k c)"), in_=wout_d[:, :], max_dma_last_dim=8192), writes=["wout"])
            for m in range(8):
                wsl = m % 2
                P.dma("pool", "wg4_%d" % wsl, lambda e, m=m, wsl=wsl: e.dma_start(out=wg4[wsl].rearrange("p k c -> p (k c)"), in_=wg4_d[:, m * 2048:(m + 1) * 2048], max_dma_last_dim=8192),
                      writes=[("wg4", wsl)])
                P.dma("pool", "wbr_%d" % wsl, lambda e, m=m, wsl=wsl: e.dma_start(out=wbr[wsl].rearrange("p a k c -> p (a k c)"), in_=wbr_d[:, m * 1024:(m + 1) * 1024], max_dma_last_dim=8192),
                      writes=[("wbr", wsl)])
                for c in range(NCH):
                    cs = slice(c * 512, (c + 1) * 512)
                    banks = {}
                    for gi_, nm in enumerate(("ga", "gb")):
                        pb = nb([0, 1, 2, 3, 4, 5, 6, 7])
                        banks[nm] = pb
                        for k in range(8):
                            P.op("pe", lambda e, pb=pb, k=k, gi_=gi_, wsl=wsl, cs=cs: e.matmul(PS(pb)[:, :], lhsT=wg4[wsl][:, k, gi_ * 128:(gi_ + 1) * 128], rhs=hT[:, k, cs],
                                                                                        start=(k == 0), stop=(k == 7)),
                                 reads=[("wg4", wsl), ("hT", c)], writes=[("ps", pb)])
                    for a_, (nm, oT, ores) in enumerate((("ya", oaT, "oaT"), ("yb", obT, "obT"))):
                        pb = nb([0, 1, 2, 3, 4, 5, 6, 7])
                        banks[nm] = pb
                        for k in range(4):
                            P.op("pe", lambda e, pb=pb, k=k, a_=a_, wsl=wsl, oT=oT, cs=cs: e.matmul(PS(pb)[:, :], lhsT=wbr[wsl][:, a_, k, :], rhs=oT[:, k, cs], start=(k == 0), stop=(k == 3)),
                                 reads=[("wbr", wsl), (ores, c)], writes=[("ps", pb)])
                    i0 = 0
                    sa, sb_ = sgb_[i0], sgb_[i0 + 1]
                    P.op("act", lambda e, sa=sa, pb=banks["ga"]: e.activation(out=sa, in_=PS(pb)[:, :], func=AF.Sigmoid), reads=[("ps", banks["ga"])], writes=[("sg", i0)])
                    P.op("act", lambda e, sb_=sb_, pb=banks["gb"]: e.activation(out=sb_, in_=PS(pb)[:, :], func=AF.Sigmoid), reads=[("ps", banks["gb"])], writes=[("sg", i0 + 1)])
                    P.op("dve", lambda e, sa=sa, pb=banks["ya"]: e.tensor_tensor(out=t12[0], in0=sa, in1=PS(pb)[:, :], op=ALU.mult), reads=[("sg", i0), ("ps", banks["ya"])], writes=["t1"])
                    P.op("dve", lambda e, sb_=sb_, pb=banks["yb"]: e.tensor_tensor(out=t12[1], in0=sb_, in1=PS(pb)[:, :], op=ALU.mult), reads=[("sg", i0 + 1), ("ps", banks["yb"])], writes=["t2"])
                    P.op("dve", lambda e, m=m, cs=cs: e.tensor_tensor(out=mergedT[:, m, cs], in0=t12[0], in1=t12[1], op=ALU.add), reads=["t1", "t2"], writes=[("mergedT", c)])
            phase_barrier()
            for i in range(NT):
                P.dma("sp", "x1ld%d" % (i % 4), lambda e, i=i: e.dma_start(out=x1[:, i, :], in_=x_d[b, i * 128:(i + 1) * 128, :]), writes=[("x1", i)])
                pb0 = (i % 4) * 2
                for half in range(2):
                    pb = pb0 + half
                    for k in range(8):
                        P.op("pe", lambda e, pb=pb, k=k, i=i, half=half: e.matmul(PS(pb)[:, :], lhsT=mergedT[:, k, i * 128:(i + 1) * 128], rhs=wout[:, k, half * 512:(half + 1) * 512],
                                                                               start=(k == 0), stop=(k == 7)),
                             reads=[("mergedT", i // 4), "wout"], writes=[("ps", pb)])
                    P.op("dve", lambda e, pb=pb, i=i, half=half: e.tensor_tensor(out=x1[:, i, half * 512:(half + 1) * 512], in0=x1[:, i, half * 512:(half + 1) * 512], in1=PS(pb)[:, :], op=ALU.add),
                         reads=[("ps", pb), ("x1", i)], writes=[("x1", i)])
            phase_barrier()
            if dbg and b == 0:
                P.dma("sp", "dbg", lambda e: e.dma_start(out=dbg_d["d_x1"][:, :], in_=x1.rearrange("p i d -> p (i d)")), reads=[("x1", i) for i in range(NT)])
                phase_barrier()
            if stop_after == "P4":
                break

            h2T = hT
            xnf2 = [dD(i * 4 * KB, 4 * KB, F32) for i in range(2)]
            hTf2 = [dD(8 * KB + i * 4 * KB, 4 * KB, F32, "p (k t) -> p k t", k=8) for i in range(2)]
            comb = dD(16 * KB, 2 * KB, F32, "p (i e) -> p i e", i=16)
            elm_all = dD(18 * KB, 2304, F32, "p (i e) -> p i e", i=16)
            eqA = dD(21 * KB, 2 * KB, F32, "p (i e) -> p i e", i=16)
            eqB = dD(23 * KB, 2 * KB, F32, "p (i e) -> p i e", i=16)
            rsm = dD(25 * KB, 1 * KB, F32, "p (a i) -> p a i", a=16)
            sgm = [dD(26 * KB + i * KB, 1 * KB, BF16) for i in range(4)]
            hid = [dD(30 * KB + i * 2 * KB, 2 * KB, BF16, "p (f t) -> p f t", f=2) for i in range(2)]

            def ffn_side(i, xt, xres, rsc, xn, xnres):
                sl = i % 2
                xnf, hTf = xnf2[sl], hTf2[sl]
                P.op("dve", lambda e: e.tensor_scalar(out=xnf, in0=xt, scalar1=rsc, scalar2=None, op0=ALU.mult), reads=[xres, ("rs", i)], writes=[("xnf", sl)])
                for half in range(2):
                    pb = (4 + half) if sl == 0 else (2 + half)
                    for kk in range(4):
                        k = half * 4 + kk
                        P.op("pe", lambda e, kk=kk, k=k: e.transpose(out=PS(pb)[:, kk * 128:(kk + 1) * 128], in_=xnf[:, k * 128:(k + 1) * 128], identity=ident_f[:]),
                             reads=[("xnf", sl), "c_ident"], writes=[("ps", pb)])
                    pv = PS(pb)[:, :].rearrange("p (k t) -> p k t", k=4)
                    gf = gpk[:, 8 + half * 4:8 + half * 4 + 4]
                    P.op("dve", lambda e: e.tensor_tensor(out=h2T[:, half * 4:half * 4 + 4, i * 128:(i + 1) * 128], in0=pv, in1=gB[1][:, half * 4:half * 4 + 4, :], op=ALU.mult),
                         reads=[("ps", pb), "gB"], writes=[("hT", i // 4)])
                    P.op("dve", lambda e: e.tensor_tensor(out=hTf[:, half * 4:half * 4 + 4, :], in0=pv, in1=gf.unsqueeze(2).broadcast_to([128, 4, 128]), op=ALU.mult),
                         reads=[("ps", pb), "c_g1"], writes=[("hTf", sl)])
                rb = 6 + sl
                for k in range(8):
                    P.op("pe", lambda e, k=k: e.matmul(PS(rb)[:, 0:36], lhsT=hTf[:, k, :], rhs=wr_f[:, k, :], start=(k == 0), stop=(k == 7)),
                         reads=[("hTf", sl), "c_wr"], writes=[("ps", rb)])
                P.op("dve", lambda e: e.tensor_tensor(out=elm_all[:, i, :], in0=PS(rb)[:, 0:36], in1=brb[:], op=ALU.add), reads=[("ps", rb), "c_br"], writes=["elm_all"])

            def router_batched():
                GL = elm_all[:, :, 0:4]
                EL = elm_all[:, :, 4:36]
                R_ = lambda a: rsm[:, a, :]
                bc = lambda ap, n: ap.unsqueeze(2).broadcast_to([128, 16, n])
                seq = []
                D = lambda fn: seq.append(("dve", fn))
                A_ = lambda fn: seq.append(("act", fn))
                gmax, gs, v1, v2, w1, w2, dd = R_(0), R_(1), R_(2), R_(3), R_(4), R_(5), R_(6)
                oh = eqA[:, :, 0:4]
                ngm = eqA[:, :, 4:8]
                gd = eqA[:, :, 8:12]
                D(lambda e: e.tensor_reduce(out=gmax, in_=GL, axis=AX.X, op=ALU.max))
                D(lambda e: e.tensor_tensor(out=oh, in0=GL, in1=bc(gmax, 4), op=ALU.is_ge))
                D(lambda e: e.tensor_scalar(out=ngm, in0=oh, scalar1=-1.0, scalar2=-NEG, op0=ALU.add, op1=ALU.mult))
                D(lambda e: e.tensor_tensor(out=gd, in0=GL, in1=bc(gmax, 4), op=ALU.subtract))
                A_(lambda e: e.activation(out=gd, in_=gd, func=AF.Exp))
                D(lambda e: e.tensor_reduce(out=gs, in_=gd, axis=AX.X, op=ALU.add))
                D(lambda e: e.reciprocal(out=gs, in_=gs))
                D(lambda e: e.tensor_tensor(out=EL.rearrange("p i (g x) -> p i g x", g=4), in0=EL.rearrange("p i (g x) -> p i g x", g=4),
                                            in1=ngm.unsqueeze(3).broadcast_to([128, 16, 4, 8]), op=ALU.add))
                D(lambda e: e.tensor_reduce(out=v1, in_=EL, axis=AX.X, op=ALU.max))
                D(lambda e: e.tensor_tensor(out=eqA[:, :, :], in0=EL, in1=bc(v1, 32), op=ALU.is_equal))
                D(lambda e: e.scalar_tensor_tensor(out=eqB[:, :, :], in0=eqA[:, :, :], scalar=NEG, in1=EL, op0=ALU.mult, op1=ALU.add))
                D(lambda e: e.tensor_reduce(out=v2, in_=eqB[:, :, :], axis=AX.X, op=ALU.max))
                D(lambda e: e.tensor_tensor(out=eqB[:, :, :], in0=eqB[:, :, :], in1=bc(v2, 32), op=ALU.is_equal))
                D(lambda e: e.tensor_tensor(out=dd, in0=v2, in1=v1, op=ALU.subtract))
                A_(lambda e: e.activation(out=dd, in_=dd, func=AF.Exp))
                D(lambda e: e.tensor_scalar(out=dd, in0=dd, scalar1=1.0, scalar2=None, op0=ALU.add))
                D(lambda e: e.reciprocal(out=w1, in_=dd))
                D(lambda e: e.tensor_scalar(out=w2, in0=w1, scalar1=-1.0, scalar2=1.0, op0=ALU.mult, op1=ALU.add))
                D(lambda e: e.tensor_tensor(out=w1, in0=w1, in1=gs, op=ALU.mult))
                D(lambda e: e.tensor_tensor(out=w2, in0=w2, in1=gs, op=ALU.mult))
                D(lambda e: e.tensor_tensor(out=eqA[:, :, :], in0=eqA[:, :, :], in1=bc(w1, 32), op=ALU.mult))
                D(lambda e: e.tensor_tensor(out=eqB[:, :, :], in0=eqB[:, :, :], in1=bc(w2, 32), op=ALU.mult))
                D(lambda e: e.tensor_tensor(out=comb[:, :, :], in0=eqA[:, :, :], in1=eqB[:, :, :], op=ALU.add))
                for eng, fn in seq:
                    P.op(eng, fn, reads=["elm_all", "router"], writes=["router", "comb"])

            build_gB(1)
            norm_transpose(b, "x1", 1, h2T, "hT", None, [dD(34 * KB, 2 * KB, BF16), dD(36 * KB, 2 * KB, BF16)], None, f32_side=ffn_side)
            router_batched()
            phase_barrier()
            def wviews(ex):
                wm = wmoe[ex % 2]
                return (wm[:, 0:2048].rearrange("p (k c) -> p k c", k=8), wm[:, 2048:4096].rearrange("p (k c) -> p k c", k=8),
                        wm[:, 4096:6144].rearrange("p (f c) -> p f c", f=2))

            def moe_dma(ex):
                wsl = ex % 2
                P.dma("pool", "wmoe%d" % wsl, lambda e, wsl=wsl, ex=ex: e.dma_start(out=wmoe[wsl], in_=wmoe_d[ex, :, :], max_dma_last_dim=8192), writes=[("wmoe", wsl)])

            def GU(n, f):
                ex, c = n // NCH, n % NCH
                wsl = ex % 2
                wgv, wuv_, wdv = wviews(ex)
                cs = slice(c * 512, (c + 1) * 512)
                hs = n % 2
                gb_, ub_ = f * 2, f * 2 + 1
                for k in range(8):
                    P.op("pe", lambda e, k=k: e.matmul(PS(gb_)[:, :], lhsT=wgv[:, k, f * 128:(f + 1) * 128], rhs=h2T[:, k, cs], start=(k == 0), stop=(k == 7)),
                         reads=[("wmoe", wsl), ("hT", c)], writes=[("ps", gb_)])
                for k in range(8):
                    P.op("pe", lambda e, k=k: e.matmul(PS(ub_)[:, :], lhsT=wuv_[:, k, f * 128:(f + 1) * 128], rhs=h2T[:, k, cs], start=(k == 0), stop=(k == 7)),
                         reads=[("wmoe", wsl), ("hT", c)], writes=[("ps", ub_)])
                sgi = hs * 2 + f
                P.op("act", lambda e: e.activation(out=sgm[sgi], in_=PS(gb_)[:, :], func=AF.Silu), reads=[("ps", gb_)], writes=[("sgm", sgi)])
                P.op("dve", lambda e: e.tensor_tensor(out=hid[hs][:, f, :], in0=sgm[sgi], in1=PS(ub_)[:, :], op=ALU.mult),
                     reads=[("sgm", sgi), ("ps", ub_)], writes=[("hid", hs, f)])

            def DOWN(n, tiles):
                ex, c = n // NCH, n % NCH
                wsl = ex % 2
                wgv, wuv_, wdv = wviews(ex)
                hs = n % 2
                for tl in tiles:
                    i = c * 4 + tl
                    for half in range(2):
                        pb = 4 + (rr[0] % 4)
                        rr[0] += 1
                        for f in range(2):
                            P.op("pe", lambda e, f=f: e.matmul(PS(pb)[:, :], lhsT=hid[hs][:, f, tl * 128:(tl + 1) * 128], rhs=wdv[:, f, half * 512:(half + 1) * 512],
                                                             start=(f == 0), stop=(f == 1)),
                                 reads=[("hid", hs, f), ("wmoe", wsl)], writes=[("ps", pb)])
                        P.op("dve", lambda e: e.scalar_tensor_tensor(out=x1[:, i, half * 512:(half + 1) * 512], in0=PS(pb)[:, :], scalar=comb[:, i, ex:ex + 1],
                                                                      in1=x1[:, i, half * 512:(half + 1) * 512], op0=ALU.mult, op1=ALU.add),
                             reads=[("ps", pb), "comb", ("x1", i)], writes=[("x1", i)])

            NSTEP = N_EXP * NCH
            moe_dma(0)
            GU(0, 0)
            GU(0, 1)
            for n in range(NSTEP):
                if n % NCH == 0 and n // NCH + 1 < N_EXP:
                    moe_dma(n // NCH + 1)
                if n + 1 < NSTEP:
                    GU(n + 1, 0)
                DOWN(n, (0, 1))
                if n + 1 < NSTEP:
                    GU(n + 1, 1)
                DOWN(n, (2, 3))
            phase_barrier()
            if stop_after == "P5":
                break

            h3T = hT
            P.dma("pool", "wpg", lambda e: e.dma_start(out=wpg.rearrange("p k c -> p (k c)"), in_=wpg_d[:, :], max_dma_last_dim=8192), writes=["wpg"])
            P.dma("pool", "wpl", lambda e: e.dma_start(out=wpl.rearrange("p k c -> p (k c)"), in_=wpl_d[:, :], max_dma_last_dim=8192), writes=["wpl"])
            build_gB(2)
            norm_transpose(b, "x1", 2, h3T, "hT", None, [dD(0, 2 * KB, BF16), dD(2 * KB, 2 * KB, BF16)], [6, 7])
            pt_b = [dD(4 * KB + i * KB, 1 * KB, F32) for i in range(2)]
            pT_b = [dD(6 * KB + i * 512, 512, BF16, "p (k t) -> p k t", k=2) for i in range(2)]
            sig = [dD(8 * KB + i * 2 * KB, 2 * KB, BF16) for i in range(2)]
            tmpf = [dD(12 * KB + i * 4 * KB, 4 * KB, F32) for i in range(2)]
            outb = [dD(20 * KB + i * 4 * KB, 4 * KB, F32) for i in range(2)]
            junkf = dD(28 * KB, 2 * KB, BF16)
            gfin = dD(30 * KB, 4 * KB, F32)
            P.dma("sp", "c_gfin", lambda e: e.dma_start(out=gfin, in_=gfin_d[:, :]), writes=["c_gfin"])
            for i in range(NT):
                sl = i % 2
                P.dma("sp", "pt%d" % sl, lambda e, i=i, sl=sl: e.dma_start(out=pt_b[sl], in_=p_d[b, i * 128:(i + 1) * 128, :]), writes=[("pt", sl)])
                for k in range(2):
                    P.op("pe", lambda e, k=k, sl=sl: e.transpose(out=PS(5)[:, k * 128:(k + 1) * 128], in_=pt_b[sl][:, k * 128:(k + 1) * 128], identity=ident_f[:]),
                         reads=[("pt", sl), "c_ident"], writes=[("ps", 5)])
                P.op("act", lambda e, sl=sl: e.activation(out=pT_b[sl].rearrange("p k t -> p (k t)"), in_=PS(5)[:, 0:256], func=AF.Copy), reads=[("ps", 5)], writes=[("pT", sl)])
                for half in range(2):
                    gbk = half
                    pbk = 2 + half
                    for k in range(8):
                        P.op("pe", lambda e, gbk=gbk, k=k, i=i, half=half: e.matmul(PS(gbk)[:, :], lhsT=h3T[:, k, i * 128:(i + 1) * 128], rhs=wpg[:, k, half * 512:(half + 1) * 512], start=(k == 0), stop=(k == 7)),
                             reads=[("hT", i // 4), "wpg"], writes=[("ps", gbk)])
                    for k in range(2):
                        P.op("pe", lambda e, pbk=pbk, k=k, sl=sl, half=half: e.matmul(PS(pbk)[:, :], lhsT=pT_b[sl][:, k, :], rhs=wpl[:, k, half * 512:(half + 1) * 512], start=(k == 0), stop=(k == 1)),
                             reads=[("pT", sl), "wpl"], writes=[("ps", pbk)])
                    hsl = slice(half * 512, (half + 1) * 512)
                    P.op("act", lambda e, gbk=gbk, sl=sl, hsl=hsl: e.activation(out=sig[sl][:, hsl], in_=PS(gbk)[:, :], func=AF.Sigmoid), reads=[("ps", gbk)], writes=[("sig", sl, half)])
                    P.op("dve", lambda e, pbk=pbk, sl=sl, hsl=hsl: e.tensor_tensor(out=tmpf[sl][:, hsl], in0=sig[sl][:, hsl], in1=PS(pbk)[:, :], op=ALU.mult),
                         reads=[("sig", sl, half), ("ps", pbk)], writes=[("tmpf", sl, half)])
                    P.op("dve", lambda e, sl=sl, hsl=hsl, i=i: e.tensor_tensor(out=tmpf[sl][:, hsl], in0=tmpf[sl][:, hsl], in1=x1[:, i, hsl], op=ALU.add),
                         reads=[("tmpf", sl, half), ("x1", i)], writes=[("tmpf", sl, half)])
                ssc = small[:, 64 + i:65 + i]
                P.op("act", lambda e, sl=sl, ssc=ssc: e.activation(out=junkf, in_=tmpf[sl], func=AF.Square, accum_out=ssc), reads=[("tmpf", sl, 0), ("tmpf", sl, 1)], writes=["junkf", ("fs", i)])
                P.op("dve", lambda e, ssc=ssc: e.tensor_scalar(out=ssc, in0=ssc, scalar1=1.0 / D, scalar2=EPS, op0=ALU.mult, op1=ALU.add), reads=[("fs", i)], writes=[("fs", i)])
                P.op("act", lambda e, ssc=ssc: e.activation(out=ssc, in_=ssc, func=AF.Sqrt), reads=[("fs", i)], writes=[("fs", i)])
                P.op("dve", lambda e, ssc=ssc: e.reciprocal(out=ssc, in_=ssc), reads=[("fs", i)], writes=[("fs", i)])
                P.op("dve", lambda e, sl=sl, ssc=ssc: e.scalar_tensor_tensor(out=outb[sl], in0=tmpf[sl], scalar=ssc, in1=gfin, op0=ALU.mult, op1=ALU.mult),
                     reads=[("tmpf", sl, 0), ("tmpf", sl, 1), ("fs", i), "c_gfin"], writes=[("outb", sl)])
                tok = P.dma("sp", "out%d" % sl, lambda e, sl=sl, i=i: e.dma_start(out=out_d[b, i * 128:(i + 1) * 128, :], in_=outb[sl]), reads=[("outb", sl)])
                last_out_tokens.append(tok)
            phase_barrier()

        finals = {}
        for tok in last_out_tokens:
            finals[tok[1]] = max(finals.get(tok[1], 0), tok[2])
        for k in P.dma_keys:
            if k == "dbg":
                finals[k] = P.dma_count[k]
        P.emit(final_wait_tokens=[("d", k, n) for k, n in finals.items()])
    return nc


def _pk(w, ncols=None):
    K = w.shape[0] // 128
    return np.ascontiguousarray(w.reshape(K, 128, -1).transpose(1, 0, 2).reshape(128, -1))


def _t5_bucket_np(d):
    d = np.maximum(d, 0)
    d_f = np.maximum(d, 1).astype(np.float32)
    large = 16 + (np.log(d_f / np.float32(16)) / np.float32(math.log(128 / 16)) * np.float32(16)).astype(np.int32)
    large = np.minimum(large, 31)
    return np.where(d < 16, d, large)


def prep_weights(inp):
    f = lambda a: np.ascontiguousarray(a, dtype=np.float32)
    w_in = inp["w_in"][0]
    o = {}
    c0 = 0
    sec = {}
    for nm, wdt in (("q_a", 512), ("c_kv", 128), ("q_idx", 512), ("k_idx", 64), ("w_idx", 8), ("qkv_b", 1536), ("gate_a", 1024), ("gate_b", 1024)):
        sec[nm] = w_in[:, c0:c0 + wdt]
        c0 += wdt
    w1 = np.concatenate([sec["q_a"], sec["q_idx"], sec["c_kv"], sec["k_idx"], sec["k_idx"]], axis=1)
    o["w1"] = _pk(w1)
    o["widx"] = _pk(sec["w_idx"])
    o["w3"] = _pk(sec["qkv_b"])
    ga = sec["gate_a"].reshape(1024, 8, 128)
    gb = sec["gate_b"].reshape(1024, 8, 128)
    g4 = np.concatenate([ga, gb], axis=2)
    g4 = g4.reshape(8, 128, 8, 256).transpose(1, 2, 0, 3)
    o["wg4"] = f(g4.reshape(128, -1))
    wa = inp["w_branch_a"][0].reshape(4, 128, 8, 128)
    wb = inp["w_branch_b"][0].reshape(4, 128, 8, 128)
    wbr = np.stack([wa, wb], axis=0).transpose(2, 3, 0, 1, 4)
    o["wbr"] = f(wbr.reshape(128, -1))
    o["wout"] = _pk(inp["w_out"][0])
    wuk = inp["w_uk"][0]
    wukT = wuk.transpose(0, 2, 1).reshape(4, 2, 64, 128).transpose(1, 2, 0, 3)
    o["wuk"] = f(wukT.reshape(128, 512))
    o["wuv"] = f(inp["w_uv"][0].transpose(1, 0, 2).reshape(128, 512))
    wg = inp["w_gate"][0].reshape(N_EXP, 8, 128, 256).transpose(0, 2, 1, 3).reshape(N_EXP, 128, 2048)
    wu = inp["w_up"][0].reshape(N_EXP, 8, 128, 256).transpose(0, 2, 1, 3).reshape(N_EXP, 128, 2048)
    wd = inp["w_down"][0].reshape(N_EXP, 2, 128, 1024).transpose(0, 2, 1, 3).reshape(N_EXP, 128, 2048)
    o["wmoe"] = f(np.concatenate([wg, wu, wd], axis=2))
    wr = np.concatenate([inp["w_r1"][0], inp["w_r2"][0].transpose(1, 0, 2).reshape(1024, 32)], axis=1)
    o["wr"] = _pk(wr)
    br = np.concatenate([inp["b_r1"][0], inp["b_r2"][0].reshape(32)])
    o["br"] = f(np.broadcast_to(br[None, :], (128, 36)))
    o["wpg"] = _pk(inp["w_ple_gate"][0])
    o["wpl"] = _pk(inp["w_ple"][0])
    o["g_attn"] = f(inp["attn_norm"][0].reshape(8, 128).T)
    o["g_ffn"] = f(inp["ffn_norm"][0].reshape(8, 128).T)
    o["g_ple"] = f(inp["ple_norm"][0].reshape(8, 128).T)
    o["g_fin"] = f(np.broadcast_to(inp["final_norm"][None, :], (128, 1024)))
    o["g_kv"] = f(inp["kv_norm"][0].reshape(128, 1))
    rb = inp["rel_bias"]
    s_l = np.arange(128)[:, None]
    u = np.arange(640)[None, :]
    bidx = _t5_bucket_np(u - s_l)
    o["btoep"] = f(rb[bidx].transpose(0, 2, 1).reshape(128, 8 * 640))
    o["b31"] = f(np.broadcast_to(rb[31][None, :], (128, 8)))
    o["ident"] = np.eye(128, dtype=np.float32)
    tt = np.arange(128)[:, None]
    ss = np.arange(128)[None, :]
    o["cneg"] = np.where(ss <= tt, 0.0, NEG).astype(np.float32)
    o["smask"] = (tt < ss).astype(np.float32)
    o["uinc"] = (tt >= ss).astype(np.float32)
    sel = np.zeros((128, 256), np.float32)
    sel[64, 0:128] = 1.0
    sel[63, 128:256] = 1.0
    o["sel"] = sel
    return o


_NC_CACHE = {}


def kernel(**inputs):
    inp = {k: np.asarray(v) for k, v in inputs.items()}
    n = 8
    NB = 2
    wts = prep_weights(inp)
    x = np.ascontiguousarray(inp["x"], dtype=np.float32)
    p = np.ascontiguousarray(inp["p"][0], dtype=np.float32)
    if "nc" not in _NC_CACHE:
        _NC_CACHE["nc"] = build_nc(NB=NB)
    nc = _NC_CACHE["nc"]
    in_maps = []
    for c in range(n):
        m = dict(wts)
        m["x"] = x[c * NB:(c + 1) * NB]
        m["p"] = p[c * NB:(c + 1) * NB]
        in_maps.append(m)
    res = run_bass_kernel_spmd(nc, in_maps, core_ids=list(range(n)))
    out = np.concatenate([r["out"] for r in res.results], axis=0)
    return out.astype(np.float32)
```

```python
import math
import types
import contextlib
import numpy as np
import concourse.bass as bass
import concourse.mybir as mybir
from concourse.bass_utils import run_bass_kernel_spmd

F32 = mybir.dt.float32
BF16 = mybir.dt.bfloat16
AF = mybir.ActivationFunctionType
ALU = mybir.AluOpType
AX = mybir.AxisListType

S = 2048
D = 1024
NT = S // 128
NCH = S // 512
ATTN_SCALE = 64 ** -0.5
IDX_SCALE = (8 ** -0.5) * (64 ** -0.5)
EPS = 1e-6
NEG = -1.0e30
N_BISECT = 12
N_EXP = 32
import os as _os
SBE = _os.environ.get("SBE", "pool")
SBLN = _os.environ.get("SBLN", "1") == "1"


class Prog:
    ENGS = ("pe", "act", "dve", "pool", "sp")

    def __init__(self, nc, same_eng_sync=True):
        self.nc = nc
        self.ops = {e: [] for e in self.ENGS}
        self.last_w = {}
        self.last_r = {}
        self.clock = {e: {} for e in self.ENGS}
        self.opclock = {}
        self.dma_count = {}
        self.dma_keys = []
        self.signaling = set()
        self.same_eng_sync = same_eng_sync
        self._bar = 0

    def _add(self, eng, fn, reads, writes, dma_key=None, n_dma=1):
        idx = len(self.ops[eng]) + 1
        deps = {}

        def need(tok):
            kind, who, n = tok
            if kind == "e" and who == eng:
                if eng in ("pe", "sp") or not self.same_eng_sync:
                    return
            k = (kind, who)
            if deps.get(k, 0) < n:
                deps[k] = n

        for r in reads:
            for tok in self.last_w.get(r, {}).values():
                need(tok)
        for w in writes:
            for tok in self.last_w.get(w, {}).values():
                need(tok)
            for tok in self.last_r.get(w, {}).values():
                need(tok)
        if dma_key is not None:
            if dma_key not in self.dma_count:
                self.dma_count[dma_key] = 0
                self.dma_keys.append(dma_key)
            prev = self.dma_count[dma_key]
            if prev > 0:
                need(("d", dma_key, prev))
            self.dma_count[dma_key] = prev + n_dma
            mytok = ("d", dma_key, prev + n_dma)
        else:
            mytok = ("e", eng, idx)
        clk = self.clock[eng]
        final = []
        for k, n in deps.items():
            if clk.get(k, 0) >= n:
                continue
            final.append((k[0], k[1], n))
        for kind, who, n in final:
            oc = self.opclock.get((kind, who, n))
            if oc:
                for k2, n2 in oc.items():
                    if clk.get(k2, 0) < n2:
                        clk[k2] = n2
            if clk.get((kind, who), 0) < n:
                clk[(kind, who)] = n
            if kind == "e":
                self.signaling.add((who, n))
        snap = dict(clk)
        if mytok[0] == "e":
            snap[("e", eng)] = idx
        self.opclock[mytok] = snap
        self.ops[eng].append((fn, final, dma_key, mytok))
        if fn is not None:
            for r in reads:
                self.last_r.setdefault(r, {})[(mytok[0], mytok[1])] = mytok
        for w in writes:
            self.last_w[w] = {(mytok[0], mytok[1]): mytok}
            self.last_r[w] = {}
        return mytok

    @staticmethod
    def _freeze(fn):
        if fn is None or getattr(fn, "__closure__", None) is None:
            return fn
        cells = []
        for c in fn.__closure__:
            try:
                cells.append(types.CellType(c.cell_contents))
            except ValueError:
                cells.append(c)
        return types.FunctionType(fn.__code__, fn.__globals__, fn.__name__, fn.__defaults__, tuple(cells))

    def op(self, eng, fn, reads=(), writes=()):
        return self._add(eng, self._freeze(fn), tuple(reads), tuple(writes))

    def dma(self, eng, key, fns, reads=(), writes=()):
        if not isinstance(fns, (list, tuple)):
            fns = [fns]
        return self._add(eng, [self._freeze(f) for f in fns], tuple(reads), tuple(writes), dma_key=key, n_dma=len(fns))

    def barrier(self, tiny_fn):
        self._bar += 1
        res = ("__barrier__", self._bar)
        allres = list(set(list(self.last_w.keys()) + list(self.last_r.keys())))
        self._add("dve", self._freeze(tiny_fn), tuple(), tuple(allres) + (res,))
        for e in ("pe", "act", "pool", "sp"):
            self._add(e, None, (res,), tuple())

    def emit(self, final_wait_tokens=()):
        nc = self.nc
        sigval = {}
        for e in self.ENGS:
            s = 0
            for i in range(1, len(self.ops[e]) + 1):
                if (e, i) in self.signaling:
                    s += 1
                    sigval[(e, i)] = s
        engobj = {"pe": nc.tensor, "act": nc.scalar, "dve": nc.vector, "pool": nc.gpsimd, "sp": nc.sync}
        with contextlib.ExitStack() as st:
            esem = {e: st.enter_context(nc.semaphore("sem_" + e)) for e in self.ENGS}
            dsem = {k: st.enter_context(nc.semaphore("dsem_%d" % i)) for i, k in enumerate(self.dma_keys)}
            block = st.enter_context(nc.Block())

            def run(e):
                eng = engobj[e]
                for i, (fn, deps, dma_key, mytok) in enumerate(self.ops[e], start=1):
                    for kind, who, n in deps:
                        if kind == "e":
                            eng.wait_ge(esem[who], sigval[(who, n)])
                        else:
                            eng.wait_ge(dsem[who], 16 * n)
                    if fn is None:
                        assert (e, i) not in self.signaling
                        continue
                    if dma_key is not None:
                        for f in fn:
                            f(eng).then_inc(dsem[dma_key], 16)
                    else:
                        ins = fn(eng)
                        if (e, i) in self.signaling:
                            ins.then_inc(esem[e], 1)
                if e == "sp":
                    for k in self.dma_keys:
                        eng.wait_ge(dsem[k], 16 * self.dma_count[k])

            block.tensor(lambda eng: run("pe"))
            block.scalar(lambda eng: run("act"))
            block.vector(lambda eng: run("dve"))
            block.gpsimd(lambda eng: run("pool"))
            block.sync(lambda eng: run("sp"))


def build_nc(NB=2, stop_after=None, dbg=False):
    nc = bass.Bass("TRN2", target_bir_lowering=False)

    def din(name, shape, dt=F32):
        return nc.dram_tensor(name, list(shape), dt, kind="ExternalInput").ap()

    x_d = din("x", [NB, S, D])
    p_d = din("p", [NB, S, 256])
    w1_d = din("w1", [128, 8 * 1280])
    widx_d = din("widx", [128, 8 * 8])
    w3_d = din("w3", [128, 8 * 1536])
    wg4_d = din("wg4", [128, 8 * 8 * 256])
    wbr_d = din("wbr", [128, 8 * 1024])
    wout_d = din("wout", [128, 8 * 1024])
    wuk_d = din("wuk", [128, 512])
    wuv_d = din("wuv", [128, 512])
    wmoe_d = din("wmoe", [N_EXP, 128, 6144])
    wr_d = din("wr", [128, 8 * 36])
    br_d = din("br", [128, 36])
    wpg_d = din("wpg", [128, 8 * 1024])
    wpl_d = din("wpl", [128, 2 * 1024])
    gat_d = din("g_attn", [128, 8])
    gff_d = din("g_ffn", [128, 8])
    gpl_d = din("g_ple", [128, 8])
    gfin_d = din("g_fin", [128, 1024])
    gkv_d = din("g_kv", [128, 1])
    btoep_d = din("btoep", [128, 8 * 640])
    b31_d = din("b31", [128, 8])
    ident_d = din("ident", [128, 128])
    cneg_d = din("cneg", [128, 128])
    smask_d = din("smask", [128, 128])
    uinc_d = din("uinc", [128, 128])
    sel_d = din("sel", [128, 256])
    out_d = nc.dram_tensor("out", [NB, S, D], F32, kind="ExternalOutput").ap()
    dbg_d = {}
    if dbg:
        for nm, shp in (("d_oaT", [128, 4 * S]), ("d_obT", [128, 4 * S]), ("d_x1", [128, NT * D])):
            dbg_d[nm] = nc.dram_tensor(nm, shp, F32, kind="ExternalOutput").ap()

    st = contextlib.ExitStack()
    with st:
        def sb(name, shape, dt):
            return st.enter_context(nc.sbuf_tensor("s_" + name, list(shape), dt))

        arA = sb("arA", [128, 16384], BF16)
        arB = sb("arB", [128, 32768], BF16)
        arC = sb("arC", [128, 16384], BF16)
        arD = sb("arD", [128, 33792], BF16)
        ident_f = sb("ident_f", [128, 128], F32)
        ident_b = sb("ident_b", [128, 128], BF16)
        cneg = sb("cneg", [128, 128], F32)
        smask = sb("smask", [128, 128], BF16)
        uinc = sb("uinc", [128, 128], BF16)
        ones_b = sb("ones_b", [128, 128], BF16)
        sel_f = sb("sel_f", [128, 256], F32)
        gB1 = sb("gB", [128, 8, 128], BF16)
        gB = [gB1, gB1, gB1]
        gpk = sb("gpk", [128, 24], F32)
        gkv = sb("gkv", [128, 1], F32)
        b31 = sb("b31", [128, 8], F32)
        brb = sb("brb", [128, 36], F32)
        wr_f = sb("wr_f", [128, 8, 36], F32)
        wuk = sb("wuk", [128, 4, 128], BF16)
        wuv = sb("wuv", [128, 512], BF16)
        widx_w = sb("widx_w", [128, 8, 8], BF16)
        small = sb("small", [128, 256], F32)
        tiny = sb("tiny", [128, 2], F32)

        psb = [st.enter_context(nc.psum_tensor("ps%d" % i, [128, 512], F32)) for i in range(8)]

        P = Prog(nc)

        def carve(ar, off_bytes, nbytes, dt, pattern=None, **kw):
            e0 = off_bytes // 2
            ap = ar[:, e0:e0 + nbytes // 2]
            if dt == F32:
                ap = ap.bitcast(F32)
            if pattern:
                ap = ap.rearrange(pattern, **kw)
            return ap

        KB = 1024
        hT = carve(arA, 0, 32 * KB, BF16, "p (k t) -> p k t", k=8)
        qaT = carve(arB, 0, 16 * KB, BF16, "p (k t) -> p k t", k=4)
        qiT = carve(arB, 16 * KB, 16 * KB, BF16, "p (k t) -> p k t", k=4)
        ckvT = carve(arB, 32 * KB, 4 * KB, BF16)
        kiT = carve(arB, 36 * KB, 4 * KB, BF16)
        Vp = carve(arB, 40 * KB, 16 * 4 * 130 * 2, BF16, "p (j m c) -> p j m c", j=16, m=4)
        widx_tm = carve(arB, 40 * KB + 16640, 512, F32, "p (i h) -> p i h", i=16)
        qbT = carve(arB, 0, 16 * KB, BF16, "p (k t) -> p k t", k=4)
        kbT = carve(arB, 16 * KB, 16 * KB, BF16, "p (k t) -> p k t", k=4)
        vb = carve(arB, 32 * KB, 16 * KB, BF16, "p (j c) -> p j c", j=16)
        qbTn = carve(arB, 48 * KB, 16 * KB, BF16, "p (k t) -> p k t", k=4)
        x1 = carve(arB, 0, 64 * KB, F32, "p (i d) -> p i d", i=16)
        oaT = carve(arC, 0, 16 * KB, BF16, "p (k t) -> p k t", k=4)
        obT = carve(arC, 16 * KB, 16 * KB, BF16, "p (k t) -> p k t", k=4)
        wmoe = [carve(arC, i * 12 * KB, 12 * KB, BF16) for i in range(2)]
        wpg = carve(arC, 0, 16 * KB, BF16, "p (k c) -> p k c", k=8)
        wpl = carve(arC, 16 * KB, 4 * KB, BF16, "p (k c) -> p k c", k=2)
        def dD(off, nbytes, dt, pattern=None, **kw):
            assert off + nbytes <= 66 * KB, (off, nbytes)
            return carve(arD, off, nbytes, dt, pattern, **kw)

        PS = lambda i: psb[i]

        def psbf(i):
            return psb[i][:].bitcast(BF16)

        def ld(key, dst, src, eng="sp", res=None, **kw):
            P.dma(eng, key, lambda e: e.dma_start(out=dst, in_=src, **kw), writes=[res or key])

        ld("c_ident", ident_f[:], ident_d[:, :])
        ld("c_cneg", cneg[:], cneg_d[:, :])
        ld("c_sel", sel_f[:], sel_d[:, :])
        ld("c_gkv", gkv[:], gkv_d[:, :])
        ld("c_b31", b31[:], b31_d[:, :])
        ld("c_br", brb[:], br_d[:, :])
        ld("c_wr", wr_f[:].rearrange("p k c -> p (k c)"), wr_d[:, :])
        ld("c_g0", gpk[:, 0:8], gat_d[:, :])
        ld("c_g1", gpk[:, 8:16], gff_d[:, :])
        ld("c_g2", gpk[:, 16:24], gpl_d[:, :])
        ld("c_smask", smask[:], smask_d[:, :], eng="pool")
        ld("c_uinc", uinc[:], uinc_d[:, :], eng="pool")
        ld("c_wuk", wuk[:].rearrange("p k c -> p (k c)"), wuk_d[:, :], eng="pool")
        ld("c_wuv", wuv[:], wuv_d[:, :], eng="pool")
        ld("c_widx", widx_w[:].rearrange("p k c -> p (k c)"), widx_d[:, :], eng="pool")
        P.op("dve", lambda e: e.tensor_copy(out=ident_b[:], in_=ident_f[:]), reads=["c_ident"], writes=["ident_b"])
        P.op("dve", lambda e: e.memset(ones_b[:], 1.0), writes=["ones_b"])
        P.op("dve", lambda e: e.memset(tiny[:, 0:1], 0.0), writes=["tiny"])
        P.op("dve", lambda e: e.memset(tiny[:, 1:2], -0.5), writes=["mhalf"])
        def build_gB(gi):
            for k in range(8):
                P.op("dve", lambda e, gi=gi, k=k: e.tensor_scalar(out=gB1[:, k, :], in0=ones_b[:], scalar1=gpk[:, gi * 8 + k:gi * 8 + k + 1],
                                                                  scalar2=None, op0=ALU.mult),
                     reads=["ones_b", "c_g%d" % gi], writes=["gB"])

        evac_rr = [0]

        def evac(out, in_, reads, writes, scale=None, eng=None):
            if eng is None:
                eng = ("act", "dve")[evac_rr[0] % 2]
                evac_rr[0] += 1
            if eng == "act":
                if scale is None:
                    P.op("act", lambda e: e.activation(out=out, in_=in_, func=AF.Copy), reads=reads, writes=writes)
                else:
                    P.op("act", lambda e: e.activation(out=out, in_=in_, func=AF.Copy, scale=float(scale)), reads=reads, writes=writes)
            else:
                if scale is None:
                    P.op("dve", lambda e: e.tensor_copy(out=out, in_=in_), reads=reads, writes=writes)
                else:
                    P.op("dve", lambda e: e.tensor_scalar(out=out, in0=in_, scalar1=float(scale), scalar2=None, op0=ALU.mult), reads=reads, writes=writes)

        def phase_barrier():
            P.barrier(lambda e: e.memset(tiny[:, 0:1], 0.0))

        def norm_transpose(b, src, gi, dstT, dstres, xt_bufs, xn_bufs, ps_banks, f32_side=None):
            for i in range(NT):
                sl = i % 2
                if src == "x":
                    xt = xt_bufs[sl]
                    xres = "xt%d" % sl
                    P.dma("sp", xres, lambda e, xt=xt, i=i: e.dma_start(out=xt, in_=x_d[b, i * 128:(i + 1) * 128, :]), writes=[xres])
                else:
                    xt = x1[:, i, :]
                    xres = ("x1", i)
                xn = xn_bufs[sl]
                xnres = "xn%d" % sl
                ssc = small[:, i:i + 1]
                rsc = small[:, 16 + i:17 + i]
                P.op("act", lambda e, xt=xt, xn=xn, ssc=ssc: e.activation(out=xn, in_=xt, func=AF.Square, accum_out=ssc),
                     reads=[xres], writes=[xnres, ("ss", i)])
                P.op("dve", lambda e, ssc=ssc, rsc=rsc: e.tensor_scalar(out=rsc, in0=ssc, scalar1=1.0 / D, scalar2=EPS, op0=ALU.mult, op1=ALU.add),
                     reads=[("ss", i)], writes=[("rs", i)])
                P.op("act", lambda e, rsc=rsc: e.activation(out=rsc, in_=rsc, func=AF.Sqrt), reads=[("rs", i)], writes=[("rs", i)])
                P.op("dve", lambda e, rsc=rsc: e.reciprocal(out=rsc, in_=rsc), reads=[("rs", i)], writes=[("rs", i)])
                if f32_side is None:
                    P.op("dve", lambda e, xt=xt, xn=xn, rsc=rsc: e.tensor_scalar(out=xn, in0=xt, scalar1=rsc, scalar2=None, op0=ALU.mult),
                         reads=[xres, ("rs", i)], writes=[xnres])
                    pb = ps_banks[i % len(ps_banks)]
                    pres = ("ps", pb)
                    pv = psbf(pb)[:, 0:1024].rearrange("p (k t) -> p k t", k=8)
                    for k in range(8):
                        P.op("pe", lambda e, pv=pv, xn=xn, k=k: e.transpose(out=pv[:, k, :], in_=xn[:, k * 128:(k + 1) * 128], identity=ident_b[:]),
                             reads=[xnres, "ident_b"], writes=[pres])
                    P.op("dve", lambda e, pv=pv, i=i: e.tensor_tensor(out=dstT[:, :, i * 128:(i + 1) * 128], in0=pv, in1=gB[gi][:], op=ALU.mult),
                         reads=[pres, "gB"], writes=[(dstres, i // 4)])
                else:
                    f32_side(i, xt, xres, rsc, xn, xnres)

        last_out_tokens = []
        for b in range(NB):
            xt_bufs = [dD(0, 4 * KB, F32), dD(4 * KB, 4 * KB, F32)]
            xn_bufs = [dD(8 * KB, 2 * KB, BF16), dD(10 * KB, 2 * KB, BF16)]
            w1 = dD(12 * KB, 20 * KB, BF16, "p (k c) -> p k c", k=8)
            ckv_raw = dD(32 * KB, 8 * KB, F32)
            sqb = [dD(40 * KB, 1 * KB, BF16), dD(41 * KB, 1 * KB, BF16)]
            rstd_b = [dD(42 * KB, 2 * KB, F32), dD(44 * KB, 2 * KB, F32)]
            P.dma("pool", "w1", lambda e: e.dma_start(out=w1.rearrange("p k c -> p (k c)"), in_=w1_d[:, :], max_dma_last_dim=8192), writes=["w1"])
            build_gB(0)
            norm_transpose(b, "x", 0, hT, "hT", xt_bufs, xn_bufs, [6, 7])

            if stop_after == "P0":
                break
            bank_rr = [0]

            def nb(banks):
                v = banks[bank_rr[0] % len(banks)]
                bank_rr[0] += 1
                return v

            def proj_T(w, wres, cc, c, banks):
                pb = nb(banks)
                for k in range(8):
                    P.op("pe", lambda e, pb=pb, k=k: e.matmul(PS(pb)[:, :], lhsT=w[:, k, cc * 128:(cc + 1) * 128], rhs=hT[:, k, c * 512:(c + 1) * 512],
                                                             start=(k == 0), stop=(k == 7)),
                         reads=[wres, ("hT", c)], writes=[("ps", pb)])
                return pb

            for cc in range(10):
                for c in range(NCH):
                    pb = proj_T(w1, "w1", cc, c, [0, 1, 2, 3])
                    cs = slice(c * 512, (c + 1) * 512)
                    if cc < 4:
                        evac(qaT[:, cc, cs], PS(pb)[:, :], [("ps", pb)], [("qaT", c)])
                    elif cc < 8:
                        evac(qiT[:, cc - 4, cs], PS(pb)[:, :], [("ps", pb)], [("qiT", c)])
                    elif cc == 8:
                        evac(ckv_raw[:, cs], PS(pb)[:, :], [("ps", pb)], [("ckv_raw", c)])
                    else:
                        evac(kiT[:, cs], PS(pb)[:, :], [("ps", pb)], [("kiT", c)])
            for c in range(NCH):
                cs = slice(c * 512, (c + 1) * 512)
                sq = sqb[c % 2]
                rb = rstd_b[c % 2]
                P.op("act", lambda e, sq=sq, cs=cs: e.activation(out=sq, in_=ckv_raw[:, cs], func=AF.Square), reads=[("ckv_raw", c)], writes=[("sq", c % 2)])
                pb = nb([0, 1, 2, 3])
                P.op("pe", lambda e, pb=pb, sq=sq: e.matmul(PS(pb)[:, :], lhsT=ones_b[:], rhs=sq, start=True, stop=True),
                     reads=[("sq", c % 2), "ones_b"], writes=[("ps", pb)])
                P.op("dve", lambda e, pb=pb, rb=rb: e.tensor_scalar(out=rb, in0=PS(pb)[:, :], scalar1=1.0 / 128, scalar2=EPS, op0=ALU.mult, op1=ALU.add),
                     reads=[("ps", pb)], writes=[("rb", c % 2)])
                P.op("act", lambda e, rb=rb: e.activation(out=rb, in_=rb, func=AF.Sqrt), reads=[("rb", c % 2)], writes=[("rb", c % 2)])
                P.op("dve", lambda e, rb=rb: e.reciprocal(out=rb, in_=rb), reads=[("rb", c % 2)], writes=[("rb", c % 2)])
                P.op("dve", lambda e, rb=rb, cs=cs: e.scalar_tensor_tensor(out=ckvT[:, cs], in0=ckv_raw[:, cs], scalar=gkv[:, 0:1], in1=rb, op0=ALU.mult, op1=ALU.mult),
                     reads=[("ckv_raw", c), ("rb", c % 2), "c_gkv"], writes=[("ckvT", c)])
            pbw = 4
            for i in range(NT):
                for k in range(8):
                    P.op("pe", lambda e, i=i, k=k: e.matmul(PS(pbw)[:, i * 8:(i + 1) * 8], lhsT=hT[:, k, i * 128:(i + 1) * 128], rhs=widx_w[:, k, :],
                                                         start=(k == 0), stop=(k == 7)),
                         reads=[("hT", i // 4), "c_widx"], writes=[("ps", pbw)])
            P.op("dve", lambda e: e.tensor_scalar(out=widx_tm.rearrange("p i h -> p (i h)"), in0=PS(pbw)[:, 0:128], scalar1=IDX_SCALE, scalar2=None, op0=ALU.mult),
                 reads=[("ps", pbw)], writes=["widx_tm"])
            P.op("pool", lambda e: e.memset(Vp.rearrange("p j m c -> p (j m) c")[:, :, 64:65], 1.0), writes=["Vp"])
            P.op("pool", lambda e: e.memset(Vp.rearrange("p j m c -> p (j m) c")[:, :, 129:130], 1.0), writes=["Vp"])
            for j in range(NT):
                pb = nb([0, 1, 2, 3])
                P.op("pe", lambda e, pb=pb, j=j: e.matmul(PS(pb)[:, :], lhsT=ckvT[:, j * 128:(j + 1) * 128], rhs=wuv[:], start=True, stop=True),
                     reads=[("ckvT", j // 4), "c_wuv"], writes=[("ps", pb)])
                pv = PS(pb)[:, :].rearrange("p (m h d) -> p m h d", m=4, h=2)
                evac(Vp[:, j, :, 0:64], pv[:, :, 0, :], [("ps", pb)], ["Vp"])
                evac(Vp[:, j, :, 65:129], pv[:, :, 1, :], [("ps", pb)], ["Vp"])
            phase_barrier()
            if stop_after == "P1":
                break

            NEGM = 30000.0
            score = [dD(0, 8 * KB, F32), dD(8 * KB, 8 * KB, F32), carve(arC, 16 * KB, 8 * KB, F32), carve(arC, 24 * KB, 8 * KB, F32)]
            junk = dD(16 * KB, 4 * KB, BF16)
            mask_tm = dD(20 * KB, 4 * KB, BF16)
            maskT = dD(24 * KB, 16 * KB, BF16, "p (j t) -> p j t", j=16)
            B8 = dD(40 * KB, 10 * KB, BF16, "p (h u) -> p h u", h=8)
            rbuf = [dD(50 * KB + i * KB, 1 * KB, BF16) for i in range(4)]
            Pbuf = [dD(54 * KB + i * KB, 1 * KB, BF16) for i in range(4)]
            qabs = [dD(58 * KB + i * KB, 1 * KB, BF16) for i in range(2)]
            dg = dD(60 * KB, 2 * KB, BF16, "p (h t) -> p h t", h=8)
            o_f32 = dD(62 * KB, 2 * KB, F32)
            rec = dD(64 * KB, 2 * KB, F32)
            btmp = dD(0, 10 * KB, F32, "p (h u) -> p h u", h=4)
            for hh in range(2):
                P.dma("sp", "btoep", lambda e, hh=hh: e.dma_start(out=btmp.rearrange("p h u -> p (h u)")[:, 0:2560], in_=btoep_d[:, hh * 2560:(hh + 1) * 2560]), writes=["btmp"])
                P.op("dve", lambda e, hh=hh: e.tensor_scalar(out=B8[:, hh * 4:hh * 4 + 4, :], in0=btmp[:, 0:4, :], scalar1=1.0 / ATTN_SCALE, scalar2=None, op0=ALU.mult),
                     reads=["btmp"], writes=["B8"])
            phase_barrier()

            LO, WD, MID, CNT, TMP = 40, 44, 48, 52, 56
            rr = [0]
            smr = lambda base, a0, a1: small[:, base + a0:base + a1]

            def scores_chunk(c):
                units = []
                for tl in range(4):
                    L = (4 * c + tl + 1) * 128
                    nsc = (L + 511) // 512
                    for sc in range(nsc):
                        for h in range(8):
                            units.append((tl, sc, h, nsc))
                stt = {}

                def A(u):
                    tl, sc, h, nsc = u
                    i = 4 * c + tl
                    L = (i + 1) * 128
                    ws = min(512, L - sc * 512)
                    bp = (h % 2) * 64
                    zb = nb([0, 1, 2, 3])
                    P.op("pe", lambda e: e.matmul(PS(zb)[:, 0:ws], lhsT=qiT[bp:bp + 64, h // 2, i * 128:(i + 1) * 128], rhs=kiT[bp:bp + 64, sc * 512:sc * 512 + ws], start=True, stop=True),
                         reads=[("qiT", c), ("kiT", sc)], writes=[("ps", zb)])
                    rs = rr[0] % 4
                    rr[0] += 1
                    if rs != 3:
                        P.op("act", lambda e: e.activation(out=rbuf[rs][:, 0:ws], in_=PS(zb)[:, 0:ws], func=AF.Relu), reads=[("ps", zb)], writes=[("rbuf", rs)])
                    else:
                        P.op("dve", lambda e: e.tensor_scalar(out=rbuf[rs][:, 0:ws], in0=PS(zb)[:, 0:ws], scalar1=0.0, scalar2=None, op0=ALU.max), reads=[("ps", zb)], writes=[("rbuf", rs)])
                    stt[u] = (rs, ws)

                def B(u):
                    tl, sc, h, nsc = u
                    i = 4 * c + tl
                    rs, ws = stt[u]
                    sct = score[tl]
                    spb = 4 + (sc % 2)
                    if sc == 0 and h == 0:
                        for hh in range(8):
                            P.op("dve", lambda e, hh=hh: e.tensor_scalar(out=dg[:, hh, :], in0=ident_b[:], scalar1=widx_tm[:, i, hh:hh + 1], scalar2=None, op0=ALU.mult),
                                 reads=["ident_b", "widx_tm"], writes=[("dg", hh)])
                    P.op("pe", lambda e: e.matmul(PS(spb)[:, 0:ws], lhsT=dg[:, h, :], rhs=rbuf[rs][:, 0:ws], start=(h == 0), stop=(h == 7)),
                         reads=[("dg", h), ("rbuf", rs)], writes=[("ps", spb)])
                    if h == 7:
                        last = (sc == nsc - 1)
                        wcopy = ws - 128 if last else ws
                        if wcopy > 0:
                            P.op("act", lambda e: e.activation(out=sct[:, sc * 512:sc * 512 + wcopy], in_=PS(spb)[:, 0:wcopy], func=AF.Copy),
                                 reads=[("ps", spb)], writes=[("score", tl)])
                        if last:
                            P.op("dve", lambda e: e.tensor_tensor(out=sct[:, sc * 512 + ws - 128:sc * 512 + ws], in0=PS(spb)[:, ws - 128:ws], in1=cneg[:], op=ALU.add),
                                 reads=[("ps", spb), "c_cneg"], writes=[("score", tl)])

                nu = len(units)
                for k in range(nu + 2):
                    if k < nu:
                        A(units[k])
                    if 0 <= k - 2 < nu:
                        B(units[k - 2])

            def bisect_chunk(c):
                act = [tl for tl in range(4) if 4 * c + tl >= 2]
                for tl in range(4):
                    i = 4 * c + tl
                    L = (i + 1) * 128
                    if i < 2:
                        P.op("dve", lambda e, tl=tl: e.memset(smr(LO, tl, tl + 1), -1.0e29), writes=[("lo", tl)])
                    else:
                        P.op("dve", lambda e, tl=tl, i=i: e.tensor_reduce(out=smr(LO, tl, tl + 1), in_=score[tl][:, 0:i * 128], axis=AX.X, op=ALU.min), reads=[("score", tl)], writes=[("lo", tl)])
                        P.op("dve", lambda e, tl=tl, L=L: e.tensor_reduce(out=smr(WD, tl, tl + 1), in_=score[tl][:, 0:L], axis=AX.X, op=ALU.max), reads=[("score", tl)], writes=[("wd", tl)])
                if not act:
                    return
                a0, a1 = act[0], act[-1] + 1
                R = lambda nm: [(nm, t) for t in range(a0, a1)]
                P.op("dve", lambda e: e.tensor_tensor(out=smr(WD, a0, a1), in0=smr(WD, a0, a1), in1=smr(LO, a0, a1), op=ALU.subtract), reads=R("wd") + R("lo"), writes=R("wd"))
                for it in range(N_BISECT):
                    f = 0.5 ** (it + 1)
                    P.op("dve", lambda e, f=f: e.scalar_tensor_tensor(out=smr(MID, a0, a1), in0=smr(WD, a0, a1), scalar=f, in1=smr(LO, a0, a1), op0=ALU.mult, op1=ALU.add),
                         reads=R("wd") + R("lo"), writes=R("mid"))
                    for tl in act:
                        L = (4 * c + tl + 1) * 128
                        jb, jres = ((junk, "junk"), (mask_tm, "mask_tm"))[tl % 2]
                        P.op("dve", lambda e, tl=tl, L=L, jb=jb: e.tensor_scalar(out=jb[:, 0:L], in0=score[tl][:, 0:L], scalar1=smr(MID, tl, tl + 1), scalar2=None, op0=ALU.is_ge, op1=ALU.add,
                                                                         accum_out=smr(CNT, tl, tl + 1)),
                             reads=[("score", tl), ("mid", tl)], writes=[("cnt", tl), jres])
                    P.op("dve", lambda e, f=f: e.tensor_scalar(out=smr(TMP, a0, a1), in0=smr(CNT, a0, a1), scalar1=255.5, scalar2=f, op0=ALU.is_ge, op1=ALU.mult),
                         reads=R("cnt"), writes=["tmp4"])
                    P.op("dve", lambda e: e.tensor_tensor(out=smr(TMP, a0, a1), in0=smr(TMP, a0, a1), in1=smr(WD, a0, a1), op=ALU.mult), reads=["tmp4"] + R("wd"), writes=["tmp4"])
                    P.op("dve", lambda e: e.tensor_tensor(out=smr(LO, a0, a1), in0=smr(LO, a0, a1), in1=smr(TMP, a0, a1), op=ALU.add), reads=["tmp4"] + R("lo"), writes=R("lo"))

            def maskgen_chunk(c):
                for tl in range(4):
                    i = 4 * c + tl
                    L = (i + 1) * 128
                    P.op("dve", lambda e, tl=tl, L=L: e.tensor_scalar(out=mask_tm[:, 0:L], in0=score[tl][:, 0:L], scalar1=smr(LO, tl, tl + 1), scalar2=-NEGM, op0=ALU.is_lt, op1=ALU.mult),
                         reads=[("score", tl), ("lo", tl)], writes=["mask_tm"])
                    for j0 in range(0, i + 1, 8):
                        n = min(8, i + 1 - j0)
                        tb = 6 + ((j0 // 8) % 2)
                        pv = psbf(tb)[:, 0:1024].rearrange("p (k t) -> p k t", k=8)
                        for jj in range(n):
                            j = j0 + jj
                            P.op("pe", lambda e, pv=pv, jj=jj, j=j: e.transpose(out=pv[:, jj, :], in_=mask_tm[:, j * 128:(j + 1) * 128], identity=ident_b[:]),
                                 reads=["mask_tm", "ident_b"], writes=[("ps", tb)])
                        evac(maskT[:, j0:j0 + n, tl * 128:(tl + 1) * 128], pv[:, 0:n, :], [("ps", tb)], [("maskT", tl)])

            def attention_chunk(c):
                cs0 = c * 512
                jmax = 4 * c + 3

                def emit_qabs(h):
                    bp = (h % 2) * 64
                    qb_ = nb([0, 1, 2, 3])
                    P.op("pe", lambda e, qb_=qb_, bp=bp, h=h: e.matmul(PS(qb_)[:, :], lhsT=wuk[bp:bp + 64, h // 2, :], rhs=qaT[bp:bp + 64, h // 2, cs0:cs0 + 512], start=True, stop=True),
                         reads=["c_wuk", ("qaT", c)], writes=[("ps", qb_)])
                    evac(qabs[h % 2], PS(qb_)[:, :], [("ps", qb_)], [("qabs", h % 2)], eng="act")

                def emit_tail(h):
                    bp = (h % 2) * 64
                    ob = 4 + (h % 2)
                    dbk = 6 + (h % 2)
                    P.op("act", lambda e: e.activation(out=o_f32[bp:bp + 64, :], in_=PS(ob)[bp:bp + 64, :], func=AF.Copy), reads=[("ps", ob)], writes=[("o_f32", h % 2)])
                    P.op("act", lambda e: e.activation(out=rec[bp:bp + 64, :], in_=PS(dbk)[bp:bp + 64, :], func=AF.Ln), reads=[("ps", dbk)], writes=[("rec", h % 2)])
                    P.op("act", lambda e: e.activation(out=rec[bp:bp + 64, :], in_=rec[bp:bp + 64, :], func=AF.Exp, scale=-1.0), reads=[("rec", h % 2)], writes=[("rec", h % 2)])
                    P.op("pool", lambda e: e.tensor_tensor(out=oaT[bp:bp + 64, h // 2, cs0:cs0 + 512], in0=o_f32[bp:bp + 64, :], in1=rec[bp:bp + 64, :], op=ALU.mult),
                         reads=[("o_f32", h % 2), ("rec", h % 2)], writes=[("oaT", c)])

                emit_qabs(0)
                pending_tail = None
                for h in range(8):
                    m = h // 2
                    qs = h % 2
                    ob = 4 + (h % 2)
                    if h < 7:
                        emit_qabs(h + 1)
                    stt = {}

                    def A(j):
                        col0 = max(0, j - 4 * c) * 128
                        N = 512 - col0
                        near = j >= 4 * c - 1
                        lb = nb([0, 1, 2, 3])
                        P.op("pe", lambda e, lb=lb, j=j, col0=col0, N=N: e.matmul(PS(lb)[:, 0:N], lhsT=ckvT[:, j * 128:(j + 1) * 128], rhs=qabs[qs][:, col0:512], start=True, stop=False),
                             reads=[("ckvT", j // 4), ("qabs", qs)], writes=[("ps", lb)])
                        P.op("pe", lambda e, lb=lb, j=j, col0=col0, N=N, near=near: e.matmul(PS(lb)[:, 0:N], lhsT=ident_b[:], rhs=maskT[:, j, col0:512], start=False, stop=(not near)),
                             reads=["ident_b"] + [("maskT", t) for t in range(col0 // 128, 4)], writes=[("ps", lb)])
                        if near:
                            u0 = cs0 + col0 - 128 * j
                            P.op("pe", lambda e, lb=lb, u0=u0, N=N: e.matmul(PS(lb)[:, 0:N], lhsT=ident_b[:], rhs=B8[:, h, u0:u0 + N], start=False, stop=True),
                                 reads=["ident_b", "B8"], writes=[("ps", lb)])
                        pi = rr[0] % 4
                        rr[0] += 1
                        Pt = Pbuf[pi]
                        if near:
                            P.op("act", lambda e, lb=lb, Pt=Pt, N=N: e.activation(out=Pt[:, 0:N], in_=PS(lb)[:, 0:N], func=AF.Exp, scale=ATTN_SCALE),
                                 reads=[("ps", lb)], writes=[("Pbuf", pi)])
                        else:
                            P.op("act", lambda e, lb=lb, Pt=Pt, N=N: e.activation(out=Pt[:, 0:N], in_=PS(lb)[:, 0:N], func=AF.Exp, scale=ATTN_SCALE, bias=b31[:, h:h + 1]),
                                 reads=[("ps", lb), "c_b31"], writes=[("Pbuf", pi)])
                        stt[j] = (pi, col0, N)

                    def B(j):
                        pi, col0, N = stt[j]
                        w0 = 0 if h % 2 == 0 else 1
                        P.op("pe", lambda e, j=j, w0=w0, pi=pi, col0=col0, N=N: e.matmul(PS(ob)[:, col0:512], lhsT=Vp[:, j, m, w0:w0 + 128], rhs=Pbuf[pi][:, 0:N],
                                                                                  start=(j == 0), stop=(j == jmax)),
                             reads=["Vp", ("Pbuf", pi)], writes=[("ps", ob)])
                        dbk = 6 + (h % 2)
                        P.op("pe", lambda e, j=j, pi=pi, col0=col0, N=N: e.matmul(PS(dbk)[:, col0:512], lhsT=ones_b[:], rhs=Pbuf[pi][:, 0:N], start=(j == 0), stop=(j == jmax)),
                             reads=["ones_b", ("Pbuf", pi)], writes=[("ps", dbk)])

                    for k in range(jmax + 3):
                        if k <= jmax:
                            A(k)
                        if 0 <= k - 2 <= jmax:
                            B(k - 2)
                        if k == 2 and pending_tail is not None:
                            emit_tail(pending_tail)
                            pending_tail = None
                    pending_tail = h
                emit_tail(pending_tail)

            scores_chunk(0)
            bisect_chunk(0)
            maskgen_chunk(0)
            for c in range(NCH):
                if c + 1 < NCH:
                    scores_chunk(c + 1)
                    bisect_chunk(c + 1)
                attention_chunk(c)
                if c + 1 < NCH:
                    maskgen_chunk(c + 1)
            phase_barrier()
            if stop_after == "P2":
                break

            w3 = dD(0, 24 * KB, BF16, "p (k c) -> p k c", k=8)
            ebuf = [dD(24 * KB + i * 2 * KB, 2 * KB, F32) for i in range(4)]
            spbuf = [dD(32 * KB + i * KB, 1 * KB, BF16) for i in range(6)]
            tbuf = [dD(38 * KB + i * 2 * KB, 2 * KB, F32) for i in range(3)]
            abuf = [dD(44 * KB + i * KB, 1 * KB, BF16) for i in range(4)]
            sbrr = [0, 0, 0, 0]
            P.dma("pool", "w3", lambda e: e.dma_start(out=w3.rearrange("p k c -> p (k c)"), in_=w3_d[:, :], max_dma_last_dim=8192), writes=["w3"])
            if stop_after == "P3w":
                break
            for cc in range(8):
                for c in range(NCH):
                    pb = proj_T(w3, "w3", cc, c, [0, 1, 2, 3])
                    cs = slice(c * 512, (c + 1) * 512)
                    if cc < 4:
                        evac(qbT[:, cc, cs], PS(pb)[:, :], [("ps", pb)], [("qbT", c)])
                    else:
                        evac(kbT[:, cc - 4, cs], PS(pb)[:, :], [("ps", pb)], [("kbT", c)])
            if stop_after == "P3q":
                break
            for j in range(NT):
                pb = nb([0, 1, 2, 3])
                for k in range(8):
                    P.op("pe", lambda e, pb=pb, j=j, k=k: e.matmul(PS(pb)[:, :], lhsT=hT[:, k, j * 128:(j + 1) * 128], rhs=w3[:, k, 1024:1536], start=(k == 0), stop=(k == 7)),
                         reads=["w3", ("hT", j // 4)], writes=[("ps", pb)])
                evac(vb[:, j, :], PS(pb)[:, :], [("ps", pb)], [("vb", j // 4)])
            if stop_after == "P3a":
                break
            for c in range(NCH):
                cs0 = c * 512
                jmax = 4 * c + 3
                for hp in range(4):
                    m = hp
                    units = [(j, hh) for j in range(jmax, -1, -1) for hh in range(2)]
                    stt = {}
                    prev_sp = {0: None, 1: None}

                    def S1(u):
                        j, hh = u
                        bp = hh * 64
                        col0 = max(0, j - 4 * c) * 128
                        N = 512 - col0
                        zb = nb([0, 1, 2, 5])
                        P.op("pe", lambda e, zb=zb, bp=bp, j=j, col0=col0, N=N: e.matmul(PS(zb)[:, 0:N], lhsT=kbT[bp:bp + 64, m, j * 128:(j + 1) * 128],
                                                                                    rhs=qbT[bp:bp + 64, m, cs0 + col0:cs0 + 512], start=True, stop=True),
                             reads=[("kbT", j // 4), ("qbT", c)], writes=[("ps", zb)])
                        ei = sbrr[0] % 4
                        si = sbrr[1] % 6
                        sbrr[0] += 1
                        sbrr[1] += 1
                        eb_, spt = ebuf[ei], spbuf[si]
                        P.op("act", lambda e, zb=zb, eb_=eb_, N=N: e.activation(out=eb_[:, 0:N], in_=PS(zb)[:, 0:N], func=AF.Exp, scale=ATTN_SCALE),
                             reads=[("ps", zb)], writes=[("ebuf", ei)])
                        if j >= 4 * c:
                            P.op("dve", lambda e, eb_=eb_: e.tensor_tensor(out=eb_[:, 0:128], in0=eb_[:, 0:128], in1=smask[:], op=ALU.mult),
                                 reads=[("ebuf", ei), "c_smask"], writes=[("ebuf", ei)])
                        P.op("act", lambda e, eb_=eb_, spt=spt, N=N: e.activation(out=spt[:, 0:N], in_=eb_[:, 0:N], func=AF.Ln, bias=1.0, scale=1.0),
                             reads=[("ebuf", ei)], writes=[("spbuf", si)])
                        stt[u] = dict(ei=ei, si=si, col0=col0, N=N)

                    def S2(u):
                        j, hh = u
                        d = stt[u]
                        xb = 3 + hh
                        col0, N = d["col0"], d["N"]
                        spt = spbuf[d["si"]]
                        pv = prev_sp[hh]
                        if pv is not None:
                            psi, pcol0, pN = pv
                            P.op("pe", lambda e, xb=xb, psi=psi, pcol0=pcol0, pN=pN: e.matmul(PS(xb)[:, pcol0:512], lhsT=smask[:], rhs=spbuf[psi][:, 0:pN], start=False, stop=True, skip_group_check=True),
                                 reads=["c_smask", ("spbuf", psi)], writes=[("ps", xb)])
                        P.op("pe", lambda e, xb=xb, spt=spt, col0=col0, N=N, first=(pv is None): e.matmul(PS(xb)[:, col0:512], lhsT=uinc[:], rhs=spt[:, 0:N], start=first, stop=True, skip_group_check=True),
                             reads=["c_uinc", ("spbuf", d["si"])], writes=[("ps", xb)])
                        prev_sp[hh] = (d["si"], col0, N)
                        ti = sbrr[2] % 3
                        ai = sbrr[3] % 4
                        sbrr[2] += 1
                        sbrr[3] += 1
                        d["ai"] = ai
                        P.op("act", lambda e, xb=xb, ti=ti, col0=col0, N=N: e.activation(out=tbuf[ti][:, 0:N], in_=PS(xb)[:, col0:512], func=AF.Exp, scale=-1.0),
                             reads=[("ps", xb)], writes=[("tbuf", ti)])
                        P.op("dve", lambda e, ti=ti, ai=ai, ei=d["ei"], N=N: e.tensor_tensor(out=abuf[ai][:, 0:N], in0=tbuf[ti][:, 0:N], in1=ebuf[ei][:, 0:N], op=ALU.mult),
                             reads=[("tbuf", ti), ("ebuf", d["ei"])], writes=[("abuf", ai)])

                    def S3(u):
                        j, hh = u
                        d = stt[u]
                        ob = 6 + hh
                        col0, N = d["col0"], d["N"]
                        P.op("pe", lambda e, ob=ob, j=j, ai=d["ai"], col0=col0, N=N: e.matmul(PS(ob)[:, col0:512], lhsT=vb[:, j, m * 128:(m + 1) * 128], rhs=abuf[ai][:, 0:N],
                                                                                       start=(j == jmax), stop=(j == 0), skip_group_check=True),
                             reads=[("vb", j // 4), ("abuf", d["ai"])], writes=[("ps", ob)])

                    nu = len(units)
                    for k in range(nu + 2):
                        if k < nu:
                            S1(units[k])
                        if 0 <= k - 1 < nu:
                            S2(units[k - 1])
                        if 0 <= k - 2 < nu:
                            S3(units[k - 2])
                    for hh in range(2):
                        bp = hh * 64
                        evac(obT[bp:bp + 64, m, cs0:cs0 + 512], PS(6 + hh)[bp:bp + 64, :], [("ps", 6 + hh)], [("obT", c)])
            phase_barrier()
            if dbg:
                dtmp = dD(48 * KB, 16 * KB, F32)
                for nm, src, res in (("d_oaT", oaT, "oaT"), ("d_obT", obT, "obT")):
                    for q in range(2):
                        P.op("dve", lambda e, src=src, q=q: e.tensor_copy(out=dtmp, in_=src.rearrange("p k t -> p (k t)")[:, q * 4096:(q + 1) * 4096]),
                             reads=[(res, cq) for cq in range(4)], writes=["dtmp"])
                        if b == 0:
                            P.dma("sp", "dbg", lambda e, nm=nm, q=q: e.dma_start(out=dbg_d[nm][:, q * 4096:(q + 1) * 4096], in_=dtmp), reads=["dtmp"])
                phase_barrier()
            if stop_after == "P3":
                break

            mergedT = dD(0, 32 * KB, BF16, "p (k t) -> p k t", k=8)
            wout = dD(32 * KB, 16 * KB, BF16, "p (k c) -> p k c", k=8)
            wg4 = [dD(48 * KB + i * 4 * KB, 4 * KB, BF16, "p (k c) -> p k c", k=8) for i in range(2)]
            wbr = [dD(56 * KB + i * 2 * KB, 2 * KB, BF16, "p (a k c) -> p a k c", a=2, k=4) for i in range(2)]
            sgb_ = [dD(60 * KB + i * KB, 1 * KB, BF16) for i in range(2)]
            t12 = [dD(62 * KB + i * 2 * KB, 2 * KB, F32) for i in range(2)]
            P.dma("pool", "wout", lambda e: e.dma_start(out=wout.rearrange("p k c -> p (k c)"), in_=wout_d[:, :], max_dma_last_dim=8192), writes=["wout"])
            for m in range(8):
                wsl = m % 2
                P.dma("pool", "wg4_%d" % wsl, lambda e, m=m, wsl=wsl: e.dma_start(out=wg4[wsl].rearrange("p k c -> p (k c)"), in_=wg4_d[:, m * 2048:(m + 1) * 2048], max_dma_last_dim=8192),
                      writes=[("wg4", wsl)])
                P.dma("pool", "wbr_%d" % wsl, lambda e, m=m, wsl=wsl: e.dma_start(out=wbr[wsl].rearrange("p a k c -> p (a k c)"), in_=wbr_d[:, m * 1024:(m + 1) * 1024], max_dma_last_dim=8192),
                      writes=[("wbr", wsl)])
                for c in range(NCH):
                    cs = slice(c * 512, (c + 1) * 512)
                    banks = {}
                    for gi_, nm in enumerate(("ga", "gb")):
                        pb = nb([0, 1, 2, 3, 4, 5, 6, 7])
                        banks[nm] = pb
                        for k in range(8):
                            P.op("pe", lambda e, pb=pb, k=k, gi_=gi_, wsl=wsl, cs=cs: e.matmul(PS(pb)[:, :], lhsT=wg4[wsl][:, k, gi_ * 128:(gi_ + 1) * 128], rhs=hT[:, k, cs],
                                                                                        start=(k == 0), stop=(k == 7)),
                                 reads=[("wg4", wsl), ("hT", c)], writes=[("ps", pb)])
                    for a_, (nm, oT, ores) in enumerate((("ya", oaT, "oaT"), ("yb", obT, "obT"))):
                        pb = nb([0, 1, 2, 3, 4, 5, 6, 7])
                        banks[nm] = pb
                        for k in range(4):
                            P.op("pe", lambda e, pb=pb, k=k, a_=a_, wsl=wsl, oT=oT, cs=cs: e.matmul(PS(pb)[:, :], lhsT=wbr[wsl][:, a_, k, :], rhs=oT[:, k, cs], start=(k == 0), stop=(k == 3)),
                                 reads=[("wbr", wsl), (ores, c)], writes=[("ps", pb)])
                    i0 = 0
                    sa, sb_ = sgb_[i0], sgb_[i0 + 1]
                    P.op("act", lambda e, sa=sa, pb=banks["ga"]: e.activation(out=sa, in_=PS(pb)[:, :], func=AF.Sigmoid), reads=[("ps", banks["ga"])], writes=[("sg", i0)])
                    P.op("act", lambda e, sb_=sb_, pb=banks["gb"]: e.activation(out=sb_, in_=PS(pb)[:, :], func=AF.Sigmoid), reads=[("ps", banks["gb"])], writes=[("sg", i0 + 1)])
                    P.op("dve", lambda e, sa=sa, pb=banks["ya"]: e.tensor_tensor(out=t12[0], in0=sa, in1=PS(pb)[:, :], op=ALU.mult), reads=[("sg", i0), ("ps", banks["ya"])], writes=["t1"])
                    P.op("dve", lambda e, sb_=sb_, pb=banks["yb"]: e.tensor_tensor(out=t12[1], in0=sb_, in1=PS(pb)[:, :], op=ALU.mult), reads=[("sg", i0 + 1), ("ps", banks["yb"])], writes=["t2"])
                    P.op("dve", lambda e, m=m, cs=cs: e.tensor_tensor(out=mergedT[:, m, cs], in0=t12[0], in1=t12[1], op=ALU.add), reads=["t1", "t2"], writes=[("mergedT", c)])
            phase_barrier()
            for i in range(NT):
                P.dma("sp", "x1ld%d" % (i % 4), lambda e, i=i: e.dma_start(out=x1[:, i, :], in_=x_d[b, i * 128:(i + 1) * 128, :]), writes=[("x1", i)])
                pb0 = (i % 4) * 2
                for half in range(2):
                    pb = pb0 + half
                    for k in range(8):
                        P.op("pe", lambda e, pb=pb, k=k, i=i, half=half: e.matmul(PS(pb)[:, :], lhsT=mergedT[:, k, i * 128:(i + 1) * 128], rhs=wout[:, k, half * 512:(half + 1) * 512],
                                                                               start=(k == 0), stop=(k == 7)),
                             reads=[("mergedT", i // 4), "wout"], writes=[("ps", pb)])
                    P.op("dve", lambda e, pb=pb, i=i, half=half: e.tensor_tensor(out=x1[:, i, half * 512:(half + 1) * 512], in0=x1[:, i, half * 512:(half + 1) * 512], in1=PS(pb)[:, :], op=ALU.add),
                         reads=[("ps", pb), ("x1", i)], writes=[("x1", i)])
            phase_barrier()
            if dbg and b == 0:
                P.dma("sp", "dbg", lambda e: e.dma_start(out=dbg_d["d_x1"][:, :], in_=x1.rearrange("p i d -> p (i d)")), reads=[("x1", i) for i in range(NT)])
                phase_barrier()
            if stop_after == "P4":
                break

            h2T = hT
            xnf2 = [dD(i * 4 * KB, 4 * KB, F32) for i in range(2)]
            hTf2 = [dD(8 * KB + i * 4 * KB, 4 * KB, F32, "p (k t) -> p k t", k=8) for i in range(2)]
            comb = dD(16 * KB, 2 * KB, F32, "p (i e) -> p i e", i=16)
            elm_all = dD(18 * KB, 2304, F32, "p (i e) -> p i e", i=16)
            eqA = dD(21 * KB, 2 * KB, F32, "p (i e) -> p i e", i=16)
            eqB = dD(23 * KB, 2 * KB, F32, "p (i e) -> p i e", i=16)
            rsm = dD(25 * KB, 1 * KB, F32, "p (a i) -> p a i", a=16)
            sgm = [dD(26 * KB + i * KB, 1 * KB, BF16) for i in range(4)]
            hid = [dD(30 * KB + i * 2 * KB, 2 * KB, BF16, "p (f t) -> p f t", f=2) for i in range(2)]

            def ffn_side(i, xt, xres, rsc, xn, xnres):
                sl = i % 2
                xnf, hTf = xnf2[sl], hTf2[sl]
                P.op("dve", lambda e: e.tensor_scalar(out=xnf, in0=xt, scalar1=rsc, scalar2=None, op0=ALU.mult), reads=[xres, ("rs", i)], writes=[("xnf", sl)])
                for half in range(2):
                    pb = (4 + half) if sl == 0 else (2 + half)
                    for kk in range(4):
                        k = half * 4 + kk
                        P.op("pe", lambda e, kk=kk, k=k: e.transpose(out=PS(pb)[:, kk * 128:(kk + 1) * 128], in_=xnf[:, k * 128:(k + 1) * 128], identity=ident_f[:]),
                             reads=[("xnf", sl), "c_ident"], writes=[("ps", pb)])
                    pv = PS(pb)[:, :].rearrange("p (k t) -> p k t", k=4)
                    gf = gpk[:, 8 + half * 4:8 + half * 4 + 4]
                    P.op("dve", lambda e: e.tensor_tensor(out=h2T[:, half * 4:half * 4 + 4, i * 128:(i + 1) * 128], in0=pv, in1=gB[1][:, half * 4:half * 4 + 4, :], op=ALU.mult),
                         reads=[("ps", pb), "gB"], writes=[("hT", i // 4)])
                    P.op("dve", lambda e: e.tensor_tensor(out=hTf[:, half * 4:half * 4 + 4, :], in0=pv, in1=gf.unsqueeze(2).broadcast_to([128, 4, 128]), op=ALU.mult),
                         reads=[("ps", pb), "c_g1"], writes=[("hTf", sl)])
                rb = 6 + sl
                for k in range(8):
                    P.op("pe", lambda e, k=k: e.matmul(PS(rb)[:, 0:36], lhsT=hTf[:, k, :], rhs=wr_f[:, k, :], start=(k == 0), stop=(k == 7)),
                         reads=[("hTf", sl), "c_wr"], writes=[("ps", rb)])
                P.op("dve", lambda e: e.tensor_tensor(out=elm_all[:, i, :], in0=PS(rb)[:, 0:36], in1=brb[:], op=ALU.add), reads=[("ps", rb), "c_br"], writes=["elm_all"])

            def router_batched():
                GL = elm_all[:, :, 0:4]
                EL = elm_all[:, :, 4:36]
                R_ = lambda a: rsm[:, a, :]
                bc = lambda ap, n: ap.unsqueeze(2).broadcast_to([128, 16, n])
                seq = []
                D = lambda fn: seq.append(("dve", fn))
                A_ = lambda fn: seq.append(("act", fn))
                gmax, gs, v1, v2, w1, w2, dd = R_(0), R_(1), R_(2), R_(3), R_(4), R_(5), R_(6)
                oh = eqA[:, :, 0:4]
                ngm = eqA[:, :, 4:8]
                gd = eqA[:, :, 8:12]
                D(lambda e: e.tensor_reduce(out=gmax, in_=GL, axis=AX.X, op=ALU.max))
                D(lambda e: e.tensor_tensor(out=oh, in0=GL, in1=bc(gmax, 4), op=ALU.is_ge))
                D(lambda e: e.tensor_scalar(out=ngm, in0=oh, scalar1=-1.0, scalar2=-NEG, op0=ALU.add, op1=ALU.mult))
                D(lambda e: e.tensor_tensor(out=gd, in0=GL, in1=bc(gmax, 4), op=ALU.subtract))
                A_(lambda e: e.activation(out=gd, in_=gd, func=AF.Exp))
                D(lambda e: e.tensor_reduce(out=gs, in_=gd, axis=AX.X, op=ALU.add))
                D(lambda e: e.reciprocal(out=gs, in_=gs))
                D(lambda e: e.tensor_tensor(out=EL.rearrange("p i (g x) -> p i g x", g=4), in0=EL.rearrange("p i (g x) -> p i g x", g=4),
                                            in1=ngm.unsqueeze(3).broadcast_to([128, 16, 4, 8]), op=ALU.add))
                D(lambda e: e.tensor_reduce(out=v1, in_=EL, axis=AX.X, op=ALU.max))
                D(lambda e: e.tensor_tensor(out=eqA[:, :, :], in0=EL, in1=bc(v1, 32), op=ALU.is_equal))
                D(lambda e: e.scalar_tensor_tensor(out=eqB[:, :, :], in0=eqA[:, :, :], scalar=NEG, in1=EL, op0=ALU.mult, op1=ALU.add))
                D(lambda e: e.tensor_reduce(out=v2, in_=eqB[:, :, :], axis=AX.X, op=ALU.max))
                D(lambda e: e.tensor_tensor(out=eqB[:, :, :], in0=eqB[:, :, :], in1=bc(v2, 32), op=ALU.is_equal))
                D(lambda e: e.tensor_tensor(out=dd, in0=v2, in1=v1, op=ALU.subtract))
                A_(lambda e: e.activation(out=dd, in_=dd, func=AF.Exp))
                D(lambda e: e.tensor_scalar(out=dd, in0=dd, scalar1=1.0, scalar2=None, op0=ALU.add))
                D(lambda e: e.reciprocal(out=w1, in_=dd))
                D(lambda e: e.tensor_scalar(out=w2, in0=w1, scalar1=-1.0, scalar2=1.0, op0=ALU.mult, op1=ALU.add))
                D(lambda e: e.tensor_tensor(out=w1, in0=w1, in1=gs, op=ALU.mult))
                D(lambda e: e.tensor_tensor(out=w2, in0=w2, in1=gs, op=ALU.mult))
                D(lambda e: e.tensor_tensor(out=eqA[:, :, :], in0=eqA[:, :, :], in1=bc(w1, 32), op=ALU.mult))
                D(lambda e: e.tensor_tensor(out=eqB[:, :, :], in0=eqB[:, :, :], in1=bc(w2, 32), op=ALU.mult))
                D(lambda e: e.tensor_tensor(out=comb[:, :, :], in0=eqA[:, :, :], in1=eqB[:, :, :], op=ALU.add))
                for eng, fn in seq:
                    P.op(eng, fn, reads=["elm_all", "router"], writes=["router", "comb"])

            build_gB(1)
            norm_transpose(b, "x1", 1, h2T, "hT", None, [dD(34 * KB, 2 * KB, BF16), dD(36 * KB, 2 * KB, BF16)], None, f32_side=ffn_side)
            router_batched()
            phase_barrier()
            def wviews(ex):
                wm = wmoe[ex % 2]
                return (wm[:, 0:2048].rearrange("p (k c) -> p k c", k=8), wm[:, 2048:4096].rearrange("p (k c) -> p k c", k=8),
                        wm[:, 4096:6144].rearrange("p (f c) -> p f c", f=2))

            def moe_dma(ex):
                wsl = ex % 2
                P.dma("pool", "wmoe%d" % wsl, lambda e, wsl=wsl, ex=ex: e.dma_start(out=wmoe[wsl], in_=wmoe_d[ex, :, :], max_dma_last_dim=8192), writes=[("wmoe", wsl)])

            def GU(n, f):
                ex, c = n // NCH, n % NCH
                wsl = ex % 2
                wgv, wuv_, wdv = wviews(ex)
                cs = slice(c * 512, (c + 1) * 512)
                hs = n % 2
                gb_, ub_ = f * 2, f * 2 + 1
                for k in range(8):
                    P.op("pe", lambda e, k=k: e.matmul(PS(gb_)[:, :], lhsT=wgv[:, k, f * 128:(f + 1) * 128], rhs=h2T[:, k, cs], start=(k == 0), stop=(k == 7)),
                         reads=[("wmoe", wsl), ("hT", c)], writes=[("ps", gb_)])
                for k in range(8):
                    P.op("pe", lambda e, k=k: e.matmul(PS(ub_)[:, :], lhsT=wuv_[:, k, f * 128:(f + 1) * 128], rhs=h2T[:, k, cs], start=(k == 0), stop=(k == 7)),
                         reads=[("wmoe", wsl), ("hT", c)], writes=[("ps", ub_)])
                sgi = hs * 2 + f
                P.op("act", lambda e: e.activation(out=sgm[sgi], in_=PS(gb_)[:, :], func=AF.Silu), reads=[("ps", gb_)], writes=[("sgm", sgi)])
                P.op("dve", lambda e: e.tensor_tensor(out=hid[hs][:, f, :], in0=sgm[sgi], in1=PS(ub_)[:, :], op=ALU.mult),
                     reads=[("sgm", sgi), ("ps", ub_)], writes=[("hid", hs, f)])

            def DOWN(n, tiles):
                ex, c = n // NCH, n % NCH
                wsl = ex % 2
                wgv, wuv_, wdv = wviews(ex)
                hs = n % 2
                for tl in tiles:
                    i = c * 4 + tl
                    for half in range(2):
                        pb = 4 + (rr[0] % 4)
                        rr[0] += 1
                        for f in range(2):
                            P.op("pe", lambda e, f=f: e.matmul(PS(pb)[:, :], lhsT=hid[hs][:, f, tl * 128:(tl + 1) * 128], rhs=wdv[:, f, half * 512:(half + 1) * 512],
                                                             start=(f == 0), stop=(f == 1)),
                                 reads=[("hid", hs, f), ("wmoe", wsl)], writes=[("ps", pb)])
                        P.op("dve", lambda e: e.scalar_tensor_tensor(out=x1[:, i, half * 512:(half + 1) * 512], in0=PS(pb)[:, :], scalar=comb[:, i, ex:ex + 1],
                                                                      in1=x1[:, i, half * 512:(half + 1) * 512], op0=ALU.mult, op1=ALU.add),
                             reads=[("ps", pb), "comb", ("x1", i)], writes=[("x1", i)])

            NSTEP = N_EXP * NCH
            moe_dma(0)
            GU(0, 0)
            GU(0, 1)
            for n in range(NSTEP):
                if n % NCH == 0 and n // NCH + 1 < N_EXP:
                    moe_dma(n // NCH + 1)
                if n + 1 < NSTEP:
                    GU(n + 1, 0)
                DOWN(n, (0, 1))
                if n + 1 < NSTEP:
                    GU(n + 1, 1)
                DOWN(n, (2, 3))
            phase_barrier()
            if stop_after == "P5":
                break

            h3T = hT
            P.dma("pool", "wpg", lambda e: e.dma_start(out=wpg.rearrange("p k c -> p (k c)"), in_=wpg_d[:, :], max_dma_last_dim=8192), writes=["wpg"])
            P.dma("pool", "wpl", lambda e: e.dma_start(out=wpl.rearrange("p k c -> p (k c)"), in_=wpl_d[:, :], max_dma_last_dim=8192), writes=["wpl"])
            build_gB(2)
            norm_transpose(b, "x1", 2, h3T, "hT", None, [dD(0, 2 * KB, BF16), dD(2 * KB, 2 * KB, BF16)], [6, 7])
            pt_b = [dD(4 * KB + i * KB, 1 * KB, F32) for i in range(2)]
            pT_b = [dD(6 * KB + i * 512, 512, BF16, "p (k t) -> p k t", k=2) for i in range(2)]
            sig = [dD(8 * KB + i * 2 * KB, 2 * KB, BF16) for i in range(2)]
            tmpf = [dD(12 * KB + i * 4 * KB, 4 * KB, F32) for i in range(2)]
            outb = [dD(20 * KB + i * 4 * KB, 4 * KB, F32) for i in range(2)]
            junkf = dD(28 * KB, 2 * KB, BF16)
            gfin = dD(30 * KB, 4 * KB, F32)
            P.dma("sp", "c_gfin", lambda e: e.dma_start(out=gfin, in_=gfin_d[:, :]), writes=["c_gfin"])
            def ple_main(i):
                sl = i % 2
                P.dma("sp", "pt%d" % sl, lambda e: e.dma_start(out=pt_b[sl], in_=p_d[b, i * 128:(i + 1) * 128, :]), writes=[("pt", sl)])
                for k in range(2):
                    P.op("pe", lambda e, k=k: e.transpose(out=PS(5)[:, k * 128:(k + 1) * 128], in_=pt_b[sl][:, k * 128:(k + 1) * 128], identity=ident_f[:]),
                         reads=[("pt", sl), "c_ident"], writes=[("ps", 5)])
                P.op("act", lambda e: e.activation(out=pT_b[sl].rearrange("p k t -> p (k t)"), in_=PS(5)[:, 0:256], func=AF.Copy), reads=[("ps", 5)], writes=[("pT", sl)])
                for half in range(2):
                    gbk = half + 6 * (i % 2)
                    pbk = 2 + half
                    for k in range(8):
                        P.op("pe", lambda e, k=k: e.matmul(PS(gbk)[:, :], lhsT=h3T[:, k, i * 128:(i + 1) * 128], rhs=wpg[:, k, half * 512:(half + 1) * 512], start=(k == 0), stop=(k == 7)),
                             reads=[("hT", i // 4), "wpg"], writes=[("ps", gbk)])
                    for k in range(2):
                        P.op("pe", lambda e, k=k: e.matmul(PS(pbk)[:, :], lhsT=pT_b[sl][:, k, :], rhs=wpl[:, k, half * 512:(half + 1) * 512], start=(k == 0), stop=(k == 1)),
                             reads=[("pT", sl), "wpl"], writes=[("ps", pbk)])
                    hsl = slice(half * 512, (half + 1) * 512)
                    P.op("act", lambda e: e.activation(out=sig[sl][:, hsl], in_=PS(gbk)[:, :], func=AF.Sigmoid), reads=[("ps", gbk)], writes=[("sig", sl, half)])
                    P.op("dve", lambda e: e.tensor_tensor(out=tmpf[sl][:, hsl], in0=sig[sl][:, hsl], in1=PS(pbk)[:, :], op=ALU.mult),
                         reads=[("sig", sl, half), ("ps", pbk)], writes=[("tmpf", sl, half)])
                    P.op("dve", lambda e: e.tensor_tensor(out=tmpf[sl][:, hsl], in0=tmpf[sl][:, hsl], in1=x1[:, i, hsl], op=ALU.add),
                         reads=[("tmpf", sl, half), ("x1", i)], writes=[("tmpf", sl, half)])

            def ple_tail(i):
                sl = i % 2
                ssc = small[:, 64 + i:65 + i]
                P.op("act", lambda e: e.activation(out=junkf, in_=tmpf[sl], func=AF.Square, accum_out=ssc), reads=[("tmpf", sl, 0), ("tmpf", sl, 1)], writes=["junkf", ("fs", i)])
                P.op("pool", lambda e: e.tensor_scalar(out=ssc, in0=ssc, scalar1=1.0 / D, scalar2=EPS, op0=ALU.mult, op1=ALU.add), reads=[("fs", i)], writes=[("fs", i)])
                P.op("pool", lambda e: e.tensor_tensor(out=ssc, in0=ssc, in1=tiny[:, 1:2], op=ALU.pow), reads=[("fs", i), "mhalf"], writes=[("fs", i)])
                P.op("dve", lambda e: e.scalar_tensor_tensor(out=outb[sl], in0=tmpf[sl], scalar=ssc, in1=gfin, op0=ALU.mult, op1=ALU.mult),
                     reads=[("tmpf", sl, 0), ("tmpf", sl, 1), ("fs", i), "c_gfin"], writes=[("outb", sl)])
                tok = P.dma("sp", "out%d" % sl, lambda e: e.dma_start(out=out_d[b, i * 128:(i + 1) * 128, :], in_=outb[sl]), reads=[("outb", sl)])
                last_out_tokens.append(tok)

            for k in range(NT + 1):
                if k < NT:
                    ple_main(k)
                if k >= 1:
                    ple_tail(k - 1)
            phase_barrier()

        finals = {}
        for tok in last_out_tokens:
            finals[tok[1]] = max(finals.get(tok[1], 0), tok[2])
        for k in P.dma_keys:
            if k == "dbg":
                finals[k] = P.dma_count[k]
        P.emit(final_wait_tokens=[("d", k, n) for k, n in finals.items()])
    return nc


def _pk(w, ncols=None):
    K = w.shape[0] // 128
    return np.ascontiguousarray(w.reshape(K, 128, -1).transpose(1, 0, 2).reshape(128, -1))


def _t5_bucket_np(d):
    d = np.maximum(d, 0)
    d_f = np.maximum(d, 1).astype(np.float32)
    large = 16 + (np.log(d_f / np.float32(16)) / np.float32(math.log(128 / 16)) * np.float32(16)).astype(np.int32)
    large = np.minimum(large, 31)
    return np.where(d < 16, d, large)


def prep_weights(inp):
    f = lambda a: np.ascontiguousarray(a, dtype=np.float32)
    w_in = inp["w_in"][0]
    o = {}
    c0 = 0
    sec = {}
    for nm, wdt in (("q_a", 512), ("c_kv", 128), ("q_idx", 512), ("k_idx", 64), ("w_idx", 8), ("qkv_b", 1536), ("gate_a", 1024), ("gate_b", 1024)):
        sec[nm] = w_in[:, c0:c0 + wdt]
        c0 += wdt
    w1 = np.concatenate([sec["q_a"], sec["q_idx"], sec["c_kv"], sec["k_idx"], sec["k_idx"]], axis=1)
    o["w1"] = _pk(w1)
    o["widx"] = _pk(sec["w_idx"])
    o["w3"] = _pk(sec["qkv_b"])
    ga = sec["gate_a"].reshape(1024, 8, 128)
    gb = sec["gate_b"].reshape(1024, 8, 128)
    g4 = np.concatenate([ga, gb], axis=2)
    g4 = g4.reshape(8, 128, 8, 256).transpose(1, 2, 0, 3)
    o["wg4"] = f(g4.reshape(128, -1))
    wa = inp["w_branch_a"][0].reshape(4, 128, 8, 128)
    wb = inp["w_branch_b"][0].reshape(4, 128, 8, 128)
    wbr = np.stack([wa, wb], axis=0).transpose(2, 3, 0, 1, 4)
    o["wbr"] = f(wbr.reshape(128, -1))
    o["wout"] = _pk(inp["w_out"][0])
    wuk = inp["w_uk"][0]
    wukT = wuk.transpose(0, 2, 1).reshape(4, 2, 64, 128).transpose(1, 2, 0, 3)
    o["wuk"] = f(wukT.reshape(128, 512))
    o["wuv"] = f(inp["w_uv"][0].transpose(1, 0, 2).reshape(128, 512))
    wg = inp["w_gate"][0].reshape(N_EXP, 8, 128, 256).transpose(0, 2, 1, 3).reshape(N_EXP, 128, 2048)
    wu = inp["w_up"][0].reshape(N_EXP, 8, 128, 256).transpose(0, 2, 1, 3).reshape(N_EXP, 128, 2048)
    wd = inp["w_down"][0].reshape(N_EXP, 2, 128, 1024).transpose(0, 2, 1, 3).reshape(N_EXP, 128, 2048)
    o["wmoe"] = f(np.concatenate([wg, wu, wd], axis=2))
    wr = np.concatenate([inp["w_r1"][0], inp["w_r2"][0].transpose(1, 0, 2).reshape(1024, 32)], axis=1)
    o["wr"] = _pk(wr)
    br = np.concatenate([inp["b_r1"][0], inp["b_r2"][0].reshape(32)])
    o["br"] = f(np.broadcast_to(br[None, :], (128, 36)))
    o["wpg"] = _pk(inp["w_ple_gate"][0])
    o["wpl"] = _pk(inp["w_ple"][0])
    o["g_attn"] = f(inp["attn_norm"][0].reshape(8, 128).T)
    o["g_ffn"] = f(inp["ffn_norm"][0].reshape(8, 128).T)
    o["g_ple"] = f(inp["ple_norm"][0].reshape(8, 128).T)
    o["g_fin"] = f(np.broadcast_to(inp["final_norm"][None, :], (128, 1024)))
    o["g_kv"] = f(inp["kv_norm"][0].reshape(128, 1))
    rb = inp["rel_bias"]
    s_l = np.arange(128)[:, None]
    u = np.arange(640)[None, :]
    bidx = _t5_bucket_np(u - s_l)
    o["btoep"] = f(rb[bidx].transpose(0, 2, 1).reshape(128, 8 * 640))
    o["b31"] = f(np.broadcast_to(rb[31][None, :], (128, 8)))
    o["ident"] = np.eye(128, dtype=np.float32)
    tt = np.arange(128)[:, None]
    ss = np.arange(128)[None, :]
    o["cneg"] = np.where(ss <= tt, 0.0, NEG).astype(np.float32)
    o["smask"] = (tt < ss).astype(np.float32)
    o["uinc"] = (tt >= ss).astype(np.float32)
    sel = np.zeros((128, 256), np.float32)
    sel[64, 0:128] = 1.0
    sel[63, 128:256] = 1.0
    o["sel"] = sel
    return o


_NC_CACHE = {}


def kernel(**inputs):
    inp = {k: np.asarray(v) for k, v in inputs.items()}
    n = 8
    NB = 2
    wts = prep_weights(inp)
    x = np.ascontiguousarray(inp["x"], dtype=np.float32)
    p = np.ascontiguousarray(inp["p"][0], dtype=np.float32)
    if "nc" not in _NC_CACHE:
        _NC_CACHE["nc"] = build_nc(NB=NB)
    nc = _NC_CACHE["nc"]
    in_maps = []
    for c in range(n):
        m = dict(wts)
        m["x"] = x[c * NB:(c + 1) * NB]
        m["p"] = p[c * NB:(c + 1) * NB]
        in_maps.append(m)
    res = run_bass_kernel_spmd(nc, in_maps, core_ids=list(range(n)))
    out = np.concatenate([r["out"] for r in res.results], axis=0)
    return out.astype(np.float32)
```

```python
import math
import types
import contextlib
import numpy as np
import concourse.bass as bass
import concourse.mybir as mybir
from concourse.bass_utils import run_bass_kernel_spmd

F32 = mybir.dt.float32
BF16 = mybir.dt.bfloat16
AF = mybir.ActivationFunctionType
ALU = mybir.AluOpType
AX = mybir.AxisListType

S = 2048
D = 1024
NT = S // 128
NCH = S // 512
ATTN_SCALE = 64 ** -0.5
IDX_SCALE = (8 ** -0.5) * (64 ** -0.5)
EPS = 1e-6
NEG = -1.0e30
N_BISECT = 12
N_EXP = 32
import os as _os
SBE = _os.environ.get("SBE", "pool")
SBLN = _os.environ.get("SBLN", "1") == "1"


class Prog:
    ENGS = ("pe", "act", "dve", "pool", "sp")

    def __init__(self, nc, same_eng_sync=True):
        self.nc = nc
        self.ops = {e: [] for e in self.ENGS}
        self.last_w = {}
        self.last_r = {}
        self.clock = {e: {} for e in self.ENGS}
        self.opclock = {}
        self.dma_count = {}
        self.dma_keys = []
        self.signaling = set()
        self.same_eng_sync = same_eng_sync
        self._bar = 0

    def _add(self, eng, fn, reads, writes, dma_key=None, n_dma=1):
        idx = len(self.ops[eng]) + 1
        deps = {}

        def need(tok):
            kind, who, n = tok
            if kind == "e" and who == eng:
                if eng in ("pe", "sp") or not self.same_eng_sync:
                    return
            k = (kind, who)
            if deps.get(k, 0) < n:
                deps[k] = n

        for r in reads:
            for tok in self.last_w.get(r, {}).values():
                need(tok)
        for w in writes:
            for tok in self.last_w.get(w, {}).values():
                need(tok)
            for tok in self.last_r.get(w, {}).values():
                need(tok)
        if dma_key is not None:
            if dma_key not in self.dma_count:
                self.dma_count[dma_key] = 0
                self.dma_keys.append(dma_key)
            prev = self.dma_count[dma_key]
            if prev > 0:
                need(("d", dma_key, prev))
            self.dma_count[dma_key] = prev + n_dma
            mytok = ("d", dma_key, prev + n_dma)
        else:
            mytok = ("e", eng, idx)
        clk = self.clock[eng]
        final = []
        for k, n in deps.items():
            if clk.get(k, 0) >= n:
                continue
            final.append((k[0], k[1], n))
        for kind, who, n in final:
            oc = self.opclock.get((kind, who, n))
            if oc:
                for k2, n2 in oc.items():
                    if clk.get(k2, 0) < n2:
                        clk[k2] = n2
            if clk.get((kind, who), 0) < n:
                clk[(kind, who)] = n
            if kind == "e":
                self.signaling.add((who, n))
        snap = dict(clk)
        if mytok[0] == "e":
            snap[("e", eng)] = idx
        self.opclock[mytok] = snap
        self.ops[eng].append((fn, final, dma_key, mytok))
        if fn is not None:
            for r in reads:
                self.last_r.setdefault(r, {})[(mytok[0], mytok[1])] = mytok
        for w in writes:
            self.last_w[w] = {(mytok[0], mytok[1]): mytok}
            self.last_r[w] = {}
        return mytok

    @staticmethod
    def _freeze(fn):
        if fn is None or getattr(fn, "__closure__", None) is None:
            return fn
        cells = []
        for c in fn.__closure__:
            try:
                cells.append(types.CellType(c.cell_contents))
            except ValueError:
                cells.append(c)
        return types.FunctionType(fn.__code__, fn.__globals__, fn.__name__, fn.__defaults__, tuple(cells))

    def op(self, eng, fn, reads=(), writes=()):
        return self._add(eng, self._freeze(fn), tuple(reads), tuple(writes))

    def dma(self, eng, key, fns, reads=(), writes=()):
        if not isinstance(fns, (list, tuple)):
            fns = [fns]
        return self._add(eng, [self._freeze(f) for f in fns], tuple(reads), tuple(writes), dma_key=key, n_dma=len(fns))

    def barrier(self, tiny_fn):
        self._bar += 1
        res = ("__barrier__", self._bar)
        allres = list(set(list(self.last_w.keys()) + list(self.last_r.keys())))
        self._add("dve", self._freeze(tiny_fn), tuple(), tuple(allres) + (res,))
        for e in ("pe", "act", "pool", "sp"):
            self._add(e, None, (res,), tuple())

    def emit(self, final_wait_tokens=()):
        nc = self.nc
        sigval = {}
        for e in self.ENGS:
            s = 0
            for i in range(1, len(self.ops[e]) + 1):
                if (e, i) in self.signaling:
                    s += 1
                    sigval[(e, i)] = s
        engobj = {"pe": nc.tensor, "act": nc.scalar, "dve": nc.vector, "pool": nc.gpsimd, "sp": nc.sync}
        with contextlib.ExitStack() as st:
            esem = {e: st.enter_context(nc.semaphore("sem_" + e)) for e in self.ENGS}
            dsem = {k: st.enter_context(nc.semaphore("dsem_%d" % i)) for i, k in enumerate(self.dma_keys)}
            block = st.enter_context(nc.Block())

            def run(e):
                eng = engobj[e]
                for i, (fn, deps, dma_key, mytok) in enumerate(self.ops[e], start=1):
                    for kind, who, n in deps:
                        if kind == "e":
                            eng.wait_ge(esem[who], sigval[(who, n)])
                        else:
                            eng.wait_ge(dsem[who], 16 * n)
                    if fn is None:
                        assert (e, i) not in self.signaling
                        continue
                    if dma_key is not None:
                        for f in fn:
                            f(eng).then_inc(dsem[dma_key], 16)
                    else:
                        ins = fn(eng)
                        if (e, i) in self.signaling:
                            ins.then_inc(esem[e], 1)
                if e == "sp":
                    for k in self.dma_keys:
                        eng.wait_ge(dsem[k], 16 * self.dma_count[k])

            block.tensor(lambda eng: run("pe"))
            block.scalar(lambda eng: run("act"))
            block.vector(lambda eng: run("dve"))
            block.gpsimd(lambda eng: run("pool"))
            block.sync(lambda eng: run("sp"))


def build_nc(NB=2, stop_after=None, dbg=False):
    nc = bass.Bass("TRN2", target_bir_lowering=False)

    def din(name, shape, dt=F32):
        return nc.dram_tensor(name, list(shape), dt, kind="ExternalInput").ap()

    x_d = din("x", [NB, S, D])
    p_d = din("p", [NB, S, 256])
    w1_d = din("w1", [128, 8 * 1280])
    widx_d = din("widx", [128, 8 * 8])
    w3_d = din("w3", [128, 8 * 1536])
    wg4_d = din("wg4", [128, 8 * 8 * 256])
    wbr_d = din("wbr", [128, 8 * 1024])
    wout_d = din("wout", [128, 8 * 1024])
    wuk_d = din("wuk", [128, 512])
    wuv_d = din("wuv", [128, 512])
    wmoe_d = din("wmoe", [N_EXP, 128, 6144])
    wr_d = din("wr", [128, 8 * 36])
    br_d = din("br", [128, 36])
    wpg_d = din("wpg", [128, 8 * 1024])
    wpl_d = din("wpl", [128, 2 * 1024])
    gat_d = din("g_attn", [128, 8])
    gff_d = din("g_ffn", [128, 8])
    gpl_d = din("g_ple", [128, 8])
    gfin_d = din("g_fin", [128, 1024])
    gkv_d = din("g_kv", [128, 1])
    btoep_d = din("btoep", [128, 8 * 640])
    b31_d = din("b31", [128, 8])
    ident_d = din("ident", [128, 128])
    cneg_d = din("cneg", [128, 128])
    smask_d = din("smask", [128, 128])
    uinc_d = din("uinc", [128, 128])
    sel_d = din("sel", [128, 256])
    out_d = nc.dram_tensor("out", [NB, S, D], F32, kind="ExternalOutput").ap()
    dbg_d = {}
    if dbg:
        for nm, shp in (("d_oaT", [128, 4 * S]), ("d_obT", [128, 4 * S]), ("d_x1", [128, NT * D])):
            dbg_d[nm] = nc.dram_tensor(nm, shp, F32, kind="ExternalOutput").ap()

    st = contextlib.ExitStack()
    with st:
        def sb(name, shape, dt):
            return st.enter_context(nc.sbuf_tensor("s_" + name, list(shape), dt))

        arA = sb("arA", [128, 16384], BF16)
        arB = sb("arB", [128, 32768], BF16)
        arC = sb("arC", [128, 16384], BF16)
        arD = sb("arD", [128, 33792], BF16)
        ident_f = sb("ident_f", [128, 128], F32)
        ident_b = sb("ident_b", [128, 128], BF16)
        cneg = sb("cneg", [128, 128], F32)
        smask = sb("smask", [128, 128], BF16)
        uinc = sb("uinc", [128, 128], BF16)
        ones_b = sb("ones_b", [128, 128], BF16)
        sel_f = sb("sel_f", [128, 256], F32)
        gB1 = sb("gB", [128, 8, 128], BF16)
        gB = [gB1, gB1, gB1]
        gpk = sb("gpk", [128, 24], F32)
        gkv = sb("gkv", [128, 1], F32)
        b31 = sb("b31", [128, 8], F32)
        brb = sb("brb", [128, 36], F32)
        wr_f = sb("wr_f", [128, 8, 36], F32)
        wuk = sb("wuk", [128, 4, 128], BF16)
        wuv = sb("wuv", [128, 512], BF16)
        widx_w = sb("widx_w", [128, 8, 8], BF16)
        small = sb("small", [128, 256], F32)
        tiny = sb("tiny", [128, 2], F32)

        psb = [st.enter_context(nc.psum_tensor("ps%d" % i, [128, 512], F32)) for i in range(8)]

        P = Prog(nc)

        def carve(ar, off_bytes, nbytes, dt, pattern=None, **kw):
            e0 = off_bytes // 2
            ap = ar[:, e0:e0 + nbytes // 2]
            if dt == F32:
                ap = ap.bitcast(F32)
            if pattern:
                ap = ap.rearrange(pattern, **kw)
            return ap

        KB = 1024
        hT = carve(arA, 0, 32 * KB, BF16, "p (k t) -> p k t", k=8)
        qaT = carve(arB, 0, 16 * KB, BF16, "p (k t) -> p k t", k=4)
        qiT = carve(arB, 16 * KB, 16 * KB, BF16, "p (k t) -> p k t", k=4)
        ckvT = carve(arB, 32 * KB, 4 * KB, BF16)
        kiT = carve(arB, 36 * KB, 4 * KB, BF16)
        Vp = carve(arB, 40 * KB, 16 * 4 * 130 * 2, BF16, "p (j m c) -> p j m c", j=16, m=4)
        widx_tm = carve(arB, 40 * KB + 16640, 512, F32, "p (i h) -> p i h", i=16)
        qbT = carve(arB, 0, 16 * KB, BF16, "p (k t) -> p k t", k=4)
        kbT = carve(arB, 16 * KB, 16 * KB, BF16, "p (k t) -> p k t", k=4)
        vb = carve(arB, 32 * KB, 16 * KB, BF16, "p (j c) -> p j c", j=16)
        qbTn = carve(arB, 48 * KB, 16 * KB, BF16, "p (k t) -> p k t", k=4)
        x1 = carve(arB, 0, 64 * KB, F32, "p (i d) -> p i d", i=16)
        oaT = carve(arC, 0, 16 * KB, BF16, "p (k t) -> p k t", k=4)
        obT = carve(arC, 16 * KB, 16 * KB, BF16, "p (k t) -> p k t", k=4)
        wmoe = [carve(arC, i * 12 * KB, 12 * KB, BF16) for i in range(2)]
        wpg = carve(arC, 0, 16 * KB, BF16, "p (k c) -> p k c", k=8)
        wpl = carve(arC, 16 * KB, 4 * KB, BF16, "p (k c) -> p k c", k=2)
        def dD(off, nbytes, dt, pattern=None, **kw):
            assert off + nbytes <= 66 * KB, (off, nbytes)
            return carve(arD, off, nbytes, dt, pattern, **kw)

        PS = lambda i: psb[i]

        def psbf(i):
            return psb[i][:].bitcast(BF16)

        def ld(key, dst, src, eng="sp", res=None, **kw):
            P.dma(eng, key, lambda e: e.dma_start(out=dst, in_=src, **kw), writes=[res or key])

        ld("c_ident", ident_f[:], ident_d[:, :])
        ld("c_cneg", cneg[:], cneg_d[:, :])
        ld("c_sel", sel_f[:], sel_d[:, :])
        ld("c_gkv", gkv[:], gkv_d[:, :])
        ld("c_b31", b31[:], b31_d[:, :])
        ld("c_br", brb[:], br_d[:, :])
        ld("c_wr", wr_f[:].rearrange("p k c -> p (k c)"), wr_d[:, :])
        ld("c_g0", gpk[:, 0:8], gat_d[:, :])
        ld("c_g1", gpk[:, 8:16], gff_d[:, :])
        ld("c_g2", gpk[:, 16:24], gpl_d[:, :])
        ld("c_smask", smask[:], smask_d[:, :], eng="pool")
        ld("c_uinc", uinc[:], uinc_d[:, :], eng="pool")
        ld("c_wuk", wuk[:].rearrange("p k c -> p (k c)"), wuk_d[:, :], eng="pool")
        ld("c_wuv", wuv[:], wuv_d[:, :], eng="pool")
        ld("c_widx", widx_w[:].rearrange("p k c -> p (k c)"), widx_d[:, :], eng="pool")
        P.op("dve", lambda e: e.tensor_copy(out=ident_b[:], in_=ident_f[:]), reads=["c_ident"], writes=["ident_b"])
        P.op("dve", lambda e: e.memset(ones_b[:], 1.0), writes=["ones_b"])
        P.op("dve", lambda e: e.memset(tiny[:, 0:1], 0.0), writes=["tiny"])
        P.op("dve", lambda e: e.memset(tiny[:, 1:2], -0.5), writes=["mhalf"])
        def build_gB(gi):
            for k in range(8):
                P.op("dve", lambda e, gi=gi, k=k: e.tensor_scalar(out=gB1[:, k, :], in0=ones_b[:], scalar1=gpk[:, gi * 8 + k:gi * 8 + k + 1],
                                                                  scalar2=None, op0=ALU.mult),
                     reads=["ones_b", "c_g%d" % gi], writes=["gB"])

        evac_rr = [0]

        def evac(out, in_, reads, writes, scale=None, eng=None):
            if eng is None:
                eng = ("act", "dve")[evac_rr[0] % 2]
                evac_rr[0] += 1
            if eng == "act":
                if scale is None:
                    P.op("act", lambda e: e.activation(out=out, in_=in_, func=AF.Copy), reads=reads, writes=writes)
                else:
                    P.op("act", lambda e: e.activation(out=out, in_=in_, func=AF.Copy, scale=float(scale)), reads=reads, writes=writes)
            else:
                if scale is None:
                    P.op("dve", lambda e: e.tensor_copy(out=out, in_=in_), reads=reads, writes=writes)
                else:
                    P.op("dve", lambda e: e.tensor_scalar(out=out, in0=in_, scalar1=float(scale), scalar2=None, op0=ALU.mult), reads=reads, writes=writes)

        def phase_barrier():
            P.barrier(lambda e: e.memset(tiny[:, 0:1], 0.0))

        def norm_transpose(b, src, gi, dstT, dstres, xt_bufs, xn_bufs, ps_banks, f32_side=None):
            for i in range(NT):
                sl = i % 2
                if src == "x":
                    xt = xt_bufs[sl]
                    xres = "xt%d" % sl
                    P.dma("sp", xres, lambda e, xt=xt, i=i: e.dma_start(out=xt, in_=x_d[b, i * 128:(i + 1) * 128, :]), writes=[xres])
                else:
                    xt = x1[:, i, :]
                    xres = ("x1", i)
                xn = xn_bufs[sl]
                xnres = "xn%d" % sl
                ssc = small[:, i:i + 1]
                rsc = small[:, 16 + i:17 + i]
                P.op("act", lambda e, xt=xt, xn=xn, ssc=ssc: e.activation(out=xn, in_=xt, func=AF.Square, accum_out=ssc),
                     reads=[xres], writes=[xnres, ("ss", i)])
                P.op("dve", lambda e, ssc=ssc, rsc=rsc: e.tensor_scalar(out=rsc, in0=ssc, scalar1=1.0 / D, scalar2=EPS, op0=ALU.mult, op1=ALU.add),
                     reads=[("ss", i)], writes=[("rs", i)])
                P.op("act", lambda e, rsc=rsc: e.activation(out=rsc, in_=rsc, func=AF.Sqrt), reads=[("rs", i)], writes=[("rs", i)])
                P.op("dve", lambda e, rsc=rsc: e.reciprocal(out=rsc, in_=rsc), reads=[("rs", i)], writes=[("rs", i)])
                if f32_side is None:
                    P.op("dve", lambda e, xt=xt, xn=xn, rsc=rsc: e.tensor_scalar(out=xn, in0=xt, scalar1=rsc, scalar2=None, op0=ALU.mult),
                         reads=[xres, ("rs", i)], writes=[xnres])
                    pb = ps_banks[i % len(ps_banks)]
                    pres = ("ps", pb)
                    pv = psbf(pb)[:, 0:1024].rearrange("p (k t) -> p k t", k=8)
                    for k in range(8):
                        P.op("pe", lambda e, pv=pv, xn=xn, k=k: e.transpose(out=pv[:, k, :], in_=xn[:, k * 128:(k + 1) * 128], identity=ident_b[:]),
                             reads=[xnres, "ident_b"], writes=[pres])
                    P.op("dve", lambda e, pv=pv, i=i: e.tensor_tensor(out=dstT[:, :, i * 128:(i + 1) * 128], in0=pv, in1=gB[gi][:], op=ALU.mult),
                         reads=[pres, "gB"], writes=[(dstres, i // 4)])
                else:
                    f32_side(i, xt, xres, rsc, xn, xnres)

        last_out_tokens = []
        for b in range(NB):
            xt_bufs = [dD(0, 4 * KB, F32), dD(4 * KB, 4 * KB, F32)]
            xn_bufs = [dD(8 * KB, 2 * KB, BF16), dD(10 * KB, 2 * KB, BF16)]
            w1 = dD(12 * KB, 20 * KB, BF16, "p (k c) -> p k c", k=8)
            ckv_raw = dD(32 * KB, 8 * KB, F32)
            sqb = [dD(40 * KB, 1 * KB, BF16), dD(41 * KB, 1 * KB, BF16)]
            rstd_b = [dD(42 * KB, 2 * KB, F32), dD(44 * KB, 2 * KB, F32)]
            P.dma("pool", "w1", lambda e: e.dma_start(out=w1.rearrange("p k c -> p (k c)"), in_=w1_d[:, :], max_dma_last_dim=8192), writes=["w1"])
            build_gB(0)
            norm_transpose(b, "x", 0, hT, "hT", xt_bufs, xn_bufs, [6, 7])

            if stop_after == "P0":
                break
            bank_rr = [0]

            def nb(banks):
                v = banks[bank_rr[0] % len(banks)]
                bank_rr[0] += 1
                return v

            def proj_T(w, wres, cc, c, banks):
                pb = nb(banks)
                for k in range(8):
                    P.op("pe", lambda e, pb=pb, k=k: e.matmul(PS(pb)[:, :], lhsT=w[:, k, cc * 128:(cc + 1) * 128], rhs=hT[:, k, c * 512:(c + 1) * 512],
                                                             start=(k == 0), stop=(k == 7)),
                         reads=[wres, ("hT", c)], writes=[("ps", pb)])
                return pb

            for cc in range(10):
                for c in range(NCH):
                    pb = proj_T(w1, "w1", cc, c, [0, 1, 2, 3])
                    cs = slice(c * 512, (c + 1) * 512)
                    if cc < 4:
                        evac(qaT[:, cc, cs], PS(pb)[:, :], [("ps", pb)], [("qaT", c)])
                    elif cc < 8:
                        evac(qiT[:, cc - 4, cs], PS(pb)[:, :], [("ps", pb)], [("qiT", c)])
                    elif cc == 8:
                        evac(ckv_raw[:, cs], PS(pb)[:, :], [("ps", pb)], [("ckv_raw", c)])
                    else:
                        evac(kiT[:, cs], PS(pb)[:, :], [("ps", pb)], [("kiT", c)])
            for c in range(NCH):
                cs = slice(c * 512, (c + 1) * 512)
                sq = sqb[c % 2]
                rb = rstd_b[c % 2]
                P.op("act", lambda e, sq=sq, cs=cs: e.activation(out=sq, in_=ckv_raw[:, cs], func=AF.Square), reads=[("ckv_raw", c)], writes=[("sq", c % 2)])
                pb = nb([0, 1, 2, 3])
                P.op("pe", lambda e, pb=pb, sq=sq: e.matmul(PS(pb)[:, :], lhsT=ones_b[:], rhs=sq, start=True, stop=True),
                     reads=[("sq", c % 2), "ones_b"], writes=[("ps", pb)])
                P.op("dve", lambda e, pb=pb, rb=rb: e.tensor_scalar(out=rb, in0=PS(pb)[:, :], scalar1=1.0 / 128, scalar2=EPS, op0=ALU.mult, op1=ALU.add),
                     reads=[("ps", pb)], writes=[("rb", c % 2)])
                P.op("act", lambda e, rb=rb: e.activation(out=rb, in_=rb, func=AF.Sqrt), reads=[("rb", c % 2)], writes=[("rb", c % 2)])
                P.op("dve", lambda e, rb=rb: e.reciprocal(out=rb, in_=rb), reads=[("rb", c % 2)], writes=[("rb", c % 2)])
                P.op("dve", lambda e, rb=rb, cs=cs: e.scalar_tensor_tensor(out=ckvT[:, cs], in0=ckv_raw[:, cs], scalar=gkv[:, 0:1], in1=rb, op0=ALU.mult, op1=ALU.mult),
                     reads=[("ckv_raw", c), ("rb", c % 2), "c_gkv"], writes=[("ckvT", c)])
            pbw = 4
            for i in range(NT):
                for k in range(8):
                    P.op("pe", lambda e, i=i, k=k: e.matmul(PS(pbw)[:, i * 8:(i + 1) * 8], lhsT=hT[:, k, i * 128:(i + 1) * 128], rhs=widx_w[:, k, :],
                                                         start=(k == 0), stop=(k == 7)),
                         reads=[("hT", i // 4), "c_widx"], writes=[("ps", pbw)])
            P.op("dve", lambda e: e.tensor_scalar(out=widx_tm.rearrange("p i h -> p (i h)"), in0=PS(pbw)[:, 0:128], scalar1=IDX_SCALE, scalar2=None, op0=ALU.mult),
                 reads=[("ps", pbw)], writes=["widx_tm"])
            P.op("pool", lambda e: e.memset(Vp.rearrange("p j m c -> p (j m) c")[:, :, 64:65], 1.0), writes=["Vp"])
            P.op("pool", lambda e: e.memset(Vp.rearrange("p j m c -> p (j m) c")[:, :, 129:130], 1.0), writes=["Vp"])
            for j in range(NT):
                pb = nb([0, 1, 2, 3])
                P.op("pe", lambda e, pb=pb, j=j: e.matmul(PS(pb)[:, :], lhsT=ckvT[:, j * 128:(j + 1) * 128], rhs=wuv[:], start=True, stop=True),
                     reads=[("ckvT", j // 4), "c_wuv"], writes=[("ps", pb)])
                pv = PS(pb)[:, :].rearrange("p (m h d) -> p m h d", m=4, h=2)
                evac(Vp[:, j, :, 0:64], pv[:, :, 0, :], [("ps", pb)], ["Vp"])
                evac(Vp[:, j, :, 65:129], pv[:, :, 1, :], [("ps", pb)], ["Vp"])
            phase_barrier()
            if stop_after == "P1":
                break

            NEGM = 30000.0
            score = [dD(0, 8 * KB, F32), dD(8 * KB, 8 * KB, F32), carve(arC, 16 * KB, 8 * KB, F32), carve(arC, 24 * KB, 8 * KB, F32)]
            junk = dD(16 * KB, 4 * KB, BF16)
            mask_tm = dD(20 * KB, 4 * KB, BF16)
            maskT = dD(24 * KB, 16 * KB, BF16, "p (j t) -> p j t", j=16)
            B8 = dD(40 * KB, 10 * KB, BF16, "p (h u) -> p h u", h=8)
            rbuf = [dD(50 * KB + i * KB, 1 * KB, BF16) for i in range(4)]
            Pbuf = [dD(54 * KB + i * KB, 1 * KB, BF16) for i in range(4)]
            qabs = [dD(58 * KB + i * KB, 1 * KB, BF16) for i in range(2)]
            dg = dD(60 * KB, 2 * KB, BF16, "p (h t) -> p h t", h=8)
            o_f32 = dD(62 * KB, 2 * KB, F32)
            rec = dD(64 * KB, 2 * KB, F32)
            btmp = dD(0, 10 * KB, F32, "p (h u) -> p h u", h=4)
            for hh in range(2):
                P.dma("sp", "btoep", lambda e, hh=hh: e.dma_start(out=btmp.rearrange("p h u -> p (h u)")[:, 0:2560], in_=btoep_d[:, hh * 2560:(hh + 1) * 2560]), writes=["btmp"])
                P.op("dve", lambda e, hh=hh: e.tensor_scalar(out=B8[:, hh * 4:hh * 4 + 4, :], in0=btmp[:, 0:4, :], scalar1=1.0 / ATTN_SCALE, scalar2=None, op0=ALU.mult),
                     reads=["btmp"], writes=["B8"])
            phase_barrier()

            LO, WD, MID, CNT, TMP = 40, 44, 48, 52, 56
            rr = [0]
            smr = lambda base, a0, a1: small[:, base + a0:base + a1]

            def scores_chunk(c):
                units = []
                for tl in range(4):
                    L = (4 * c + tl + 1) * 128
                    nsc = (L + 511) // 512
                    for sc in range(nsc):
                        for h in range(8):
                            units.append((tl, sc, h, nsc))
                stt = {}

                def A(u):
                    tl, sc, h, nsc = u
                    i = 4 * c + tl
                    L = (i + 1) * 128
                    ws = min(512, L - sc * 512)
                    bp = (h % 2) * 64
                    zb = nb([0, 1, 2, 3])
                    P.op("pe", lambda e: e.matmul(PS(zb)[:, 0:ws], lhsT=qiT[bp:bp + 64, h // 2, i * 128:(i + 1) * 128], rhs=kiT[bp:bp + 64, sc * 512:sc * 512 + ws], start=True, stop=True),
                         reads=[("qiT", c), ("kiT", sc)], writes=[("ps", zb)])
                    rs = rr[0] % 4
                    rr[0] += 1
                    if rs != 3:
                        P.op("act", lambda e: e.activation(out=rbuf[rs][:, 0:ws], in_=PS(zb)[:, 0:ws], func=AF.Relu), reads=[("ps", zb)], writes=[("rbuf", rs)])
                    else:
                        P.op("dve", lambda e: e.tensor_scalar(out=rbuf[rs][:, 0:ws], in0=PS(zb)[:, 0:ws], scalar1=0.0, scalar2=None, op0=ALU.max), reads=[("ps", zb)], writes=[("rbuf", rs)])
                    stt[u] = (rs, ws)

                def B(u):
                    tl, sc, h, nsc = u
                    i = 4 * c + tl
                    rs, ws = stt[u]
                    sct = score[tl]
                    spb = 4 + (sc % 2)
                    if sc == 0 and h == 0:
                        for hh in range(8):
                            P.op("dve", lambda e, hh=hh: e.tensor_scalar(out=dg[:, hh, :], in0=ident_b[:], scalar1=widx_tm[:, i, hh:hh + 1], scalar2=None, op0=ALU.mult),
                                 reads=["ident_b", "widx_tm"], writes=[("dg", hh)])
                    P.op("pe", lambda e: e.matmul(PS(spb)[:, 0:ws], lhsT=dg[:, h, :], rhs=rbuf[rs][:, 0:ws], start=(h == 0), stop=(h == 7)),
                         reads=[("dg", h), ("rbuf", rs)], writes=[("ps", spb)])
                    if h == 7:
                        last = (sc == nsc - 1)
                        wcopy = ws - 128 if last else ws
                        if wcopy > 0:
                            P.op("act", lambda e: e.activation(out=sct[:, sc * 512:sc * 512 + wcopy], in_=PS(spb)[:, 0:wcopy], func=AF.Copy),
                                 reads=[("ps", spb)], writes=[("score", tl)])
                        if last:
                            P.op("dve", lambda e: e.tensor_tensor(out=sct[:, sc * 512 + ws - 128:sc * 512 + ws], in0=PS(spb)[:, ws - 128:ws], in1=cneg[:], op=ALU.add),
                                 reads=[("ps", spb), "c_cneg"], writes=[("score", tl)])

                nu = len(units)
                for k in range(nu + 2):
                    if k < nu:
                        A(units[k])
                    if 0 <= k - 2 < nu:
                        B(units[k - 2])

            def bisect_chunk(c):
                act = [tl for tl in range(4) if 4 * c + tl >= 2]
                for tl in range(4):
                    i = 4 * c + tl
                    L = (i + 1) * 128
                    if i < 2:
                        P.op("dve", lambda e, tl=tl: e.memset(smr(LO, tl, tl + 1), -1.0e29), writes=[("lo", tl)])
                    else:
                        P.op("dve", lambda e, tl=tl, i=i: e.tensor_reduce(out=smr(LO, tl, tl + 1), in_=score[tl][:, 0:i * 128], axis=AX.X, op=ALU.min), reads=[("score", tl)], writes=[("lo", tl)])
                        P.op("dve", lambda e, tl=tl, L=L: e.tensor_reduce(out=smr(WD, tl, tl + 1), in_=score[tl][:, 0:L], axis=AX.X, op=ALU.max), reads=[("score", tl)], writes=[("wd", tl)])
                if not act:
                    return
                a0, a1 = act[0], act[-1] + 1
                R = lambda nm: [(nm, t) for t in range(a0, a1)]
                P.op("dve", lambda e: e.tensor_tensor(out=smr(WD, a0, a1), in0=smr(WD, a0, a1), in1=smr(LO, a0, a1), op=ALU.subtract), reads=R("wd") + R("lo"), writes=R("wd"))
                for it in range(N_BISECT):
                    f = 0.5 ** (it + 1)
                    P.op("dve", lambda e, f=f: e.scalar_tensor_tensor(out=smr(MID, a0, a1), in0=smr(WD, a0, a1), scalar=f, in1=smr(LO, a0, a1), op0=ALU.mult, op1=ALU.add),
                         reads=R("wd") + R("lo"), writes=R("mid"))
                    for tl in act:
                        L = (4 * c + tl + 1) * 128
                        jb, jres = ((junk, "junk"), (mask_tm, "mask_tm"))[tl % 2]
                        P.op("dve", lambda e, tl=tl, L=L, jb=jb: e.tensor_scalar(out=jb[:, 0:L], in0=score[tl][:, 0:L], scalar1=smr(MID, tl, tl + 1), scalar2=None, op0=ALU.is_ge, op1=ALU.add,
                                                                         accum_out=smr(CNT, tl, tl + 1)),
                             reads=[("score", tl), ("mid", tl)], writes=[("cnt", tl), jres])
                    P.op("dve", lambda e, f=f: e.tensor_scalar(out=smr(TMP, a0, a1), in0=smr(CNT, a0, a1), scalar1=255.5, scalar2=f, op0=ALU.is_ge, op1=ALU.mult),
                         reads=R("cnt"), writes=["tmp4"])
                    P.op("dve", lambda e: e.tensor_tensor(out=smr(TMP, a0, a1), in0=smr(TMP, a0, a1), in1=smr(WD, a0, a1), op=ALU.mult), reads=["tmp4"] + R("wd"), writes=["tmp4"])
                    P.op("dve", lambda e: e.tensor_tensor(out=smr(LO, a0, a1), in0=smr(LO, a0, a1), in1=smr(TMP, a0, a1), op=ALU.add), reads=["tmp4"] + R("lo"), writes=R("lo"))

            def maskgen_chunk(c):
                for tl in range(4):
                    i = 4 * c + tl
                    L = (i + 1) * 128
                    P.op("dve", lambda e, tl=tl, L=L: e.tensor_scalar(out=mask_tm[:, 0:L], in0=score[tl][:, 0:L], scalar1=smr(LO, tl, tl + 1), scalar2=-NEGM, op0=ALU.is_lt, op1=ALU.mult),
                         reads=[("score", tl), ("lo", tl)], writes=["mask_tm"])
                    for j0 in range(0, i + 1, 8):
                        n = min(8, i + 1 - j0)
                        tb = 6 + ((j0 // 8) % 2)
                        pv = psbf(tb)[:, 0:1024].rearrange("p (k t) -> p k t", k=8)
                        for jj in range(n):
                            j = j0 + jj
                            P.op("pe", lambda e, pv=pv, jj=jj, j=j: e.transpose(out=pv[:, jj, :], in_=mask_tm[:, j * 128:(j + 1) * 128], identity=ident_b[:]),
                                 reads=["mask_tm", "ident_b"], writes=[("ps", tb)])
                        evac(maskT[:, j0:j0 + n, tl * 128:(tl + 1) * 128], pv[:, 0:n, :], [("ps", tb)], [("maskT", tl)])

            def attention_chunk(c):
                cs0 = c * 512
                jmax = 4 * c + 3

                def emit_qabs(h):
                    bp = (h % 2) * 64
                    qb_ = nb([0, 1, 2, 3])
                    P.op("pe", lambda e, qb_=qb_, bp=bp, h=h: e.matmul(PS(qb_)[:, :], lhsT=wuk[bp:bp + 64, h // 2, :], rhs=qaT[bp:bp + 64, h // 2, cs0:cs0 + 512], start=True, stop=True),
                         reads=["c_wuk", ("qaT", c)], writes=[("ps", qb_)])
                    evac(qabs[h % 2], PS(qb_)[:, :], [("ps", qb_)], [("qabs", h % 2)], eng="act")

                def emit_tail(h):
                    bp = (h % 2) * 64
                    ob = 4 + (h % 2)
                    dbk = 6 + (h % 2)
                    P.op("act", lambda e: e.activation(out=o_f32[bp:bp + 64, :], in_=PS(ob)[bp:bp + 64, :], func=AF.Copy), reads=[("ps", ob)], writes=[("o_f32", h % 2)])
                    P.op("act", lambda e: e.activation(out=rec[bp:bp + 64, :], in_=PS(dbk)[bp:bp + 64, :], func=AF.Ln), reads=[("ps", dbk)], writes=[("rec", h % 2)])
                    P.op("act", lambda e: e.activation(out=rec[bp:bp + 64, :], in_=rec[bp:bp + 64, :], func=AF.Exp, scale=-1.0), reads=[("rec", h % 2)], writes=[("rec", h % 2)])
                    P.op("pool", lambda e: e.tensor_tensor(out=oaT[bp:bp + 64, h // 2, cs0:cs0 + 512], in0=o_f32[bp:bp + 64, :], in1=rec[bp:bp + 64, :], op=ALU.mult),
                         reads=[("o_f32", h % 2), ("rec", h % 2)], writes=[("oaT", c)])

                emit_qabs(0)
                pending_tail = None
                for h in range(8):
                    m = h // 2
                    qs = h % 2
                    ob = 4 + (h % 2)
                    if h < 7:
                        emit_qabs(h + 1)
                    stt = {}

                    def A(j):
                        col0 = max(0, j - 4 * c) * 128
                        N = 512 - col0
                        near = j >= 4 * c - 1
                        lb = nb([0, 1, 2, 3])
                        P.op("pe", lambda e, lb=lb, j=j, col0=col0, N=N: e.matmul(PS(lb)[:, 0:N], lhsT=ckvT[:, j * 128:(j + 1) * 128], rhs=qabs[qs][:, col0:512], start=True, stop=False),
                             reads=[("ckvT", j // 4), ("qabs", qs)], writes=[("ps", lb)])
                        P.op("pe", lambda e, lb=lb, j=j, col0=col0, N=N, near=near: e.matmul(PS(lb)[:, 0:N], lhsT=ident_b[:], rhs=maskT[:, j, col0:512], start=False, stop=(not near)),
                             reads=["ident_b"] + [("maskT", t) for t in range(col0 // 128, 4)], writes=[("ps", lb)])
                        if near:
                            u0 = cs0 + col0 - 128 * j
                            P.op("pe", lambda e, lb=lb, u0=u0, N=N: e.matmul(PS(lb)[:, 0:N], lhsT=ident_b[:], rhs=B8[:, h, u0:u0 + N], start=False, stop=True),
                                 reads=["ident_b", "B8"], writes=[("ps", lb)])
                        pi = rr[0] % 4
                        rr[0] += 1
                        Pt = Pbuf[pi]
                        if near:
                            P.op("act", lambda e, lb=lb, Pt=Pt, N=N: e.activation(out=Pt[:, 0:N], in_=PS(lb)[:, 0:N], func=AF.Exp, scale=ATTN_SCALE),
                                 reads=[("ps", lb)], writes=[("Pbuf", pi)])
                        else:
                            P.op("act", lambda e, lb=lb, Pt=Pt, N=N: e.activation(out=Pt[:, 0:N], in_=PS(lb)[:, 0:N], func=AF.Exp, scale=ATTN_SCALE, bias=b31[:, h:h + 1]),
                                 reads=[("ps", lb), "c_b31"], writes=[("Pbuf", pi)])
                        stt[j] = (pi, col0, N)

                    def B(j):
                        pi, col0, N = stt[j]
                        w0 = 0 if h % 2 == 0 else 1
                        P.op("pe", lambda e, j=j, w0=w0, pi=pi, col0=col0, N=N: e.matmul(PS(ob)[:, col0:512], lhsT=Vp[:, j, m, w0:w0 + 128], rhs=Pbuf[pi][:, 0:N],
                                                                                  start=(j == 0), stop=(j == jmax)),
                             reads=["Vp", ("Pbuf", pi)], writes=[("ps", ob)])
                        dbk = 6 + (h % 2)
                        P.op("pe", lambda e, j=j, pi=pi, col0=col0, N=N: e.matmul(PS(dbk)[:, col0:512], lhsT=ones_b[:], rhs=Pbuf[pi][:, 0:N], start=(j == 0), stop=(j == jmax)),
                             reads=["ones_b", ("Pbuf", pi)], writes=[("ps", dbk)])

                    for k in range(jmax + 3):
                        if k <= jmax:
                            A(k)
                        if 0 <= k - 2 <= jmax:
                            B(k - 2)
                        if k == 2 and pending_tail is not None:
                            emit_tail(pending_tail)
                            pending_tail = None
                    pending_tail = h
                emit_tail(pending_tail)

            scores_chunk(0)
            bisect_chunk(0)
            maskgen_chunk(0)
            for c in range(NCH):
                if c + 1 < NCH:
                    scores_chunk(c + 1)
                    bisect_chunk(c + 1)
                attention_chunk(c)
                if c + 1 < NCH:
                    maskgen_chunk(c + 1)
            phase_barrier()
            if stop_after == "P2":
                break

            w3 = dD(0, 24 * KB, BF16, "p (k c) -> p k c", k=8)
            ebuf = [dD(24 * KB + i * 2 * KB, 2 * KB, F32) for i in range(4)]
            spbuf = [dD(32 * KB + i * KB, 1 * KB, BF16) for i in range(6)]
            tbuf = [dD(38 * KB + i * 2 * KB, 2 * KB, F32) for i in range(3)]
            abuf = [dD(44 * KB + i * KB, 1 * KB, BF16) for i in range(4)]
            sbrr = [0, 0, 0, 0]
            P.dma("pool", "w3", lambda e: e.dma_start(out=w3.rearrange("p k c -> p (k c)"), in_=w3_d[:, :], max_dma_last_dim=8192), writes=["w3"])
            if stop_after == "P3w":
                break
            for cc in range(8):
                for c in range(NCH):
                    pb = proj_T(w3, "w3", cc, c, [0, 1, 2, 3])
                    cs = slice(c * 512, (c + 1) * 512)
                    if cc < 4:
                        evac(qbT[:, cc, cs], PS(pb)[:, :], [("ps", pb)], [("qbT", c)])
                    else:
                        evac(kbT[:, cc - 4, cs], PS(pb)[:, :], [("ps", pb)], [("kbT", c)])
            if stop_after == "P3q":
                break
            for j in range(NT):
                pb = nb([0, 1, 2, 3])
                for k in range(8):
                    P.op("pe", lambda e, pb=pb, j=j, k=k: e.matmul(PS(pb)[:, :], lhsT=hT[:, k, j * 128:(j + 1) * 128], rhs=w3[:, k, 1024:1536], start=(k == 0), stop=(k == 7)),
                         reads=["w3", ("hT", j // 4)], writes=[("ps", pb)])
                evac(vb[:, j, :], PS(pb)[:, :], [("ps", pb)], [("vb", j // 4)])
            if stop_after == "P3a":
                break
            for c in range(NCH):
                cs0 = c * 512
                jmax = 4 * c + 3
                for hp in range(4):
                    m = hp
                    units = [(j, hh) for j in range(jmax, -1, -1) for hh in range(2)]
                    stt = {}
                    prev_sp = {0: None, 1: None}

                    def S1(u):
                        j, hh = u
                        bp = hh * 64
                        col0 = max(0, j - 4 * c) * 128
                        N = 512 - col0
                        zb = nb([0, 1, 2, 5])
                        P.op("pe", lambda e, zb=zb, bp=bp, j=j, col0=col0, N=N: e.matmul(PS(zb)[:, 0:N], lhsT=kbT[bp:bp + 64, m, j * 128:(j + 1) * 128],
                                                                                    rhs=qbT[bp:bp + 64, m, cs0 + col0:cs0 + 512], start=True, stop=True),
                             reads=[("kbT", j // 4), ("qbT", c)], writes=[("ps", zb)])
                        ei = sbrr[0] % 4
                        si = sbrr[1] % 6
                        sbrr[0] += 1
                        sbrr[1] += 1
                        eb_, spt = ebuf[ei], spbuf[si]
                        P.op("act", lambda e, zb=zb, eb_=eb_, N=N: e.activation(out=eb_[:, 0:N], in_=PS(zb)[:, 0:N], func=AF.Exp, scale=ATTN_SCALE),
                             reads=[("ps", zb)], writes=[("ebuf", ei)])
                        if j >= 4 * c:
                            P.op("dve", lambda e, eb_=eb_: e.tensor_tensor(out=eb_[:, 0:128], in0=eb_[:, 0:128], in1=smask[:], op=ALU.mult),
                                 reads=[("ebuf", ei), "c_smask"], writes=[("ebuf", ei)])
                        P.op("act", lambda e, eb_=eb_, spt=spt, N=N: e.activation(out=spt[:, 0:N], in_=eb_[:, 0:N], func=AF.Ln, bias=1.0, scale=1.0),
                             reads=[("ebuf", ei)], writes=[("spbuf", si)])
                        stt[u] = dict(ei=ei, si=si, col0=col0, N=N)

                    def S2(u):
                        j, hh = u
                        d = stt[u]
                        xb = 3 + hh
                        col0, N = d["col0"], d["N"]
                        spt = spbuf[d["si"]]
                        pv = prev_sp[hh]
                        if pv is not None:
                            psi, pcol0, pN = pv
                            P.op("pe", lambda e, xb=xb, psi=psi, pcol0=pcol0, pN=pN: e.matmul(PS(xb)[:, pcol0:512], lhsT=smask[:], rhs=spbuf[psi][:, 0:pN], start=False, stop=True, skip_group_check=True),
                                 reads=["c_smask", ("spbuf", psi)], writes=[("ps", xb)])
                        P.op("pe", lambda e, xb=xb, spt=spt, col0=col0, N=N, first=(pv is None): e.matmul(PS(xb)[:, col0:512], lhsT=uinc[:], rhs=spt[:, 0:N], start=first, stop=True, skip_group_check=True),
                             reads=["c_uinc", ("spbuf", d["si"])], writes=[("ps", xb)])
                        prev_sp[hh] = (d["si"], col0, N)
                        ti = sbrr[2] % 3
                        ai = sbrr[3] % 4
                        sbrr[2] += 1
                        sbrr[3] += 1
                        d["ai"] = ai
                        P.op("act", lambda e, xb=xb, ti=ti, col0=col0, N=N: e.activation(out=tbuf[ti][:, 0:N], in_=PS(xb)[:, col0:512], func=AF.Exp, scale=-1.0),
                             reads=[("ps", xb)], writes=[("tbuf", ti)])
                        P.op("dve", lambda e, ti=ti, ai=ai, ei=d["ei"], N=N: e.tensor_tensor(out=abuf[ai][:, 0:N], in0=tbuf[ti][:, 0:N], in1=ebuf[ei][:, 0:N], op=ALU.mult),
                             reads=[("tbuf", ti), ("ebuf", d["ei"])], writes=[("abuf", ai)])

                    def S3(u):
                        j, hh = u
                        d = stt[u]
                        ob = 6 + hh
                        col0, N = d["col0"], d["N"]
                        P.op("pe", lambda e, ob=ob, j=j, ai=d["ai"], col0=col0, N=N: e.matmul(PS(ob)[:, col0:512], lhsT=vb[:, j, m * 128:(m + 1) * 128], rhs=abuf[ai][:, 0:N],
                                                                                       start=(j == jmax), stop=(j == 0), skip_group_check=True),
                             reads=[("vb", j // 4), ("abuf", d["ai"])], writes=[("ps", ob)])

                    nu = len(units)
                    for k in range(nu + 2):
                        if k < nu:
                            S1(units[k])
                        if 0 <= k - 1 < nu:
                            S2(units[k - 1])
                        if 0 <= k - 2 < nu:
                            S3(units[k - 2])
                    for hh in range(2):
                        bp = hh * 64
                        evac(obT[bp:bp + 64, m, cs0:cs0 + 512], PS(6 + hh)[bp:bp + 64, :], [("ps", 6 + hh)], [("obT", c)])
            phase_barrier()
            if dbg:
                dtmp = dD(48 * KB, 16 * KB, F32)
                for nm, src, res in (("d_oaT", oaT, "oaT"), ("d_obT", obT, "obT")):
                    for q in range(2):
                        P.op("dve", lambda e, src=src, q=q: e.tensor_copy(out=dtmp, in_=src.rearrange("p k t -> p (k t)")[:, q * 4096:(q + 1) * 4096]),
                             reads=[(res, cq) for cq in range(4)], writes=["dtmp"])
                        if b == 0:
                            P.dma("sp", "dbg", lambda e, nm=nm, q=q: e.dma_start(out=dbg_d[nm][:, q * 4096:(q + 1) * 4096], in_=dtmp), reads=["dtmp"])
                phase_barrier()
            if stop_after == "P3":
                break

            mergedT = dD(0, 32 * KB, BF16, "p (k t) -> p k t", k=8)
            wout = dD(32 * KB, 16 * KB, BF16, "p (k c) -> p k c", k=8)
            wg4 = [dD(48 * KB + i * 4 * KB, 4 * KB, BF16, "p (k c) -> p k c", k=8) for i in range(2)]
            wbr = [dD(56 * KB + i * 2 * KB, 2 * KB, BF16, "p (a k c) -> p a k c", a=2, k=4) for i in range(2)]
            sgb_ = [dD(60 * KB + i * KB, 1 * KB, BF16) for i in range(2)]
            t12 = [dD(62 * KB + i * 2 * KB, 2 * KB, F32) for i in range(2)]
            P.dma("pool", "wout", lambda e: e.dma_start(out=wout.rearrange("p k c -> p (k c)"), in_=wout_d[:, :], max_dma_last_dim=8192), writes=["wout"])
            for m in range(8):
                wsl = m % 2
                P.dma("pool", "wg4_%d" % wsl, lambda e, m=m, wsl=wsl: e.dma_start(out=wg4[wsl].rearrange("p k c -> p (k c)"), in_=wg4_d[:, m * 2048:(m + 1) * 2048], max_dma_last_dim=8192),
                      writes=[("wg4", wsl)])
                P.dma("pool", "wbr_%d" % wsl, lambda e, m=m, wsl=wsl: e.dma_start(out=wbr[wsl].rearrange("p a k c -> p (a k c)"), in_=wbr_d[:, m * 1024:(m + 1) * 1024], max_dma_last_dim=8192),
                      writes=[("wbr", wsl)])
                for c in range(NCH):
                    cs = slice(c * 512, (c + 1) * 512)
                    banks = {}
                    for gi_, nm in enumerate(("ga", "gb")):
                        pb = nb([0, 1, 2, 3, 4, 5, 6, 7])
                        banks[nm] = pb
                        for k in range(8):
                            P.op("pe", lambda e, pb=pb, k=k, gi_=gi_, wsl=wsl, cs=cs: e.matmul(PS(pb)[:, :], lhsT=wg4[wsl][:, k, gi_ * 128:(gi_ + 1) * 128], rhs=hT[:, k, cs],
                                                                                        start=(k == 0), stop=(k == 7)),
                                 reads=[("wg4", wsl), ("hT", c)], writes=[("ps", pb)])
                    for a_, (nm, oT, ores) in enumerate((("ya", oaT, "oaT"), ("yb", obT, "obT"))):
                        pb = nb([0, 1, 2, 3, 4, 5, 6, 7])
                        banks[nm] = pb
                        for k in range(4):
                            P.op("pe", lambda e, pb=pb, k=k, a_=a_, wsl=wsl, oT=oT, cs=cs: e.matmul(PS(pb)[:, :], lhsT=wbr[wsl][:, a_, k, :], rhs=oT[:, k, cs], start=(k == 0), stop=(k == 3)),
                                 reads=[("wbr", wsl), (ores, c)], writes=[("ps", pb)])
                    i0 = 0
                    sa, sb_ = sgb_[i0], sgb_[i0 + 1]
                    P.op("act", lambda e, sa=sa, pb=banks["ga"]: e.activation(out=sa, in_=PS(pb)[:, :], func=AF.Sigmoid), reads=[("ps", banks["ga"])], writes=[("sg", i0)])
                    P.op("act", lambda e, sb_=sb_, pb=banks["gb"]: e.activation(out=sb_, in_=PS(pb)[:, :], func=AF.Sigmoid), reads=[("ps", banks["gb"])], writes=[("sg", i0 + 1)])
                    P.op("dve", lambda e, sa=sa, pb=banks["ya"]: e.tensor_tensor(out=t12[0], in0=sa, in1=PS(pb)[:, :], op=ALU.mult), reads=[("sg", i0), ("ps", banks["ya"])], writes=["t1"])
                    P.op("dve", lambda e, sb_=sb_, pb=banks["yb"]: e.tensor_tensor(out=t12[1], in0=sb_, in1=PS(pb)[:, :], op=ALU.mult), reads=[("sg", i0 + 1), ("ps", banks["yb"])], writes=["t2"])
                    P.op("dve", lambda e, m=m, cs=cs: e.tensor_tensor(out=mergedT[:, m, cs], in0=t12[0], in1=t12[1], op=ALU.add), reads=["t1", "t2"], writes=[("mergedT", c)])
            phase_barrier()
            for i in range(NT):
                P.dma("sp", "x1ld%d" % (i % 4), lambda e, i=i: e.dma_start(out=x1[:, i, :], in_=x_d[b, i * 128:(i + 1) * 128, :]), writes=[("x1", i)])
                pb0 = (i % 4) * 2
                for half in range(2):
                    pb = pb0 + half
                    for k in range(8):
                        P.op("pe", lambda e, pb=pb, k=k, i=i, half=half: e.matmul(PS(pb)[:, :], lhsT=mergedT[:, k, i * 128:(i + 1) * 128], rhs=wout[:, k, half * 512:(half + 1) * 512],
                                                                               start=(k == 0), stop=(k == 7)),
                             reads=[("mergedT", i // 4), "wout"], writes=[("ps", pb)])
                    P.op("dve", lambda e, pb=pb, i=i, half=half: e.tensor_tensor(out=x1[:, i, half * 512:(half + 1) * 512], in0=x1[:, i, half * 512:(half + 1) * 512], in1=PS(pb)[:, :], op=ALU.add),
                         reads=[("ps", pb), ("x1", i)], writes=[("x1", i)])
            phase_barrier()
            if dbg and b == 0:
                P.dma("sp", "dbg", lambda e: e.dma_start(out=dbg_d["d_x1"][:, :], in_=x1.rearrange("p i d -> p (i d)")), reads=[("x1", i) for i in range(NT)])
                phase_barrier()
            if stop_after == "P4":
                break

            h2T = hT
            xnf2 = [dD(i * 4 * KB, 4 * KB, F32) for i in range(2)]
            hTf2 = [dD(8 * KB + i * 4 * KB, 4 * KB, F32, "p (k t) -> p k t", k=8) for i in range(2)]
            comb = dD(16 * KB, 2 * KB, F32, "p (i e) -> p i e", i=16)
            elm_all = dD(18 * KB, 2304, F32, "p (i e) -> p i e", i=16)
            eqA = dD(21 * KB, 2 * KB, F32, "p (i e) -> p i e", i=16)
            eqB = dD(23 * KB, 2 * KB, F32, "p (i e) -> p i e", i=16)
            rsm = dD(25 * KB, 1 * KB, F32, "p (a i) -> p a i", a=16)
            sgm = [dD(26 * KB + i * KB, 1 * KB, BF16) for i in range(4)]
            hid = [dD(30 * KB + i * 2 * KB, 2 * KB, BF16, "p (f t) -> p f t", f=2) for i in range(2)]

            def ffn_side(i, xt, xres, rsc, xn, xnres):
                sl = i % 2
                xnf, hTf = xnf2[sl], hTf2[sl]
                P.op("dve", lambda e: e.tensor_scalar(out=xnf, in0=xt, scalar1=rsc, scalar2=None, op0=ALU.mult), reads=[xres, ("rs", i)], writes=[("xnf", sl)])
                for half in range(2):
                    pb = (4 + half) if sl == 0 else (2 + half)
                    for kk in range(4):
                        k = half * 4 + kk
                        P.op("pe", lambda e, kk=kk, k=k: e.transpose(out=PS(pb)[:, kk * 128:(kk + 1) * 128], in_=xnf[:, k * 128:(k + 1) * 128], identity=ident_f[:]),
                             reads=[("xnf", sl), "c_ident"], writes=[("ps", pb)])
                    pv = PS(pb)[:, :].rearrange("p (k t) -> p k t", k=4)
                    gf = gpk[:, 8 + half * 4:8 + half * 4 + 4]
                    P.op("dve", lambda e: e.tensor_tensor(out=h2T[:, half * 4:half * 4 + 4, i * 128:(i + 1) * 128], in0=pv, in1=gB[1][:, half * 4:half * 4 + 4, :], op=ALU.mult),
                         reads=[("ps", pb), "gB"], writes=[("hT", i // 4)])
                    P.op("dve", lambda e: e.tensor_tensor(out=hTf[:, half * 4:half * 4 + 4, :], in0=pv, in1=gf.unsqueeze(2).broadcast_to([128, 4, 128]), op=ALU.mult),
                         reads=[("ps", pb), "c_g1"], writes=[("hTf", sl)])
                rb = 6 + sl
                for k in range(8):
                    P.op("pe", lambda e, k=k: e.matmul(PS(rb)[:, 0:36], lhsT=hTf[:, k, :], rhs=wr_f[:, k, :], start=(k == 0), stop=(k == 7)),
                         reads=[("hTf", sl), "c_wr"], writes=[("ps", rb)])
                P.op("dve", lambda e: e.tensor_tensor(out=elm_all[:, i, :], in0=PS(rb)[:, 0:36], in1=brb[:], op=ALU.add), reads=[("ps", rb), "c_br"], writes=["elm_all"])

            def router_batched():
                GL = elm_all[:, :, 0:4]
                EL = elm_all[:, :, 4:36]
                R_ = lambda a: rsm[:, a, :]
                bc = lambda ap, n: ap.unsqueeze(2).broadcast_to([128, 16, n])
                seq = []
                D = lambda fn: seq.append(("dve", fn))
                A_ = lambda fn: seq.append(("act", fn))
                gmax, gs, v1, v2, w1, w2, dd = R_(0), R_(1), R_(2), R_(3), R_(4), R_(5), R_(6)
                oh = eqA[:, :, 0:4]
                ngm = eqA[:, :, 4:8]
                gd = eqA[:, :, 8:12]
                D(lambda e: e.tensor_reduce(out=gmax, in_=GL, axis=AX.X, op=ALU.max))
                D(lambda e: e.tensor_tensor(out=oh, in0=GL, in1=bc(gmax, 4), op=ALU.is_ge))
                D(lambda e: e.tensor_scalar(out=ngm, in0=oh, scalar1=-1.0, scalar2=-NEG, op0=ALU.add, op1=ALU.mult))
                D(lambda e: e.tensor_tensor(out=gd, in0=GL, in1=bc(gmax, 4), op=ALU.subtract))
                A_(lambda e: e.activation(out=gd, in_=gd, func=AF.Exp))
                D(lambda e: e.tensor_reduce(out=gs, in_=gd, axis=AX.X, op=ALU.add))
                D(lambda e: e.reciprocal(out=gs, in_=gs))
                D(lambda e: e.tensor_tensor(out=EL.rearrange("p i (g x) -> p i g x", g=4), in0=EL.rearrange("p i (g x) -> p i g x", g=4),
                                            in1=ngm.unsqueeze(3).broadcast_to([128, 16, 4, 8]), op=ALU.add))
                D(lambda e: e.tensor_reduce(out=v1, in_=EL, axis=AX.X, op=ALU.max))
                D(lambda e: e.tensor_tensor(out=eqA[:, :, :], in0=EL, in1=bc(v1, 32), op=ALU.is_equal))
                D(lambda e: e.scalar_tensor_tensor(out=eqB[:, :, :], in0=eqA[:, :, :], scalar=NEG, in1=EL, op0=ALU.mult, op1=ALU.add))
                D(lambda e: e.tensor_reduce(out=v2, in_=eqB[:, :, :], axis=AX.X, op=ALU.max))
                D(lambda e: e.tensor_tensor(out=eqB[:, :, :], in0=eqB[:, :, :], in1=bc(v2, 32), op=ALU.is_equal))
                D(lambda e: e.tensor_tensor(out=dd, in0=v2, in1=v1, op=ALU.subtract))
                A_(lambda e: e.activation(out=dd, in_=dd, func=AF.Exp))
                D(lambda e: e.tensor_scalar(out=dd, in0=dd, scalar1=1.0, scalar2=None, op0=ALU.add))
                D(lambda e: e.reciprocal(out=w1, in_=dd))
                D(lambda e: e.tensor_scalar(out=w2, in0=w1, scalar1=-1.0, scalar2=1.0, op0=ALU.mult, op1=ALU.add))
                D(lambda e: e.tensor_tensor(out=w1, in0=w1, in1=gs, op=ALU.mult))
                D(lambda e: e.tensor_tensor(out=w2, in0=w2, in1=gs, op=ALU.mult))
                D(lambda e: e.tensor_tensor(out=eqA[:, :, :], in0=eqA[:, :, :], in1=bc(w1, 32), op=ALU.mult))
                D(lambda e: e.tensor_tensor(out=eqB[:, :, :], in0=eqB[:, :, :], in1=bc(w2, 32), op=ALU.mult))
                D(lambda e: e.tensor_tensor(out=comb[:, :, :], in0=eqA[:, :, :], in1=eqB[:, :, :], op=ALU.add))
                for eng, fn in seq:
                    P.op(eng, fn, reads=["elm_all", "router"], writes=["router", "comb"])

            build_gB(1)
            norm_transpose(b, "x1", 1, h2T, "hT", None, [dD(34 * KB, 2 * KB, BF16), dD(36 * KB, 2 * KB, BF16)], None, f32_side=ffn_side)
            router_batched()
            phase_barrier()
            def wviews(ex):
                wm = wmoe[ex % 2]
                return (wm[:, 0:2048].rearrange("p (k c) -> p k c", k=8), wm[:, 2048:4096].rearrange("p (k c) -> p k c", k=8),
                        wm[:, 4096:6144].rearrange("p (f c) -> p f c", f=2))

            def moe_dma(ex):
                wsl = ex % 2
                P.dma("pool", "wmoe%d" % wsl, lambda e, wsl=wsl, ex=ex: e.dma_start(out=wmoe[wsl], in_=wmoe_d[ex, :, :], max_dma_last_dim=8192), writes=[("wmoe", wsl)])

            def GU(n, f):
                ex, c = n // NCH, n % NCH
                wsl = ex % 2
                wgv, wuv_, wdv = wviews(ex)
                cs = slice(c * 512, (c + 1) * 512)
                hs = n % 2
                gb_, ub_ = f * 2, f * 2 + 1
                for k in range(8):
                    P.op("pe", lambda e, k=k: e.matmul(PS(gb_)[:, :], lhsT=wgv[:, k, f * 128:(f + 1) * 128], rhs=h2T[:, k, cs], start=(k == 0), stop=(k == 7)),
                         reads=[("wmoe", wsl), ("hT", c)], writes=[("ps", gb_)])
                for k in range(8):
                    P.op("pe", lambda e, k=k: e.matmul(PS(ub_)[:, :], lhsT=wuv_[:, k, f * 128:(f + 1) * 128], rhs=h2T[:, k, cs], start=(k == 0), stop=(k == 7)),
                         reads=[("wmoe", wsl), ("hT", c)], writes=[("ps", ub_)])
                sgi = hs * 2 + f
                P.op("act", lambda e: e.activation(out=sgm[sgi], in_=PS(gb_)[:, :], func=AF.Silu), reads=[("ps", gb_)], writes=[("sgm", sgi)])
                P.op("dve", lambda e: e.tensor_tensor(out=hid[hs][:, f, :], in0=sgm[sgi], in1=PS(ub_)[:, :], op=ALU.mult),
                     reads=[("sgm", sgi), ("ps", ub_)], writes=[("hid", hs, f)])

            def DOWN(n, tiles):
                ex, c = n // NCH, n % NCH
                wsl = ex % 2
                wgv, wuv_, wdv = wviews(ex)
                hs = n % 2
                for tl in tiles:
                    i = c * 4 + tl
                    for half in range(2):
                        pb = 4 + (rr[0] % 4)
                        rr[0] += 1
                        for f in range(2):
                            P.op("pe", lambda e, f=f: e.matmul(PS(pb)[:, :], lhsT=hid[hs][:, f, tl * 128:(tl + 1) * 128], rhs=wdv[:, f, half * 512:(half + 1) * 512],
                                                             start=(f == 0), stop=(f == 1)),
                                 reads=[("hid", hs, f), ("wmoe", wsl)], writes=[("ps", pb)])
                        P.op("dve", lambda e: e.scalar_tensor_tensor(out=x1[:, i, half * 512:(half + 1) * 512], in0=PS(pb)[:, :], scalar=comb[:, i, ex:ex + 1],
                                                                      in1=x1[:, i, half * 512:(half + 1) * 512], op0=ALU.mult, op1=ALU.add),
                             reads=[("ps", pb), "comb", ("x1", i)], writes=[("x1", i)])

            NSTEP = N_EXP * NCH
            moe_dma(0)
            GU(0, 0)
            GU(0, 1)
            for n in range(NSTEP):
                if n % NCH == 0 and n // NCH + 1 < N_EXP:
                    moe_dma(n // NCH + 1)
                if n + 1 < NSTEP:
                    GU(n + 1, 0)
                DOWN(n, (0, 1))
                if n + 1 < NSTEP:
                    GU(n + 1, 1)
                DOWN(n, (2, 3))
            phase_barrier()
            if stop_after == "P5":
                break

            h3T = hT
            P.dma("pool", "wpg", lambda e: e.dma_start(out=wpg.rearrange("p k c -> p (k c)"), in_=wpg_d[:, :], max_dma_last_dim=8192), writes=["wpg"])
            P.dma("pool", "wpl", lambda e: e.dma_start(out=wpl.rearrange("p k c -> p (k c)"), in_=wpl_d[:, :], max_dma_last_dim=8192), writes=["wpl"])
            build_gB(2)
            norm_transpose(b, "x1", 2, h3T, "hT", None, [dD(0, 2 * KB, BF16), dD(2 * KB, 2 * KB, BF16)], [6, 7])
            pt_b = [dD(4 * KB + i * KB, 1 * KB, F32) for i in range(2)]
            pT_b = [dD(6 * KB + i * 512, 512, BF16, "p (k t) -> p k t", k=2) for i in range(2)]
            sig = [dD(8 * KB + i * 2 * KB, 2 * KB, BF16) for i in range(2)]
            tmpf = [dD(12 * KB + i * 4 * KB, 4 * KB, F32) for i in range(2)]
            outb = [dD(20 * KB + i * 4 * KB, 4 * KB, F32) for i in range(2)]
            junkf = dD(28 * KB, 2 * KB, BF16)
            gfin = dD(30 * KB, 4 * KB, F32)
            P.dma("sp", "c_gfin", lambda e: e.dma_start(out=gfin, in_=gfin_d[:, :]), writes=["c_gfin"])
            def ple_main(i):
                sl = i % 2
                P.dma("sp", "pt%d" % sl, lambda e: e.dma_start(out=pt_b[sl], in_=p_d[b, i * 128:(i + 1) * 128, :]), writes=[("pt", sl)])
                for k in range(2):
                    P.op("pe", lambda e, k=k: e.transpose(out=PS(5)[:, k * 128:(k + 1) * 128], in_=pt_b[sl][:, k * 128:(k + 1) * 128], identity=ident_f[:]),
                         reads=[("pt", sl), "c_ident"], writes=[("ps", 5)])
                P.op("act", lambda e: e.activation(out=pT_b[sl].rearrange("p k t -> p (k t)"), in_=PS(5)[:, 0:256], func=AF.Copy), reads=[("ps", 5)], writes=[("pT", sl)])
                for half in range(2):
                    gbk = half + 6 * (i % 2)
                    pbk = 2 + half
                    for k in range(8):
                        P.op("pe", lambda e, k=k: e.matmul(PS(gbk)[:, :], lhsT=h3T[:, k, i * 128:(i + 1) * 128], rhs=wpg[:, k, half * 512:(half + 1) * 512], start=(k == 0), stop=(k == 7)),
                             reads=[("hT", i // 4), "wpg"], writes=[("ps", gbk)])
                    for k in range(2):
                        P.op("pe", lambda e, k=k: e.matmul(PS(pbk)[:, :], lhsT=pT_b[sl][:, k, :], rhs=wpl[:, k, half * 512:(half + 1) * 512], start=(k == 0), stop=(k == 1)),
                             reads=[("pT", sl), "wpl"], writes=[("ps", pbk)])
                    hsl = slice(half * 512, (half + 1) * 512)
                    P.op("act", lambda e: e.activation(out=sig[sl][:, hsl], in_=PS(gbk)[:, :], func=AF.Sigmoid), reads=[("ps", gbk)], writes=[("sig", sl, half)])
                    P.op("dve", lambda e: e.tensor_tensor(out=tmpf[sl][:, hsl], in0=sig[sl][:, hsl], in1=PS(pbk)[:, :], op=ALU.mult),
                         reads=[("sig", sl, half), ("ps", pbk)], writes=[("tmpf", sl, half)])
                    P.op("dve", lambda e: e.tensor_tensor(out=x1[:, i, hsl], in0=tmpf[sl][:, hsl], in1=x1[:, i, hsl], op=ALU.add),
                         reads=[("tmpf", sl, half), ("x1", i)], writes=[("x1", i)])

            for k in range(NT):
                ple_main(k)
            for i in range(NT):
                P.op("act", lambda e, i=i: e.activation(out=junkf, in_=x1[:, i, :], func=AF.Square, accum_out=small[:, 64 + i:65 + i]), reads=[("x1", i)], writes=["junkf", "fs16"])
            fs16 = small[:, 64:80]
            P.op("dve", lambda e: e.tensor_scalar(out=fs16, in0=fs16, scalar1=1.0 / D, scalar2=EPS, op0=ALU.mult, op1=ALU.add), reads=["fs16"], writes=["fs16"])
            P.op("act", lambda e: e.activation(out=fs16, in_=fs16, func=AF.Sqrt), reads=["fs16"], writes=["fs16"])
            P.op("dve", lambda e: e.reciprocal(out=fs16, in_=fs16), reads=["fs16"], writes=["fs16"])
            for i in range(NT):
                sl = i % 2
                P.op("dve", lambda e, i=i, sl=sl: e.scalar_tensor_tensor(out=outb[sl], in0=x1[:, i, :], scalar=small[:, 64 + i:65 + i], in1=gfin, op0=ALU.mult, op1=ALU.mult),
                     reads=[("x1", i), "fs16", "c_gfin"], writes=[("outb", sl)])
                tok = P.dma("sp", "out%d" % sl, lambda e, i=i, sl=sl: e.dma_start(out=out_d[b, i * 128:(i + 1) * 128, :], in_=outb[sl]), reads=[("outb", sl)])
                last_out_tokens.append(tok)
            phase_barrier()

        finals = {}
        for tok in last_out_tokens:
            finals[tok[1]] = max(finals.get(tok[1], 0), tok[2])
        for k in P.dma_keys:
            if k == "dbg":
                finals[k] = P.dma_count[k]
        P.emit(final_wait_tokens=[("d", k, n) for k, n in finals.items()])
    return nc


def _pk(w, ncols=None):
    K = w.shape[0] // 128
    return np.ascontiguousarray(w.reshape(K, 128, -1).transpose(1, 0, 2).reshape(128, -1))


def _t5_bucket_np(d):
    d = np.maximum(d, 0)
    d_f = np.maximum(d, 1).astype(np.float32)
    large = 16 + (np.log(d_f / np.float32(16)) / np.float32(math.log(128 / 16)) * np.float32(16)).astype(np.int32)
    large = np.minimum(large, 31)
    return np.where(d < 16, d, large)


def prep_weights(inp):
    f = lambda a: np.ascontiguousarray(a, dtype=np.float32)
    w_in = inp["w_in"][0]
    o = {}
    c0 = 0
    sec = {}
    for nm, wdt in (("q_a", 512), ("c_kv", 128), ("q_idx", 512), ("k_idx", 64), ("w_idx", 8), ("qkv_b", 1536), ("gate_a", 1024), ("gate_b", 1024)):
        sec[nm] = w_in[:, c0:c0 + wdt]
        c0 += wdt
    w1 = np.concatenate([sec["q_a"], sec["q_idx"], sec["c_kv"], sec["k_idx"], sec["k_idx"]], axis=1)
    o["w1"] = _pk(w1)
    o["widx"] = _pk(sec["w_idx"])
    o["w3"] = _pk(sec["qkv_b"])
    ga = sec["gate_a"].reshape(1024, 8, 128)
    gb = sec["gate_b"].reshape(1024, 8, 128)
    g4 = np.concatenate([ga, gb], axis=2)
    g4 = g4.reshape(8, 128, 8, 256).transpose(1, 2, 0, 3)
    o["wg4"] = f(g4.reshape(128, -1))
    wa = inp["w_branch_a"][0].reshape(4, 128, 8, 128)
    wb = inp["w_branch_b"][0].reshape(4, 128, 8, 128)
    wbr = np.stack([wa, wb], axis=0).transpose(2, 3, 0, 1, 4)
    o["wbr"] = f(wbr.reshape(128, -1))
    o["wout"] = _pk(inp["w_out"][0])
    wuk = inp["w_uk"][0]
    wukT = wuk.transpose(0, 2, 1).reshape(4, 2, 64, 128).transpose(1, 2, 0, 3)
    o["wuk"] = f(wukT.reshape(128, 512))
    o["wuv"] = f(inp["w_uv"][0].transpose(1, 0, 2).reshape(128, 512))
    wg = inp["w_gate"][0].reshape(N_EXP, 8, 128, 256).transpose(0, 2, 1, 3).reshape(N_EXP, 128, 2048)
    wu = inp["w_up"][0].reshape(N_EXP, 8, 128, 256).transpose(0, 2, 1, 3).reshape(N_EXP, 128, 2048)
    wd = inp["w_down"][0].reshape(N_EXP, 2, 128, 1024).transpose(0, 2, 1, 3).reshape(N_EXP, 128, 2048)
    o["wmoe"] = f(np.concatenate([wg, wu, wd], axis=2))
    wr = np.concatenate([inp["w_r1"][0], inp["w_r2"][0].transpose(1, 0, 2).reshape(1024, 32)], axis=1)
    o["wr"] = _pk(wr)
    br = np.concatenate([inp["b_r1"][0], inp["b_r2"][0].reshape(32)])
    o["br"] = f(np.broadcast_to(br[None, :], (128, 36)))
    o["wpg"] = _pk(inp["w_ple_gate"][0])
    o["wpl"] = _pk(inp["w_ple"][0])
    o["g_attn"] = f(inp["attn_norm"][0].reshape(8, 128).T)
    o["g_ffn"] = f(inp["ffn_norm"][0].reshape(8, 128).T)
    o["g_ple"] = f(inp["ple_norm"][0].reshape(8, 128).T)
    o["g_fin"] = f(np.broadcast_to(inp["final_norm"][None, :], (128, 1024)))
    o["g_kv"] = f(inp["kv_norm"][0].reshape(128, 1))
    rb = inp["rel_bias"]
    s_l = np.arange(128)[:, None]
    u = np.arange(640)[None, :]
    bidx = _t5_bucket_np(u - s_l)
    o["btoep"] = f(rb[bidx].transpose(0, 2, 1).reshape(128, 8 * 640))
    o["b31"] = f(np.broadcast_to(rb[31][None, :], (128, 8)))
    o["ident"] = np.eye(128, dtype=np.float32)
    tt = np.arange(128)[:, None]
    ss = np.arange(128)[None, :]
    o["cneg"] = np.where(ss <= tt, 0.0, NEG).astype(np.float32)
    o["smask"] = (tt < ss).astype(np.float32)
    o["uinc"] = (tt >= ss).astype(np.float32)
    sel = np.zeros((128, 256), np.float32)
    sel[64, 0:128] = 1.0
    sel[63, 128:256] = 1.0
    o["sel"] = sel
    return o


_NC_CACHE = {}


def kernel(**inputs):
    inp = {k: np.asarray(v) for k, v in inputs.items()}
    n = 8
    NB = 2
    wts = prep_weights(inp)
    x = np.ascontiguousarray(inp["x"], dtype=np.float32)
    p = np.ascontiguousarray(inp["p"][0], dtype=np.float32)
    if "nc" not in _NC_CACHE:
        _NC_CACHE["nc"] = build_nc(NB=NB)
    nc = _NC_CACHE["nc"]
    in_maps = []
    for c in range(n):
        m = dict(wts)
        m["x"] = x[c * NB:(c + 1) * NB]
        m["p"] = p[c * NB:(c + 1) * NB]
        in_maps.append(m)
    res = run_bass_kernel_spmd(nc, in_maps, core_ids=list(range(n)))
    out = np.concatenate([r["out"] for r in res.results], axis=0)
    return out.astype(np.float32)
```
